# Optimizing a Trainium2 kernel written in Bass

```python
import math
import jax, jax.numpy as jnp
from jax import lax
import numpy as np

D_MODEL = 1024
BATCH = 32
SEQ = 2048
DEPTH = 1

N_META = 16
EPS = 1e-6
BLOCK = 128
D_RNN = D_MODEL
RG_BLOCKS = 8
RG_BS = D_RNN // RG_BLOCKS
CONV_W = 4
RG_C = 8.0
MLA_HEADS = 16
Q_LORA = 384
KV_LORA = 256
QK_NOPE = 64
QK_ROPE = 32
V_DIM = 64
ROPE_BASE = 10000.0
PEER_HEADS = 8
PEER_NKEYS = 128
PEER_EXPERTS = PEER_NKEYS * PEER_NKEYS
PEER_QDIM = 256
PEER_TOPK = 16

IN_SIZES = (D_RNN, D_RNN, Q_LORA, KV_LORA, QK_ROPE, D_MODEL, D_MODEL)
IN_SPLITS = tuple(int(s) for s in np.cumsum(IN_SIZES)[:-1])
D_IN = int(sum(IN_SIZES))

kernel_name = 'hybrid_rglru_mla_peer_block'


def _rmsnorm(x, g):
    xf = x.astype(jnp.float32)
    xf = xf * lax.rsqrt(jnp.mean(xf * xf, axis=-1, keepdims=True) + EPS)
    return xf.astype(x.dtype) * g


def _rope(x, pos):
    half = QK_ROPE // 2
    inv = jnp.power(ROPE_BASE, -jnp.arange(half, dtype=jnp.float32) * (2.0 / QK_ROPE))
    ang = pos[:, None] * inv[None, :]
    cos = jnp.cos(ang)[None, :, None, :].astype(x.dtype)
    sin = jnp.sin(ang)[None, :, None, :].astype(x.dtype)
    x1, x2 = x[..., :half], x[..., half:]
    return jnp.concatenate([x1 * cos - x2 * sin, x1 * sin + x2 * cos], axis=-1)


def _causal_conv(x, w, b):
    T = x.shape[1]
    xp = jnp.pad(x, ((0, 0), (CONV_W - 1, 0), (0, 0)))
    y = xp[:, 0:T] * w[0] + b
    for k in range(1, CONV_W):
        y = y + xp[:, k:k + T] * w[k]
    return y


def _lin_combine(e1, e2):
    a1, b1 = e1
    a2, b2 = e2
    return (a1 * a2, a2 * b1 + b2)


def _rg_lru(x, w_a, b_a, w_x, b_x, lam):
    B, T, _ = x.shape
    xb = x.reshape(B, T, RG_BLOCKS, RG_BS)
    r = jax.nn.sigmoid(jnp.einsum('btnc,ncd->btnd', xb, w_a).reshape(B, T, D_RNN) + b_a)
    i = jax.nn.sigmoid(jnp.einsum('btnc,ncd->btnd', xb, w_x).reshape(B, T, D_RNN) + b_x)
    log_a = RG_C * r.astype(jnp.float32) * jax.nn.log_sigmoid(lam.astype(jnp.float32))
    a = jnp.exp(log_a)
    u = jnp.sqrt(-jnp.expm1(2.0 * log_a)) * (i * x).astype(jnp.float32)
    _, h = lax.associative_scan(_lin_combine, (a, u), axis=1)
    return h.astype(x.dtype)


def _mla_attention(q_nope, q_rope, k_nope, k_rope, v):
    B, T, H, _ = q_nope.shape
    nb = T // BLOCK
    scale = (QK_NOPE + QK_ROPE) ** -0.5
    key_pos = jnp.arange(T)

    def to_blocks(t):
        return jnp.swapaxes(t.reshape((B, nb, BLOCK) + t.shape[2:]), 0, 1)

    def one_block(args):
        qn, qr, bi = args
        q_pos = bi * BLOCK + jnp.arange(BLOCK)
        s = (jnp.einsum('bqhd,bkhd->bhqk', qn, k_nope)
             + jnp.einsum('bqhd,bkd->bhqk', qr, k_rope)).astype(jnp.float32) * scale
        s = jnp.where(key_pos[None, :] <= q_pos[:, None], s, -1e30)
        p = jax.nn.softmax(s, axis=-1).astype(v.dtype)
        return jnp.einsum('bhqk,bkhd->bqhd', p, v)

    o = lax.map(one_block, (to_blocks(q_nope), to_blocks(q_rope), jnp.arange(nb)))
    return jnp.swapaxes(o, 0, 1).reshape(B, T, H * V_DIM)


def _peer(x, w_q, keys1, keys2, u, v):
    B, T, D = x.shape
    half = PEER_QDIM // 2
    xc = x.reshape(B * T // BLOCK, BLOCK, D)

    def one_chunk(xt):
        q = (xt @ w_q).reshape(BLOCK, PEER_HEADS, PEER_QDIM)
        s1 = jnp.einsum('chd,nd->chn', q[..., :half], keys1).astype(jnp.float32)
        s2 = jnp.einsum('chd,nd->chn', q[..., half:], keys2).astype(jnp.float32)
        t1, i1 = lax.top_k(s1, PEER_TOPK)
        t2, i2 = lax.top_k(s2, PEER_TOPK)
        cand = (t1[..., :, None] + t2[..., None, :]).reshape(BLOCK, PEER_HEADS, PEER_TOPK * PEER_TOPK)
        cidx = (i1[..., :, None] * PEER_NKEYS + i2[..., None, :]).reshape(BLOCK, PEER_HEADS, PEER_TOPK * PEER_TOPK)
        ts, sel = lax.top_k(cand, PEER_TOPK)
        idx = jnp.take_along_axis(cidx, sel, axis=-1)
        g = jax.nn.softmax(ts, axis=-1)
        act = jax.nn.gelu(jnp.einsum('chkd,cd->chk', u[idx], xt))
        coef = (g * act.astype(jnp.float32)).astype(xt.dtype)
        return jnp.einsum('chk,chkd->cd', coef, v[idx])

    return lax.map(one_chunk, xc).reshape(B, T, D)


def setup_inputs(seed: int = 0) -> dict:
    key = jax.random.key(seed)
    ks = jax.random.split(key, 26)
    f32 = jnp.float32
    L = DEPTH

    def nrm(k, shape, scale):
        return jax.random.normal(k, shape, f32) * scale

    def gain(k, shape):
        return 1.0 + 0.01 * jax.random.normal(k, shape, f32)

    a_c = jax.random.uniform(ks[10], (L, D_RNN), f32, 0.9, 0.999)
    s = a_c ** (1.0 / RG_C)
    rg_lambda = jnp.log(s) - jnp.log1p(-s)
    return {
        'x': nrm(ks[0], (BATCH, SEQ, D_MODEL), 1.0),
        'meta_tokens': nrm(ks[1], (N_META, D_MODEL), 1.0),
        'norm1_g': gain(ks[2], (L, D_MODEL)),
        'w_in': nrm(ks[3], (L, D_MODEL, D_IN), D_MODEL ** -0.5),
        'conv_w': nrm(ks[4], (L, CONV_W, D_RNN), CONV_W ** -0.5),
        'conv_b': nrm(ks[5], (L, D_RNN), 0.01),
        'rg_wa': nrm(ks[6], (L, RG_BLOCKS, RG_BS, RG_BS), RG_BS ** -0.5),
        'rg_ba': nrm(ks[7], (L, D_RNN), 0.01),
        'rg_wx': nrm(ks[8], (L, RG_BLOCKS, RG_BS, RG_BS), RG_BS ** -0.5),
        'rg_bx': nrm(ks[9], (L, D_RNN), 0.01),
        'rg_lambda': rg_lambda,
        'w_rnn_out': nrm(ks[11], (L, D_RNN, D_MODEL), D_RNN ** -0.5),
        'q_norm_g': gain(ks[12], (L, Q_LORA)),
        'w_uq': nrm(ks[13], (L, Q_LORA, MLA_HEADS * (QK_NOPE + QK_ROPE)), Q_LORA ** -0.5),
        'kv_norm_g': gain(ks[14], (L, KV_LORA)),
        'w_ukv': nrm(ks[15], (L, KV_LORA, MLA_HEADS * (QK_NOPE + V_DIM)), KV_LORA ** -0.5),
        'w_attn_out': nrm(ks[16], (L, MLA_HEADS * V_DIM, D_MODEL), (MLA_HEADS * V_DIM) ** -0.5),
        'w_out': nrm(ks[17], (L, D_MODEL, D_MODEL), D_MODEL ** -0.5),
        'norm2_g': gain(ks[18], (L, D_MODEL)),
        'peer_wq': nrm(ks[19], (L, D_MODEL, PEER_HEADS * PEER_QDIM), D_MODEL ** -0.5),
        'peer_keys1': nrm(ks[20], (L, PEER_NKEYS, PEER_QDIM // 2), (PEER_QDIM // 2) ** -0.5),
        'peer_keys2': nrm(ks[21], (L, PEER_NKEYS, PEER_QDIM // 2), (PEER_QDIM // 2) ** -0.5),
        'peer_u': nrm(ks[22], (L, PEER_EXPERTS, D_MODEL), D_MODEL ** -0.5),
        'peer_v': nrm(ks[23], (L, PEER_EXPERTS, D_MODEL), PEER_HEADS ** -0.5),
        'final_g': gain(ks[24], (D_MODEL,)),
    }


def reference(x, meta_tokens, norm1_g, w_in, conv_w, conv_b, rg_wa, rg_ba, rg_wx, rg_bx, rg_lambda,
              w_rnn_out, q_norm_g, w_uq, kv_norm_g, w_ukv, w_attn_out, w_out, norm2_g,
              peer_wq, peer_keys1, peer_keys2, peer_u, peer_v, final_g):
    B, S, D = x.shape
    T = N_META + S
    Tp = -(-T // BLOCK) * BLOCK
    meta = jnp.broadcast_to(meta_tokens[None].astype(x.dtype), (B, N_META, D))
    h = jnp.concatenate([meta, x], axis=1)
    h = jnp.pad(h, ((0, 0), (0, Tp - T), (0, 0)))
    pos = jnp.arange(Tp, dtype=jnp.float32)
    for l in range(DEPTH):
        n1 = _rmsnorm(h, norm1_g[l])
        xr, gr, cq, ckv, kr, g_rnn, g_att = jnp.split(n1 @ w_in[l], IN_SPLITS, axis=-1)
        hr = _rg_lru(_causal_conv(xr, conv_w[l], conv_b[l]), rg_wa[l], rg_ba[l], rg_wx[l], rg_bx[l], rg_lambda[l])
        y_rnn = (hr * jax.nn.gelu(gr)) @ w_rnn_out[l]
        q = (_rmsnorm(cq, q_norm_g[l]) @ w_uq[l]).reshape(B, Tp, MLA_HEADS, QK_NOPE + QK_ROPE)
        kv = (_rmsnorm(ckv, kv_norm_g[l]) @ w_ukv[l]).reshape(B, Tp, MLA_HEADS, QK_NOPE + V_DIM)
        q_rope = _rope(q[..., QK_NOPE:], pos)
        k_rope = _rope(kr[:, :, None, :], pos)[:, :, 0, :]
        o = _mla_attention(q[..., :QK_NOPE], q_rope, kv[..., :QK_NOPE], k_rope, kv[..., QK_NOPE:])
        y_att = o @ w_attn_out[l]
        mixed = jax.nn.sigmoid(g_rnn) * y_rnn + jax.nn.sigmoid(g_att) * y_att
        h = h + mixed @ w_out[l]
        h = h + _peer(_rmsnorm(h, norm2_g[l]), peer_wq[l], peer_keys1[l], peer_keys2[l], peer_u[l], peer_v[l])
    h = _rmsnorm(h, final_g)
    return h[:, N_META:N_META + S]
```

```python
from contextlib import ExitStack
import numpy as np
import concourse.bass as bass
import concourse.mybir as mybir
from concourse.bass_utils import run_bass_kernel_spmd

F32 = mybir.dt.float32
BF16 = mybir.dt.bfloat16
I32 = mybir.dt.int32
U32 = mybir.dt.uint32
AF = mybir.ActivationFunctionType
ALU = mybir.AluOpType
AX = mybir.AxisListType

SEQC = 2176
NREAL = 2048
EPS = 1e-6
GROUPS = [(0, 128)] + [(128 + 512 * g, 512) for g in range(4)]


class T:
    def __init__(self, h, name, accum=False):
        self.h = h
        self.name = name
        self.w = {}
        self.r = {}
        self.accum = accum

    def __getitem__(self, k):
        return self.h[k]


class B:
    def __init__(self, nc):
        self.nc = nc
        self.E = {"pe": nc.tensor, "act": nc.scalar, "dve": nc.vector, "pool": nc.gpsimd, "sp": nc.sync}
        self.sems = {}
        self.cnt = {}
        self.seen = {k: {} for k in self.E}
        for k in self.E:
            self.sems[k] = nc.alloc_semaphore("s_" + k)
            self.cnt[k] = 0
        self.nins = 0

    def newsem(self, name):
        self.sems[name] = self.nc.alloc_semaphore("s_" + name)
        self.cnt[name] = 0
        return name

    def _wait(self, eng, evs):
        for key, val in evs.items():
            if val <= 0 or (eng == "pe" and key == "pe"):
                continue
            if self.seen[eng].get(key, 0) >= val:
                continue
            self.E[eng].wait_ge(self.sems[key], val)
            self.seen[eng][key] = val
            self.nins += 1

    @staticmethod
    def _merge(d, e):
        for k, v in e.items():
            if d.get(k, 0) < v:
                d[k] = v

    def _deps(self, reads, writes):
        evs = {}
        for t in reads:
            self._merge(evs, t.w)
        for t in writes:
            if not t.accum:
                self._merge(evs, t.w)
                self._merge(evs, t.r)
        return evs

    def _commit(self, ev, reads, writes):
        for t in reads:
            if not t.accum:
                self._merge(t.r, ev)
        for t in writes:
            if t.accum:
                self._merge(t.w, ev)
            else:
                t.w = dict(ev)
                t.r = {}

    def op(self, eng, fn, reads=(), writes=()):
        self._wait(eng, self._deps(reads, writes))
        ins = fn(self.E[eng])
        self.cnt[eng] += 1
        ins.then_inc(self.sems[eng], 1)
        self.nins += 1
        self._commit({eng: self.cnt[eng]}, reads, writes)

    def dma(self, q, sem, fn, reads=(), writes=()):
        own = [t for t in writes if not t.accum] or [t for t in reads if not t.accum]
        t0 = own[0]
        if getattr(t0, "sem", None) is None:
            t0.sem = self.newsem("d%d" % len(self.sems))
        sem = t0.sem
        self._wait(q, self._deps(reads, writes))
        ins = fn(self.E[q])
        self.cnt[sem] += 16
        ins.then_inc(self.sems[sem], 16)
        self.nins += 1
        self._commit({sem: self.cnt[sem]}, reads, writes)

    def barrier(self):
        allev = {k: v for k, v in self.cnt.items() if v > 0}
        for e in self.E:
            self._wait(e, allev)


def build(NSEQ, phases=(1, 2, 3, 4, 5), debug=False, p5tiles=None):
    nc = bass.Bass("TRN2", target_bir_lowering=False)
    b = B(nc)
    NT = NSEQ * SEQC
    NR = NSEQ * NREAL
    skind = "ExternalOutput" if debug else "Internal"

    def din(name, shape, dt=F32):
        return nc.dram_tensor(name, list(shape), dt, kind="ExternalInput")

    def dscr(name, shape, dt=F32):
        return T(nc.dram_tensor(name, list(shape), dt, kind=skind), name, accum=True)

    x_d = din("x", [NSEQ, NREAL, 1024])
    meta_d = din("meta", [16, 1024])
    g1_d = din("g1", [128, 8])
    win_d = din("w_in", [1024, 4992])
    cw_d = din("convw", [128, 8, 4])
    cb_d = din("convb", [128, 8])
    rgwa_d = din("rg_wa", [128, 8, 128])
    rgwx_d = din("rg_wx", [128, 8, 128])
    rgba_d = din("rg_ba", [128, 8])
    rgbx_d = din("rg_bx", [128, 8])
    lam_d = din("rg_lam", [128, 8])
    wro_d = din("w_rnn_out", [1024, 1024])
    qg_d = din("qg", [128, 3])
    kvg_d = din("kvg", [128, 2])
    wuq_d = din("w_uq", [384, 4096])
    wukv_d = din("w_ukv", [256, 2048])
    wao_d = din("w_attn_out", [1024, 1024])
    wo_d = din("w_out", [1024, 1024])
    g2_d = din("g2", [1, 1024])
    wq_d = din("peer_wq", [1024, 2048])
    k1T_d = din("keys1T", [128, 128])
    k2T_d = din("keys2T", [128, 128])
    pu_d = din("peer_u", [16384, 1024])
    pv_d = din("peer_v", [16384, 1024])
    gf_d = din("gf", [1, 1024])
    cos_d = din("cos2", [128, SEQC])
    sin_d = din("sin2s", [128, SEQC])
    out_d = nc.dram_tensor("out", [NSEQ, NREAL, 1024], F32, kind="ExternalOutput")
    OUT = T(out_d, "out", accum=True)

    S_xr = dscr("S_xr", [1024, NT])
    S_gr = dscr("S_gr", [1024, NT])
    S_cq = dscr("S_cq", [384, NT])
    S_ckv = dscr("S_ckv", [256, NT])
    S_kr = dscr("S_kr", [128, NT])
    S_krs = dscr("S_krs", [128, NT])
    S_grnn = dscr("S_grnn", [1024, NT])
    S_gatt = dscr("S_gatt", [1024, NT])
    S_mrnn = dscr("S_mrnn", [1024, NT])
    S_oT = dscr("S_oT", [1024, NT], BF16)
    S_h2 = dscr("S_h2", [NR, 1024])

    ld = b.newsem("ld")
    st = b.newsem("st")
    wl = b.newsem("wl")

    def mk(es, pre):
        def sb(name, shape, dt):
            return T(es.enter_context(nc.sbuf_tensor(pre + name, list(shape), dt)), name)

        def ps(name, shape, dt):
            return T(es.enter_context(nc.psum_tensor(pre + name, list(shape), dt)), name)
        return sb, ps

    def make_ident(sb):
        identf = sb("identf", [128, 128], F32)
        ident = sb("ident", [128, 128], BF16)
        b.op("pool", lambda e: e.memset(identf[:], 1.0), writes=[identf])
        b.op("pool", lambda e: e.affine_select(out=identf[:], in_=identf[:], pattern=[[-1, 128]],
                                               compare_op=ALU.is_equal, fill=0.0, base=0, channel_multiplier=1),
             reads=[identf], writes=[identf])
        b.op("dve", lambda e: e.tensor_copy(out=ident[:], in_=identf[:]), reads=[identf], writes=[ident])
        return ident, identf

    def load_small(sb, name, d, shape):
        t = sb(name, shape, F32)
        b.dma("sp", wl, lambda e: e.dma_start(out=t[:], in_=d.ap()), writes=[t])
        return t

    def load_w_cast(dst, dst_ap, src_ap):
        b.dma("pool", wl, lambda e: e.dma_start(out=dst_ap, in_=src_ap), writes=[dst])

    def phase1():
        with ExitStack() as es:
            sb, ps = mk(es, "p1_")
            W = sb("W1", [128, 8, 4992], BF16)
            g1 = load_small(sb, "g1s", g1_d, [128, 8])
            stg = [sb("wstg%d" % i, [128, 4992], F32) for i in range(2)]
            for kc in range(8):
                s_ = stg[kc % 2]
                b.dma("sp", wl, lambda e: e.dma_start(out=s_[:], in_=win_d.ap()[kc * 128:(kc + 1) * 128, :]), writes=[s_])
                b.op("dve", lambda e: e.tensor_scalar(out=W[:, kc, :], in0=s_[:], scalar1=g1[:, kc:kc + 1], scalar2=None,
                                                      op0=ALU.mult), reads=[s_, g1], writes=[W])
            ident, _ = make_ident(sb)
            xt = [sb("xt%d" % i, [128, 1024], F32) for i in range(2)]
            junk = sb("junk", [128, 1024], BF16)
            ss = [sb("ss%d" % i, [128, 1], F32) for i in range(2)]
            xb = [sb("xb%d" % i, [128, 1024], BF16) for i in range(2)]
            n1T = [sb("n1T%d" % i, [128, 8, 512], BF16) for i in range(2)]
            pT = [ps("pT%d" % i, [128, 1024], BF16) for i in range(2)]
            pa = [ps("pa%d" % i, [128, 512], F32) for i in range(4)]
            stage = [sb("stage%d" % i, [128, 512], F32) for i in range(4)]
            chunks = []
            for i in range(8):
                chunks.append((S_xr, i * 128, None, 128))
            for i in range(8):
                chunks.append((S_gr, i * 128, AF.Gelu_apprx_tanh, 128))
            for i in range(3):
                chunks.append((S_cq, i * 128, None, 128))
            for i in range(2):
                chunks.append((S_ckv, i * 128, None, 128))
            chunks.append((S_kr, 0, None, 128))
            chunks.append((S_krs, 0, None, 128))
            for i in range(8):
                chunks.append((S_grnn, i * 128, AF.Sigmoid, 128))
            for i in range(8):
                chunks.append((S_gatt, i * 128, AF.Sigmoid, 128))
            ti = 0
            gi = 0
            ci = 0
            for s in range(NSEQ):
                for (c0, n) in GROUPS:
                    nT = n1T[gi % 2]
                    gi += 1
                    for j in range(n // 128):
                        t = c0 // 128 + j
                        X = xt[ti % 2]
                        SS = ss[ti % 2]
                        XB = xb[ti % 2]
                        PT = pT[ti % 2]
                        ti += 1
                        if t == 0:
                            b.op("pool", lambda e: e.memset(X[:], 0.0), writes=[X])
                            b.dma("sp", ld, lambda e: e.dma_start(out=X[112:128, :], in_=meta_d.ap()), writes=[X])
                        else:
                            b.dma("sp", ld, lambda e: e.dma_start(out=X[:], in_=x_d.ap()[s, (t - 1) * 128:t * 128, :]), writes=[X])
                        b.op("act", lambda e: e.activation(out=junk[:], in_=X[:], func=AF.Square, accum_out=SS[:]),
                             reads=[X], writes=[junk, SS])
                        b.op("dve", lambda e: e.tensor_scalar(out=SS[:], in0=SS[:], scalar1=1.0 / 1024, scalar2=EPS,
                                                              op0=ALU.mult, op1=ALU.add), reads=[SS], writes=[SS])
                        b.op("act", lambda e: e.activation(out=SS[:], in_=SS[:], func=AF.Sqrt), reads=[SS], writes=[SS])
                        b.op("dve", lambda e: e.reciprocal(out=SS[:], in_=SS[:]), reads=[SS], writes=[SS])
                        b.op("act", lambda e: e.activation(out=XB[:], in_=X[:], func=AF.Copy, scale=SS[:]),
                             reads=[X, SS], writes=[XB])
                        for c in range(8):
                            b.op("pe", lambda e: e.transpose(out=PT[:, c * 128:(c + 1) * 128], in_=XB[:, c * 128:(c + 1) * 128],
                                                             identity=ident[:]), reads=[XB, ident], writes=[PT])
                        b.op("dve", lambda e: e.tensor_copy(out=nT[:, :, j * 128:(j + 1) * 128],
                                                            in_=PT[:].rearrange("p (c t) -> p c t", c=8)),
                             reads=[PT], writes=[nT])
                    for oc, (S_, r0, fn, m) in enumerate(chunks):
                        PA = pa[ci % 4]
                        SG = stage[ci % 4]
                        ci += 1
                        for kc in range(8):
                            b.op("pe", lambda e: e.matmul(PA[:, 0:n], lhsT=W[:, kc, oc * 128:(oc + 1) * 128], rhs=nT[:, kc, 0:n],
                                                          start=(kc == 0), stop=(kc == 7)), reads=[W, nT], writes=[PA])
                        if fn is None:
                            b.op("dve", lambda e: e.tensor_copy(out=SG[:, 0:n], in_=PA[:, 0:n]), reads=[PA], writes=[SG])
                        else:
                            b.op("act", lambda e: e.activation(out=SG[:, 0:n], in_=PA[:, 0:n], func=fn), reads=[PA], writes=[SG])
                        col = s * SEQC + c0
                        b.dma("pool", st, lambda e: e.dma_start(out=S_.h.ap()[r0:r0 + 128, col:col + n], in_=SG[:, 0:n]),
                              reads=[SG], writes=[S_])
        b.barrier()

    def phase2():
        with ExitStack() as es:
            sb, ps = mk(es, "p2_")
            WA = sb("WA", [128, 8, 128], BF16)
            WX = sb("WX", [128, 8, 128], BF16)
            WRO = sb("WRO", [128, 8, 1024], BF16)
            load_w_cast(WA, WA[:], rgwa_d.ap())
            load_w_cast(WX, WX[:], rgwx_d.ap())
            load_w_cast(WRO, WRO[:], wro_d.ap().rearrange("(k p) f -> p k f", p=128))
            cw = load_small(sb, "cw", cw_d, [128, 8, 4])
            cb = load_small(sb, "cb", cb_d, [128, 8])
            ba = load_small(sb, "ba", rgba_d, [128, 8])
            bx = load_small(sb, "bx", rgbx_d, [128, 8])
            lam = load_small(sb, "lam", lam_d, [128, 8])
            c8 = sb("c8", [128, 8], F32)
            c16 = sb("c16", [128, 8], F32)
            b.op("act", lambda e: e.activation(out=c8[:], in_=lam[:], func=AF.Exp, scale=-1.0), reads=[lam], writes=[c8])
            b.op("act", lambda e: e.activation(out=c8[:], in_=c8[:], func=AF.Ln, bias=1.0), reads=[c8], writes=[c8])
            b.op("dve", lambda e: e.tensor_scalar(out=c16[:], in0=c8[:], scalar1=-16.0, scalar2=None, op0=ALU.mult),
                 reads=[c8], writes=[c16])
            b.op("dve", lambda e: e.tensor_scalar(out=c8[:], in0=c8[:], scalar1=-8.0, scalar2=None, op0=ALU.mult),
                 reads=[c8, c16], writes=[c8])
            XR = sb("XR", [128, SEQC], F32)
            Y = sb("Y", [128, SEQC], F32)
            YB = sb("YB", [128, SEQC], BF16)
            A = sb("A", [128, SEQC], F32)
            U = sb("U", [128, SEQC], F32)
            H = sb("H", [128, SEQC], F32)
            GR = sb("GR", [128, SEQC], F32)
            ZT = sb("ZT", [128, 8, SEQC], BF16)
            tr = sb("tr", [128, 512], F32)
            ta2 = sb("ta2", [128, 512], F32)
            ti_ = sb("ti", [128, 512], F32)
            gs = [sb("gs%d" % i, [128, 512], F32) for i in range(2)]
            stage = [sb("stage%d" % i, [128, 512], F32) for i in range(2)]
            pA = ps("pA", [128, 512], F32)
            pX = ps("pX", [128, 512], F32)
            pO = [ps("pO%d" % i, [128, 512], F32) for i in range(2)]
            V0 = 112
            NV = SEQC - V0
            k = 0
            for s in range(NSEQ):
                sc = s * SEQC
                for n_ in range(8):
                    r0 = n_ * 128
                    b.dma("sp", ld, lambda e: e.dma_start(out=XR[:], in_=S_xr.h.ap()[r0:r0 + 128, sc:sc + SEQC]),
                          reads=[S_xr], writes=[XR])
                    b.dma("sp", ld, lambda e: e.dma_start(out=GR[:], in_=S_gr.h.ap()[r0:r0 + 128, sc:sc + SEQC]),
                          reads=[S_gr], writes=[GR])
                    b.op("dve", lambda e: e.tensor_scalar(out=Y[:, V0:SEQC], in0=XR[:, V0 - 3:SEQC - 3], scalar1=cw[:, n_, 0:1],
                                                          scalar2=cb[:, n_:n_ + 1], op0=ALU.mult, op1=ALU.add),
                         reads=[XR, cw, cb], writes=[Y])
                    for kk in range(1, 4):
                        b.op("dve", lambda e: e.scalar_tensor_tensor(out=Y[:, V0:SEQC], in0=XR[:, V0 - 3 + kk:SEQC - 3 + kk],
                                                                     scalar=cw[:, n_, kk:kk + 1], in1=Y[:, V0:SEQC],
                                                                     op0=ALU.mult, op1=ALU.add), reads=[XR, cw, Y], writes=[Y])
                    b.op("act", lambda e: e.activation(out=YB[:, V0:SEQC], in_=Y[:, V0:SEQC], func=AF.Copy), reads=[Y], writes=[YB])
                    for (c0, n) in GROUPS:
                        if c0 == 0:
                            c0, n = V0, 16
                        cs = slice(c0, c0 + n)
                        b.op("pe", lambda e: e.matmul(pA[:, 0:n], lhsT=WA[:, n_, :], rhs=YB[:, cs], start=True, stop=True),
                             reads=[WA, YB], writes=[pA])
                        b.op("pe", lambda e: e.matmul(pX[:, 0:n], lhsT=WX[:, n_, :], rhs=YB[:, cs], start=True, stop=True),
                             reads=[WX, YB], writes=[pX])
                        b.op("act", lambda e: e.activation(out=tr[:, 0:n], in_=pA[:, 0:n], func=AF.Sigmoid, bias=ba[:, n_:n_ + 1]),
                             reads=[pA, ba], writes=[tr])
                        b.op("act", lambda e: e.activation(out=ti_[:, 0:n], in_=pX[:, 0:n], func=AF.Sigmoid, bias=bx[:, n_:n_ + 1]),
                             reads=[pX, bx], writes=[ti_])
                        b.op("act", lambda e: e.activation(out=A[:, cs], in_=tr[:, 0:n], func=AF.Exp, scale=c8[:, n_:n_ + 1]),
                             reads=[tr, c8], writes=[A])
                        b.op("act", lambda e: e.activation(out=ta2[:, 0:n], in_=tr[:, 0:n], func=AF.Exp, scale=c16[:, n_:n_ + 1]),
                             reads=[tr, c16], writes=[ta2])
                        b.op("dve", lambda e: e.tensor_scalar(out=ta2[:, 0:n], in0=ta2[:, 0:n], scalar1=-1.0, scalar2=1.0,
                                                              op0=ALU.mult, op1=ALU.add), reads=[ta2], writes=[ta2])
                        b.op("act", lambda e: e.activation(out=ta2[:, 0:n], in_=ta2[:, 0:n], func=AF.Sqrt), reads=[ta2], writes=[ta2])
                        b.op("dve", lambda e: e.tensor_tensor(out=ti_[:, 0:n], in0=ti_[:, 0:n], in1=Y[:, cs], op=ALU.mult),
                             reads=[ti_, Y], writes=[ti_])
                        b.op("dve", lambda e: e.tensor_tensor(out=U[:, cs], in0=ti_[:, 0:n], in1=ta2[:, 0:n], op=ALU.mult),
                             reads=[ti_, ta2], writes=[U])
                    b.op("dve", lambda e: e.tensor_tensor_scan(out=H[:, V0:SEQC], data0=A[:, V0:SEQC], data1=U[:, V0:SEQC],
                                                               initial=0.0, op0=ALU.mult, op1=ALU.add), reads=[A, U], writes=[H])
                    b.op("dve", lambda e: e.tensor_tensor(out=ZT[:, n_, V0:SEQC], in0=H[:, V0:SEQC], in1=GR[:, V0:SEQC], op=ALU.mult),
                         reads=[H, GR], writes=[ZT])
                for (c0, n) in GROUPS[1:]:
                    cs = slice(c0, c0 + n)
                    for dc in range(8):
                        PO = pO[k % 2]
                        G = gs[k % 2]
                        SG = stage[k % 2]
                        k += 1
                        b.dma("sp", ld, lambda e: e.dma_start(out=G[:, 0:n], in_=S_grnn.h.ap()[dc * 128:(dc + 1) * 128, sc + c0:sc + c0 + n]),
                              reads=[S_grnn], writes=[G])
                        for kc in range(8):
                            b.op("pe", lambda e: e.matmul(PO[:, 0:n], lhsT=WRO[:, kc, dc * 128:(dc + 1) * 128], rhs=ZT[:, kc, cs],
                                                          start=(kc == 0), stop=(kc == 7)), reads=[WRO, ZT], writes=[PO])
                        b.op("dve", lambda e: e.tensor_tensor(out=SG[:, 0:n], in0=PO[:, 0:n], in1=G[:, 0:n], op=ALU.mult),
                             reads=[PO, G], writes=[SG])
                        b.dma("pool", st, lambda e: e.dma_start(out=S_mrnn.h.ap()[dc * 128:(dc + 1) * 128, sc + c0:sc + c0 + n],
                                                                in_=SG[:, 0:n]), reads=[SG], writes=[S_mrnn])
        b.barrier()


    def phase3a():
        with ExitStack() as es:
            sb, ps = mk(es, "p3_")
            WUQ = sb("WUQ", [128, 3, 3072], BF16)
            WUKV = sb("WUKV", [128, 2, 2048], BF16)
            qg = load_small(sb, "qg", qg_d, [128, 3])
            kvg = load_small(sb, "kvg", kvg_d, [128, 2])
            wst = sb("wst", [128, 3072], F32)
            for kc in range(3):
                b.dma("sp", wl, lambda e: e.dma_start(out=wst[:], in_=wuq_d.ap()[kc * 128:(kc + 1) * 128, 0:3072]), writes=[wst])
                b.op("dve", lambda e: e.tensor_scalar(out=WUQ[:, kc, :], in0=wst[:], scalar1=qg[:, kc:kc + 1], scalar2=None,
                                                      op0=ALU.mult), reads=[wst, qg], writes=[WUQ])
            for kc in range(2):
                b.dma("sp", wl, lambda e: e.dma_start(out=wst[:, 0:2048], in_=wukv_d.ap()[kc * 128:(kc + 1) * 128, :]), writes=[wst])
                b.op("dve", lambda e: e.tensor_scalar(out=WUKV[:, kc, :], in0=wst[:, 0:2048], scalar1=kvg[:, kc:kc + 1], scalar2=None,
                                                      op0=ALU.mult), reads=[wst, kvg], writes=[WUKV])
            ident, identf = make_ident(sb)
            ones = sb("ones", [128, 128], F32)
            b.op("pool", lambda e: e.memset(ones[:], 1.0), writes=[ones])
            tri = sb("tri", [128, 128], BF16)
            b.op("pool", lambda e: e.memset(identf[:], 1.0), reads=[identf], writes=[identf])
            b.op("pool", lambda e: e.affine_select(out=identf[:], in_=identf[:], pattern=[[1, 128]], compare_op=ALU.is_ge,
                                                   fill=0.0, base=0, channel_multiplier=-1), reads=[identf], writes=[identf])
            b.op("dve", lambda e: e.tensor_copy(out=tri[:], in_=identf[:]), reads=[identf], writes=[tri])
            Kn = sb("Kn", [128, 8, SEQC], BF16)
            Kr2 = sb("Kr2", [128, SEQC], BF16)
            V = sb("V", [128, 17, 16, 65], BF16)
            b.op("pool", lambda e: e.memset(V[:], 1.0), writes=[V])
            cq_t = sb("cq_t", [128, 3, 512], F32)
            ckv_t = sb("ckv_t", [128, 2, 512], F32)
            sq = sb("sq", [128, 3, 512], F32)
            rstd = sb("rstd", [128, 512], F32)
            cqn = sb("cqn", [128, 3, 512], BF16)
            ckvn = sb("ckvn", [128, 2, 512], BF16)
            kr_t = sb("kr_t", [128, 512], F32)
            krs_t = sb("krs_t", [128, 512], F32)
            cos_t = sb("cos_t", [128, 512], F32)
            sin_t = sb("sin_t", [128, 512], F32)
            tmp1 = sb("tmp1", [128, 512], F32)
            tmp2 = sb("tmp2", [128, 512], F32)
            Qn = sb("Qn", [128, 8, 512], BF16)
            Qr = sb("Qr", [128, 8, 512], BF16)
            PTs = [sb("PTs%d" % i, [128, 512], BF16) for i in range(3)]
            o_tm = sb("o_tm", [128, 4, 1024], BF16)
            oT = sb("oT", [128, 8, 512], BF16)
            rec = sb("rec", [128, 4], F32)
            pn = ps("pn", [128, 512], F32)
            pk = [ps("pk%d" % i, [128, 512], F32) for i in range(2)]
            pS = [ps("pS%d" % i, [128, 512], F32) for i in range(2)]
            pO = [ps("pO%d" % i, [128, 512], F32) for i in range(2)]
            pT = ps("pT", [128, 1024], BF16)
            scale = float(96 ** -0.5)
            ki = [0]

            def nextpk():
                ki[0] += 1
                return pk[ki[0] % 2]

            def rmsn(src, nch, dst, nfeat, n):
                b.op("act", lambda e: e.activation(out=sq[:, 0:nch, 0:n], in_=src[:, :, 0:n], func=AF.Square), reads=[src], writes=[sq])
                for c in range(nch):
                    b.op("pe", lambda e: e.matmul(pn[:, 0:n], lhsT=ones[:], rhs=sq[:, c, 0:n], start=(c == 0), stop=(c == nch - 1)),
                         reads=[ones, sq], writes=[pn])
                b.op("dve", lambda e: e.tensor_scalar(out=rstd[:, 0:n], in0=pn[:, 0:n], scalar1=1.0 / nfeat, scalar2=EPS,
                                                      op0=ALU.mult, op1=ALU.add), reads=[pn], writes=[rstd])
                b.op("act", lambda e: e.activation(out=rstd[:, 0:n], in_=rstd[:, 0:n], func=AF.Sqrt), reads=[rstd], writes=[rstd])
                b.op("dve", lambda e: e.reciprocal(out=rstd[:, 0:n], in_=rstd[:, 0:n]), reads=[rstd], writes=[rstd])
                for c in range(nch):
                    b.op("dve", lambda e: e.tensor_tensor(out=dst[:, c, 0:n], in0=src[:, c, 0:n], in1=rstd[:, 0:n], op=ALU.mult),
                         reads=[src, rstd], writes=[dst])

            si = 0
            for s in range(NSEQ):
                sc = s * SEQC
                for (c0, n) in GROUPS:
                    col = sc + c0
                    b.dma("sp", ld, lambda e: e.dma_start(out=cq_t[:, :, 0:n],
                                                          in_=S_cq.h.ap()[:, col:col + n].rearrange("(c p) n -> p c n", p=128)),
                          reads=[S_cq], writes=[cq_t])
                    b.dma("sp", ld, lambda e: e.dma_start(out=ckv_t[:, :, 0:n],
                                                          in_=S_ckv.h.ap()[:, col:col + n].rearrange("(c p) n -> p c n", p=128)),
                          reads=[S_ckv], writes=[ckv_t])
                    b.dma("sp", ld, lambda e: e.dma_start(out=kr_t[:, 0:n], in_=S_kr.h.ap()[:, col:col + n]), reads=[S_kr], writes=[kr_t])
                    b.dma("sp", ld, lambda e: e.dma_start(out=krs_t[:, 0:n], in_=S_krs.h.ap()[:, col:col + n]), reads=[S_krs], writes=[krs_t])
                    b.dma("sp", ld, lambda e: e.dma_start(out=cos_t[:, 0:n], in_=cos_d.ap()[:, c0:c0 + n]), writes=[cos_t])
                    b.dma("sp", ld, lambda e: e.dma_start(out=sin_t[:, 0:n], in_=sin_d.ap()[:, c0:c0 + n]), writes=[sin_t])
                    rmsn(cq_t, 3, cqn, 384.0, n)
                    rmsn(ckv_t, 2, ckvn, 256.0, n)
                    for j in range(8):
                        P_ = nextpk()
                        for kc in range(2):
                            b.op("pe", lambda e: e.matmul(P_[:, 0:n], lhsT=WUKV[:, kc, j * 128:(j + 1) * 128], rhs=ckvn[:, kc, 0:n],
                                                          start=(kc == 0), stop=(kc == 1)), reads=[WUKV, ckvn], writes=[P_])
                        b.op("act", lambda e: e.activation(out=Kn[:, j, c0:c0 + n], in_=P_[:, 0:n], func=AF.Copy), reads=[P_], writes=[Kn])
                    b.op("dve", lambda e: e.tensor_tensor(out=tmp1[:, 0:n], in0=kr_t[:, 0:n], in1=cos_t[:, 0:n], op=ALU.mult),
                         reads=[kr_t, cos_t], writes=[tmp1])
                    b.op("dve", lambda e: e.tensor_tensor(out=tmp2[:, 0:n], in0=krs_t[:, 0:n], in1=sin_t[:, 0:n], op=ALU.mult),
                         reads=[krs_t, sin_t], writes=[tmp2])
                    b.op("dve", lambda e: e.tensor_tensor(out=Kr2[:, c0:c0 + n], in0=tmp1[:, 0:n], in1=tmp2[:, 0:n], op=ALU.add),
                         reads=[tmp1, tmp2], writes=[Kr2])
                    for jt in range(n // 128):
                        t = c0 // 128 + jt
                        if t == 0:
                            lo, M = 112, 16
                        else:
                            lo, M = jt * 128, 128
                        for half in range(2):
                            P_ = nextpk()
                            for kc in range(2):
                                b.op("pe", lambda e: e.matmul(P_[0:M, :], lhsT=ckvn[:, kc, lo:lo + M],
                                                              rhs=WUKV[:, kc, 1024 + half * 512:1024 + (half + 1) * 512],
                                                              start=(kc == 0), stop=(kc == 1)), reads=[WUKV, ckvn], writes=[P_])
                            b.op("act", lambda e: e.activation(out=V[0:M, t, half * 8:(half + 1) * 8, 0:64],
                                                               in_=P_[0:M, :].rearrange("p (h d) -> p h d", h=8), func=AF.Copy),
                                 reads=[P_], writes=[V])
                    if c0 == 0:
                        continue
                    for j in range(8):
                        P_ = nextpk()
                        for kc in range(3):
                            b.op("pe", lambda e: e.matmul(P_[:, 0:n], lhsT=WUQ[:, kc, j * 128:(j + 1) * 128], rhs=cqn[:, kc, 0:n],
                                                          start=(kc == 0), stop=(kc == 2)), reads=[WUQ, cqn], writes=[P_])
                        b.op("act", lambda e: e.activation(out=Qn[:, j, 0:n], in_=P_[:, 0:n], func=AF.Copy), reads=[P_], writes=[Qn])
                    for j in range(8):
                        P1 = nextpk()
                        for kc in range(3):
                            b.op("pe", lambda e: e.matmul(P1[:, 0:n], lhsT=WUQ[:, kc, 1024 + j * 128:1024 + (j + 1) * 128], rhs=cqn[:, kc, 0:n],
                                                          start=(kc == 0), stop=(kc == 2)), reads=[WUQ, cqn], writes=[P1])
                        b.op("dve", lambda e: e.tensor_tensor(out=tmp1[:, 0:n], in0=P1[:, 0:n], in1=cos_t[:, 0:n], op=ALU.mult),
                             reads=[P1, cos_t], writes=[tmp1])
                        P2 = nextpk()
                        for kc in range(3):
                            b.op("pe", lambda e: e.matmul(P2[:, 0:n], lhsT=WUQ[:, kc, 2048 + j * 128:2048 + (j + 1) * 128], rhs=cqn[:, kc, 0:n],
                                                          start=(kc == 0), stop=(kc == 2)), reads=[WUQ, cqn], writes=[P2])
                        b.op("dve", lambda e: e.tensor_tensor(out=tmp2[:, 0:n], in0=P2[:, 0:n], in1=sin_t[:, 0:n], op=ALU.mult),
                             reads=[P2, sin_t], writes=[tmp2])
                        b.op("dve", lambda e: e.tensor_tensor(out=Qr[:, j, 0:n], in0=tmp1[:, 0:n], in1=tmp2[:, 0:n], op=ALU.add),
                             reads=[tmp1, tmp2], writes=[Qr])
                    g0t = c0 // 128
                    for h in range(16):
                        j = h // 2
                        p0 = (h % 2) * 64
                        PO = pO[h % 2]
                        POv = PO[:, 0:260].rearrange("p (q d) -> p q d", q=4)
                        for kt in range(0, g0t + 4):
                            if kt == 0:
                                k0, M = 112, 16
                            else:
                                k0, M = kt * 128, 128
                            qlo = max(0, kt - g0t)
                            q0 = qlo * 128
                            PS_ = pS[si % 2]
                            PTb = PTs[si % 3]
                            si += 1
                            b.op("pe", lambda e: e.matmul(PS_[0:M, q0:512], lhsT=Kn[p0:p0 + 64, j, k0:k0 + M], rhs=Qn[p0:p0 + 64, j, q0:512],
                                                          start=True, stop=False), reads=[Kn, Qn], writes=[PS_])
                            b.op("pe", lambda e: e.matmul(PS_[0:M, q0:512], lhsT=Kr2[p0:p0 + 32, k0:k0 + M], rhs=Qr[p0:p0 + 32, j, q0:512],
                                                          start=False, stop=True), reads=[Kr2, Qr], writes=[PS_])
                            b.op("act", lambda e: e.activation(out=PTb[0:M, q0:512], in_=PS_[0:M, q0:512], func=AF.Exp, scale=scale),
                                 reads=[PS_], writes=[PTb])
                            if kt >= g0t:
                                b.op("dve", lambda e: e.tensor_tensor(out=PTb[:, q0:q0 + 128], in0=PTb[:, q0:q0 + 128], in1=tri[:], op=ALU.mult),
                                     reads=[PTb, tri], writes=[PTb])
                            for qt in range(qlo, 4):
                                b.op("pe", lambda e: e.matmul(POv[:, qt, :], lhsT=PTb[0:M, qt * 128:(qt + 1) * 128], rhs=V[0:M, kt, h, :],
                                                              start=(kt == 0 and qt == 0), stop=(kt == g0t + qt), skip_group_check=True),
                                     reads=[PTb, V], writes=[PO])
                        b.op("dve", lambda e: e.reciprocal(out=rec[:], in_=POv[:, :, 64]), reads=[PO], writes=[rec])
                        for qt in range(4):
                            b.op("dve", lambda e: e.tensor_scalar(out=o_tm[:, qt, h * 64:(h + 1) * 64], in0=POv[:, qt, 0:64],
                                                                  scalar1=rec[:, qt:qt + 1], scalar2=None, op0=ALU.mult),
                                 reads=[PO, rec], writes=[o_tm])
                    for qt in range(4):
                        for c in range(8):
                            b.op("pe", lambda e: e.transpose(out=pT[:, c * 128:(c + 1) * 128], in_=o_tm[:, qt, c * 128:(c + 1) * 128],
                                                             identity=ident[:]), reads=[o_tm, ident], writes=[pT])
                        b.op("dve", lambda e: e.tensor_copy(out=oT[:, :, qt * 128:(qt + 1) * 128],
                                                            in_=pT[:].rearrange("p (c t) -> p c t", c=8)), reads=[pT], writes=[oT])
                    b.dma("pool", st, lambda e: e.dma_start(out=S_oT.h.ap()[:, col:col + n].rearrange("(c p) n -> p c n", p=128), in_=oT[:]),
                          reads=[oT], writes=[S_oT])
        b.barrier()

    def phase3b():
        with ExitStack() as es:
            sb, ps = mk(es, "p3b_")
            WAO = sb("WAO", [128, 8, 1024], BF16)
            WO = sb("WO", [128, 8, 1024], BF16)
            load_w_cast(WAO, WAO[:], wao_d.ap().rearrange("(k p) f -> p k f", p=128))
            load_w_cast(WO, WO[:], wo_d.ap().rearrange("(k p) f -> p k f", p=128))
            oT = [sb("oT%d" % i, [128, 8, 512], BF16) for i in range(2)]
            ga = [sb("ga%d" % i, [128, 512], F32) for i in range(2)]
            mr = [sb("mr%d" % i, [128, 512], F32) for i in range(2)]
            tmp = sb("tmp", [128, 512], F32)
            mixT = sb("mixT", [128, 8, 512], BF16)
            xt = [sb("xt%d" % i, [128, 1024], F32) for i in range(2)]
            h2 = [sb("h2%d" % i, [128, 1024], F32) for i in range(2)]
            pk = [ps("pk%d" % i, [128, 512], F32) for i in range(4)]
            gi = 0
            k = 0
            ti = 0
            for s in range(NSEQ):
                sc = s * SEQC
                for g, (c0, n) in enumerate(GROUPS):
                    if g == 0:
                        continue
                    col = sc + c0
                    OT = oT[gi % 2]
                    gi += 1
                    b.dma("sp", ld, lambda e: e.dma_start(out=OT[:], in_=S_oT.h.ap()[:, col:col + n].rearrange("(c p) n -> p c n", p=128)),
                          reads=[S_oT], writes=[OT])
                    for dc in range(8):
                        GA = ga[k % 2]
                        MR = mr[k % 2]
                        PK = pk[k % 4]
                        k += 1
                        b.dma("sp", ld, lambda e: e.dma_start(out=GA[:], in_=S_gatt.h.ap()[dc * 128:(dc + 1) * 128, col:col + n]),
                              reads=[S_gatt], writes=[GA])
                        b.dma("sp", ld, lambda e: e.dma_start(out=MR[:], in_=S_mrnn.h.ap()[dc * 128:(dc + 1) * 128, col:col + n]),
                              reads=[S_mrnn], writes=[MR])
                        for kc in range(8):
                            b.op("pe", lambda e: e.matmul(PK[:], lhsT=WAO[:, kc, dc * 128:(dc + 1) * 128], rhs=OT[:, kc, :],
                                                          start=(kc == 0), stop=(kc == 7)), reads=[WAO, OT], writes=[PK])
                        b.op("dve", lambda e: e.tensor_tensor(out=tmp[:], in0=PK[:], in1=GA[:], op=ALU.mult), reads=[PK, GA], writes=[tmp])
                        b.op("dve", lambda e: e.tensor_tensor(out=mixT[:, dc, :], in0=tmp[:], in1=MR[:], op=ALU.add),
                             reads=[tmp, MR], writes=[mixT])
                    for qt in range(4):
                        X = xt[ti % 2]
                        H2 = h2[ti % 2]
                        ti += 1
                        r0 = (g - 1) * 512 + qt * 128
                        b.dma("sp", ld, lambda e: e.dma_start(out=X[:], in_=x_d.ap()[s, r0:r0 + 128, :]), writes=[X])
                        for half in range(2):
                            PK = pk[k % 4]
                            k += 1
                            for kc in range(8):
                                b.op("pe", lambda e: e.matmul(PK[:], lhsT=mixT[:, kc, qt * 128:(qt + 1) * 128],
                                                              rhs=WO[:, kc, half * 512:(half + 1) * 512],
                                                              start=(kc == 0), stop=(kc == 7)), reads=[WO, mixT], writes=[PK])
                            b.op("dve", lambda e: e.tensor_tensor(out=H2[:, half * 512:(half + 1) * 512], in0=PK[:],
                                                                  in1=X[:, half * 512:(half + 1) * 512], op=ALU.add),
                                 reads=[PK, X], writes=[H2])
                        b.dma("pool", st, lambda e: e.dma_start(out=S_h2.h.ap()[s * NREAL + r0:s * NREAL + r0 + 128, :], in_=H2[:]),
                              reads=[H2], writes=[S_h2])
        b.barrier()


    def phase5():
        with ExitStack() as es:
            sb, ps = mk(es, "p5_")
            gq = b.newsem("gq")
            WQ = sb("WQ", [128, 8, 2048], BF16)
            K1T = sb("K1T", [128, 128], BF16)
            K2T = sb("K2T", [128, 128], BF16)
            load_w_cast(WQ, WQ[:], wq_d.ap().rearrange("(k p) f -> p k f", p=128))
            load_w_cast(K1T, K1T[:], k1T_d.ap())
            load_w_cast(K2T, K2T[:], k2T_d.ap())
            g2b = sb("g2b", [128, 1024], F32)
            gfb = sb("gfb", [128, 1024], F32)
            b.dma("sp", wl, lambda e: e.dma_start(out=g2b[:], in_=g2_d.ap().to_broadcast([128, 1024])), writes=[g2b])
            b.dma("sp", wl, lambda e: e.dma_start(out=gfb[:], in_=gf_d.ap().to_broadcast([128, 1024])), writes=[gfb])
            ident, identf = make_ident(sb)
            iota_i = sb("iota_i", [128, 16], I32)
            iota16 = sb("iota16", [128, 16], F32)
            b.op("pool", lambda e: e.iota(iota_i[:], pattern=[[1, 16]], base=0, channel_multiplier=0), writes=[iota_i])
            b.op("dve", lambda e: e.tensor_copy(out=iota16[:], in_=iota_i[:]), reads=[iota_i], writes=[iota16])
            L = sb("L", [128, 128, 128], BF16)
            b.op("pool", lambda e: e.memset(L[:], 0.0), writes=[L])
            Lflat = L[:].rearrange("p a b -> p (a b)")
            X = sb("X", [128, 1024], F32)
            ss = sb("ss", [128, 1], F32)
            junkb = sb("junkb", [128, 1024], BF16)
            xn = sb("xn", [128, 1024], F32)
            xnb = sb("xnb", [128, 1024], BF16)
            xT = sb("xT", [128, 8, 128], BF16)
            qT = sb("qT", [128, 16, 128], BF16)
            S = sb("S", [128, 16, 128], F32)
            S2 = sb("S2", [128, 256], F32)
            T16 = sb("T16", [128, 16, 16], F32)
            I16 = sb("I16", [128, 16, 16], U32)
            I16f = sb("I16f", [128, 16, 16], F32)
            cand = sb("cand", [128, 8, 256], F32)
            TS = sb("TS", [128, 8, 16], F32)
            CI = sb("CI", [128, 8, 16], U32)
            CIa = sb("CIa", [128, 8, 16], U32)
            CIb = sb("CIb", [128, 8, 16], U32)
            Af = sb("Af", [128, 8, 16], F32)
            Bf = sb("Bf", [128, 8, 16], F32)
            eq = sb("eq", [128, 8, 16, 16], F32)
            i1s = sb("i1s", [128, 128], F32)
            i2s = sb("i2s", [128, 128], F32)
            i1b = sb("i1b", [128, 128], BF16)
            i2b = sb("i2b", [128, 128], BF16)
            idxf = sb("idxf", [128, 128], F32)
            IDX = sb("IDX", [128, 128], U32)
            iTf = sb("iTf", [128, 2, 128], F32)
            IDXT = sb("IDXT", [128, 128], U32)
            E = sb("E", [128, 8, 16], F32)
            Z = sb("Z", [128, 8], F32)
            G = sb("G", [128, 8, 16], F32)
            ACTV = sb("ACTV", [128, 128], F32)
            coefb = sb("coefb", [128, 128], BF16)
            coefT = sb("coefT", [128, 128], BF16)
            UG = [sb("UG%d" % i, [128, 8, 1024], BF16) for i in range(2)]
            VG = [sb("VG%d" % i, [128, 8, 1024], BF16) for i in range(2)]
            h3 = sb("h3", [128, 1024], F32)
            ot = sb("ot", [128, 1024], F32)
            pT = ps("pT", [128, 1024], BF16)
            pq = [ps("pq%d" % i, [128, 512], F32) for i in range(2)]
            pOut = [ps("pOut%d" % i, [128, 512], F32) for i in range(2)]
            T16v = T16[:].rearrange("p (h two) a -> p h two a", two=2)
            I16fv = I16f[:].rearrange("p (h two) a -> p h two a", two=2)
            B4 = [128, 8, 16, 16]
            ui = 0
            vi = 0
            qi = 0
            for i in range(NR // 128 if p5tiles is None else p5tiles):
                s, r0 = divmod(i * 128, NREAL)
                b.dma("sp", ld, lambda e: e.dma_start(out=X[:], in_=S_h2.h.ap()[i * 128:(i + 1) * 128, :]), reads=[S_h2], writes=[X])
                b.op("act", lambda e: e.activation(out=junkb[:], in_=X[:], func=AF.Square, accum_out=ss[:]), reads=[X], writes=[junkb, ss])
                b.op("dve", lambda e: e.tensor_scalar(out=ss[:], in0=ss[:], scalar1=1.0 / 1024, scalar2=EPS, op0=ALU.mult, op1=ALU.add),
                     reads=[ss], writes=[ss])
                b.op("act", lambda e: e.activation(out=ss[:], in_=ss[:], func=AF.Sqrt), reads=[ss], writes=[ss])
                b.op("dve", lambda e: e.reciprocal(out=ss[:], in_=ss[:]), reads=[ss], writes=[ss])
                b.op("dve", lambda e: e.scalar_tensor_tensor(out=xn[:], in0=X[:], scalar=ss[:, 0:1], in1=g2b[:], op0=ALU.mult, op1=ALU.mult),
                     reads=[X, ss, g2b], writes=[xn])
                b.op("act", lambda e: e.activation(out=xnb[:], in_=xn[:], func=AF.Copy), reads=[xn], writes=[xnb])
                for c in range(8):
                    b.op("pe", lambda e: e.transpose(out=pT[:, c * 128:(c + 1) * 128], in_=xnb[:, c * 128:(c + 1) * 128], identity=ident[:]),
                         reads=[xnb, ident], writes=[pT])
                b.op("dve", lambda e: e.tensor_copy(out=xT[:], in_=pT[:].rearrange("p (c t) -> p c t", c=8)), reads=[pT], writes=[xT])
                for bq in range(4):
                    PQ = pq[qi % 2]
                    qi += 1
                    for j in range(4):
                        hh = bq * 4 + j
                        for kc in range(8):
                            b.op("pe", lambda e: e.matmul(PQ[:, j * 128:(j + 1) * 128], lhsT=WQ[:, kc, hh * 128:(hh + 1) * 128], rhs=xT[:, kc, :],
                                                          start=(kc == 0), stop=(kc == 7), skip_group_check=True), reads=[WQ, xT], writes=[PQ])
                    b.op("act", lambda e: e.activation(out=qT[:, bq * 4:(bq + 1) * 4, :], in_=PQ[:].rearrange("p (j t) -> p j t", j=4), func=AF.Copy),
                         reads=[PQ], writes=[qT])
                for bq in range(4):
                    PQ = pq[qi % 2]
                    qi += 1
                    for j in range(4):
                        hh = bq * 4 + j
                        KT = K1T if hh % 2 == 0 else K2T
                        b.op("pe", lambda e: e.matmul(PQ[:, j * 128:(j + 1) * 128], lhsT=qT[:, hh, :], rhs=KT[:], start=True, stop=True,
                                                      skip_group_check=True), reads=[qT, KT], writes=[PQ])
                    b.op("dve", lambda e: e.tensor_copy(out=S[:, bq * 4:(bq + 1) * 4, :], in_=PQ[:].rearrange("p (j t) -> p j t", j=4)),
                         reads=[PQ], writes=[S])

                def top16(vals, nv, tv, iv, g):
                    vals2 = S2
                    b.op("dve", lambda e: e.max(out=tv[:, g, 0:8], in_=vals[:, g, :]), reads=[vals], writes=[tv])
                    b.op("dve", lambda e: e.max_index(out=iv[:, g, 0:8], in_max=tv[:, g, 0:8], in_values=vals[:, g, :]), reads=[vals, tv], writes=[iv])
                    b.op("dve", lambda e: e.match_replace(out=vals2[:, 0:nv], in_to_replace=tv[:, g, 0:8], in_values=vals[:, g, :], imm_value=-1e30),
                         reads=[vals, tv], writes=[vals2])
                    b.op("dve", lambda e: e.max(out=tv[:, g, 8:16], in_=vals2[:, 0:nv]), reads=[vals2], writes=[tv])
                    b.op("dve", lambda e: e.max_index(out=iv[:, g, 8:16], in_max=tv[:, g, 8:16], in_values=vals2[:, 0:nv]), reads=[vals2, tv], writes=[iv])

                for hh in range(16):
                    top16(S, 128, T16, I16, hh)
                b.op("dve", lambda e: e.tensor_copy(out=I16f[:], in_=I16[:]), reads=[I16], writes=[I16f])
                b.op("dve", lambda e: e.tensor_tensor(out=cand[:].rearrange("p h (a c) -> p h a c", a=16),
                                                      in0=T16v[:, :, 0, :].unsqueeze(3).to_broadcast(B4),
                                                      in1=T16v[:, :, 1, :].unsqueeze(2).to_broadcast(B4), op=ALU.add),
                     reads=[T16], writes=[cand])
                for h in range(8):
                    top16(cand, 256, TS, CI, h)
                b.op("dve", lambda e: e.tensor_single_scalar(out=CIa[:], in_=CI[:], scalar=4, op=ALU.logical_shift_right), reads=[CI], writes=[CIa])
                b.op("dve", lambda e: e.tensor_single_scalar(out=CIb[:], in_=CI[:], scalar=15, op=ALU.bitwise_and), reads=[CI], writes=[CIb])
                b.op("dve", lambda e: e.tensor_copy(out=Af[:], in_=CIa[:]), reads=[CIa], writes=[Af])
                b.op("dve", lambda e: e.tensor_copy(out=Bf[:], in_=CIb[:]), reads=[CIb], writes=[Bf])
                for (SEL, half, dst) in ((Af, 0, i1s), (Bf, 1, i2s)):
                    b.op("dve", lambda e: e.tensor_tensor(out=eq[:], in0=SEL[:].unsqueeze(3).to_broadcast(B4),
                                                          in1=iota16[:].unsqueeze(1).unsqueeze(1).to_broadcast(B4), op=ALU.is_equal),
                         reads=[SEL, iota16], writes=[eq])
                    b.op("dve", lambda e: e.tensor_tensor(out=eq[:], in0=eq[:], in1=I16fv[:, :, half, :].unsqueeze(2).to_broadcast(B4), op=ALU.mult),
                         reads=[eq, I16f], writes=[eq])
                    b.op("dve", lambda e: e.tensor_reduce(out=dst[:].rearrange("p (h k) -> p h k", h=8), in_=eq[:], axis=AX.X, op=ALU.add),
                         reads=[eq], writes=[dst])
                b.op("dve", lambda e: e.scalar_tensor_tensor(out=idxf[:], in0=i1s[:], scalar=128.0, in1=i2s[:], op0=ALU.mult, op1=ALU.add),
                     reads=[i1s, i2s], writes=[idxf])
                b.op("dve", lambda e: e.tensor_copy(out=IDX[:], in_=idxf[:]), reads=[idxf], writes=[IDX])
                b.op("act", lambda e: e.activation(out=i1b[:], in_=i1s[:], func=AF.Copy), reads=[i1s], writes=[i1b])
                b.op("act", lambda e: e.activation(out=i2b[:], in_=i2s[:], func=AF.Copy), reads=[i2s], writes=[i2b])
                b.op("pe", lambda e: e.transpose(out=pT[:, 0:128], in_=i1b[:], identity=ident[:]), reads=[i1b, ident], writes=[pT])
                b.op("pe", lambda e: e.transpose(out=pT[:, 128:256], in_=i2b[:], identity=ident[:]), reads=[i2b, ident], writes=[pT])
                b.op("dve", lambda e: e.tensor_copy(out=iTf[:], in_=pT[:, 0:256].rearrange("p (a t) -> p a t", a=2)), reads=[pT], writes=[iTf])
                b.op("dve", lambda e: e.scalar_tensor_tensor(out=idxf[:], in0=iTf[:, 0, :], scalar=128.0, in1=iTf[:, 1, :], op0=ALU.mult, op1=ALU.add),
                     reads=[iTf, idxf], writes=[idxf])
                b.op("dve", lambda e: e.tensor_copy(out=IDXT[:], in_=idxf[:]), reads=[idxf], writes=[IDXT])
                b.op("dve", lambda e: e.tensor_tensor(out=E[:], in0=TS[:], in1=TS[:, :, 0:1].to_broadcast([128, 8, 16]), op=ALU.subtract),
                     reads=[TS], writes=[E])
                b.op("act", lambda e: e.activation(out=E[:], in_=E[:], func=AF.Exp), reads=[E], writes=[E])
                b.op("dve", lambda e: e.tensor_reduce(out=Z[:], in_=E[:], axis=AX.X, op=ALU.add), reads=[E], writes=[Z])
                b.op("dve", lambda e: e.reciprocal(out=Z[:], in_=Z[:]), reads=[Z], writes=[Z])
                b.op("dve", lambda e: e.tensor_tensor(out=G[:], in0=E[:], in1=Z[:].unsqueeze(2).to_broadcast([128, 8, 16]), op=ALU.mult),
                     reads=[E, Z], writes=[G])
                for sbi in range(16):
                    UGb = UG[ui % 2]
                    ui += 1
                    for q in range(8):
                        slot = sbi * 8 + q
                        b.dma("pool", gq, lambda e: e.indirect_dma_start(out=UGb[:, q, :], out_offset=None, in_=pu_d.ap(),
                                                                         in_offset=bass.IndirectOffsetOnAxis(ap=IDX[:, slot:slot + 1], axis=0)),
                              reads=[IDX], writes=[UGb])
                    for q in range(8):
                        slot = sbi * 8 + q
                        b.op("dve", lambda e: e.scalar_tensor_tensor(out=junkb[:], in0=UGb[:, q, :], scalar=1.0, in1=xnb[:], op0=ALU.mult,
                                                                     op1=ALU.mult, accum_out=ACTV[:, slot:slot + 1]),
                             reads=[UGb, xnb], writes=[junkb, ACTV])
                b.op("act", lambda e: e.activation(out=ACTV[:], in_=ACTV[:], func=AF.Gelu_apprx_tanh), reads=[ACTV], writes=[ACTV])
                b.op("dve", lambda e: e.tensor_tensor(out=coefb[:], in0=ACTV[:], in1=G[:].rearrange("p h k -> p (h k)"), op=ALU.mult),
                     reads=[ACTV, G], writes=[coefb])
                b.op("pe", lambda e: e.transpose(out=pT[:, 0:128], in_=coefb[:], identity=ident[:]), reads=[coefb, ident], writes=[pT])
                b.op("dve", lambda e: e.tensor_copy(out=coefT[:], in_=pT[:, 0:128]), reads=[pT], writes=[coefT])
                b.op("dve", lambda e: e.tensor_copy(out=Lflat[:, 0:16384:129], in_=coefT[:]), reads=[coefT], writes=[L])
                for tb in range(16):
                    VGb = VG[vi % 2]
                    vi += 1
                    for q in range(8):
                        t = tb * 8 + q
                        b.dma("pool", gq, lambda e: e.indirect_dma_start(out=VGb[:, q, :], out_offset=None, in_=pv_d.ap(),
                                                                         in_offset=bass.IndirectOffsetOnAxis(ap=IDXT[:, t:t + 1], axis=0)),
                              reads=[IDXT], writes=[VGb])
                    for q in range(8):
                        t = tb * 8 + q
                        for half in range(2):
                            b.op("pe", lambda e: e.matmul(pOut[half][:], lhsT=L[:, t, :], rhs=VGb[:, q, half * 512:(half + 1) * 512],
                                                          start=(t == 0), stop=(t == 127)), reads=[L, VGb], writes=[pOut[half]])
                for half in range(2):
                    b.op("dve", lambda e: e.tensor_tensor(out=h3[:, half * 512:(half + 1) * 512], in0=pOut[half][:],
                                                          in1=X[:, half * 512:(half + 1) * 512], op=ALU.add), reads=[pOut[half], X], writes=[h3])
                b.op("act", lambda e: e.activation(out=junkb[:], in_=h3[:], func=AF.Square, accum_out=ss[:]), reads=[h3], writes=[junkb, ss])
                b.op("dve", lambda e: e.tensor_scalar(out=ss[:], in0=ss[:], scalar1=1.0 / 1024, scalar2=EPS, op0=ALU.mult, op1=ALU.add),
                     reads=[ss], writes=[ss])
                b.op("act", lambda e: e.activation(out=ss[:], in_=ss[:], func=AF.Sqrt), reads=[ss], writes=[ss])
                b.op("dve", lambda e: e.reciprocal(out=ss[:], in_=ss[:]), reads=[ss], writes=[ss])
                b.op("dve", lambda e: e.scalar_tensor_tensor(out=ot[:], in0=h3[:], scalar=ss[:, 0:1], in1=gfb[:], op0=ALU.mult, op1=ALU.mult),
                     reads=[h3, ss, gfb], writes=[ot])
                b.dma("sp", st, lambda e: e.dma_start(out=out_d.ap()[s, r0:r0 + 128, :], in_=ot[:]), reads=[ot], writes=[OUT])
        b.barrier()

    progs = {1: phase1, 2: phase2, 3: phase3a, 4: phase3b, 5: phase5}
    for p in phases:
        if p in progs:
            progs[p]()
    b.barrier()
    return nc


def _pc(v, nchunk):
    return np.ascontiguousarray(np.asarray(v, np.float32).reshape(nchunk, 128).T)


def prep_common(inp):
    f = lambda a: np.asarray(a, np.float32)
    w_in = f(inp["w_in"])[0]
    z32 = np.zeros((1024, 32), np.float32)
    kr = w_in[:, 2688:2720]
    krs = np.concatenate([kr[:, 16:], kr[:, :16]], axis=1)
    w_in_r = np.concatenate([
        w_in[:, 0:1024], w_in[:, 1024:2048], w_in[:, 2048:2432], w_in[:, 2432:2688],
        kr, z32, kr, z32, krs, z32, krs, z32,
        w_in[:, 2720:3744], w_in[:, 3744:4768]], axis=1)
    assert w_in_r.shape == (1024, 4992)
    conv_w = f(inp["conv_w"])[0]
    cw = np.ascontiguousarray(conv_w.reshape(4, 8, 128).transpose(2, 1, 0))
    w_uq = f(inp["w_uq"])[0].reshape(384, 16, 96)
    nope = w_uq[:, :, :64].reshape(384, 1024)
    rope = w_uq[:, :, 64:]
    ropes = np.concatenate([rope[:, :, 16:], rope[:, :, :16]], axis=2)
    z = np.zeros((384, 16, 32), np.float32)
    rope_p = np.concatenate([rope, z], axis=2).reshape(384, 1024)
    ropes_p = np.concatenate([ropes, z], axis=2).reshape(384, 1024)
    w_uq_r = np.concatenate([nope, rope_p, ropes_p, np.zeros((384, 1024), np.float32)], axis=1)
    w_ukv = f(inp["w_ukv"])[0].reshape(256, 16, 128)
    w_ukv_r = np.concatenate([w_ukv[:, :, :64].reshape(256, 1024), w_ukv[:, :, 64:].reshape(256, 1024)], axis=1)
    pos = (np.arange(SEQC, dtype=np.float32) - 112.0).astype(np.float32)
    inv = np.power(np.float32(10000.0), -np.arange(16, dtype=np.float32) * np.float32(2.0 / 32)).astype(np.float32)
    ang = (pos[None, :] * inv[:, None]).astype(np.float32)
    c, s_ = np.cos(ang).astype(np.float32), np.sin(ang).astype(np.float32)
    cos32 = np.concatenate([c, c], axis=0)
    sin32 = np.concatenate([-s_, s_], axis=0)
    zz = np.zeros((32, SEQC), np.float32)
    cos2 = np.concatenate([cos32, zz, cos32, zz], axis=0)
    sin2s = np.concatenate([sin32, zz, sin32, zz], axis=0)
    return {
        "meta": f(inp["meta_tokens"]),
        "g1": _pc(inp["norm1_g"][0], 8),
        "w_in": np.ascontiguousarray(w_in_r),
        "convw": cw,
        "convb": _pc(inp["conv_b"][0], 8),
        "rg_wa": np.ascontiguousarray(f(inp["rg_wa"])[0].transpose(1, 0, 2)),
        "rg_wx": np.ascontiguousarray(f(inp["rg_wx"])[0].transpose(1, 0, 2)),
        "rg_ba": _pc(inp["rg_ba"][0], 8),
        "rg_bx": _pc(inp["rg_bx"][0], 8),
        "rg_lam": _pc(inp["rg_lambda"][0], 8),
        "w_rnn_out": f(inp["w_rnn_out"])[0],
        "qg": _pc(inp["q_norm_g"][0], 3),
        "kvg": _pc(inp["kv_norm_g"][0], 2),
        "w_uq": np.ascontiguousarray(w_uq_r),
        "w_ukv": np.ascontiguousarray(w_ukv_r),
        "w_attn_out": f(inp["w_attn_out"])[0],
        "w_out": f(inp["w_out"])[0],
        "g2": f(inp["norm2_g"]).reshape(1, 1024),
        "peer_wq": f(inp["peer_wq"])[0],
        "keys1T": np.ascontiguousarray(f(inp["peer_keys1"])[0].T),
        "keys2T": np.ascontiguousarray(f(inp["peer_keys2"])[0].T),
        "peer_u": f(inp["peer_u"])[0],
        "peer_v": f(inp["peer_v"])[0],
        "gf": f(inp["final_g"]).reshape(1, 1024),
        "cos2": cos2,
        "sin2s": sin2s,
    }


def kernel(**inputs):
    NC = 8
    NSEQ = 4
    common = prep_common(inputs)
    x = np.asarray(inputs["x"], np.float32)
    nc = build(NSEQ)
    in_maps = []
    for c in range(NC):
        m = dict(common)
        m["x"] = np.ascontiguousarray(x[c * NSEQ:(c + 1) * NSEQ])
        in_maps.append(m)
    res = run_bass_kernel_spmd(nc, in_maps, core_ids=list(range(NC)))
    return np.concatenate([r["out"] for r in res.results], axis=0)
```

```python
from contextlib import ExitStack
import numpy as np
import concourse.bass as bass
import concourse.mybir as mybir
from concourse.bass_utils import run_bass_kernel_spmd

F32 = mybir.dt.float32
BF16 = mybir.dt.bfloat16
I32 = mybir.dt.int32
U32 = mybir.dt.uint32
AF = mybir.ActivationFunctionType
ALU = mybir.AluOpType
AX = mybir.AxisListType

SEQC = 2176
NREAL = 2048
EPS = 1e-6
GROUPS = [(0, 128)] + [(128 + 512 * g, 512) for g in range(4)]


class T:
    def __init__(self, h, name, accum=False):
        self.h = h
        self.name = name
        self.w = {}
        self.r = {}
        self.accum = accum

    def __getitem__(self, k):
        return self.h[k]


class B:
    def __init__(self, nc):
        self.nc = nc
        self.E = {"pe": nc.tensor, "act": nc.scalar, "dve": nc.vector, "pool": nc.gpsimd, "sp": nc.sync}
        self.sems = {}
        self.cnt = {}
        self.seen = {k: {} for k in self.E}
        for k in self.E:
            self.sems[k] = nc.alloc_semaphore("s_" + k)
            self.cnt[k] = 0
        self.nins = 0

    def newsem(self, name):
        self.sems[name] = self.nc.alloc_semaphore("s_" + name)
        self.cnt[name] = 0
        return name

    def _wait(self, eng, evs):
        for key, val in evs.items():
            if val <= 0 or (eng == "pe" and key == "pe"):
                continue
            if self.seen[eng].get(key, 0) >= val:
                continue
            self.E[eng].wait_ge(self.sems[key], val)
            self.seen[eng][key] = val
            self.nins += 1

    @staticmethod
    def _merge(d, e):
        for k, v in e.items():
            if d.get(k, 0) < v:
                d[k] = v

    def _deps(self, reads, writes):
        evs = {}
        for t in reads:
            self._merge(evs, t.w)
        for t in writes:
            if not t.accum:
                self._merge(evs, t.w)
                self._merge(evs, t.r)
        return evs

    def _commit(self, ev, reads, writes):
        for t in reads:
            if not t.accum:
                self._merge(t.r, ev)
        for t in writes:
            if t.accum:
                self._merge(t.w, ev)
            else:
                t.w = dict(ev)
                t.r = {}

    def op(self, eng, fn, reads=(), writes=()):
        self._wait(eng, self._deps(reads, writes))
        ins = fn(self.E[eng])
        self.cnt[eng] += 1
        ins.then_inc(self.sems[eng], 1)
        self.nins += 1
        self._commit({eng: self.cnt[eng]}, reads, writes)

    def dma(self, q, sem, fn, reads=(), writes=()):
        own = [t for t in writes if not t.accum] or [t for t in reads if not t.accum]
        t0 = own[0]
        if getattr(t0, "sem", None) is None:
            t0.sem = self.newsem("d%d" % len(self.sems))
        sem = t0.sem
        deps = self._deps(reads, writes)
        if writes and not writes[0].accum:
            wr = {}
            for t in writes:
                self._merge(wr, t.r)
            if deps.get(sem, 0) > wr.get(sem, 0):
                v = wr.get(sem, 0)
                if v > 0:
                    deps[sem] = v
                else:
                    deps.pop(sem)
        self._wait(q, deps)
        ins = fn(self.E[q])
        self.cnt[sem] += 16
        ins.then_inc(self.sems[sem], 16)
        self.nins += 1
        self._commit({sem: self.cnt[sem]}, reads, writes)

    def barrier(self):
        allev = {k: v for k, v in self.cnt.items() if v > 0}
        for e in self.E:
            self._wait(e, allev)


def build(NSEQ, phases=(1, 2, 3, 4, 5), debug=False, p5tiles=None):
    nc = bass.Bass("TRN2", target_bir_lowering=False)
    b = B(nc)
    NT = NSEQ * SEQC
    NR = NSEQ * NREAL
    skind = "ExternalOutput" if debug else "Internal"

    def din(name, shape, dt=F32):
        return nc.dram_tensor(name, list(shape), dt, kind="ExternalInput")

    def dscr(name, shape, dt=F32):
        return T(nc.dram_tensor(name, list(shape), dt, kind=skind), name, accum=True)

    x_d = din("x", [NSEQ, NREAL, 1024])
    meta_d = din("meta", [16, 1024])
    g1_d = din("g1", [128, 8])
    win_d = din("w_in", [1024, 4992])
    cw_d = din("convw", [128, 8, 4])
    cb_d = din("convb", [128, 8])
    rgwa_d = din("rg_wa", [128, 8, 128])
    rgwx_d = din("rg_wx", [128, 8, 128])
    rgba_d = din("rg_ba", [128, 8])
    rgbx_d = din("rg_bx", [128, 8])
    lam_d = din("rg_lam", [128, 8])
    wro_d = din("w_rnn_out", [1024, 1024])
    qg_d = din("qg", [128, 3])
    kvg_d = din("kvg", [128, 2])
    wuq_d = din("w_uq", [384, 4096])
    wukv_d = din("w_ukv", [256, 2048])
    wao_d = din("w_attn_out", [1024, 1024])
    wo_d = din("w_out", [1024, 1024])
    g2_d = din("g2", [1, 1024])
    wq_d = din("peer_wq", [1024, 2048])
    k1T_d = din("keys1T", [128, 128])
    k2T_d = din("keys2T", [128, 128])
    pu_d = din("peer_u", [16384, 1024])
    pv_d = din("peer_v", [16384, 1024])
    gf_d = din("gf", [1, 1024])
    cos_d = din("cos2", [128, SEQC])
    sin_d = din("sin2s", [128, SEQC])
    out_d = nc.dram_tensor("out", [NSEQ, NREAL, 1024], F32, kind="ExternalOutput")
    OUT = T(out_d, "out", accum=True)

    S_xr = dscr("S_xr", [1024, NT])
    S_gr = dscr("S_gr", [1024, NT])
    S_cq = dscr("S_cq", [384, NT])
    S_ckv = dscr("S_ckv", [256, NT])
    S_kr = dscr("S_kr", [128, NT])
    S_krs = dscr("S_krs", [128, NT])
    S_grnn = dscr("S_grnn", [1024, NT])
    S_gatt = dscr("S_gatt", [1024, NT])
    S_mrnn = dscr("S_mrnn", [1024, NT])
    S_oT = dscr("S_oT", [1024, NT], BF16)
    S_h2 = dscr("S_h2", [NR, 1024])

    ld = b.newsem("ld")
    st = b.newsem("st")
    wl = b.newsem("wl")

    def mk(es, pre):
        def sb(name, shape, dt):
            return T(es.enter_context(nc.sbuf_tensor(pre + name, list(shape), dt)), name)

        def ps(name, shape, dt):
            return T(es.enter_context(nc.psum_tensor(pre + name, list(shape), dt)), name)
        return sb, ps

    def make_ident(sb):
        identf = sb("identf", [128, 128], F32)
        ident = sb("ident", [128, 128], BF16)
        b.op("pool", lambda e: e.memset(identf[:], 1.0), writes=[identf])
        b.op("pool", lambda e: e.affine_select(out=identf[:], in_=identf[:], pattern=[[-1, 128]],
                                               compare_op=ALU.is_equal, fill=0.0, base=0, channel_multiplier=1),
             reads=[identf], writes=[identf])
        b.op("dve", lambda e: e.tensor_copy(out=ident[:], in_=identf[:]), reads=[identf], writes=[ident])
        return ident, identf

    def load_small(sb, name, d, shape):
        t = sb(name, shape, F32)
        b.dma("sp", wl, lambda e: e.dma_start(out=t[:], in_=d.ap()), writes=[t])
        return t

    def load_w_cast(dst, dst_ap, src_ap):
        b.dma("pool", wl, lambda e: e.dma_start(out=dst_ap, in_=src_ap), writes=[dst])

    def phase1():
        with ExitStack() as es:
            sb, ps = mk(es, "p1_")
            W = sb("W1", [128, 8, 4992], BF16)
            g1 = load_small(sb, "g1s", g1_d, [128, 8])
            stg = [sb("wstg%d" % i, [128, 4992], F32) for i in range(2)]
            for kc in range(8):
                s_ = stg[kc % 2]
                b.dma("sp", wl, lambda e: e.dma_start(out=s_[:], in_=win_d.ap()[kc * 128:(kc + 1) * 128, :]), writes=[s_])
                b.op("dve", lambda e: e.tensor_scalar(out=W[:, kc, :], in0=s_[:], scalar1=g1[:, kc:kc + 1], scalar2=None,
                                                      op0=ALU.mult), reads=[s_, g1], writes=[W])
            ident, _ = make_ident(sb)
            xt = [sb("xt%d" % i, [128, 1024], F32) for i in range(2)]
            junk = sb("junk", [128, 1024], BF16)
            ss = [sb("ss%d" % i, [128, 1], F32) for i in range(2)]
            xb = [sb("xb%d" % i, [128, 1024], BF16) for i in range(2)]
            n1T = [sb("n1T%d" % i, [128, 8, 512], BF16) for i in range(2)]
            pT = [ps("pT%d" % i, [128, 1024], BF16) for i in range(2)]
            pa = [ps("pa%d" % i, [128, 512], F32) for i in range(4)]
            stage = [sb("stage%d" % i, [128, 512], F32) for i in range(4)]
            chunks = []
            for i in range(8):
                chunks.append((S_xr, i * 128, None, 128))
            for i in range(8):
                chunks.append((S_gr, i * 128, AF.Gelu_apprx_tanh, 128))
            for i in range(3):
                chunks.append((S_cq, i * 128, None, 128))
            for i in range(2):
                chunks.append((S_ckv, i * 128, None, 128))
            chunks.append((S_kr, 0, None, 128))
            chunks.append((S_krs, 0, None, 128))
            for i in range(8):
                chunks.append((S_grnn, i * 128, AF.Sigmoid, 128))
            for i in range(8):
                chunks.append((S_gatt, i * 128, AF.Sigmoid, 128))
            ti = 0
            gi = 0
            ci = 0
            for s in range(NSEQ):
                for (c0, n) in GROUPS:
                    nT = n1T[gi % 2]
                    gi += 1
                    for j in range(n // 128):
                        t = c0 // 128 + j
                        X = xt[ti % 2]
                        SS = ss[ti % 2]
                        XB = xb[ti % 2]
                        PT = pT[ti % 2]
                        ti += 1
                        if t == 0:
                            b.op("pool", lambda e: e.memset(X[:], 0.0), writes=[X])
                            b.dma("sp", ld, lambda e: e.dma_start(out=X[112:128, :], in_=meta_d.ap()), writes=[X])
                        else:
                            b.dma("sp", ld, lambda e: e.dma_start(out=X[:], in_=x_d.ap()[s, (t - 1) * 128:t * 128, :]), writes=[X])
                        b.op("act", lambda e: e.activation(out=junk[:], in_=X[:], func=AF.Square, accum_out=SS[:]),
                             reads=[X], writes=[junk, SS])
                        b.op("dve", lambda e: e.tensor_scalar(out=SS[:], in0=SS[:], scalar1=1.0 / 1024, scalar2=EPS,
                                                              op0=ALU.mult, op1=ALU.add), reads=[SS], writes=[SS])
                        b.op("act", lambda e: e.activation(out=SS[:], in_=SS[:], func=AF.Sqrt), reads=[SS], writes=[SS])
                        b.op("dve", lambda e: e.reciprocal(out=SS[:], in_=SS[:]), reads=[SS], writes=[SS])
                        b.op("act", lambda e: e.activation(out=XB[:], in_=X[:], func=AF.Copy, scale=SS[:]),
                             reads=[X, SS], writes=[XB])
                        for c in range(8):
                            b.op("pe", lambda e: e.transpose(out=PT[:, c * 128:(c + 1) * 128], in_=XB[:, c * 128:(c + 1) * 128],
                                                             identity=ident[:]), reads=[XB, ident], writes=[PT])
                        b.op("dve", lambda e: e.tensor_copy(out=nT[:, :, j * 128:(j + 1) * 128],
                                                            in_=PT[:].rearrange("p (c t) -> p c t", c=8)),
                             reads=[PT], writes=[nT])
                    for oc, (S_, r0, fn, m) in enumerate(chunks):
                        PA = pa[ci % 4]
                        SG = stage[ci % 4]
                        ci += 1
                        for kc in range(8):
                            b.op("pe", lambda e: e.matmul(PA[:, 0:n], lhsT=W[:, kc, oc * 128:(oc + 1) * 128], rhs=nT[:, kc, 0:n],
                                                          start=(kc == 0), stop=(kc == 7)), reads=[W, nT], writes=[PA])
                        if fn is None:
                            b.op("dve", lambda e: e.tensor_copy(out=SG[:, 0:n], in_=PA[:, 0:n]), reads=[PA], writes=[SG])
                        else:
                            b.op("act", lambda e: e.activation(out=SG[:, 0:n], in_=PA[:, 0:n], func=fn), reads=[PA], writes=[SG])
                        col = s * SEQC + c0
                        b.dma("pool", st, lambda e: e.dma_start(out=S_.h.ap()[r0:r0 + 128, col:col + n], in_=SG[:, 0:n]),
                              reads=[SG], writes=[S_])
        b.barrier()

    def phase2():
        with ExitStack() as es:
            sb, ps = mk(es, "p2_")
            WA = sb("WA", [128, 8, 128], BF16)
            WX = sb("WX", [128, 8, 128], BF16)
            WRO = sb("WRO", [128, 8, 1024], BF16)
            load_w_cast(WA, WA[:], rgwa_d.ap())
            load_w_cast(WX, WX[:], rgwx_d.ap())
            load_w_cast(WRO, WRO[:], wro_d.ap().rearrange("(k p) f -> p k f", p=128))
            cw = load_small(sb, "cw", cw_d, [128, 8, 4])
            cb = load_small(sb, "cb", cb_d, [128, 8])
            ba = load_small(sb, "ba", rgba_d, [128, 8])
            bx = load_small(sb, "bx", rgbx_d, [128, 8])
            lam = load_small(sb, "lam", lam_d, [128, 8])
            c8 = sb("c8", [128, 8], F32)
            c16 = sb("c16", [128, 8], F32)
            b.op("act", lambda e: e.activation(out=c8[:], in_=lam[:], func=AF.Exp, scale=-1.0), reads=[lam], writes=[c8])
            b.op("act", lambda e: e.activation(out=c8[:], in_=c8[:], func=AF.Ln, bias=1.0), reads=[c8], writes=[c8])
            b.op("dve", lambda e: e.tensor_scalar(out=c16[:], in0=c8[:], scalar1=-16.0, scalar2=None, op0=ALU.mult),
                 reads=[c8], writes=[c16])
            b.op("dve", lambda e: e.tensor_scalar(out=c8[:], in0=c8[:], scalar1=-8.0, scalar2=None, op0=ALU.mult),
                 reads=[c8, c16], writes=[c8])
            XR = sb("XR", [128, SEQC], F32)
            Y = sb("Y", [128, SEQC], F32)
            YB = sb("YB", [128, SEQC], BF16)
            A = sb("A", [128, SEQC], F32)
            U = sb("U", [128, SEQC], F32)
            H = sb("H", [128, SEQC], F32)
            GR = sb("GR", [128, SEQC], F32)
            ZT = sb("ZT", [128, 8, SEQC], BF16)
            tr = sb("tr", [128, 512], F32)
            ta2 = sb("ta2", [128, 512], F32)
            ti_ = sb("ti", [128, 512], F32)
            gs = [sb("gs%d" % i, [128, 512], F32) for i in range(2)]
            stage = [sb("stage%d" % i, [128, 512], F32) for i in range(2)]
            pA = ps("pA", [128, 512], F32)
            pX = ps("pX", [128, 512], F32)
            pO = [ps("pO%d" % i, [128, 512], F32) for i in range(2)]
            V0 = 112
            NV = SEQC - V0
            k = 0
            for s in range(NSEQ):
                sc = s * SEQC
                for n_ in range(8):
                    r0 = n_ * 128
                    b.dma("sp", ld, lambda e: e.dma_start(out=XR[:], in_=S_xr.h.ap()[r0:r0 + 128, sc:sc + SEQC]),
                          reads=[S_xr], writes=[XR])
                    b.dma("sp", ld, lambda e: e.dma_start(out=GR[:], in_=S_gr.h.ap()[r0:r0 + 128, sc:sc + SEQC]),
                          reads=[S_gr], writes=[GR])
                    b.op("dve", lambda e: e.tensor_scalar(out=Y[:, V0:SEQC], in0=XR[:, V0 - 3:SEQC - 3], scalar1=cw[:, n_, 0:1],
                                                          scalar2=cb[:, n_:n_ + 1], op0=ALU.mult, op1=ALU.add),
                         reads=[XR, cw, cb], writes=[Y])
                    for kk in range(1, 4):
                        b.op("dve", lambda e: e.scalar_tensor_tensor(out=Y[:, V0:SEQC], in0=XR[:, V0 - 3 + kk:SEQC - 3 + kk],
                                                                     scalar=cw[:, n_, kk:kk + 1], in1=Y[:, V0:SEQC],
                                                                     op0=ALU.mult, op1=ALU.add), reads=[XR, cw, Y], writes=[Y])
                    b.op("act", lambda e: e.activation(out=YB[:, V0:SEQC], in_=Y[:, V0:SEQC], func=AF.Copy), reads=[Y], writes=[YB])
                    for (c0, n) in GROUPS:
                        if c0 == 0:
                            c0, n = V0, 16
                        cs = slice(c0, c0 + n)
                        b.op("pe", lambda e: e.matmul(pA[:, 0:n], lhsT=WA[:, n_, :], rhs=YB[:, cs], start=True, stop=True),
                             reads=[WA, YB], writes=[pA])
                        b.op("pe", lambda e: e.matmul(pX[:, 0:n], lhsT=WX[:, n_, :], rhs=YB[:, cs], start=True, stop=True),
                             reads=[WX, YB], writes=[pX])
                        b.op("act", lambda e: e.activation(out=tr[:, 0:n], in_=pA[:, 0:n], func=AF.Sigmoid, bias=ba[:, n_:n_ + 1]),
                             reads=[pA, ba], writes=[tr])
                        b.op("act", lambda e: e.activation(out=ti_[:, 0:n], in_=pX[:, 0:n], func=AF.Sigmoid, bias=bx[:, n_:n_ + 1]),
                             reads=[pX, bx], writes=[ti_])
                        b.op("act", lambda e: e.activation(out=A[:, cs], in_=tr[:, 0:n], func=AF.Exp, scale=c8[:, n_:n_ + 1]),
                             reads=[tr, c8], writes=[A])
                        b.op("act", lambda e: e.activation(out=ta2[:, 0:n], in_=tr[:, 0:n], func=AF.Exp, scale=c16[:, n_:n_ + 1]),
                             reads=[tr, c16], writes=[ta2])
                        b.op("dve", lambda e: e.tensor_scalar(out=ta2[:, 0:n], in0=ta2[:, 0:n], scalar1=-1.0, scalar2=1.0,
                                                              op0=ALU.mult, op1=ALU.add), reads=[ta2], writes=[ta2])
                        b.op("act", lambda e: e.activation(out=ta2[:, 0:n], in_=ta2[:, 0:n], func=AF.Sqrt), reads=[ta2], writes=[ta2])
                        b.op("dve", lambda e: e.tensor_tensor(out=ti_[:, 0:n], in0=ti_[:, 0:n], in1=Y[:, cs], op=ALU.mult),
                             reads=[ti_, Y], writes=[ti_])
                        b.op("dve", lambda e: e.tensor_tensor(out=U[:, cs], in0=ti_[:, 0:n], in1=ta2[:, 0:n], op=ALU.mult),
                             reads=[ti_, ta2], writes=[U])
                    b.op("dve", lambda e: e.tensor_tensor_scan(out=H[:, V0:SEQC], data0=A[:, V0:SEQC], data1=U[:, V0:SEQC],
                                                               initial=0.0, op0=ALU.mult, op1=ALU.add), reads=[A, U], writes=[H])
                    b.op("dve", lambda e: e.tensor_tensor(out=ZT[:, n_, V0:SEQC], in0=H[:, V0:SEQC], in1=GR[:, V0:SEQC], op=ALU.mult),
                         reads=[H, GR], writes=[ZT])
                for (c0, n) in GROUPS[1:]:
                    cs = slice(c0, c0 + n)
                    for dc in range(8):
                        PO = pO[k % 2]
                        G = gs[k % 2]
                        SG = stage[k % 2]
                        k += 1
                        b.dma("sp", ld, lambda e: e.dma_start(out=G[:, 0:n], in_=S_grnn.h.ap()[dc * 128:(dc + 1) * 128, sc + c0:sc + c0 + n]),
                              reads=[S_grnn], writes=[G])
                        for kc in range(8):
                            b.op("pe", lambda e: e.matmul(PO[:, 0:n], lhsT=WRO[:, kc, dc * 128:(dc + 1) * 128], rhs=ZT[:, kc, cs],
                                                          start=(kc == 0), stop=(kc == 7)), reads=[WRO, ZT], writes=[PO])
                        b.op("dve", lambda e: e.tensor_tensor(out=SG[:, 0:n], in0=PO[:, 0:n], in1=G[:, 0:n], op=ALU.mult),
                             reads=[PO, G], writes=[SG])
                        b.dma("pool", st, lambda e: e.dma_start(out=S_mrnn.h.ap()[dc * 128:(dc + 1) * 128, sc + c0:sc + c0 + n],
                                                                in_=SG[:, 0:n]), reads=[SG], writes=[S_mrnn])
        b.barrier()


    def phase3a():
        with ExitStack() as es:
            sb, ps = mk(es, "p3_")
            WUQ = sb("WUQ", [128, 3, 3072], BF16)
            WUKV = sb("WUKV", [128, 2, 2048], BF16)
            qg = load_small(sb, "qg", qg_d, [128, 3])
            kvg = load_small(sb, "kvg", kvg_d, [128, 2])
            wst = sb("wst", [128, 3072], F32)
            for kc in range(3):
                b.dma("sp", wl, lambda e: e.dma_start(out=wst[:], in_=wuq_d.ap()[kc * 128:(kc + 1) * 128, 0:3072]), writes=[wst])
                b.op("dve", lambda e: e.tensor_scalar(out=WUQ[:, kc, :], in0=wst[:], scalar1=qg[:, kc:kc + 1], scalar2=None,
                                                      op0=ALU.mult), reads=[wst, qg], writes=[WUQ])
            for kc in range(2):
                b.dma("sp", wl, lambda e: e.dma_start(out=wst[:, 0:2048], in_=wukv_d.ap()[kc * 128:(kc + 1) * 128, :]), writes=[wst])
                b.op("dve", lambda e: e.tensor_scalar(out=WUKV[:, kc, :], in0=wst[:, 0:2048], scalar1=kvg[:, kc:kc + 1], scalar2=None,
                                                      op0=ALU.mult), reads=[wst, kvg], writes=[WUKV])
            ident, identf = make_ident(sb)
            ones = sb("ones", [128, 128], F32)
            b.op("pool", lambda e: e.memset(ones[:], 1.0), writes=[ones])
            tri = sb("tri", [128, 128], BF16)
            b.op("pool", lambda e: e.memset(identf[:], 1.0), reads=[identf], writes=[identf])
            b.op("pool", lambda e: e.affine_select(out=identf[:], in_=identf[:], pattern=[[1, 128]], compare_op=ALU.is_ge,
                                                   fill=0.0, base=0, channel_multiplier=-1), reads=[identf], writes=[identf])
            b.op("dve", lambda e: e.tensor_copy(out=tri[:], in_=identf[:]), reads=[identf], writes=[tri])
            Kn = sb("Kn", [128, 8, SEQC], BF16)
            Kr2 = sb("Kr2", [128, SEQC], BF16)
            V = sb("V", [128, 17, 16, 65], BF16)
            b.op("pool", lambda e: e.memset(V[:], 1.0), writes=[V])
            cq_t = sb("cq_t", [128, 3, 512], F32)
            ckv_t = sb("ckv_t", [128, 2, 512], F32)
            sq = sb("sq", [128, 3, 512], F32)
            rstd = sb("rstd", [128, 512], F32)
            cqn = sb("cqn", [128, 3, 512], BF16)
            ckvn = sb("ckvn", [128, 2, 512], BF16)
            kr_t = sb("kr_t", [128, 512], F32)
            krs_t = sb("krs_t", [128, 512], F32)
            cos_t = sb("cos_t", [128, 512], F32)
            sin_t = sb("sin_t", [128, 512], F32)
            tmp1 = sb("tmp1", [128, 512], F32)
            tmp2 = sb("tmp2", [128, 512], F32)
            Qn = sb("Qn", [128, 8, 512], BF16)
            Qr = sb("Qr", [128, 8, 512], BF16)
            PTs = [sb("PTs%d" % i, [128, 512], BF16) for i in range(3)]
            o_tm = sb("o_tm", [128, 4, 1024], BF16)
            oT = sb("oT", [128, 8, 512], BF16)
            rec = sb("rec", [128, 4], F32)
            pn = ps("pn", [128, 512], F32)
            pk = [ps("pk%d" % i, [128, 512], F32) for i in range(2)]
            pS = [ps("pS%d" % i, [128, 512], F32) for i in range(2)]
            pO = [ps("pO%d" % i, [128, 512], F32) for i in range(2)]
            pT = ps("pT", [128, 1024], BF16)
            scale = float(96 ** -0.5)
            ki = [0]

            def nextpk():
                ki[0] += 1
                return pk[ki[0] % 2]

            def rmsn(src, nch, dst, nfeat, n):
                b.op("act", lambda e: e.activation(out=sq[:, 0:nch, 0:n], in_=src[:, :, 0:n], func=AF.Square), reads=[src], writes=[sq])
                for c in range(nch):
                    b.op("pe", lambda e: e.matmul(pn[:, 0:n], lhsT=ones[:], rhs=sq[:, c, 0:n], start=(c == 0), stop=(c == nch - 1)),
                         reads=[ones, sq], writes=[pn])
                b.op("dve", lambda e: e.tensor_scalar(out=rstd[:, 0:n], in0=pn[:, 0:n], scalar1=1.0 / nfeat, scalar2=EPS,
                                                      op0=ALU.mult, op1=ALU.add), reads=[pn], writes=[rstd])
                b.op("act", lambda e: e.activation(out=rstd[:, 0:n], in_=rstd[:, 0:n], func=AF.Sqrt), reads=[rstd], writes=[rstd])
                b.op("dve", lambda e: e.reciprocal(out=rstd[:, 0:n], in_=rstd[:, 0:n]), reads=[rstd], writes=[rstd])
                for c in range(nch):
                    b.op("dve", lambda e: e.tensor_tensor(out=dst[:, c, 0:n], in0=src[:, c, 0:n], in1=rstd[:, 0:n], op=ALU.mult),
                         reads=[src, rstd], writes=[dst])

            si = 0
            for s in range(NSEQ):
                sc = s * SEQC
                for (c0, n) in GROUPS:
                    col = sc + c0
                    b.dma("sp", ld, lambda e: e.dma_start(out=cq_t[:, :, 0:n],
                                                          in_=S_cq.h.ap()[:, col:col + n].rearrange("(c p) n -> p c n", p=128)),
                          reads=[S_cq], writes=[cq_t])
                    b.dma("sp", ld, lambda e: e.dma_start(out=ckv_t[:, :, 0:n],
                                                          in_=S_ckv.h.ap()[:, col:col + n].rearrange("(c p) n -> p c n", p=128)),
                          reads=[S_ckv], writes=[ckv_t])
                    b.dma("sp", ld, lambda e: e.dma_start(out=kr_t[:, 0:n], in_=S_kr.h.ap()[:, col:col + n]), reads=[S_kr], writes=[kr_t])
                    b.dma("sp", ld, lambda e: e.dma_start(out=krs_t[:, 0:n], in_=S_krs.h.ap()[:, col:col + n]), reads=[S_krs], writes=[krs_t])
                    b.dma("sp", ld, lambda e: e.dma_start(out=cos_t[:, 0:n], in_=cos_d.ap()[:, c0:c0 + n]), writes=[cos_t])
                    b.dma("sp", ld, lambda e: e.dma_start(out=sin_t[:, 0:n], in_=sin_d.ap()[:, c0:c0 + n]), writes=[sin_t])
                    rmsn(cq_t, 3, cqn, 384.0, n)
                    rmsn(ckv_t, 2, ckvn, 256.0, n)
                    for j in range(8):
                        P_ = nextpk()
                        for kc in range(2):
                            b.op("pe", lambda e: e.matmul(P_[:, 0:n], lhsT=WUKV[:, kc, j * 128:(j + 1) * 128], rhs=ckvn[:, kc, 0:n],
                                                          start=(kc == 0), stop=(kc == 1)), reads=[WUKV, ckvn], writes=[P_])
                        b.op("act", lambda e: e.activation(out=Kn[:, j, c0:c0 + n], in_=P_[:, 0:n], func=AF.Copy), reads=[P_], writes=[Kn])
                    b.op("dve", lambda e: e.tensor_tensor(out=tmp1[:, 0:n], in0=kr_t[:, 0:n], in1=cos_t[:, 0:n], op=ALU.mult),
                         reads=[kr_t, cos_t], writes=[tmp1])
                    b.op("dve", lambda e: e.tensor_tensor(out=tmp2[:, 0:n], in0=krs_t[:, 0:n], in1=sin_t[:, 0:n], op=ALU.mult),
                         reads=[krs_t, sin_t], writes=[tmp2])
                    b.op("dve", lambda e: e.tensor_tensor(out=Kr2[:, c0:c0 + n], in0=tmp1[:, 0:n], in1=tmp2[:, 0:n], op=ALU.add),
                         reads=[tmp1, tmp2], writes=[Kr2])
                    for jt in range(n // 128):
                        t = c0 // 128 + jt
                        if t == 0:
                            lo, M = 112, 16
                        else:
                            lo, M = jt * 128, 128
                        for half in range(2):
                            P_ = nextpk()
                            for kc in range(2):
                                b.op("pe", lambda e: e.matmul(P_[0:M, :], lhsT=ckvn[:, kc, lo:lo + M],
                                                              rhs=WUKV[:, kc, 1024 + half * 512:1024 + (half + 1) * 512],
                                                              start=(kc == 0), stop=(kc == 1)), reads=[WUKV, ckvn], writes=[P_])
                            b.op("act", lambda e: e.activation(out=V[0:M, t, half * 8:(half + 1) * 8, 0:64],
                                                               in_=P_[0:M, :].rearrange("p (h d) -> p h d", h=8), func=AF.Copy),
                                 reads=[P_], writes=[V])
                    if c0 == 0:
                        continue
                    for j in range(8):
                        P_ = nextpk()
                        for kc in range(3):
                            b.op("pe", lambda e: e.matmul(P_[:, 0:n], lhsT=WUQ[:, kc, j * 128:(j + 1) * 128], rhs=cqn[:, kc, 0:n],
                                                          start=(kc == 0), stop=(kc == 2)), reads=[WUQ, cqn], writes=[P_])
                        b.op("act", lambda e: e.activation(out=Qn[:, j, 0:n], in_=P_[:, 0:n], func=AF.Copy), reads=[P_], writes=[Qn])
                    for j in range(8):
                        P1 = nextpk()
                        for kc in range(3):
                            b.op("pe", lambda e: e.matmul(P1[:, 0:n], lhsT=WUQ[:, kc, 1024 + j * 128:1024 + (j + 1) * 128], rhs=cqn[:, kc, 0:n],
                                                          start=(kc == 0), stop=(kc == 2)), reads=[WUQ, cqn], writes=[P1])
                        b.op("dve", lambda e: e.tensor_tensor(out=tmp1[:, 0:n], in0=P1[:, 0:n], in1=cos_t[:, 0:n], op=ALU.mult),
                             reads=[P1, cos_t], writes=[tmp1])
                        P2 = nextpk()
                        for kc in range(3):
                            b.op("pe", lambda e: e.matmul(P2[:, 0:n], lhsT=WUQ[:, kc, 2048 + j * 128:2048 + (j + 1) * 128], rhs=cqn[:, kc, 0:n],
                                                          start=(kc == 0), stop=(kc == 2)), reads=[WUQ, cqn], writes=[P2])
                        b.op("dve", lambda e: e.tensor_tensor(out=tmp2[:, 0:n], in0=P2[:, 0:n], in1=sin_t[:, 0:n], op=ALU.mult),
                             reads=[P2, sin_t], writes=[tmp2])
                        b.op("dve", lambda e: e.tensor_tensor(out=Qr[:, j, 0:n], in0=tmp1[:, 0:n], in1=tmp2[:, 0:n], op=ALU.add),
                             reads=[tmp1, tmp2], writes=[Qr])
                    g0t = c0 // 128
                    for h in range(16):
                        j = h // 2
                        p0 = (h % 2) * 64
                        PO = pO[h % 2]
                        POv = PO[:, 0:260].rearrange("p (q d) -> p q d", q=4)
                        for kt in range(0, g0t + 4):
                            if kt == 0:
                                k0, M = 112, 16
                            else:
                                k0, M = kt * 128, 128
                            qlo = max(0, kt - g0t)
                            q0 = qlo * 128
                            PS_ = pS[si % 2]
                            PTb = PTs[si % 3]
                            si += 1
                            b.op("pe", lambda e: e.matmul(PS_[0:M, q0:512], lhsT=Kn[p0:p0 + 64, j, k0:k0 + M], rhs=Qn[p0:p0 + 64, j, q0:512],
                                                          start=True, stop=False), reads=[Kn, Qn], writes=[PS_])
                            b.op("pe", lambda e: e.matmul(PS_[0:M, q0:512], lhsT=Kr2[p0:p0 + 32, k0:k0 + M], rhs=Qr[p0:p0 + 32, j, q0:512],
                                                          start=False, stop=True), reads=[Kr2, Qr], writes=[PS_])
                            b.op("act", lambda e: e.activation(out=PTb[0:M, q0:512], in_=PS_[0:M, q0:512], func=AF.Exp, scale=scale),
                                 reads=[PS_], writes=[PTb])
                            if kt >= g0t:
                                b.op("dve", lambda e: e.tensor_tensor(out=PTb[:, q0:q0 + 128], in0=PTb[:, q0:q0 + 128], in1=tri[:], op=ALU.mult),
                                     reads=[PTb, tri], writes=[PTb])
                            for qt in range(qlo, 4):
                                b.op("pe", lambda e: e.matmul(POv[:, qt, :], lhsT=PTb[0:M, qt * 128:(qt + 1) * 128], rhs=V[0:M, kt, h, :],
                                                              start=(kt == 0 and qt == 0), stop=(kt == g0t + qt), skip_group_check=True),
                                     reads=[PTb, V], writes=[PO])
                        b.op("dve", lambda e: e.reciprocal(out=rec[:], in_=POv[:, :, 64]), reads=[PO], writes=[rec])
                        for qt in range(4):
                            b.op("dve", lambda e: e.tensor_scalar(out=o_tm[:, qt, h * 64:(h + 1) * 64], in0=POv[:, qt, 0:64],
                                                                  scalar1=rec[:, qt:qt + 1], scalar2=None, op0=ALU.mult),
                                 reads=[PO, rec], writes=[o_tm])
                    for qt in range(4):
                        for c in range(8):
                            b.op("pe", lambda e: e.transpose(out=pT[:, c * 128:(c + 1) * 128], in_=o_tm[:, qt, c * 128:(c + 1) * 128],
                                                             identity=ident[:]), reads=[o_tm, ident], writes=[pT])
                        b.op("dve", lambda e: e.tensor_copy(out=oT[:, :, qt * 128:(qt + 1) * 128],
                                                            in_=pT[:].rearrange("p (c t) -> p c t", c=8)), reads=[pT], writes=[oT])
                    b.dma("pool", st, lambda e: e.dma_start(out=S_oT.h.ap()[:, col:col + n].rearrange("(c p) n -> p c n", p=128), in_=oT[:]),
                          reads=[oT], writes=[S_oT])
        b.barrier()

    def phase3b():
        with ExitStack() as es:
            sb, ps = mk(es, "p3b_")
            WAO = sb("WAO", [128, 8, 1024], BF16)
            WO = sb("WO", [128, 8, 1024], BF16)
            load_w_cast(WAO, WAO[:], wao_d.ap().rearrange("(k p) f -> p k f", p=128))
            load_w_cast(WO, WO[:], wo_d.ap().rearrange("(k p) f -> p k f", p=128))
            oT = [sb("oT%d" % i, [128, 8, 512], BF16) for i in range(2)]
            ga = [sb("ga%d" % i, [128, 512], F32) for i in range(2)]
            mr = [sb("mr%d" % i, [128, 512], F32) for i in range(2)]
            tmp = sb("tmp", [128, 512], F32)
            mixT = sb("mixT", [128, 8, 512], BF16)
            xt = [sb("xt%d" % i, [128, 1024], F32) for i in range(2)]
            h2 = [sb("h2%d" % i, [128, 1024], F32) for i in range(2)]
            pk = [ps("pk%d" % i, [128, 512], F32) for i in range(4)]
            gi = 0
            k = 0
            ti = 0
            for s in range(NSEQ):
                sc = s * SEQC
                for g, (c0, n) in enumerate(GROUPS):
                    if g == 0:
                        continue
                    col = sc + c0
                    OT = oT[gi % 2]
                    gi += 1
                    b.dma("sp", ld, lambda e: e.dma_start(out=OT[:], in_=S_oT.h.ap()[:, col:col + n].rearrange("(c p) n -> p c n", p=128)),
                          reads=[S_oT], writes=[OT])
                    for dc in range(8):
                        GA = ga[k % 2]
                        MR = mr[k % 2]
                        PK = pk[k % 4]
                        k += 1
                        b.dma("sp", ld, lambda e: e.dma_start(out=GA[:], in_=S_gatt.h.ap()[dc * 128:(dc + 1) * 128, col:col + n]),
                              reads=[S_gatt], writes=[GA])
                        b.dma("sp", ld, lambda e: e.dma_start(out=MR[:], in_=S_mrnn.h.ap()[dc * 128:(dc + 1) * 128, col:col + n]),
                              reads=[S_mrnn], writes=[MR])
                        for kc in range(8):
                            b.op("pe", lambda e: e.matmul(PK[:], lhsT=WAO[:, kc, dc * 128:(dc + 1) * 128], rhs=OT[:, kc, :],
                                                          start=(kc == 0), stop=(kc == 7)), reads=[WAO, OT], writes=[PK])
                        b.op("dve", lambda e: e.tensor_tensor(out=tmp[:], in0=PK[:], in1=GA[:], op=ALU.mult), reads=[PK, GA], writes=[tmp])
                        b.op("dve", lambda e: e.tensor_tensor(out=mixT[:, dc, :], in0=tmp[:], in1=MR[:], op=ALU.add),
                             reads=[tmp, MR], writes=[mixT])
                    for qt in range(4):
                        X = xt[ti % 2]
                        H2 = h2[ti % 2]
                        ti += 1
                        r0 = (g - 1) * 512 + qt * 128
                        b.dma("sp", ld, lambda e: e.dma_start(out=X[:], in_=x_d.ap()[s, r0:r0 + 128, :]), writes=[X])
                        for half in range(2):
                            PK = pk[k % 4]
                            k += 1
                            for kc in range(8):
                                b.op("pe", lambda e: e.matmul(PK[:], lhsT=mixT[:, kc, qt * 128:(qt + 1) * 128],
                                                              rhs=WO[:, kc, half * 512:(half + 1) * 512],
                                                              start=(kc == 0), stop=(kc == 7)), reads=[WO, mixT], writes=[PK])
                            b.op("dve", lambda e: e.tensor_tensor(out=H2[:, half * 512:(half + 1) * 512], in0=PK[:],
                                                                  in1=X[:, half * 512:(half + 1) * 512], op=ALU.add),
                                 reads=[PK, X], writes=[H2])
                        b.dma("pool", st, lambda e: e.dma_start(out=S_h2.h.ap()[s * NREAL + r0:s * NREAL + r0 + 128, :], in_=H2[:]),
                              reads=[H2], writes=[S_h2])
        b.barrier()


    def phase5():
        with ExitStack() as es:
            sb, ps = mk(es, "p5_")
            gq = b.newsem("gq")
            WQ = sb("WQ", [128, 8, 2048], BF16)
            K1T = sb("K1T", [128, 128], BF16)
            K2T = sb("K2T", [128, 128], BF16)
            load_w_cast(WQ, WQ[:], wq_d.ap().rearrange("(k p) f -> p k f", p=128))
            load_w_cast(K1T, K1T[:], k1T_d.ap())
            load_w_cast(K2T, K2T[:], k2T_d.ap())
            g2b = sb("g2b", [128, 1024], F32)
            gfb = sb("gfb", [128, 1024], F32)
            b.dma("sp", wl, lambda e: e.dma_start(out=g2b[:], in_=g2_d.ap().to_broadcast([128, 1024])), writes=[g2b])
            b.dma("sp", wl, lambda e: e.dma_start(out=gfb[:], in_=gf_d.ap().to_broadcast([128, 1024])), writes=[gfb])
            ident, identf = make_ident(sb)
            iota_i = sb("iota_i", [128, 16], I32)
            iota16 = sb("iota16", [128, 16], F32)
            b.op("pool", lambda e: e.iota(iota_i[:], pattern=[[1, 16]], base=0, channel_multiplier=0), writes=[iota_i])
            b.op("dve", lambda e: e.tensor_copy(out=iota16[:], in_=iota_i[:]), reads=[iota_i], writes=[iota16])
            L = sb("L", [128, 128, 128], BF16)
            b.op("pool", lambda e: e.memset(L[:], 0.0), writes=[L])
            Lflat = L[:].rearrange("p a b -> p (a b)")
            X = sb("X", [128, 1024], F32)
            ss = sb("ss", [128, 1], F32)
            junkb = sb("junkb", [128, 1024], BF16)
            xn = sb("xn", [128, 1024], F32)
            xnb = sb("xnb", [128, 1024], BF16)
            xT = sb("xT", [128, 8, 128], BF16)
            qT = sb("qT", [128, 16, 128], BF16)
            S = sb("S", [128, 16, 128], F32)
            S2 = sb("S2", [128, 256], F32)
            T16 = sb("T16", [128, 16, 16], F32)
            I16 = sb("I16", [128, 16, 16], U32)
            I16f = sb("I16f", [128, 16, 16], F32)
            cand = sb("cand", [128, 8, 256], F32)
            TS = sb("TS", [128, 8, 16], F32)
            CI = sb("CI", [128, 8, 16], U32)
            CIa = sb("CIa", [128, 8, 16], U32)
            CIb = sb("CIb", [128, 8, 16], U32)
            Af = sb("Af", [128, 8, 16], F32)
            Bf = sb("Bf", [128, 8, 16], F32)
            eq = sb("eq", [128, 8, 16, 16], F32)
            i1s = sb("i1s", [128, 128], F32)
            i2s = sb("i2s", [128, 128], F32)
            i1b = sb("i1b", [128, 128], BF16)
            i2b = sb("i2b", [128, 128], BF16)
            idxf = sb("idxf", [128, 128], F32)
            IDX = sb("IDX", [128, 128], U32)
            iTf = sb("iTf", [128, 2, 128], F32)
            IDXT = sb("IDXT", [128, 128], U32)
            E = sb("E", [128, 8, 16], F32)
            Z = sb("Z", [128, 8], F32)
            G = sb("G", [128, 8, 16], F32)
            ACTV = sb("ACTV", [128, 128], F32)
            coefb = sb("coefb", [128, 128], BF16)
            coefT = sb("coefT", [128, 128], BF16)
            UG = [sb("UG%d" % i, [128, 8, 1024], BF16) for i in range(2)]
            VG = [sb("VG%d" % i, [128, 8, 1024], BF16) for i in range(2)]
            h3 = sb("h3", [128, 1024], F32)
            ot = sb("ot", [128, 1024], F32)
            pT = ps("pT", [128, 1024], BF16)
            pq = [ps("pq%d" % i, [128, 512], F32) for i in range(2)]
            pOut = [ps("pOut%d" % i, [128, 512], F32) for i in range(2)]
            T16v = T16[:].rearrange("p (h two) a -> p h two a", two=2)
            I16fv = I16f[:].rearrange("p (h two) a -> p h two a", two=2)
            B4 = [128, 8, 16, 16]
            ui = 0
            vi = 0
            qi = 0
            for i in range(NR // 128 if p5tiles is None else p5tiles):
                s, r0 = divmod(i * 128, NREAL)
                b.dma("sp", ld, lambda e: e.dma_start(out=X[:], in_=S_h2.h.ap()[i * 128:(i + 1) * 128, :]), reads=[S_h2], writes=[X])
                b.op("act", lambda e: e.activation(out=junkb[:], in_=X[:], func=AF.Square, accum_out=ss[:]), reads=[X], writes=[junkb, ss])
                b.op("dve", lambda e: e.tensor_scalar(out=ss[:], in0=ss[:], scalar1=1.0 / 1024, scalar2=EPS, op0=ALU.mult, op1=ALU.add),
                     reads=[ss], writes=[ss])
                b.op("act", lambda e: e.activation(out=ss[:], in_=ss[:], func=AF.Sqrt), reads=[ss], writes=[ss])
                b.op("dve", lambda e: e.reciprocal(out=ss[:], in_=ss[:]), reads=[ss], writes=[ss])
                b.op("dve", lambda e: e.scalar_tensor_tensor(out=xn[:], in0=X[:], scalar=ss[:, 0:1], in1=g2b[:], op0=ALU.mult, op1=ALU.mult),
                     reads=[X, ss, g2b], writes=[xn])
                b.op("act", lambda e: e.activation(out=xnb[:], in_=xn[:], func=AF.Copy), reads=[xn], writes=[xnb])
                for c in range(8):
                    b.op("pe", lambda e: e.transpose(out=pT[:, c * 128:(c + 1) * 128], in_=xnb[:, c * 128:(c + 1) * 128], identity=ident[:]),
                         reads=[xnb, ident], writes=[pT])
                b.op("dve", lambda e: e.tensor_copy(out=xT[:], in_=pT[:].rearrange("p (c t) -> p c t", c=8)), reads=[pT], writes=[xT])
                for bq in range(4):
                    PQ = pq[qi % 2]
                    qi += 1
                    for j in range(4):
                        hh = bq * 4 + j
                        for kc in range(8):
                            b.op("pe", lambda e: e.matmul(PQ[:, j * 128:(j + 1) * 128], lhsT=WQ[:, kc, hh * 128:(hh + 1) * 128], rhs=xT[:, kc, :],
                                                          start=(kc == 0), stop=(kc == 7), skip_group_check=True), reads=[WQ, xT], writes=[PQ])
                    b.op("act", lambda e: e.activation(out=qT[:, bq * 4:(bq + 1) * 4, :], in_=PQ[:].rearrange("p (j t) -> p j t", j=4), func=AF.Copy),
                         reads=[PQ], writes=[qT])
                for bq in range(4):
                    PQ = pq[qi % 2]
                    qi += 1
                    for j in range(4):
                        hh = bq * 4 + j
                        KT = K1T if hh % 2 == 0 else K2T
                        b.op("pe", lambda e: e.matmul(PQ[:, j * 128:(j + 1) * 128], lhsT=qT[:, hh, :], rhs=KT[:], start=True, stop=True,
                                                      skip_group_check=True), reads=[qT, KT], writes=[PQ])
                    b.op("dve", lambda e: e.tensor_copy(out=S[:, bq * 4:(bq + 1) * 4, :], in_=PQ[:].rearrange("p (j t) -> p j t", j=4)),
                         reads=[PQ], writes=[S])

                def top16(vals, nv, tv, iv, g):
                    vals2 = S2
                    b.op("dve", lambda e: e.max(out=tv[:, g, 0:8], in_=vals[:, g, :]), reads=[vals], writes=[tv])
                    b.op("dve", lambda e: e.max_index(out=iv[:, g, 0:8], in_max=tv[:, g, 0:8], in_values=vals[:, g, :]), reads=[vals, tv], writes=[iv])
                    b.op("dve", lambda e: e.match_replace(out=vals2[:, 0:nv], in_to_replace=tv[:, g, 0:8], in_values=vals[:, g, :], imm_value=-1e30),
                         reads=[vals, tv], writes=[vals2])
                    b.op("dve", lambda e: e.max(out=tv[:, g, 8:16], in_=vals2[:, 0:nv]), reads=[vals2], writes=[tv])
                    b.op("dve", lambda e: e.max_index(out=iv[:, g, 8:16], in_max=tv[:, g, 8:16], in_values=vals2[:, 0:nv]), reads=[vals2, tv], writes=[iv])

                for hh in range(16):
                    top16(S, 128, T16, I16, hh)
                b.op("dve", lambda e: e.tensor_copy(out=I16f[:], in_=I16[:]), reads=[I16], writes=[I16f])
                b.op("dve", lambda e: e.tensor_tensor(out=cand[:].rearrange("p h (a c) -> p h a c", a=16),
                                                      in0=T16v[:, :, 0, :].unsqueeze(3).to_broadcast(B4),
                                                      in1=T16v[:, :, 1, :].unsqueeze(2).to_broadcast(B4), op=ALU.add),
                     reads=[T16], writes=[cand])
                for h in range(8):
                    top16(cand, 256, TS, CI, h)
                b.op("dve", lambda e: e.tensor_single_scalar(out=CIa[:], in_=CI[:], scalar=4, op=ALU.logical_shift_right), reads=[CI], writes=[CIa])
                b.op("dve", lambda e: e.tensor_single_scalar(out=CIb[:], in_=CI[:], scalar=15, op=ALU.bitwise_and), reads=[CI], writes=[CIb])
                b.op("dve", lambda e: e.tensor_copy(out=Af[:], in_=CIa[:]), reads=[CIa], writes=[Af])
                b.op("dve", lambda e: e.tensor_copy(out=Bf[:], in_=CIb[:]), reads=[CIb], writes=[Bf])
                for (SEL, half, dst) in ((Af, 0, i1s), (Bf, 1, i2s)):
                    b.op("dve", lambda e: e.tensor_tensor(out=eq[:], in0=SEL[:].unsqueeze(3).to_broadcast(B4),
                                                          in1=iota16[:].unsqueeze(1).unsqueeze(1).to_broadcast(B4), op=ALU.is_equal),
                         reads=[SEL, iota16], writes=[eq])
                    b.op("dve", lambda e: e.tensor_tensor(out=eq[:], in0=eq[:], in1=I16fv[:, :, half, :].unsqueeze(2).to_broadcast(B4), op=ALU.mult),
                         reads=[eq, I16f], writes=[eq])
                    b.op("dve", lambda e: e.tensor_reduce(out=dst[:].rearrange("p (h k) -> p h k", h=8), in_=eq[:], axis=AX.X, op=ALU.add),
                         reads=[eq], writes=[dst])
                b.op("dve", lambda e: e.scalar_tensor_tensor(out=idxf[:], in0=i1s[:], scalar=128.0, in1=i2s[:], op0=ALU.mult, op1=ALU.add),
                     reads=[i1s, i2s], writes=[idxf])
                b.op("dve", lambda e: e.tensor_copy(out=IDX[:], in_=idxf[:]), reads=[idxf], writes=[IDX])
                b.op("act", lambda e: e.activation(out=i1b[:], in_=i1s[:], func=AF.Copy), reads=[i1s], writes=[i1b])
                b.op("act", lambda e: e.activation(out=i2b[:], in_=i2s[:], func=AF.Copy), reads=[i2s], writes=[i2b])
                b.op("pe", lambda e: e.transpose(out=pT[:, 0:128], in_=i1b[:], identity=ident[:]), reads=[i1b, ident], writes=[pT])
                b.op("pe", lambda e: e.transpose(out=pT[:, 128:256], in_=i2b[:], identity=ident[:]), reads=[i2b, ident], writes=[pT])
                b.op("dve", lambda e: e.tensor_copy(out=iTf[:], in_=pT[:, 0:256].rearrange("p (a t) -> p a t", a=2)), reads=[pT], writes=[iTf])
                b.op("dve", lambda e: e.scalar_tensor_tensor(out=idxf[:], in0=iTf[:, 0, :], scalar=128.0, in1=iTf[:, 1, :], op0=ALU.mult, op1=ALU.add),
                     reads=[iTf, idxf], writes=[idxf])
                b.op("dve", lambda e: e.tensor_copy(out=IDXT[:], in_=idxf[:]), reads=[idxf], writes=[IDXT])
                b.op("dve", lambda e: e.tensor_tensor(out=E[:], in0=TS[:], in1=TS[:, :, 0:1].to_broadcast([128, 8, 16]), op=ALU.subtract),
                     reads=[TS], writes=[E])
                b.op("act", lambda e: e.activation(out=E[:], in_=E[:], func=AF.Exp), reads=[E], writes=[E])
                b.op("dve", lambda e: e.tensor_reduce(out=Z[:], in_=E[:], axis=AX.X, op=ALU.add), reads=[E], writes=[Z])
                b.op("dve", lambda e: e.reciprocal(out=Z[:], in_=Z[:]), reads=[Z], writes=[Z])
                b.op("dve", lambda e: e.tensor_tensor(out=G[:], in0=E[:], in1=Z[:].unsqueeze(2).to_broadcast([128, 8, 16]), op=ALU.mult),
                     reads=[E, Z], writes=[G])
                for sbi in range(16):
                    UGb = UG[ui % 2]
                    ui += 1
                    for q in range(8):
                        slot = sbi * 8 + q
                        b.dma("pool", gq, lambda e: e.indirect_dma_start(out=UGb[:, q, :], out_offset=None, in_=pu_d.ap(),
                                                                         in_offset=bass.IndirectOffsetOnAxis(ap=IDX[:, slot:slot + 1], axis=0)),
                              reads=[IDX], writes=[UGb])
                    for q in range(8):
                        slot = sbi * 8 + q
                        b.op("dve", lambda e: e.scalar_tensor_tensor(out=junkb[:], in0=UGb[:, q, :], scalar=1.0, in1=xnb[:], op0=ALU.mult,
                                                                     op1=ALU.mult, accum_out=ACTV[:, slot:slot + 1]),
                             reads=[UGb, xnb], writes=[junkb, ACTV])
                b.op("act", lambda e: e.activation(out=ACTV[:], in_=ACTV[:], func=AF.Gelu_apprx_tanh), reads=[ACTV], writes=[ACTV])
                b.op("dve", lambda e: e.tensor_tensor(out=coefb[:], in0=ACTV[:], in1=G[:].rearrange("p h k -> p (h k)"), op=ALU.mult),
                     reads=[ACTV, G], writes=[coefb])
                b.op("pe", lambda e: e.transpose(out=pT[:, 0:128], in_=coefb[:], identity=ident[:]), reads=[coefb, ident], writes=[pT])
                b.op("dve", lambda e: e.tensor_copy(out=coefT[:], in_=pT[:, 0:128]), reads=[pT], writes=[coefT])
                b.op("dve", lambda e: e.tensor_copy(out=Lflat[:, 0:16384:129], in_=coefT[:]), reads=[coefT], writes=[L])
                for tb in range(16):
                    VGb = VG[vi % 2]
                    vi += 1
                    for q in range(8):
                        t = tb * 8 + q
                        b.dma("pool", gq, lambda e: e.indirect_dma_start(out=VGb[:, q, :], out_offset=None, in_=pv_d.ap(),
                                                                         in_offset=bass.IndirectOffsetOnAxis(ap=IDXT[:, t:t + 1], axis=0)),
                              reads=[IDXT], writes=[VGb])
                    for q in range(8):
                        t = tb * 8 + q
                        for half in range(2):
                            b.op("pe", lambda e: e.matmul(pOut[half][:], lhsT=L[:, t, :], rhs=VGb[:, q, half * 512:(half + 1) * 512],
                                                          start=(t == 0), stop=(t == 127)), reads=[L, VGb], writes=[pOut[half]])
                for half in range(2):
                    b.op("dve", lambda e: e.tensor_tensor(out=h3[:, half * 512:(half + 1) * 512], in0=pOut[half][:],
                                                          in1=X[:, half * 512:(half + 1) * 512], op=ALU.add), reads=[pOut[half], X], writes=[h3])
                b.op("act", lambda e: e.activation(out=junkb[:], in_=h3[:], func=AF.Square, accum_out=ss[:]), reads=[h3], writes=[junkb, ss])
                b.op("dve", lambda e: e.tensor_scalar(out=ss[:], in0=ss[:], scalar1=1.0 / 1024, scalar2=EPS, op0=ALU.mult, op1=ALU.add),
                     reads=[ss], writes=[ss])
                b.op("act", lambda e: e.activation(out=ss[:], in_=ss[:], func=AF.Sqrt), reads=[ss], writes=[ss])
                b.op("dve", lambda e: e.reciprocal(out=ss[:], in_=ss[:]), reads=[ss], writes=[ss])
                b.op("dve", lambda e: e.scalar_tensor_tensor(out=ot[:], in0=h3[:], scalar=ss[:, 0:1], in1=gfb[:], op0=ALU.mult, op1=ALU.mult),
                     reads=[h3, ss, gfb], writes=[ot])
                b.dma("sp", st, lambda e: e.dma_start(out=out_d.ap()[s, r0:r0 + 128, :], in_=ot[:]), reads=[ot], writes=[OUT])
        b.barrier()

    progs = {1: phase1, 2: phase2, 3: phase3a, 4: phase3b, 5: phase5}
    for p in phases:
        if p in progs:
            progs[p]()
    b.barrier()
    return nc


def _pc(v, nchunk):
    return np.ascontiguousarray(np.asarray(v, np.float32).reshape(nchunk, 128).T)


def prep_common(inp):
    f = lambda a: np.asarray(a, np.float32)
    w_in = f(inp["w_in"])[0]
    z32 = np.zeros((1024, 32), np.float32)
    kr = w_in[:, 2688:2720]
    krs = np.concatenate([kr[:, 16:], kr[:, :16]], axis=1)
    w_in_r = np.concatenate([
        w_in[:, 0:1024], w_in[:, 1024:2048], w_in[:, 2048:2432], w_in[:, 2432:2688],
        kr, z32, kr, z32, krs, z32, krs, z32,
        w_in[:, 2720:3744], w_in[:, 3744:4768]], axis=1)
    assert w_in_r.shape == (1024, 4992)
    conv_w = f(inp["conv_w"])[0]
    cw = np.ascontiguousarray(conv_w.reshape(4, 8, 128).transpose(2, 1, 0))
    w_uq = f(inp["w_uq"])[0].reshape(384, 16, 96)
    nope = w_uq[:, :, :64].reshape(384, 1024)
    rope = w_uq[:, :, 64:]
    ropes = np.concatenate([rope[:, :, 16:], rope[:, :, :16]], axis=2)
    z = np.zeros((384, 16, 32), np.float32)
    rope_p = np.concatenate([rope, z], axis=2).reshape(384, 1024)
    ropes_p = np.concatenate([ropes, z], axis=2).reshape(384, 1024)
    w_uq_r = np.concatenate([nope, rope_p, ropes_p, np.zeros((384, 1024), np.float32)], axis=1)
    w_ukv = f(inp["w_ukv"])[0].reshape(256, 16, 128)
    w_ukv_r = np.concatenate([w_ukv[:, :, :64].reshape(256, 1024), w_ukv[:, :, 64:].reshape(256, 1024)], axis=1)
    pos = (np.arange(SEQC, dtype=np.float32) - 112.0).astype(np.float32)
    inv = np.power(np.float32(10000.0), -np.arange(16, dtype=np.float32) * np.float32(2.0 / 32)).astype(np.float32)
    ang = (pos[None, :] * inv[:, None]).astype(np.float32)
    c, s_ = np.cos(ang).astype(np.float32), np.sin(ang).astype(np.float32)
    cos32 = np.concatenate([c, c], axis=0)
    sin32 = np.concatenate([-s_, s_], axis=0)
    zz = np.zeros((32, SEQC), np.float32)
    cos2 = np.concatenate([cos32, zz, cos32, zz], axis=0)
    sin2s = np.concatenate([sin32, zz, sin32, zz], axis=0)
    return {
        "meta": f(inp["meta_tokens"]),
        "g1": _pc(inp["norm1_g"][0], 8),
        "w_in": np.ascontiguousarray(w_in_r),
        "convw": cw,
        "convb": _pc(inp["conv_b"][0], 8),
        "rg_wa": np.ascontiguousarray(f(inp["rg_wa"])[0].transpose(1, 0, 2)),
        "rg_wx": np.ascontiguousarray(f(inp["rg_wx"])[0].transpose(1, 0, 2)),
        "rg_ba": _pc(inp["rg_ba"][0], 8),
        "rg_bx": _pc(inp["rg_bx"][0], 8),
        "rg_lam": _pc(inp["rg_lambda"][0], 8),
        "w_rnn_out": f(inp["w_rnn_out"])[0],
        "qg": _pc(inp["q_norm_g"][0], 3),
        "kvg": _pc(inp["kv_norm_g"][0], 2),
        "w_uq": np.ascontiguousarray(w_uq_r),
        "w_ukv": np.ascontiguousarray(w_ukv_r),
        "w_attn_out": f(inp["w_attn_out"])[0],
        "w_out": f(inp["w_out"])[0],
        "g2": f(inp["norm2_g"]).reshape(1, 1024),
        "peer_wq": f(inp["peer_wq"])[0],
        "keys1T": np.ascontiguousarray(f(inp["peer_keys1"])[0].T),
        "keys2T": np.ascontiguousarray(f(inp["peer_keys2"])[0].T),
        "peer_u": f(inp["peer_u"])[0],
        "peer_v": f(inp["peer_v"])[0],
        "gf": f(inp["final_g"]).reshape(1, 1024),
        "cos2": cos2,
        "sin2s": sin2s,
    }


def kernel(**inputs):
    NC = 8
    NSEQ = 4
    common = prep_common(inputs)
    x = np.asarray(inputs["x"], np.float32)
    nc = build(NSEQ)
    in_maps = []
    for c in range(NC):
        m = dict(common)
        m["x"] = np.ascontiguousarray(x[c * NSEQ:(c + 1) * NSEQ])
        in_maps.append(m)
    res = run_bass_kernel_spmd(nc, in_maps, core_ids=list(range(NC)))
    return np.concatenate([r["out"] for r in res.results], axis=0)
```

```python
from contextlib import ExitStack
import numpy as np
import concourse.bass as bass
import concourse.mybir as mybir
from concourse.bass_utils import run_bass_kernel_spmd

F32 = mybir.dt.float32
BF16 = mybir.dt.bfloat16
I32 = mybir.dt.int32
U32 = mybir.dt.uint32
AF = mybir.ActivationFunctionType
ALU = mybir.AluOpType
AX = mybir.AxisListType

SEQC = 2176
NREAL = 2048
EPS = 1e-6
GROUPS = [(0, 128)] + [(128 + 512 * g, 512) for g in range(4)]


class T:
    def __init__(self, h, name, accum=False):
        self.h = h
        self.name = name
        self.w = {}
        self.r = {}
        self.accum = accum

    def __getitem__(self, k):
        return self.h[k]


class B:
    def __init__(self, nc):
        self.nc = nc
        self.E = {"pe": nc.tensor, "act": nc.scalar, "dve": nc.vector, "pool": nc.gpsimd, "sp": nc.sync}
        self.sems = {}
        self.cnt = {}
        self.seen = {k: {} for k in self.E}
        for k in self.E:
            self.sems[k] = nc.alloc_semaphore("s_" + k)
            self.cnt[k] = 0
        self.nins = 0

    def newsem(self, name):
        self.sems[name] = self.nc.alloc_semaphore("s_" + name)
        self.cnt[name] = 0
        return name

    def _wait(self, eng, evs):
        for key, val in evs.items():
            if val <= 0 or (eng == "pe" and key == "pe"):
                continue
            if self.seen[eng].get(key, 0) >= val:
                continue
            self.E[eng].wait_ge(self.sems[key], val)
            self.seen[eng][key] = val
            self.nins += 1

    @staticmethod
    def _merge(d, e):
        for k, v in e.items():
            if d.get(k, 0) < v:
                d[k] = v

    def _deps(self, reads, writes):
        evs = {}
        for t in reads:
            self._merge(evs, t.w)
        for t in writes:
            if not t.accum:
                self._merge(evs, t.w)
                self._merge(evs, t.r)
        return evs

    def _commit(self, ev, reads, writes):
        for t in reads:
            if not t.accum:
                self._merge(t.r, ev)
        for t in writes:
            if t.accum:
                self._merge(t.w, ev)
            else:
                t.w = dict(ev)
                t.r = {}

    def op(self, eng, fn, reads=(), writes=()):
        self._wait(eng, self._deps(reads, writes))
        ins = fn(self.E[eng])
        self.cnt[eng] += 1
        ins.then_inc(self.sems[eng], 1)
        self.nins += 1
        self._commit({eng: self.cnt[eng]}, reads, writes)

    def dma(self, q, sem, fn, reads=(), writes=()):
        own = [t for t in writes if not t.accum] or [t for t in reads if not t.accum]
        t0 = own[0]
        if getattr(t0, "sem", None) is None:
            t0.sem = self.newsem("d%d" % len(self.sems))
        sem = t0.sem
        deps = self._deps(reads, writes)
        if writes and not writes[0].accum:
            wr = {}
            for t in writes:
                self._merge(wr, t.r)
            if deps.get(sem, 0) > wr.get(sem, 0):
                v = wr.get(sem, 0)
                if v > 0:
                    deps[sem] = v
                else:
                    deps.pop(sem)
        self._wait(q, deps)
        ins = fn(self.E[q])
        self.cnt[sem] += 16
        ins.then_inc(self.sems[sem], 16)
        self.nins += 1
        self._commit({sem: self.cnt[sem]}, reads, writes)

    def barrier(self):
        allev = {k: v for k, v in self.cnt.items() if v > 0}
        for e in self.E:
            self._wait(e, allev)


def build(NSEQ, phases=(1, 2, 3, 4, 5), debug=False, p5tiles=None):
    nc = bass.Bass("TRN2", target_bir_lowering=False)
    b = B(nc)
    NT = NSEQ * SEQC
    NR = NSEQ * NREAL
    skind = "ExternalOutput" if debug else "Internal"

    def din(name, shape, dt=F32):
        return nc.dram_tensor(name, list(shape), dt, kind="ExternalInput")

    def dscr(name, shape, dt=F32):
        return T(nc.dram_tensor(name, list(shape), dt, kind=skind), name, accum=True)

    x_d = din("x", [NSEQ, NREAL, 1024])
    meta_d = din("meta", [16, 1024])
    g1_d = din("g1", [128, 8])
    win_d = din("w_in", [1024, 4992])
    cw_d = din("convw", [128, 8, 4])
    cb_d = din("convb", [128, 8])
    rgwa_d = din("rg_wa", [128, 8, 128])
    rgwx_d = din("rg_wx", [128, 8, 128])
    rgba_d = din("rg_ba", [128, 8])
    rgbx_d = din("rg_bx", [128, 8])
    lam_d = din("rg_lam", [128, 8])
    wro_d = din("w_rnn_out", [1024, 1024])
    qg_d = din("qg", [128, 3])
    kvg_d = din("kvg", [128, 2])
    wuq_d = din("w_uq", [384, 4096])
    wukv_d = din("w_ukv", [256, 2048])
    wao_d = din("w_attn_out", [1024, 1024])
    wo_d = din("w_out", [1024, 1024])
    g2_d = din("g2", [1, 1024])
    wq_d = din("peer_wq", [1024, 2048])
    k1T_d = din("keys1T", [128, 128])
    k2T_d = din("keys2T", [128, 128])
    pu_d = din("peer_u", [16384, 1024])
    pv_d = din("peer_v", [16384, 1024])
    gf_d = din("gf", [1, 1024])
    cos_d = din("cos2", [128, SEQC])
    sin_d = din("sin2s", [128, SEQC])
    out_d = nc.dram_tensor("out", [NSEQ, NREAL, 1024], F32, kind="ExternalOutput")
    OUT = T(out_d, "out", accum=True)

    S_xr = dscr("S_xr", [1024, NT])
    S_gr = dscr("S_gr", [1024, NT])
    S_cq = dscr("S_cq", [384, NT])
    S_ckv = dscr("S_ckv", [256, NT])
    S_kr = dscr("S_kr", [128, NT])
    S_krs = dscr("S_krs", [128, NT])
    S_grnn = dscr("S_grnn", [1024, NT])
    S_gatt = dscr("S_gatt", [1024, NT])
    S_mrnn = dscr("S_mrnn", [1024, NT])
    S_oT = dscr("S_oT", [1024, NT], BF16)
    S_h2 = dscr("S_h2", [NR, 1024])
    PU16 = T(nc.dram_tensor("PU16", [16384, 1024], BF16, kind="Internal"), "PU16", accum=True)
    PV16 = T(nc.dram_tensor("PV16", [16384, 1024], BF16, kind="Internal"), "PV16", accum=True)

    ld = b.newsem("ld")
    st = b.newsem("st")
    wl = b.newsem("wl")

    def mk(es, pre):
        def sb(name, shape, dt):
            return T(es.enter_context(nc.sbuf_tensor(pre + name, list(shape), dt)), name)

        def ps(name, shape, dt):
            return T(es.enter_context(nc.psum_tensor(pre + name, list(shape), dt)), name)
        return sb, ps

    def make_ident(sb):
        identf = sb("identf", [128, 128], F32)
        ident = sb("ident", [128, 128], BF16)
        b.op("pool", lambda e: e.memset(identf[:], 1.0), writes=[identf])
        b.op("pool", lambda e: e.affine_select(out=identf[:], in_=identf[:], pattern=[[-1, 128]],
                                               compare_op=ALU.is_equal, fill=0.0, base=0, channel_multiplier=1),
             reads=[identf], writes=[identf])
        b.op("dve", lambda e: e.tensor_copy(out=ident[:], in_=identf[:]), reads=[identf], writes=[ident])
        return ident, identf

    def load_small(sb, name, d, shape):
        t = sb(name, shape, F32)
        b.dma("sp", wl, lambda e: e.dma_start(out=t[:], in_=d.ap()), writes=[t])
        return t

    def load_w_cast(dst, dst_ap, src_ap):
        b.dma("pool", wl, lambda e: e.dma_start(out=dst_ap, in_=src_ap), writes=[dst])

    def phase1():
        with ExitStack() as es:
            sb, ps = mk(es, "p1_")
            W = sb("W1", [128, 8, 4992], BF16)
            g1 = load_small(sb, "g1s", g1_d, [128, 8])
            stg = [sb("wstg%d" % i, [128, 4992], F32) for i in range(2)]
            for kc in range(8):
                s_ = stg[kc % 2]
                b.dma("sp", wl, lambda e: e.dma_start(out=s_[:], in_=win_d.ap()[kc * 128:(kc + 1) * 128, :]), writes=[s_])
                b.op("dve", lambda e: e.tensor_scalar(out=W[:, kc, :], in0=s_[:], scalar1=g1[:, kc:kc + 1], scalar2=None,
                                                      op0=ALU.mult), reads=[s_, g1], writes=[W])
            ident, _ = make_ident(sb)
            xt = [sb("xt%d" % i, [128, 1024], F32) for i in range(2)]
            junk = sb("junk", [128, 1024], BF16)
            ss = [sb("ss%d" % i, [128, 1], F32) for i in range(2)]
            xb = [sb("xb%d" % i, [128, 1024], BF16) for i in range(2)]
            n1T = [sb("n1T%d" % i, [128, 8, 512], BF16) for i in range(2)]
            pT = [ps("pT%d" % i, [128, 1024], BF16) for i in range(2)]
            pa = [ps("pa%d" % i, [128, 512], F32) for i in range(4)]
            stage = [sb("stage%d" % i, [128, 512], F32) for i in range(4)]
            chunks = []
            for i in range(8):
                chunks.append((S_xr, i * 128, None, 128))
            for i in range(8):
                chunks.append((S_gr, i * 128, AF.Gelu_apprx_tanh, 128))
            for i in range(3):
                chunks.append((S_cq, i * 128, None, 128))
            for i in range(2):
                chunks.append((S_ckv, i * 128, None, 128))
            chunks.append((S_kr, 0, None, 128))
            chunks.append((S_krs, 0, None, 128))
            for i in range(8):
                chunks.append((S_grnn, i * 128, AF.Sigmoid, 128))
            for i in range(8):
                chunks.append((S_gatt, i * 128, AF.Sigmoid, 128))
            ti = 0
            gi = 0
            ci = 0
            cv = [sb("cv%d" % i, [128, 8, 1024], BF16) for i in range(2)]
            conv = [(src, dst, c) for (src, dst) in ((pu_d, PU16), (pv_d, PV16)) for c in range(16)]
            cvi = [0]

            def convert_some(k):
                for _ in range(k):
                    if cvi[0] >= len(conv):
                        return
                    src, dst, c = conv[cvi[0]]
                    CV = cv[cvi[0] % 2]
                    cvi[0] += 1
                    b.dma("pool", None, lambda e: e.dma_start(out=CV[:], in_=src.ap()[c * 1024:(c + 1) * 1024, :].rearrange("(p r) d -> p r d", p=128)),
                          writes=[CV])
                    b.dma("sp", None, lambda e: e.dma_start(out=dst.h.ap()[c * 1024:(c + 1) * 1024, :].rearrange("(p r) d -> p r d", p=128), in_=CV[:]),
                          reads=[CV], writes=[dst])

            for s in range(NSEQ):
                for (c0, n) in GROUPS:
                    nT = n1T[gi % 2]
                    gi += 1
                    convert_some(8 if NSEQ == 1 else 2)
                    for j in range(n // 128):
                        t = c0 // 128 + j
                        X = xt[ti % 2]
                        SS = ss[ti % 2]
                        XB = xb[ti % 2]
                        PT = pT[ti % 2]
                        ti += 1
                        if t == 0:
                            b.op("pool", lambda e: e.memset(X[:], 0.0), writes=[X])
                            b.dma("sp", ld, lambda e: e.dma_start(out=X[112:128, :], in_=meta_d.ap()), writes=[X])
                        else:
                            b.dma("sp", ld, lambda e: e.dma_start(out=X[:], in_=x_d.ap()[s, (t - 1) * 128:t * 128, :]), writes=[X])
                        b.op("act", lambda e: e.activation(out=junk[:], in_=X[:], func=AF.Square, accum_out=SS[:]),
                             reads=[X], writes=[junk, SS])
                        b.op("dve", lambda e: e.tensor_scalar(out=SS[:], in0=SS[:], scalar1=1.0 / 1024, scalar2=EPS,
                                                              op0=ALU.mult, op1=ALU.add), reads=[SS], writes=[SS])
                        b.op("act", lambda e: e.activation(out=SS[:], in_=SS[:], func=AF.Sqrt), reads=[SS], writes=[SS])
                        b.op("dve", lambda e: e.reciprocal(out=SS[:], in_=SS[:]), reads=[SS], writes=[SS])
                        b.op("act", lambda e: e.activation(out=XB[:], in_=X[:], func=AF.Copy, scale=SS[:]),
                             reads=[X, SS], writes=[XB])
                        for c in range(8):
                            b.op("pe", lambda e: e.transpose(out=PT[:, c * 128:(c + 1) * 128], in_=XB[:, c * 128:(c + 1) * 128],
                                                             identity=ident[:]), reads=[XB, ident], writes=[PT])
                        b.op("dve", lambda e: e.tensor_copy(out=nT[:, :, j * 128:(j + 1) * 128],
                                                            in_=PT[:].rearrange("p (c t) -> p c t", c=8)),
                             reads=[PT], writes=[nT])
                    for oc, (S_, r0, fn, m) in enumerate(chunks):
                        PA = pa[ci % 4]
                        SG = stage[ci % 4]
                        ci += 1
                        for kc in range(8):
                            b.op("pe", lambda e: e.matmul(PA[:, 0:n], lhsT=W[:, kc, oc * 128:(oc + 1) * 128], rhs=nT[:, kc, 0:n],
                                                          start=(kc == 0), stop=(kc == 7)), reads=[W, nT], writes=[PA])
                        if fn is None:
                            b.op("dve", lambda e: e.tensor_copy(out=SG[:, 0:n], in_=PA[:, 0:n]), reads=[PA], writes=[SG])
                        else:
                            b.op("act", lambda e: e.activation(out=SG[:, 0:n], in_=PA[:, 0:n], func=fn), reads=[PA], writes=[SG])
                        col = s * SEQC + c0
                        b.dma("pool", st, lambda e: e.dma_start(out=S_.h.ap()[r0:r0 + 128, col:col + n], in_=SG[:, 0:n]),
                              reads=[SG], writes=[S_])
            convert_some(len(conv))
        b.barrier()

    def phase2():
        with ExitStack() as es:
            sb, ps = mk(es, "p2_")
            WA = sb("WA", [128, 8, 128], BF16)
            WX = sb("WX", [128, 8, 128], BF16)
            WRO = sb("WRO", [128, 8, 1024], BF16)
            load_w_cast(WA, WA[:], rgwa_d.ap())
            load_w_cast(WX, WX[:], rgwx_d.ap())
            load_w_cast(WRO, WRO[:], wro_d.ap().rearrange("(k p) f -> p k f", p=128))
            cw = load_small(sb, "cw", cw_d, [128, 8, 4])
            cb = load_small(sb, "cb", cb_d, [128, 8])
            ba = load_small(sb, "ba", rgba_d, [128, 8])
            bx = load_small(sb, "bx", rgbx_d, [128, 8])
            lam = load_small(sb, "lam", lam_d, [128, 8])
            c8 = sb("c8", [128, 8], F32)
            c16 = sb("c16", [128, 8], F32)
            b.op("act", lambda e: e.activation(out=c8[:], in_=lam[:], func=AF.Exp, scale=-1.0), reads=[lam], writes=[c8])
            b.op("act", lambda e: e.activation(out=c8[:], in_=c8[:], func=AF.Ln, bias=1.0), reads=[c8], writes=[c8])
            b.op("dve", lambda e: e.tensor_scalar(out=c16[:], in0=c8[:], scalar1=-16.0, scalar2=None, op0=ALU.mult),
                 reads=[c8], writes=[c16])
            b.op("dve", lambda e: e.tensor_scalar(out=c8[:], in0=c8[:], scalar1=-8.0, scalar2=None, op0=ALU.mult),
                 reads=[c8, c16], writes=[c8])
            XR = sb("XR", [128, SEQC], F32)
            Y = sb("Y", [128, SEQC], F32)
            YB = sb("YB", [128, SEQC], BF16)
            A = sb("A", [128, SEQC], F32)
            U = sb("U", [128, SEQC], F32)
            H = sb("H", [128, SEQC], F32)
            GR = sb("GR", [128, SEQC], F32)
            ZT = sb("ZT", [128, 8, SEQC], BF16)
            tr = sb("tr", [128, 512], F32)
            ta2 = sb("ta2", [128, 512], F32)
            ti_ = sb("ti", [128, 512], F32)
            gs = [sb("gs%d" % i, [128, 512], F32) for i in range(2)]
            stage = [sb("stage%d" % i, [128, 512], F32) for i in range(2)]
            pA = ps("pA", [128, 512], F32)
            pX = ps("pX", [128, 512], F32)
            pO = [ps("pO%d" % i, [128, 512], F32) for i in range(2)]
            V0 = 112
            NV = SEQC - V0
            k = 0
            for s in range(NSEQ):
                sc = s * SEQC
                for n_ in range(8):
                    r0 = n_ * 128
                    b.dma("sp", ld, lambda e: e.dma_start(out=XR[:], in_=S_xr.h.ap()[r0:r0 + 128, sc:sc + SEQC]),
                          reads=[S_xr], writes=[XR])
                    b.dma("sp", ld, lambda e: e.dma_start(out=GR[:], in_=S_gr.h.ap()[r0:r0 + 128, sc:sc + SEQC]),
                          reads=[S_gr], writes=[GR])
                    b.op("dve", lambda e: e.tensor_scalar(out=Y[:, V0:SEQC], in0=XR[:, V0 - 3:SEQC - 3], scalar1=cw[:, n_, 0:1],
                                                          scalar2=cb[:, n_:n_ + 1], op0=ALU.mult, op1=ALU.add),
                         reads=[XR, cw, cb], writes=[Y])
                    for kk in range(1, 4):
                        b.op("dve", lambda e: e.scalar_tensor_tensor(out=Y[:, V0:SEQC], in0=XR[:, V0 - 3 + kk:SEQC - 3 + kk],
                                                                     scalar=cw[:, n_, kk:kk + 1], in1=Y[:, V0:SEQC],
                                                                     op0=ALU.mult, op1=ALU.add), reads=[XR, cw, Y], writes=[Y])
                    b.op("act", lambda e: e.activation(out=YB[:, V0:SEQC], in_=Y[:, V0:SEQC], func=AF.Copy), reads=[Y], writes=[YB])
                    for (c0, n) in GROUPS:
                        if c0 == 0:
                            c0, n = V0, 16
                        cs = slice(c0, c0 + n)
                        b.op("pe", lambda e: e.matmul(pA[:, 0:n], lhsT=WA[:, n_, :], rhs=YB[:, cs], start=True, stop=True),
                             reads=[WA, YB], writes=[pA])
                        b.op("pe", lambda e: e.matmul(pX[:, 0:n], lhsT=WX[:, n_, :], rhs=YB[:, cs], start=True, stop=True),
                             reads=[WX, YB], writes=[pX])
                        b.op("act", lambda e: e.activation(out=tr[:, 0:n], in_=pA[:, 0:n], func=AF.Sigmoid, bias=ba[:, n_:n_ + 1]),
                             reads=[pA, ba], writes=[tr])
                        b.op("act", lambda e: e.activation(out=ti_[:, 0:n], in_=pX[:, 0:n], func=AF.Sigmoid, bias=bx[:, n_:n_ + 1]),
                             reads=[pX, bx], writes=[ti_])
                        b.op("act", lambda e: e.activation(out=A[:, cs], in_=tr[:, 0:n], func=AF.Exp, scale=c8[:, n_:n_ + 1]),
                             reads=[tr, c8], writes=[A])
                        b.op("act", lambda e: e.activation(out=ta2[:, 0:n], in_=tr[:, 0:n], func=AF.Exp, scale=c16[:, n_:n_ + 1]),
                             reads=[tr, c16], writes=[ta2])
                        b.op("dve", lambda e: e.tensor_scalar(out=ta2[:, 0:n], in0=ta2[:, 0:n], scalar1=-1.0, scalar2=1.0,
                                                              op0=ALU.mult, op1=ALU.add), reads=[ta2], writes=[ta2])
                        b.op("act", lambda e: e.activation(out=ta2[:, 0:n], in_=ta2[:, 0:n], func=AF.Sqrt), reads=[ta2], writes=[ta2])
                        b.op("dve", lambda e: e.tensor_tensor(out=ti_[:, 0:n], in0=ti_[:, 0:n], in1=Y[:, cs], op=ALU.mult),
                             reads=[ti_, Y], writes=[ti_])
                        b.op("dve", lambda e: e.tensor_tensor(out=U[:, cs], in0=ti_[:, 0:n], in1=ta2[:, 0:n], op=ALU.mult),
                             reads=[ti_, ta2], writes=[U])
                    b.op("dve", lambda e: e.tensor_tensor_scan(out=H[:, V0:SEQC], data0=A[:, V0:SEQC], data1=U[:, V0:SEQC],
                                                               initial=0.0, op0=ALU.mult, op1=ALU.add), reads=[A, U], writes=[H])
                    b.op("dve", lambda e: e.tensor_tensor(out=ZT[:, n_, V0:SEQC], in0=H[:, V0:SEQC], in1=GR[:, V0:SEQC], op=ALU.mult),
                         reads=[H, GR], writes=[ZT])
                for (c0, n) in GROUPS[1:]:
                    cs = slice(c0, c0 + n)
                    for dc in range(8):
                        PO = pO[k % 2]
                        G = gs[k % 2]
                        SG = stage[k % 2]
                        k += 1
                        b.dma("sp", ld, lambda e: e.dma_start(out=G[:, 0:n], in_=S_grnn.h.ap()[dc * 128:(dc + 1) * 128, sc + c0:sc + c0 + n]),
                              reads=[S_grnn], writes=[G])
                        for kc in range(8):
                            b.op("pe", lambda e: e.matmul(PO[:, 0:n], lhsT=WRO[:, kc, dc * 128:(dc + 1) * 128], rhs=ZT[:, kc, cs],
                                                          start=(kc == 0), stop=(kc == 7)), reads=[WRO, ZT], writes=[PO])
                        b.op("dve", lambda e: e.tensor_tensor(out=SG[:, 0:n], in0=PO[:, 0:n], in1=G[:, 0:n], op=ALU.mult),
                             reads=[PO, G], writes=[SG])
                        b.dma("pool", st, lambda e: e.dma_start(out=S_mrnn.h.ap()[dc * 128:(dc + 1) * 128, sc + c0:sc + c0 + n],
                                                                in_=SG[:, 0:n]), reads=[SG], writes=[S_mrnn])
        b.barrier()


    def phase3a():
        with ExitStack() as es:
            sb, ps = mk(es, "p3_")
            WUQ = sb("WUQ", [128, 3, 3072], BF16)
            WUKV = sb("WUKV", [128, 2, 2048], BF16)
            qg = load_small(sb, "qg", qg_d, [128, 3])
            kvg = load_small(sb, "kvg", kvg_d, [128, 2])
            wst = sb("wst", [128, 3072], F32)
            for kc in range(3):
                b.dma("sp", wl, lambda e: e.dma_start(out=wst[:], in_=wuq_d.ap()[kc * 128:(kc + 1) * 128, 0:3072]), writes=[wst])
                b.op("dve", lambda e: e.tensor_scalar(out=WUQ[:, kc, :], in0=wst[:], scalar1=qg[:, kc:kc + 1], scalar2=None,
                                                      op0=ALU.mult), reads=[wst, qg], writes=[WUQ])
            for kc in range(2):
                b.dma("sp", wl, lambda e: e.dma_start(out=wst[:, 0:2048], in_=wukv_d.ap()[kc * 128:(kc + 1) * 128, :]), writes=[wst])
                b.op("dve", lambda e: e.tensor_scalar(out=WUKV[:, kc, :], in0=wst[:, 0:2048], scalar1=kvg[:, kc:kc + 1], scalar2=None,
                                                      op0=ALU.mult), reads=[wst, kvg], writes=[WUKV])
            ident, identf = make_ident(sb)
            ones = sb("ones", [128, 128], F32)
            b.op("pool", lambda e: e.memset(ones[:], 1.0), writes=[ones])
            tri = sb("tri", [128, 128], BF16)
            b.op("pool", lambda e: e.memset(identf[:], 1.0), reads=[identf], writes=[identf])
            b.op("pool", lambda e: e.affine_select(out=identf[:], in_=identf[:], pattern=[[1, 128]], compare_op=ALU.is_ge,
                                                   fill=0.0, base=0, channel_multiplier=-1), reads=[identf], writes=[identf])
            b.op("dve", lambda e: e.tensor_copy(out=tri[:], in_=identf[:]), reads=[identf], writes=[tri])
            Kn = sb("Kn", [128, 8, SEQC], BF16)
            Kr2 = sb("Kr2", [128, SEQC], BF16)
            V = sb("V", [128, 17, 16, 65], BF16)
            b.op("pool", lambda e: e.memset(V[:], 1.0), writes=[V])
            cq_t = sb("cq_t", [128, 3, 512], F32)
            ckv_t = sb("ckv_t", [128, 2, 512], F32)
            sq = sb("sq", [128, 3, 512], F32)
            rstd = sb("rstd", [128, 512], F32)
            cqn = sb("cqn", [128, 3, 512], BF16)
            ckvn = sb("ckvn", [128, 2, 512], BF16)
            kr_t = sb("kr_t", [128, 512], F32)
            krs_t = sb("krs_t", [128, 512], F32)
            cos_t = sb("cos_t", [128, 512], F32)
            sin_t = sb("sin_t", [128, 512], F32)
            tmp1 = sb("tmp1", [128, 512], F32)
            tmp2 = sb("tmp2", [128, 512], F32)
            Qn = sb("Qn", [128, 8, 512], BF16)
            Qr = sb("Qr", [128, 8, 512], BF16)
            PTs = [sb("PTs%d" % i, [128, 512], BF16) for i in range(3)]
            o_tm = sb("o_tm", [128, 4, 1024], BF16)
            oT = sb("oT", [128, 8, 512], BF16)
            rec = sb("rec", [128, 4], F32)
            pn = ps("pn", [128, 512], F32)
            pk = [ps("pk%d" % i, [128, 512], F32) for i in range(2)]
            pS = [ps("pS%d" % i, [128, 512], F32) for i in range(2)]
            pO = [ps("pO%d" % i, [128, 512], F32) for i in range(2)]
            pT = ps("pT", [128, 1024], BF16)
            scale = float(96 ** -0.5)
            ki = [0]

            def nextpk():
                ki[0] += 1
                return pk[ki[0] % 2]

            def rmsn(src, nch, dst, nfeat, n):
                b.op("act", lambda e: e.activation(out=sq[:, 0:nch, 0:n], in_=src[:, :, 0:n], func=AF.Square), reads=[src], writes=[sq])
                for c in range(nch):
                    b.op("pe", lambda e: e.matmul(pn[:, 0:n], lhsT=ones[:], rhs=sq[:, c, 0:n], start=(c == 0), stop=(c == nch - 1)),
                         reads=[ones, sq], writes=[pn])
                b.op("dve", lambda e: e.tensor_scalar(out=rstd[:, 0:n], in0=pn[:, 0:n], scalar1=1.0 / nfeat, scalar2=EPS,
                                                      op0=ALU.mult, op1=ALU.add), reads=[pn], writes=[rstd])
                b.op("act", lambda e: e.activation(out=rstd[:, 0:n], in_=rstd[:, 0:n], func=AF.Sqrt), reads=[rstd], writes=[rstd])
                b.op("dve", lambda e: e.reciprocal(out=rstd[:, 0:n], in_=rstd[:, 0:n]), reads=[rstd], writes=[rstd])
                for c in range(nch):
                    b.op("dve", lambda e: e.tensor_tensor(out=dst[:, c, 0:n], in0=src[:, c, 0:n], in1=rstd[:, 0:n], op=ALU.mult),
                         reads=[src, rstd], writes=[dst])

            si = 0
            for s in range(NSEQ):
                sc = s * SEQC
                for (c0, n) in GROUPS:
                    col = sc + c0
                    b.dma("sp", ld, lambda e: e.dma_start(out=cq_t[:, :, 0:n],
                                                          in_=S_cq.h.ap()[:, col:col + n].rearrange("(c p) n -> p c n", p=128)),
                          reads=[S_cq], writes=[cq_t])
                    b.dma("sp", ld, lambda e: e.dma_start(out=ckv_t[:, :, 0:n],
                                                          in_=S_ckv.h.ap()[:, col:col + n].rearrange("(c p) n -> p c n", p=128)),
                          reads=[S_ckv], writes=[ckv_t])
                    b.dma("sp", ld, lambda e: e.dma_start(out=kr_t[:, 0:n], in_=S_kr.h.ap()[:, col:col + n]), reads=[S_kr], writes=[kr_t])
                    b.dma("sp", ld, lambda e: e.dma_start(out=krs_t[:, 0:n], in_=S_krs.h.ap()[:, col:col + n]), reads=[S_krs], writes=[krs_t])
                    b.dma("sp", ld, lambda e: e.dma_start(out=cos_t[:, 0:n], in_=cos_d.ap()[:, c0:c0 + n]), writes=[cos_t])
                    b.dma("sp", ld, lambda e: e.dma_start(out=sin_t[:, 0:n], in_=sin_d.ap()[:, c0:c0 + n]), writes=[sin_t])
                    rmsn(cq_t, 3, cqn, 384.0, n)
                    rmsn(ckv_t, 2, ckvn, 256.0, n)
                    for j in range(8):
                        P_ = nextpk()
                        for kc in range(2):
                            b.op("pe", lambda e: e.matmul(P_[:, 0:n], lhsT=WUKV[:, kc, j * 128:(j + 1) * 128], rhs=ckvn[:, kc, 0:n],
                                                          start=(kc == 0), stop=(kc == 1)), reads=[WUKV, ckvn], writes=[P_])
                        b.op("act", lambda e: e.activation(out=Kn[:, j, c0:c0 + n], in_=P_[:, 0:n], func=AF.Copy), reads=[P_], writes=[Kn])
                    b.op("dve", lambda e: e.tensor_tensor(out=tmp1[:, 0:n], in0=kr_t[:, 0:n], in1=cos_t[:, 0:n], op=ALU.mult),
                         reads=[kr_t, cos_t], writes=[tmp1])
                    b.op("dve", lambda e: e.tensor_tensor(out=tmp2[:, 0:n], in0=krs_t[:, 0:n], in1=sin_t[:, 0:n], op=ALU.mult),
                         reads=[krs_t, sin_t], writes=[tmp2])
                    b.op("dve", lambda e: e.tensor_tensor(out=Kr2[:, c0:c0 + n], in0=tmp1[:, 0:n], in1=tmp2[:, 0:n], op=ALU.add),
                         reads=[tmp1, tmp2], writes=[Kr2])
                    for jt in range(n // 128):
                        t = c0 // 128 + jt
                        if t == 0:
                            lo, M = 112, 16
                        else:
                            lo, M = jt * 128, 128
                        for half in range(2):
                            P_ = nextpk()
                            for kc in range(2):
                                b.op("pe", lambda e: e.matmul(P_[0:M, :], lhsT=ckvn[:, kc, lo:lo + M],
                                                              rhs=WUKV[:, kc, 1024 + half * 512:1024 + (half + 1) * 512],
                                                              start=(kc == 0), stop=(kc == 1)), reads=[WUKV, ckvn], writes=[P_])
                            b.op("act", lambda e: e.activation(out=V[0:M, t, half * 8:(half + 1) * 8, 0:64],
                                                               in_=P_[0:M, :].rearrange("p (h d) -> p h d", h=8), func=AF.Copy),
                                 reads=[P_], writes=[V])
                    if c0 == 0:
                        continue
                    for j in range(8):
                        P_ = nextpk()
                        for kc in range(3):
                            b.op("pe", lambda e: e.matmul(P_[:, 0:n], lhsT=WUQ[:, kc, j * 128:(j + 1) * 128], rhs=cqn[:, kc, 0:n],
                                                          start=(kc == 0), stop=(kc == 2)), reads=[WUQ, cqn], writes=[P_])
                        b.op("act", lambda e: e.activation(out=Qn[:, j, 0:n], in_=P_[:, 0:n], func=AF.Copy), reads=[P_], writes=[Qn])
                    for j in range(8):
                        P1 = nextpk()
                        for kc in range(3):
                            b.op("pe", lambda e: e.matmul(P1[:, 0:n], lhsT=WUQ[:, kc, 1024 + j * 128:1024 + (j + 1) * 128], rhs=cqn[:, kc, 0:n],
                                                          start=(kc == 0), stop=(kc == 2)), reads=[WUQ, cqn], writes=[P1])
                        b.op("dve", lambda e: e.tensor_tensor(out=tmp1[:, 0:n], in0=P1[:, 0:n], in1=cos_t[:, 0:n], op=ALU.mult),
                             reads=[P1, cos_t], writes=[tmp1])
                        P2 = nextpk()
                        for kc in range(3):
                            b.op("pe", lambda e: e.matmul(P2[:, 0:n], lhsT=WUQ[:, kc, 2048 + j * 128:2048 + (j + 1) * 128], rhs=cqn[:, kc, 0:n],
                                                          start=(kc == 0), stop=(kc == 2)), reads=[WUQ, cqn], writes=[P2])
                        b.op("dve", lambda e: e.tensor_tensor(out=tmp2[:, 0:n], in0=P2[:, 0:n], in1=sin_t[:, 0:n], op=ALU.mult),
                             reads=[P2, sin_t], writes=[tmp2])
                        b.op("dve", lambda e: e.tensor_tensor(out=Qr[:, j, 0:n], in0=tmp1[:, 0:n], in1=tmp2[:, 0:n], op=ALU.add),
                             reads=[tmp1, tmp2], writes=[Qr])
                    g0t = c0 // 128
                    for h in range(16):
                        j = h // 2
                        p0 = (h % 2) * 64
                        PO = pO[h % 2]
                        POv = PO[:, 0:260].rearrange("p (q d) -> p q d", q=4)
                        for kt in range(0, g0t + 4):
                            if kt == 0:
                                k0, M = 112, 16
                            else:
                                k0, M = kt * 128, 128
                            qlo = max(0, kt - g0t)
                            q0 = qlo * 128
                            PS_ = pS[si % 2]
                            PTb = PTs[si % 3]
                            si += 1
                            b.op("pe", lambda e: e.matmul(PS_[0:M, q0:512], lhsT=Kn[p0:p0 + 64, j, k0:k0 + M], rhs=Qn[p0:p0 + 64, j, q0:512],
                                                          start=True, stop=False), reads=[Kn, Qn], writes=[PS_])
                            b.op("pe", lambda e: e.matmul(PS_[0:M, q0:512], lhsT=Kr2[p0:p0 + 32, k0:k0 + M], rhs=Qr[p0:p0 + 32, j, q0:512],
                                                          start=False, stop=True), reads=[Kr2, Qr], writes=[PS_])
                            b.op("act", lambda e: e.activation(out=PTb[0:M, q0:512], in_=PS_[0:M, q0:512], func=AF.Exp, scale=scale),
                                 reads=[PS_], writes=[PTb])
                            if kt >= g0t:
                                b.op("dve", lambda e: e.tensor_tensor(out=PTb[:, q0:q0 + 128], in0=PTb[:, q0:q0 + 128], in1=tri[:], op=ALU.mult),
                                     reads=[PTb, tri], writes=[PTb])
                            for qt in range(qlo, 4):
                                b.op("pe", lambda e: e.matmul(POv[:, qt, :], lhsT=PTb[0:M, qt * 128:(qt + 1) * 128], rhs=V[0:M, kt, h, :],
                                                              start=(kt == 0 and qt == 0), stop=(kt == g0t + qt), skip_group_check=True),
                                     reads=[PTb, V], writes=[PO])
                        b.op("dve", lambda e: e.reciprocal(out=rec[:], in_=POv[:, :, 64]), reads=[PO], writes=[rec])
                        for qt in range(4):
                            b.op("dve", lambda e: e.tensor_scalar(out=o_tm[:, qt, h * 64:(h + 1) * 64], in0=POv[:, qt, 0:64],
                                                                  scalar1=rec[:, qt:qt + 1], scalar2=None, op0=ALU.mult),
                                 reads=[PO, rec], writes=[o_tm])
                    for qt in range(4):
                        for c in range(8):
                            b.op("pe", lambda e: e.transpose(out=pT[:, c * 128:(c + 1) * 128], in_=o_tm[:, qt, c * 128:(c + 1) * 128],
                                                             identity=ident[:]), reads=[o_tm, ident], writes=[pT])
                        b.op("dve", lambda e: e.tensor_copy(out=oT[:, :, qt * 128:(qt + 1) * 128],
                                                            in_=pT[:].rearrange("p (c t) -> p c t", c=8)), reads=[pT], writes=[oT])
                    b.dma("pool", st, lambda e: e.dma_start(out=S_oT.h.ap()[:, col:col + n].rearrange("(c p) n -> p c n", p=128), in_=oT[:]),
                          reads=[oT], writes=[S_oT])
        b.barrier()

    def phase3b():
        with ExitStack() as es:
            sb, ps = mk(es, "p3b_")
            WAO = sb("WAO", [128, 8, 1024], BF16)
            WO = sb("WO", [128, 8, 1024], BF16)
            load_w_cast(WAO, WAO[:], wao_d.ap().rearrange("(k p) f -> p k f", p=128))
            load_w_cast(WO, WO[:], wo_d.ap().rearrange("(k p) f -> p k f", p=128))
            oT = [sb("oT%d" % i, [128, 8, 512], BF16) for i in range(2)]
            ga = [sb("ga%d" % i, [128, 512], F32) for i in range(2)]
            mr = [sb("mr%d" % i, [128, 512], F32) for i in range(2)]
            tmp = sb("tmp", [128, 512], F32)
            mixT = sb("mixT", [128, 8, 512], BF16)
            xt = [sb("xt%d" % i, [128, 1024], F32) for i in range(2)]
            h2 = [sb("h2%d" % i, [128, 1024], F32) for i in range(2)]
            pk = [ps("pk%d" % i, [128, 512], F32) for i in range(4)]
            gi = 0
            k = 0
            ti = 0
            for s in range(NSEQ):
                sc = s * SEQC
                for g, (c0, n) in enumerate(GROUPS):
                    if g == 0:
                        continue
                    col = sc + c0
                    OT = oT[gi % 2]
                    gi += 1
                    b.dma("sp", ld, lambda e: e.dma_start(out=OT[:], in_=S_oT.h.ap()[:, col:col + n].rearrange("(c p) n -> p c n", p=128)),
                          reads=[S_oT], writes=[OT])
                    for dc in range(8):
                        GA = ga[k % 2]
                        MR = mr[k % 2]
                        PK = pk[k % 4]
                        k += 1
                        b.dma("sp", ld, lambda e: e.dma_start(out=GA[:], in_=S_gatt.h.ap()[dc * 128:(dc + 1) * 128, col:col + n]),
                              reads=[S_gatt], writes=[GA])
                        b.dma("sp", ld, lambda e: e.dma_start(out=MR[:], in_=S_mrnn.h.ap()[dc * 128:(dc + 1) * 128, col:col + n]),
                              reads=[S_mrnn], writes=[MR])
                        for kc in range(8):
                            b.op("pe", lambda e: e.matmul(PK[:], lhsT=WAO[:, kc, dc * 128:(dc + 1) * 128], rhs=OT[:, kc, :],
                                                          start=(kc == 0), stop=(kc == 7)), reads=[WAO, OT], writes=[PK])
                        b.op("dve", lambda e: e.tensor_tensor(out=tmp[:], in0=PK[:], in1=GA[:], op=ALU.mult), reads=[PK, GA], writes=[tmp])
                        b.op("dve", lambda e: e.tensor_tensor(out=mixT[:, dc, :], in0=tmp[:], in1=MR[:], op=ALU.add),
                             reads=[tmp, MR], writes=[mixT])
                    for qt in range(4):
                        X = xt[ti % 2]
                        H2 = h2[ti % 2]
                        ti += 1
                        r0 = (g - 1) * 512 + qt * 128
                        b.dma("sp", ld, lambda e: e.dma_start(out=X[:], in_=x_d.ap()[s, r0:r0 + 128, :]), writes=[X])
                        for half in range(2):
                            PK = pk[k % 4]
                            k += 1
                            for kc in range(8):
                                b.op("pe", lambda e: e.matmul(PK[:], lhsT=mixT[:, kc, qt * 128:(qt + 1) * 128],
                                                              rhs=WO[:, kc, half * 512:(half + 1) * 512],
                                                              start=(kc == 0), stop=(kc == 7)), reads=[WO, mixT], writes=[PK])
                            b.op("dve", lambda e: e.tensor_tensor(out=H2[:, half * 512:(half + 1) * 512], in0=PK[:],
                                                                  in1=X[:, half * 512:(half + 1) * 512], op=ALU.add),
                                 reads=[PK, X], writes=[H2])
                        b.dma("pool", st, lambda e: e.dma_start(out=S_h2.h.ap()[s * NREAL + r0:s * NREAL + r0 + 128, :], in_=H2[:]),
                              reads=[H2], writes=[S_h2])
        b.barrier()


    def phase5():
        with ExitStack() as es:
            sb, ps = mk(es, "p5_")
            WQ = sb("WQ", [128, 8, 2048], BF16)
            K1T = sb("K1T", [128, 128], BF16)
            K2T = sb("K2T", [128, 128], BF16)
            load_w_cast(WQ, WQ[:], wq_d.ap().rearrange("(k p) f -> p k f", p=128))
            load_w_cast(K1T, K1T[:], k1T_d.ap())
            load_w_cast(K2T, K2T[:], k2T_d.ap())
            g2b = sb("g2b", [128, 1024], F32)
            gfb = sb("gfb", [128, 1024], F32)
            b.dma("sp", None, lambda e: e.dma_start(out=g2b[:], in_=g2_d.ap().to_broadcast([128, 1024])), writes=[g2b])
            b.dma("sp", None, lambda e: e.dma_start(out=gfb[:], in_=gf_d.ap().to_broadcast([128, 1024])), writes=[gfb])
            ident, identf = make_ident(sb)
            iota_i = sb("iota_i", [128, 16], I32)
            iota16 = sb("iota16", [128, 16], F32)
            b.op("pool", lambda e: e.iota(iota_i[:], pattern=[[1, 16]], base=0, channel_multiplier=0), writes=[iota_i])
            b.op("dve", lambda e: e.tensor_copy(out=iota16[:], in_=iota_i[:]), reads=[iota_i], writes=[iota16])
            L = sb("L", [128, 128, 128], BF16)
            b.op("pool", lambda e: e.memset(L[:], 0.0), writes=[L])
            Lflat = L[:].rearrange("p a b -> p (a b)")
            Xs = [sb("X%d" % i, [128, 1024], F32) for i in range(2)]
            xnbs = [sb("xnb%d" % i, [128, 1024], BF16) for i in range(2)]
            IDXs = [sb("IDX%d" % i, [128, 128], U32) for i in range(2)]
            IDXTs = [sb("IDXT%d" % i, [128, 128], U32) for i in range(2)]
            Gs = [sb("G%d" % i, [128, 8, 16], F32) for i in range(2)]
            ssA = sb("ssA", [128, 1], F32)
            ssC = sb("ssC", [128, 1], F32)
            junkA = sb("junkA", [128, 1024], BF16)
            junkD = sb("junkD", [128, 1024], BF16)
            xT = sb("xT", [128, 8, 128], BF16)
            qT = sb("qT", [128, 16, 128], BF16)
            S = sb("S", [128, 16, 128], F32)
            eqv = S[:].rearrange("p (h x) (y a) -> p h (x y) a", x=2, a=16)
            T16 = sb("T16", [128, 16, 16], F32)
            I16 = sb("I16", [128, 16, 16], U32)
            I16f = sb("I16f", [128, 16, 16], F32)
            cand = sb("cand", [128, 8, 256], F32)
            TS = sb("TS", [128, 8, 16], F32)
            CI = sb("CI", [128, 8, 16], U32)
            CIa = sb("CIa", [128, 8, 16], U32)
            CIb = sb("CIb", [128, 8, 16], U32)
            Af = sb("Af", [128, 8, 16], F32)
            Bf = sb("Bf", [128, 8, 16], F32)
            i1s = sb("i1s", [128, 128], F32)
            i2s = sb("i2s", [128, 128], F32)
            i1b = sb("i1b", [128, 128], BF16)
            i2b = sb("i2b", [128, 128], BF16)
            idxf = sb("idxf", [128, 128], F32)
            idxTf = sb("idxTf", [128, 128], F32)
            iTf = sb("iTf", [128, 2, 128], F32)
            E = sb("E", [128, 8, 16], F32)
            Z = sb("Z", [128, 8], F32)
            ACTV = sb("ACTV", [128, 128], F32)
            coefb = sb("coefb", [128, 128], BF16)
            coefT = sb("coefT", [128, 128], BF16)
            UG = [sb("UG%d" % i, [128, 8, 1024], BF16) for i in range(2)]
            VG = [sb("VG%d" % i, [128, 8, 1024], BF16) for i in range(2)]
            h3 = sb("h3", [128, 1024], F32)
            pTa = ps("pTa", [128, 1024], BF16)
            pTb = ps("pTb", [128, 1024], BF16)
            pq = [ps("pq%d" % i, [128, 512], F32) for i in range(2)]
            pOut = [ps("pOut%d" % i, [128, 512], F32) for i in range(2)]
            T16v = T16[:].rearrange("p (h two) a -> p h two a", two=2)
            I16fv = I16f[:].rearrange("p (h two) a -> p h two a", two=2)
            B4 = [128, 8, 16, 16]
            cnt = {"u": 0, "v": 0, "q": 0}
            ntiles = NR // 128 if p5tiles is None else p5tiles

            def sweep(eng, fns, reads, writes):
                for k_, f in enumerate(fns):
                    w = writes if (k_ == 0 or k_ == len(fns) - 1) else ()
                    b.op(eng, f, reads=reads, writes=w)

            def top16(vals, ng, tv, iv):
                sweep("dve", [(lambda e, g=g: e.max(out=tv[:, g, 0:8], in_=vals[:, g, :])) for g in range(ng)], [vals], [tv])
                sweep("dve", [(lambda e, g=g: e.max_index(out=iv[:, g, 0:8], in_max=tv[:, g, 0:8], in_values=vals[:, g, :])) for g in range(ng)],
                      [vals, tv], [iv])
                yield
                sweep("dve", [(lambda e, g=g: e.match_replace(out=vals[:, g, :], in_to_replace=tv[:, g, 0:8], in_values=vals[:, g, :],
                                                              imm_value=-1e30)) for g in range(ng)], [tv], [vals])
                yield
                sweep("dve", [(lambda e, g=g: e.max(out=tv[:, g, 8:16], in_=vals[:, g, :])) for g in range(ng)], [vals], [tv])
                sweep("dve", [(lambda e, g=g: e.max_index(out=iv[:, g, 8:16], in_max=tv[:, g, 8:16], in_values=vals[:, g, :])) for g in range(ng)],
                      [vals, tv], [iv])
                yield

            def stageA(i):
                par = i % 2
                X, xnb, IDX, IDXT, G = Xs[par], xnbs[par], IDXs[par], IDXTs[par], Gs[par]
                b.dma("sp", None, lambda e: e.dma_start(out=X[:], in_=S_h2.h.ap()[i * 128:(i + 1) * 128, :]), reads=[S_h2], writes=[X])
                b.op("act", lambda e: e.activation(out=junkA[:], in_=X[:], func=AF.Square, accum_out=ssA[:]), reads=[X], writes=[junkA, ssA])
                b.op("dve", lambda e: e.tensor_scalar(out=ssA[:], in0=ssA[:], scalar1=1.0 / 1024, scalar2=EPS, op0=ALU.mult, op1=ALU.add),
                     reads=[ssA], writes=[ssA])
                b.op("act", lambda e: e.activation(out=ssA[:], in_=ssA[:], func=AF.Sqrt), reads=[ssA], writes=[ssA])
                b.op("dve", lambda e: e.reciprocal(out=ssA[:], in_=ssA[:]), reads=[ssA], writes=[ssA])
                b.op("dve", lambda e: e.scalar_tensor_tensor(out=xnb[:], in0=X[:], scalar=ssA[:, 0:1], in1=g2b[:], op0=ALU.mult, op1=ALU.mult),
                     reads=[X, ssA, g2b], writes=[xnb])
                yield
                for c in range(8):
                    b.op("pe", lambda e: e.transpose(out=pTa[:, c * 128:(c + 1) * 128], in_=xnb[:, c * 128:(c + 1) * 128], identity=ident[:]),
                         reads=[xnb, ident], writes=[pTa])
                b.op("act", lambda e: e.activation(out=xT[:], in_=pTa[:].rearrange("p (c t) -> p c t", c=8), func=AF.Copy), reads=[pTa], writes=[xT])
                yield
                for bq in range(4):
                    PQ = pq[cnt["q"] % 2]
                    cnt["q"] += 1
                    for j in range(4):
                        hh = bq * 4 + j
                        for kc in range(8):
                            b.op("pe", lambda e: e.matmul(PQ[:, j * 128:(j + 1) * 128], lhsT=WQ[:, kc, hh * 128:(hh + 1) * 128], rhs=xT[:, kc, :],
                                                          start=(kc == 0), stop=(kc == 7), skip_group_check=True), reads=[WQ, xT], writes=[PQ])
                    b.op("act", lambda e: e.activation(out=qT[:, bq * 4:(bq + 1) * 4, :], in_=PQ[:].rearrange("p (j t) -> p j t", j=4), func=AF.Copy),
                         reads=[PQ], writes=[qT])
                    yield
                for bq in range(4):
                    PQ = pq[cnt["q"] % 2]
                    cnt["q"] += 1
                    for j in range(4):
                        hh = bq * 4 + j
                        KT = K1T if hh % 2 == 0 else K2T
                        b.op("pe", lambda e: e.matmul(PQ[:, j * 128:(j + 1) * 128], lhsT=qT[:, hh, :], rhs=KT[:], start=True, stop=True,
                                                      skip_group_check=True), reads=[qT, KT], writes=[PQ])
                    b.op("act", lambda e: e.activation(out=S[:, bq * 4:(bq + 1) * 4, :], in_=PQ[:].rearrange("p (j t) -> p j t", j=4), func=AF.Copy),
                         reads=[PQ], writes=[S])
                    yield
                yield from top16(S, 16, T16, I16)
                b.op("dve", lambda e: e.tensor_copy(out=I16f[:], in_=I16[:]), reads=[I16], writes=[I16f])
                b.op("dve", lambda e: e.tensor_tensor(out=cand[:].rearrange("p h (a c) -> p h a c", a=16),
                                                      in0=T16v[:, :, 0, :].unsqueeze(3).to_broadcast(B4),
                                                      in1=T16v[:, :, 1, :].unsqueeze(2).to_broadcast(B4), op=ALU.add),
                     reads=[T16], writes=[cand])
                yield
                yield from top16(cand, 8, TS, CI)
                b.op("dve", lambda e: e.tensor_single_scalar(out=CIa[:], in_=CI[:], scalar=4, op=ALU.logical_shift_right), reads=[CI], writes=[CIa])
                b.op("dve", lambda e: e.tensor_single_scalar(out=CIb[:], in_=CI[:], scalar=15, op=ALU.bitwise_and), reads=[CI], writes=[CIb])
                b.op("dve", lambda e: e.tensor_copy(out=Af[:], in_=CIa[:]), reads=[CIa], writes=[Af])
                b.op("dve", lambda e: e.tensor_copy(out=Bf[:], in_=CIb[:]), reads=[CIb], writes=[Bf])
                yield
                for (SEL, half, dst) in ((Af, 0, i1s), (Bf, 1, i2s)):
                    b.op("dve", lambda e: e.tensor_tensor(out=eqv, in0=SEL[:].unsqueeze(3).to_broadcast(B4),
                                                          in1=iota16[:].unsqueeze(1).unsqueeze(1).to_broadcast(B4), op=ALU.is_equal),
                         reads=[SEL, iota16], writes=[S])
                    b.op("dve", lambda e: e.tensor_tensor(out=eqv, in0=eqv, in1=I16fv[:, :, half, :].unsqueeze(2).to_broadcast(B4), op=ALU.mult),
                         reads=[S, I16f], writes=[S])
                    b.op("dve", lambda e: e.tensor_reduce(out=dst[:].rearrange("p (h k) -> p h k", h=8), in_=eqv, axis=AX.X, op=ALU.add),
                         reads=[S], writes=[dst])
                    yield
                b.op("dve", lambda e: e.scalar_tensor_tensor(out=idxf[:], in0=i1s[:], scalar=128.0, in1=i2s[:], op0=ALU.mult, op1=ALU.add),
                     reads=[i1s, i2s], writes=[idxf])
                b.op("dve", lambda e: e.tensor_copy(out=IDX[:], in_=idxf[:]), reads=[idxf], writes=[IDX])
                b.op("act", lambda e: e.activation(out=i1b[:], in_=i1s[:], func=AF.Copy), reads=[i1s], writes=[i1b])
                b.op("act", lambda e: e.activation(out=i2b[:], in_=i2s[:], func=AF.Copy), reads=[i2s], writes=[i2b])
                b.op("pe", lambda e: e.transpose(out=pTa[:, 0:128], in_=i1b[:], identity=ident[:]), reads=[i1b, ident], writes=[pTa])
                b.op("pe", lambda e: e.transpose(out=pTa[:, 128:256], in_=i2b[:], identity=ident[:]), reads=[i2b, ident], writes=[pTa])
                b.op("act", lambda e: e.activation(out=iTf[:], in_=pTa[:, 0:256].rearrange("p (a t) -> p a t", a=2), func=AF.Copy),
                     reads=[pTa], writes=[iTf])
                yield
                b.op("dve", lambda e: e.scalar_tensor_tensor(out=idxTf[:], in0=iTf[:, 0, :], scalar=128.0, in1=iTf[:, 1, :], op0=ALU.mult, op1=ALU.add),
                     reads=[iTf], writes=[idxTf])
                b.op("dve", lambda e: e.tensor_copy(out=IDXT[:], in_=idxTf[:]), reads=[idxTf], writes=[IDXT])
                b.op("dve", lambda e: e.tensor_tensor(out=E[:], in0=TS[:], in1=TS[:, :, 0:1].to_broadcast([128, 8, 16]), op=ALU.subtract),
                     reads=[TS], writes=[E])
                b.op("act", lambda e: e.activation(out=E[:], in_=E[:], func=AF.Exp), reads=[E], writes=[E])
                b.op("dve", lambda e: e.tensor_reduce(out=Z[:], in_=E[:], axis=AX.X, op=ALU.add), reads=[E], writes=[Z])
                b.op("dve", lambda e: e.reciprocal(out=Z[:], in_=Z[:]), reads=[Z], writes=[Z])
                b.op("dve", lambda e: e.tensor_tensor(out=G[:], in0=E[:], in1=Z[:].unsqueeze(2).to_broadcast([128, 8, 16]), op=ALU.mult),
                     reads=[E, Z], writes=[G])
                yield

            def step(gen, k=1):
                if gen is None:
                    return
                for _ in range(k):
                    try:
                        next(gen)
                    except StopIteration:
                        return

            g0 = stageA(0)
            step(g0, 1000)
            for i in range(ntiles):
                par = i % 2
                X, xnb, IDX, IDXT, G = Xs[par], xnbs[par], IDXs[par], IDXTs[par], Gs[par]
                s, r0 = divmod(i * 128, NREAL)
                gN = stageA(i + 1) if i + 1 < ntiles else None
                for sbi in range(16):
                    UGb = UG[cnt["u"] % 2]
                    cnt["u"] += 1
                    for q in range(8):
                        slot = sbi * 8 + q
                        b.dma("pool", None, lambda e: e.indirect_dma_start(out=UGb[:, q, :], out_offset=None, in_=PU16.h.ap(),
                                                                           in_offset=bass.IndirectOffsetOnAxis(ap=IDX[:, slot:slot + 1], axis=0)),
                              reads=[IDX, PU16], writes=[UGb])
                    fns = []
                    for q in range(8):
                        slot = sbi * 8 + q
                        fns.append(lambda e, q=q, slot=slot: e.scalar_tensor_tensor(out=junkD[:], in0=UGb[:, q, :], scalar=1.0, in1=xnb[:],
                                                                                    op0=ALU.mult, op1=ALU.mult, accum_out=ACTV[:, slot:slot + 1]))
                    sweep("dve", fns, [UGb, xnb], [ACTV])
                    step(gN, 1)
                b.op("act", lambda e: e.activation(out=ACTV[:], in_=ACTV[:], func=AF.Gelu_apprx_tanh), reads=[ACTV], writes=[ACTV])
                b.op("dve", lambda e: e.tensor_tensor(out=coefb[:], in0=ACTV[:], in1=G[:].rearrange("p h k -> p (h k)"), op=ALU.mult),
                     reads=[ACTV, G], writes=[coefb])
                b.op("pe", lambda e: e.transpose(out=pTb[:, 0:128], in_=coefb[:], identity=ident[:]), reads=[coefb, ident], writes=[pTb])
                b.op("act", lambda e: e.activation(out=coefT[:], in_=pTb[:, 0:128], func=AF.Copy), reads=[pTb], writes=[coefT])
                b.op("dve", lambda e: e.tensor_copy(out=Lflat[:, 0:16384:129], in_=coefT[:]), reads=[coefT], writes=[L])
                for tb in range(16):
                    VGb = VG[cnt["v"] % 2]
                    cnt["v"] += 1
                    for q in range(8):
                        t = tb * 8 + q
                        b.dma("pool", None, lambda e: e.indirect_dma_start(out=VGb[:, q, :], out_offset=None, in_=PV16.h.ap(),
                                                                           in_offset=bass.IndirectOffsetOnAxis(ap=IDXT[:, t:t + 1], axis=0)),
                              reads=[IDXT, PV16], writes=[VGb])
                    for q in range(8):
                        t = tb * 8 + q
                        for half in range(2):
                            b.op("pe", lambda e: e.matmul(pOut[half][:], lhsT=L[:, t, :], rhs=VGb[:, q, half * 512:(half + 1) * 512],
                                                          start=(t == 0), stop=(t == 127)), reads=[L, VGb], writes=[pOut[half]])
                    step(gN, 1)
                for half in range(2):
                    b.op("dve", lambda e: e.tensor_tensor(out=h3[:, half * 512:(half + 1) * 512], in0=pOut[half][:],
                                                          in1=X[:, half * 512:(half + 1) * 512], op=ALU.add), reads=[pOut[half], X], writes=[h3])
                b.op("act", lambda e: e.activation(out=junkA[:], in_=h3[:], func=AF.Square, accum_out=ssC[:]), reads=[h3], writes=[junkA, ssC])
                b.op("dve", lambda e: e.tensor_scalar(out=ssC[:], in0=ssC[:], scalar1=1.0 / 1024, scalar2=EPS, op0=ALU.mult, op1=ALU.add),
                     reads=[ssC], writes=[ssC])
                b.op("act", lambda e: e.activation(out=ssC[:], in_=ssC[:], func=AF.Sqrt), reads=[ssC], writes=[ssC])
                b.op("dve", lambda e: e.reciprocal(out=ssC[:], in_=ssC[:]), reads=[ssC], writes=[ssC])
                b.op("dve", lambda e: e.scalar_tensor_tensor(out=h3[:], in0=h3[:], scalar=ssC[:, 0:1], in1=gfb[:], op0=ALU.mult, op1=ALU.mult),
                     reads=[h3, ssC, gfb], writes=[h3])
                b.dma("sp", None, lambda e: e.dma_start(out=out_d.ap()[s, r0:r0 + 128, :], in_=h3[:]), reads=[h3], writes=[OUT])
                step(gN, 1000)
        b.barrier()

    progs = {1: phase1, 2: phase2, 3: phase3a, 4: phase3b, 5: phase5}
    for p in phases:
        if p in progs:
            progs[p]()
    b.barrier()
    return nc


def _pc(v, nchunk):
    return np.ascontiguousarray(np.asarray(v, np.float32).reshape(nchunk, 128).T)


def prep_common(inp):
    f = lambda a: np.asarray(a, np.float32)
    w_in = f(inp["w_in"])[0]
    z32 = np.zeros((1024, 32), np.float32)
    kr = w_in[:, 2688:2720]
    krs = np.concatenate([kr[:, 16:], kr[:, :16]], axis=1)
    w_in_r = np.concatenate([
        w_in[:, 0:1024], w_in[:, 1024:2048], w_in[:, 2048:2432], w_in[:, 2432:2688],
        kr, z32, kr, z32, krs, z32, krs, z32,
        w_in[:, 2720:3744], w_in[:, 3744:4768]], axis=1)
    assert w_in_r.shape == (1024, 4992)
    conv_w = f(inp["conv_w"])[0]
    cw = np.ascontiguousarray(conv_w.reshape(4, 8, 128).transpose(2, 1, 0))
    w_uq = f(inp["w_uq"])[0].reshape(384, 16, 96)
    nope = w_uq[:, :, :64].reshape(384, 1024)
    rope = w_uq[:, :, 64:]
    ropes = np.concatenate([rope[:, :, 16:], rope[:, :, :16]], axis=2)
    z = np.zeros((384, 16, 32), np.float32)
    rope_p = np.concatenate([rope, z], axis=2).reshape(384, 1024)
    ropes_p = np.concatenate([ropes, z], axis=2).reshape(384, 1024)
    w_uq_r = np.concatenate([nope, rope_p, ropes_p, np.zeros((384, 1024), np.float32)], axis=1)
    w_ukv = f(inp["w_ukv"])[0].reshape(256, 16, 128)
    w_ukv_r = np.concatenate([w_ukv[:, :, :64].reshape(256, 1024), w_ukv[:, :, 64:].reshape(256, 1024)], axis=1)
    pos = (np.arange(SEQC, dtype=np.float32) - 112.0).astype(np.float32)
    inv = np.power(np.float32(10000.0), -np.arange(16, dtype=np.float32) * np.float32(2.0 / 32)).astype(np.float32)
    ang = (pos[None, :] * inv[:, None]).astype(np.float32)
    c, s_ = np.cos(ang).astype(np.float32), np.sin(ang).astype(np.float32)
    cos32 = np.concatenate([c, c], axis=0)
    sin32 = np.concatenate([-s_, s_], axis=0)
    zz = np.zeros((32, SEQC), np.float32)
    cos2 = np.concatenate([cos32, zz, cos32, zz], axis=0)
    sin2s = np.concatenate([sin32, zz, sin32, zz], axis=0)
    return {
        "meta": f(inp["meta_tokens"]),
        "g1": _pc(inp["norm1_g"][0], 8),
        "w_in": np.ascontiguousarray(w_in_r),
        "convw": cw,
        "convb": _pc(inp["conv_b"][0], 8),
        "rg_wa": np.ascontiguousarray(f(inp["rg_wa"])[0].transpose(1, 0, 2)),
        "rg_wx": np.ascontiguousarray(f(inp["rg_wx"])[0].transpose(1, 0, 2)),
        "rg_ba": _pc(inp["rg_ba"][0], 8),
        "rg_bx": _pc(inp["rg_bx"][0], 8),
        "rg_lam": _pc(inp["rg_lambda"][0], 8),
        "w_rnn_out": f(inp["w_rnn_out"])[0],
        "qg": _pc(inp["q_norm_g"][0], 3),
        "kvg": _pc(inp["kv_norm_g"][0], 2),
        "w_uq": np.ascontiguousarray(w_uq_r),
        "w_ukv": np.ascontiguousarray(w_ukv_r),
        "w_attn_out": f(inp["w_attn_out"])[0],
        "w_out": f(inp["w_out"])[0],
        "g2": f(inp["norm2_g"]).reshape(1, 1024),
        "peer_wq": f(inp["peer_wq"])[0],
        "keys1T": np.ascontiguousarray(f(inp["peer_keys1"])[0].T),
        "keys2T": np.ascontiguousarray(f(inp["peer_keys2"])[0].T),
        "peer_u": f(inp["peer_u"])[0],
        "peer_v": f(inp["peer_v"])[0],
        "gf": f(inp["final_g"]).reshape(1, 1024),
        "cos2": cos2,
        "sin2s": sin2s,
    }


def kernel(**inputs):
    NC = 8
    NSEQ = 4
    common = prep_common(inputs)
    x = np.asarray(inputs["x"], np.float32)
    nc = build(NSEQ)
    in_maps = []
    for c in range(NC):
        m = dict(common)
        m["x"] = np.ascontiguousarray(x[c * NSEQ:(c + 1) * NSEQ])
        in_maps.append(m)
    res = run_bass_kernel_spmd(nc, in_maps, core_ids=list(range(NC)))
    return np.concatenate([r["out"] for r in res.results], axis=0)
```

```python
from contextlib import ExitStack
import numpy as np
import concourse.bass as bass
import concourse.mybir as mybir
from concourse.bass_utils import run_bass_kernel_spmd

F32 = mybir.dt.float32
BF16 = mybir.dt.bfloat16
I32 = mybir.dt.int32
U32 = mybir.dt.uint32
AF = mybir.ActivationFunctionType
ALU = mybir.AluOpType
AX = mybir.AxisListType

SEQC = 2176
NREAL = 2048
EPS = 1e-6
GROUPS = [(0, 128)] + [(128 + 512 * g, 512) for g in range(4)]


class T:
    def __init__(self, h, name, accum=False):
        self.h = h
        self.name = name
        self.w = {}
        self.r = {}
        self.accum = accum

    def __getitem__(self, k):
        return self.h[k]


class B:
    def __init__(self, nc):
        self.nc = nc
        self.E = {"pe": nc.tensor, "act": nc.scalar, "dve": nc.vector, "pool": nc.gpsimd, "sp": nc.sync}
        self.sems = {}
        self.cnt = {}
        self.seen = {k: {} for k in self.E}
        for k in self.E:
            self.sems[k] = nc.alloc_semaphore("s_" + k)
            self.cnt[k] = 0
        self.nins = 0

    def newsem(self, name):
        self.sems[name] = self.nc.alloc_semaphore("s_" + name)
        self.cnt[name] = 0
        return name

    def _wait(self, eng, evs):
        for key, val in evs.items():
            if val <= 0 or (eng == "pe" and key == "pe"):
                continue
            if self.seen[eng].get(key, 0) >= val:
                continue
            self.E[eng].wait_ge(self.sems[key], val)
            self.seen[eng][key] = val
            self.nins += 1

    @staticmethod
    def _merge(d, e):
        for k, v in e.items():
            if d.get(k, 0) < v:
                d[k] = v

    def _deps(self, reads, writes):
        evs = {}
        for t in reads:
            self._merge(evs, t.w)
        for t in writes:
            if not t.accum:
                self._merge(evs, t.w)
                self._merge(evs, t.r)
        return evs

    def _commit(self, ev, reads, writes):
        for t in reads:
            if not t.accum:
                self._merge(t.r, ev)
        for t in writes:
            if t.accum:
                self._merge(t.w, ev)
            else:
                t.w = dict(ev)
                t.r = {}

    def op(self, eng, fn, reads=(), writes=()):
        self._wait(eng, self._deps(reads, writes))
        ins = fn(self.E[eng])
        self.cnt[eng] += 1
        ins.then_inc(self.sems[eng], 1)
        self.nins += 1
        self._commit({eng: self.cnt[eng]}, reads, writes)

    def dma(self, q, sem, fn, reads=(), writes=()):
        own = [t for t in writes if not t.accum] or [t for t in reads if not t.accum]
        t0 = own[0]
        if getattr(t0, "sem", None) is None:
            t0.sem = self.newsem("d%d" % len(self.sems))
        sem = t0.sem
        deps = self._deps(reads, writes)
        if writes and not writes[0].accum:
            wr = {}
            for t in writes:
                self._merge(wr, t.r)
            if deps.get(sem, 0) > wr.get(sem, 0):
                v = wr.get(sem, 0)
                if v > 0:
                    deps[sem] = v
                else:
                    deps.pop(sem)
        self._wait(q, deps)
        ins = fn(self.E[q])
        self.cnt[sem] += 16
        ins.then_inc(self.sems[sem], 16)
        self.nins += 1
        self._commit({sem: self.cnt[sem]}, reads, writes)

    def barrier(self):
        allev = {k: v for k, v in self.cnt.items() if v > 0}
        for e in self.E:
            self._wait(e, allev)


def build(NSEQ, phases=(1, 2, 3, 4, 5), debug=False, p5tiles=None):
    nc = bass.Bass("TRN2", target_bir_lowering=False)
    b = B(nc)
    NT = NSEQ * SEQC
    NR = NSEQ * NREAL
    skind = "ExternalOutput" if debug else "Internal"

    def din(name, shape, dt=F32):
        return nc.dram_tensor(name, list(shape), dt, kind="ExternalInput")

    def dscr(name, shape, dt=F32):
        return T(nc.dram_tensor(name, list(shape), dt, kind=skind), name, accum=True)

    x_d = din("x", [NSEQ, NREAL, 1024])
    meta_d = din("meta", [16, 1024])
    g1_d = din("g1", [128, 8])
    win_d = din("w_in", [1024, 4992])
    cw_d = din("convw", [128, 8, 4])
    cb_d = din("convb", [128, 8])
    rgwa_d = din("rg_wa", [128, 8, 128])
    rgwx_d = din("rg_wx", [128, 8, 128])
    rgba_d = din("rg_ba", [128, 8])
    rgbx_d = din("rg_bx", [128, 8])
    lam_d = din("rg_lam", [128, 8])
    wro_d = din("w_rnn_out", [1024, 1024])
    qg_d = din("qg", [128, 3])
    kvg_d = din("kvg", [128, 2])
    wuq_d = din("w_uq", [384, 4096])
    wukv_d = din("w_ukv", [256, 2048])
    wao_d = din("w_attn_out", [1024, 1024])
    wo_d = din("w_out", [1024, 1024])
    g2_d = din("g2", [1, 1024])
    wq_d = din("peer_wq", [1024, 2048])
    k1T_d = din("keys1T", [128, 128])
    k2T_d = din("keys2T", [128, 128])
    pu_d = din("peer_u", [16384, 1024])
    pv_d = din("peer_v", [16384, 1024])
    gf_d = din("gf", [1, 1024])
    cos_d = din("cos2", [128, SEQC])
    sin_d = din("sin2s", [128, SEQC])
    out_d = nc.dram_tensor("out", [NSEQ, NREAL, 1024], F32, kind="ExternalOutput")
    OUT = T(out_d, "out", accum=True)

    S_xr = dscr("S_xr", [1024, NT])
    S_gr = dscr("S_gr", [1024, NT])
    S_cq = dscr("S_cq", [384, NT])
    S_ckv = dscr("S_ckv", [256, NT])
    S_kr = dscr("S_kr", [128, NT])
    S_krs = dscr("S_krs", [128, NT])
    S_grnn = dscr("S_grnn", [1024, NT])
    S_gatt = dscr("S_gatt", [1024, NT])
    S_mrnn = dscr("S_mrnn", [1024, NT])
    S_oT = dscr("S_oT", [1024, NT], BF16)
    S_h2 = dscr("S_h2", [NR, 1024])
    UV16 = T(nc.dram_tensor("UV16", [16384, 2048], BF16, kind="Internal"), "UV16", accum=True)

    ld = b.newsem("ld")
    st = b.newsem("st")
    wl = b.newsem("wl")

    def mk(es, pre):
        def sb(name, shape, dt):
            return T(es.enter_context(nc.sbuf_tensor(pre + name, list(shape), dt)), name)

        def ps(name, shape, dt):
            return T(es.enter_context(nc.psum_tensor(pre + name, list(shape), dt)), name)
        return sb, ps

    def make_ident(sb):
        identf = sb("identf", [128, 128], F32)
        ident = sb("ident", [128, 128], BF16)
        b.op("pool", lambda e: e.memset(identf[:], 1.0), writes=[identf])
        b.op("pool", lambda e: e.affine_select(out=identf[:], in_=identf[:], pattern=[[-1, 128]],
                                               compare_op=ALU.is_equal, fill=0.0, base=0, channel_multiplier=1),
             reads=[identf], writes=[identf])
        b.op("dve", lambda e: e.tensor_copy(out=ident[:], in_=identf[:]), reads=[identf], writes=[ident])
        return ident, identf

    def load_small(sb, name, d, shape):
        t = sb(name, shape, F32)
        b.dma("sp", wl, lambda e: e.dma_start(out=t[:], in_=d.ap()), writes=[t])
        return t

    def load_w_cast(dst, dst_ap, src_ap):
        b.dma("pool", wl, lambda e: e.dma_start(out=dst_ap, in_=src_ap), writes=[dst])

    def phase1():
        with ExitStack() as es:
            sb, ps = mk(es, "p1_")
            W = sb("W1", [128, 8, 4992], BF16)
            g1 = load_small(sb, "g1s", g1_d, [128, 8])
            stg = [sb("wstg%d" % i, [128, 4992], F32) for i in range(2)]
            for kc in range(8):
                s_ = stg[kc % 2]
                b.dma("sp", wl, lambda e: e.dma_start(out=s_[:], in_=win_d.ap()[kc * 128:(kc + 1) * 128, :]), writes=[s_])
                b.op("dve", lambda e: e.tensor_scalar(out=W[:, kc, :], in0=s_[:], scalar1=g1[:, kc:kc + 1], scalar2=None,
                                                      op0=ALU.mult), reads=[s_, g1], writes=[W])
            ident, _ = make_ident(sb)
            xt = [sb("xt%d" % i, [128, 1024], F32) for i in range(2)]
            junk = sb("junk", [128, 1024], BF16)
            ss = [sb("ss%d" % i, [128, 1], F32) for i in range(2)]
            xb = [sb("xb%d" % i, [128, 1024], BF16) for i in range(2)]
            n1T = [sb("n1T%d" % i, [128, 8, 512], BF16) for i in range(2)]
            pT = [ps("pT%d" % i, [128, 1024], BF16) for i in range(2)]
            pa = [ps("pa%d" % i, [128, 512], F32) for i in range(4)]
            stage = [sb("stage%d" % i, [128, 512], F32) for i in range(4)]
            chunks = []
            for i in range(8):
                chunks.append((S_xr, i * 128, None, 128))
            for i in range(8):
                chunks.append((S_gr, i * 128, AF.Gelu_apprx_tanh, 128))
            for i in range(3):
                chunks.append((S_cq, i * 128, None, 128))
            for i in range(2):
                chunks.append((S_ckv, i * 128, None, 128))
            chunks.append((S_kr, 0, None, 128))
            chunks.append((S_krs, 0, None, 128))
            for i in range(8):
                chunks.append((S_grnn, i * 128, AF.Sigmoid, 128))
            for i in range(8):
                chunks.append((S_gatt, i * 128, AF.Sigmoid, 128))
            ti = 0
            gi = 0
            ci = 0
            cv = [sb("cv%d" % i, [128, 8, 1024], BF16) for i in range(2)]
            conv = [(src, off, c) for (src, off) in ((pu_d, 0), (pv_d, 1024)) for c in range(16)]
            cvi = [0]

            def convert_some(k):
                for _ in range(k):
                    if cvi[0] >= len(conv):
                        return
                    src, off, c = conv[cvi[0]]
                    dst = UV16
                    CV = cv[cvi[0] % 2]
                    cvi[0] += 1
                    b.dma("pool", None, lambda e: e.dma_start(out=CV[:], in_=src.ap()[c * 1024:(c + 1) * 1024, :].rearrange("(p r) d -> p r d", p=128)),
                          writes=[CV])
                    b.dma("sp", None, lambda e: e.dma_start(out=dst.h.ap()[c * 1024:(c + 1) * 1024, off:off + 1024].rearrange("(p r) d -> p r d", p=128), in_=CV[:]),
                          reads=[CV], writes=[dst])

            for s in range(NSEQ):
                for (c0, n) in GROUPS:
                    nT = n1T[gi % 2]
                    gi += 1
                    convert_some(8 if NSEQ == 1 else 2)
                    for j in range(n // 128):
                        t = c0 // 128 + j
                        X = xt[ti % 2]
                        SS = ss[ti % 2]
                        XB = xb[ti % 2]
                        PT = pT[ti % 2]
                        ti += 1
                        if t == 0:
                            b.op("pool", lambda e: e.memset(X[:], 0.0), writes=[X])
                            b.dma("sp", ld, lambda e: e.dma_start(out=X[112:128, :], in_=meta_d.ap()), writes=[X])
                        else:
                            b.dma("sp", ld, lambda e: e.dma_start(out=X[:], in_=x_d.ap()[s, (t - 1) * 128:t * 128, :]), writes=[X])
                        b.op("act", lambda e: e.activation(out=junk[:], in_=X[:], func=AF.Square, accum_out=SS[:]),
                             reads=[X], writes=[junk, SS])
                        b.op("dve", lambda e: e.tensor_scalar(out=SS[:], in0=SS[:], scalar1=1.0 / 1024, scalar2=EPS,
                                                              op0=ALU.mult, op1=ALU.add), reads=[SS], writes=[SS])
                        b.op("act", lambda e: e.activation(out=SS[:], in_=SS[:], func=AF.Sqrt), reads=[SS], writes=[SS])
                        b.op("dve", lambda e: e.reciprocal(out=SS[:], in_=SS[:]), reads=[SS], writes=[SS])
                        b.op("act", lambda e: e.activation(out=XB[:], in_=X[:], func=AF.Copy, scale=SS[:]),
                             reads=[X, SS], writes=[XB])
                        for c in range(8):
                            b.op("pe", lambda e: e.transpose(out=PT[:, c * 128:(c + 1) * 128], in_=XB[:, c * 128:(c + 1) * 128],
                                                             identity=ident[:]), reads=[XB, ident], writes=[PT])
                        b.op("dve", lambda e: e.tensor_copy(out=nT[:, :, j * 128:(j + 1) * 128],
                                                            in_=PT[:].rearrange("p (c t) -> p c t", c=8)),
                             reads=[PT], writes=[nT])
                    for oc, (S_, r0, fn, m) in enumerate(chunks):
                        PA = pa[ci % 4]
                        SG = stage[ci % 4]
                        ci += 1
                        for kc in range(8):
                            b.op("pe", lambda e: e.matmul(PA[:, 0:n], lhsT=W[:, kc, oc * 128:(oc + 1) * 128], rhs=nT[:, kc, 0:n],
                                                          start=(kc == 0), stop=(kc == 7)), reads=[W, nT], writes=[PA])
                        if fn is None:
                            b.op("dve", lambda e: e.tensor_copy(out=SG[:, 0:n], in_=PA[:, 0:n]), reads=[PA], writes=[SG])
                        else:
                            b.op("act", lambda e: e.activation(out=SG[:, 0:n], in_=PA[:, 0:n], func=fn), reads=[PA], writes=[SG])
                        col = s * SEQC + c0
                        b.dma("pool", st, lambda e: e.dma_start(out=S_.h.ap()[r0:r0 + 128, col:col + n], in_=SG[:, 0:n]),
                              reads=[SG], writes=[S_])
            convert_some(len(conv))
        b.barrier()

    def phase2():
        with ExitStack() as es:
            sb, ps = mk(es, "p2_")
            WA = sb("WA", [128, 8, 128], BF16)
            WX = sb("WX", [128, 8, 128], BF16)
            WRO = sb("WRO", [128, 8, 1024], BF16)
            load_w_cast(WA, WA[:], rgwa_d.ap())
            load_w_cast(WX, WX[:], rgwx_d.ap())
            load_w_cast(WRO, WRO[:], wro_d.ap().rearrange("(k p) f -> p k f", p=128))
            cw = load_small(sb, "cw", cw_d, [128, 8, 4])
            cb = load_small(sb, "cb", cb_d, [128, 8])
            ba = load_small(sb, "ba", rgba_d, [128, 8])
            bx = load_small(sb, "bx", rgbx_d, [128, 8])
            lam = load_small(sb, "lam", lam_d, [128, 8])
            c8 = sb("c8", [128, 8], F32)
            c16 = sb("c16", [128, 8], F32)
            b.op("act", lambda e: e.activation(out=c8[:], in_=lam[:], func=AF.Exp, scale=-1.0), reads=[lam], writes=[c8])
            b.op("act", lambda e: e.activation(out=c8[:], in_=c8[:], func=AF.Ln, bias=1.0), reads=[c8], writes=[c8])
            b.op("dve", lambda e: e.tensor_scalar(out=c16[:], in0=c8[:], scalar1=-16.0, scalar2=None, op0=ALU.mult),
                 reads=[c8], writes=[c16])
            b.op("dve", lambda e: e.tensor_scalar(out=c8[:], in0=c8[:], scalar1=-8.0, scalar2=None, op0=ALU.mult),
                 reads=[c8, c16], writes=[c8])
            XR = sb("XR", [128, SEQC], F32)
            Y = sb("Y", [128, SEQC], F32)
            YB = sb("YB", [128, SEQC], BF16)
            A = sb("A", [128, SEQC], F32)
            U = sb("U", [128, SEQC], F32)
            H = sb("H", [128, SEQC], F32)
            GR = sb("GR", [128, SEQC], F32)
            ZT = sb("ZT", [128, 8, SEQC], BF16)
            tr = sb("tr", [128, 512], F32)
            ta2 = sb("ta2", [128, 512], F32)
            ti_ = sb("ti", [128, 512], F32)
            gs = [sb("gs%d" % i, [128, 512], F32) for i in range(2)]
            stage = [sb("stage%d" % i, [128, 512], F32) for i in range(2)]
            pA = ps("pA", [128, 512], F32)
            pX = ps("pX", [128, 512], F32)
            pO = [ps("pO%d" % i, [128, 512], F32) for i in range(2)]
            V0 = 112
            NV = SEQC - V0
            k = 0
            for s in range(NSEQ):
                sc = s * SEQC
                for n_ in range(8):
                    r0 = n_ * 128
                    b.dma("sp", ld, lambda e: e.dma_start(out=XR[:], in_=S_xr.h.ap()[r0:r0 + 128, sc:sc + SEQC]),
                          reads=[S_xr], writes=[XR])
                    b.dma("sp", ld, lambda e: e.dma_start(out=GR[:], in_=S_gr.h.ap()[r0:r0 + 128, sc:sc + SEQC]),
                          reads=[S_gr], writes=[GR])
                    b.op("dve", lambda e: e.tensor_scalar(out=Y[:, V0:SEQC], in0=XR[:, V0 - 3:SEQC - 3], scalar1=cw[:, n_, 0:1],
                                                          scalar2=cb[:, n_:n_ + 1], op0=ALU.mult, op1=ALU.add),
                         reads=[XR, cw, cb], writes=[Y])
                    for kk in range(1, 4):
                        b.op("dve", lambda e: e.scalar_tensor_tensor(out=Y[:, V0:SEQC], in0=XR[:, V0 - 3 + kk:SEQC - 3 + kk],
                                                                     scalar=cw[:, n_, kk:kk + 1], in1=Y[:, V0:SEQC],
                                                                     op0=ALU.mult, op1=ALU.add), reads=[XR, cw, Y], writes=[Y])
                    b.op("act", lambda e: e.activation(out=YB[:, V0:SEQC], in_=Y[:, V0:SEQC], func=AF.Copy), reads=[Y], writes=[YB])
                    for (c0, n) in GROUPS:
                        if c0 == 0:
                            c0, n = V0, 16
                        cs = slice(c0, c0 + n)
                        b.op("pe", lambda e: e.matmul(pA[:, 0:n], lhsT=WA[:, n_, :], rhs=YB[:, cs], start=True, stop=True),
                             reads=[WA, YB], writes=[pA])
                        b.op("pe", lambda e: e.matmul(pX[:, 0:n], lhsT=WX[:, n_, :], rhs=YB[:, cs], start=True, stop=True),
                             reads=[WX, YB], writes=[pX])
                        b.op("act", lambda e: e.activation(out=tr[:, 0:n], in_=pA[:, 0:n], func=AF.Sigmoid, bias=ba[:, n_:n_ + 1]),
                             reads=[pA, ba], writes=[tr])
                        b.op("act", lambda e: e.activation(out=ti_[:, 0:n], in_=pX[:, 0:n], func=AF.Sigmoid, bias=bx[:, n_:n_ + 1]),
                             reads=[pX, bx], writes=[ti_])
                        b.op("act", lambda e: e.activation(out=A[:, cs], in_=tr[:, 0:n], func=AF.Exp, scale=c8[:, n_:n_ + 1]),
                             reads=[tr, c8], writes=[A])
                        b.op("act", lambda e: e.activation(out=ta2[:, 0:n], in_=tr[:, 0:n], func=AF.Exp, scale=c16[:, n_:n_ + 1]),
                             reads=[tr, c16], writes=[ta2])
                        b.op("dve", lambda e: e.tensor_scalar(out=ta2[:, 0:n], in0=ta2[:, 0:n], scalar1=-1.0, scalar2=1.0,
                                                              op0=ALU.mult, op1=ALU.add), reads=[ta2], writes=[ta2])
                        b.op("act", lambda e: e.activation(out=ta2[:, 0:n], in_=ta2[:, 0:n], func=AF.Sqrt), reads=[ta2], writes=[ta2])
                        b.op("dve", lambda e: e.tensor_tensor(out=ti_[:, 0:n], in0=ti_[:, 0:n], in1=Y[:, cs], op=ALU.mult),
                             reads=[ti_, Y], writes=[ti_])
                        b.op("dve", lambda e: e.tensor_tensor(out=U[:, cs], in0=ti_[:, 0:n], in1=ta2[:, 0:n], op=ALU.mult),
                             reads=[ti_, ta2], writes=[U])
                    b.op("dve", lambda e: e.tensor_tensor_scan(out=H[:, V0:SEQC], data0=A[:, V0:SEQC], data1=U[:, V0:SEQC],
                                                               initial=0.0, op0=ALU.mult, op1=ALU.add), reads=[A, U], writes=[H])
                    b.op("dve", lambda e: e.tensor_tensor(out=ZT[:, n_, V0:SEQC], in0=H[:, V0:SEQC], in1=GR[:, V0:SEQC], op=ALU.mult),
                         reads=[H, GR], writes=[ZT])
                for (c0, n) in GROUPS[1:]:
                    cs = slice(c0, c0 + n)
                    for dc in range(8):
                        PO = pO[k % 2]
                        G = gs[k % 2]
                        SG = stage[k % 2]
                        k += 1
                        b.dma("sp", ld, lambda e: e.dma_start(out=G[:, 0:n], in_=S_grnn.h.ap()[dc * 128:(dc + 1) * 128, sc + c0:sc + c0 + n]),
                              reads=[S_grnn], writes=[G])
                        for kc in range(8):
                            b.op("pe", lambda e: e.matmul(PO[:, 0:n], lhsT=WRO[:, kc, dc * 128:(dc + 1) * 128], rhs=ZT[:, kc, cs],
                                                          start=(kc == 0), stop=(kc == 7)), reads=[WRO, ZT], writes=[PO])
                        b.op("dve", lambda e: e.tensor_tensor(out=SG[:, 0:n], in0=PO[:, 0:n], in1=G[:, 0:n], op=ALU.mult),
                             reads=[PO, G], writes=[SG])
                        b.dma("pool", st, lambda e: e.dma_start(out=S_mrnn.h.ap()[dc * 128:(dc + 1) * 128, sc + c0:sc + c0 + n],
                                                                in_=SG[:, 0:n]), reads=[SG], writes=[S_mrnn])
        b.barrier()


    def phase3a():
        with ExitStack() as es:
            sb, ps = mk(es, "p3_")
            WUQ = sb("WUQ", [128, 3, 3072], BF16)
            WUKV = sb("WUKV", [128, 2, 2048], BF16)
            qg = load_small(sb, "qg", qg_d, [128, 3])
            kvg = load_small(sb, "kvg", kvg_d, [128, 2])
            wst = sb("wst", [128, 3072], F32)
            for kc in range(3):
                b.dma("sp", wl, lambda e: e.dma_start(out=wst[:], in_=wuq_d.ap()[kc * 128:(kc + 1) * 128, 0:3072]), writes=[wst])
                b.op("dve", lambda e: e.tensor_scalar(out=WUQ[:, kc, :], in0=wst[:], scalar1=qg[:, kc:kc + 1], scalar2=None,
                                                      op0=ALU.mult), reads=[wst, qg], writes=[WUQ])
            for kc in range(2):
                b.dma("sp", wl, lambda e: e.dma_start(out=wst[:, 0:2048], in_=wukv_d.ap()[kc * 128:(kc + 1) * 128, :]), writes=[wst])
                b.op("dve", lambda e: e.tensor_scalar(out=WUKV[:, kc, :], in0=wst[:, 0:2048], scalar1=kvg[:, kc:kc + 1], scalar2=None,
                                                      op0=ALU.mult), reads=[wst, kvg], writes=[WUKV])
            ident, identf = make_ident(sb)
            ones = sb("ones", [128, 128], F32)
            b.op("pool", lambda e: e.memset(ones[:], 1.0), writes=[ones])
            tri = sb("tri", [128, 128], BF16)
            b.op("pool", lambda e: e.memset(identf[:], 1.0), reads=[identf], writes=[identf])
            b.op("pool", lambda e: e.affine_select(out=identf[:], in_=identf[:], pattern=[[1, 128]], compare_op=ALU.is_ge,
                                                   fill=0.0, base=0, channel_multiplier=-1), reads=[identf], writes=[identf])
            b.op("dve", lambda e: e.tensor_copy(out=tri[:], in_=identf[:]), reads=[identf], writes=[tri])
            Kn = sb("Kn", [128, 8, SEQC], BF16)
            Kr2 = sb("Kr2", [128, SEQC], BF16)
            V = sb("V", [128, 17, 16, 65], BF16)
            b.op("pool", lambda e: e.memset(V[:], 1.0), writes=[V])
            cq_t = sb("cq_t", [128, 3, 512], F32)
            ckv_t = sb("ckv_t", [128, 2, 512], F32)
            sq = sb("sq", [128, 3, 512], F32)
            rstd = sb("rstd", [128, 512], F32)
            cqn = sb("cqn", [128, 3, 512], BF16)
            ckvn = sb("ckvn", [128, 2, 512], BF16)
            kr_t = sb("kr_t", [128, 512], F32)
            krs_t = sb("krs_t", [128, 512], F32)
            cos_t = sb("cos_t", [128, 512], F32)
            sin_t = sb("sin_t", [128, 512], F32)
            tmp1 = sb("tmp1", [128, 512], F32)
            tmp2 = sb("tmp2", [128, 512], F32)
            Qn = sb("Qn", [128, 8, 512], BF16)
            Qr = sb("Qr", [128, 8, 512], BF16)
            PTs = [sb("PTs%d" % i, [128, 512], BF16) for i in range(3)]
            o_tm = sb("o_tm", [128, 4, 1024], BF16)
            oT = sb("oT", [128, 8, 512], BF16)
            rec = sb("rec", [128, 4], F32)
            pn = ps("pn", [128, 512], F32)
            pk = [ps("pk%d" % i, [128, 512], F32) for i in range(2)]
            pS = [ps("pS%d" % i, [128, 512], F32) for i in range(2)]
            pO = [ps("pO%d" % i, [128, 512], F32) for i in range(2)]
            pT = ps("pT", [128, 1024], BF16)
            scale = float(96 ** -0.5)
            ki = [0]

            def nextpk():
                ki[0] += 1
                return pk[ki[0] % 2]

            def rmsn(src, nch, dst, nfeat, n):
                b.op("act", lambda e: e.activation(out=sq[:, 0:nch, 0:n], in_=src[:, :, 0:n], func=AF.Square), reads=[src], writes=[sq])
                for c in range(nch):
                    b.op("pe", lambda e: e.matmul(pn[:, 0:n], lhsT=ones[:], rhs=sq[:, c, 0:n], start=(c == 0), stop=(c == nch - 1)),
                         reads=[ones, sq], writes=[pn])
                b.op("dve", lambda e: e.tensor_scalar(out=rstd[:, 0:n], in0=pn[:, 0:n], scalar1=1.0 / nfeat, scalar2=EPS,
                                                      op0=ALU.mult, op1=ALU.add), reads=[pn], writes=[rstd])
                b.op("act", lambda e: e.activation(out=rstd[:, 0:n], in_=rstd[:, 0:n], func=AF.Sqrt), reads=[rstd], writes=[rstd])
                b.op("dve", lambda e: e.reciprocal(out=rstd[:, 0:n], in_=rstd[:, 0:n]), reads=[rstd], writes=[rstd])
                for c in range(nch):
                    b.op("dve", lambda e: e.tensor_tensor(out=dst[:, c, 0:n], in0=src[:, c, 0:n], in1=rstd[:, 0:n], op=ALU.mult),
                         reads=[src, rstd], writes=[dst])

            si = 0
            for s in range(NSEQ):
                sc = s * SEQC
                for (c0, n) in GROUPS:
                    col = sc + c0
                    b.dma("sp", ld, lambda e: e.dma_start(out=cq_t[:, :, 0:n],
                                                          in_=S_cq.h.ap()[:, col:col + n].rearrange("(c p) n -> p c n", p=128)),
                          reads=[S_cq], writes=[cq_t])
                    b.dma("sp", ld, lambda e: e.dma_start(out=ckv_t[:, :, 0:n],
                                                          in_=S_ckv.h.ap()[:, col:col + n].rearrange("(c p) n -> p c n", p=128)),
                          reads=[S_ckv], writes=[ckv_t])
                    b.dma("sp", ld, lambda e: e.dma_start(out=kr_t[:, 0:n], in_=S_kr.h.ap()[:, col:col + n]), reads=[S_kr], writes=[kr_t])
                    b.dma("sp", ld, lambda e: e.dma_start(out=krs_t[:, 0:n], in_=S_krs.h.ap()[:, col:col + n]), reads=[S_krs], writes=[krs_t])
                    b.dma("sp", ld, lambda e: e.dma_start(out=cos_t[:, 0:n], in_=cos_d.ap()[:, c0:c0 + n]), writes=[cos_t])
                    b.dma("sp", ld, lambda e: e.dma_start(out=sin_t[:, 0:n], in_=sin_d.ap()[:, c0:c0 + n]), writes=[sin_t])
                    rmsn(cq_t, 3, cqn, 384.0, n)
                    rmsn(ckv_t, 2, ckvn, 256.0, n)
                    for j in range(8):
                        P_ = nextpk()
                        for kc in range(2):
                            b.op("pe", lambda e: e.matmul(P_[:, 0:n], lhsT=WUKV[:, kc, j * 128:(j + 1) * 128], rhs=ckvn[:, kc, 0:n],
                                                          start=(kc == 0), stop=(kc == 1)), reads=[WUKV, ckvn], writes=[P_])
                        b.op("act", lambda e: e.activation(out=Kn[:, j, c0:c0 + n], in_=P_[:, 0:n], func=AF.Copy), reads=[P_], writes=[Kn])
                    b.op("dve", lambda e: e.tensor_tensor(out=tmp1[:, 0:n], in0=kr_t[:, 0:n], in1=cos_t[:, 0:n], op=ALU.mult),
                         reads=[kr_t, cos_t], writes=[tmp1])
                    b.op("dve", lambda e: e.tensor_tensor(out=tmp2[:, 0:n], in0=krs_t[:, 0:n], in1=sin_t[:, 0:n], op=ALU.mult),
                         reads=[krs_t, sin_t], writes=[tmp2])
                    b.op("dve", lambda e: e.tensor_tensor(out=Kr2[:, c0:c0 + n], in0=tmp1[:, 0:n], in1=tmp2[:, 0:n], op=ALU.add),
                         reads=[tmp1, tmp2], writes=[Kr2])
                    for jt in range(n // 128):
                        t = c0 // 128 + jt
                        if t == 0:
                            lo, M = 112, 16
                        else:
                            lo, M = jt * 128, 128
                        for half in range(2):
                            P_ = nextpk()
                            for kc in range(2):
                                b.op("pe", lambda e: e.matmul(P_[0:M, :], lhsT=ckvn[:, kc, lo:lo + M],
                                                              rhs=WUKV[:, kc, 1024 + half * 512:1024 + (half + 1) * 512],
                                                              start=(kc == 0), stop=(kc == 1)), reads=[WUKV, ckvn], writes=[P_])
                            b.op("act", lambda e: e.activation(out=V[0:M, t, half * 8:(half + 1) * 8, 0:64],
                                                               in_=P_[0:M, :].rearrange("p (h d) -> p h d", h=8), func=AF.Copy),
                                 reads=[P_], writes=[V])
                    if c0 == 0:
                        continue
                    for j in range(8):
                        P_ = nextpk()
                        for kc in range(3):
                            b.op("pe", lambda e: e.matmul(P_[:, 0:n], lhsT=WUQ[:, kc, j * 128:(j + 1) * 128], rhs=cqn[:, kc, 0:n],
                                                          start=(kc == 0), stop=(kc == 2)), reads=[WUQ, cqn], writes=[P_])
                        b.op("act", lambda e: e.activation(out=Qn[:, j, 0:n], in_=P_[:, 0:n], func=AF.Copy), reads=[P_], writes=[Qn])
                    for j in range(8):
                        P1 = nextpk()
                        for kc in range(3):
                            b.op("pe", lambda e: e.matmul(P1[:, 0:n], lhsT=WUQ[:, kc, 1024 + j * 128:1024 + (j + 1) * 128], rhs=cqn[:, kc, 0:n],
                                                          start=(kc == 0), stop=(kc == 2)), reads=[WUQ, cqn], writes=[P1])
                        b.op("dve", lambda e: e.tensor_tensor(out=tmp1[:, 0:n], in0=P1[:, 0:n], in1=cos_t[:, 0:n], op=ALU.mult),
                             reads=[P1, cos_t], writes=[tmp1])
                        P2 = nextpk()
                        for kc in range(3):
                            b.op("pe", lambda e: e.matmul(P2[:, 0:n], lhsT=WUQ[:, kc, 2048 + j * 128:2048 + (j + 1) * 128], rhs=cqn[:, kc, 0:n],
                                                          start=(kc == 0), stop=(kc == 2)), reads=[WUQ, cqn], writes=[P2])
                        b.op("dve", lambda e: e.tensor_tensor(out=tmp2[:, 0:n], in0=P2[:, 0:n], in1=sin_t[:, 0:n], op=ALU.mult),
                             reads=[P2, sin_t], writes=[tmp2])
                        b.op("dve", lambda e: e.tensor_tensor(out=Qr[:, j, 0:n], in0=tmp1[:, 0:n], in1=tmp2[:, 0:n], op=ALU.add),
                             reads=[tmp1, tmp2], writes=[Qr])
                    g0t = c0 // 128
                    for h in range(16):
                        j = h // 2
                        p0 = (h % 2) * 64
                        PO = pO[h % 2]
                        POv = PO[:, 0:260].rearrange("p (q d) -> p q d", q=4)
                        for kt in range(0, g0t + 4):
                            if kt == 0:
                                k0, M = 112, 16
                            else:
                                k0, M = kt * 128, 128
                            qlo = max(0, kt - g0t)
                            q0 = qlo * 128
                            PS_ = pS[si % 2]
                            PTb = PTs[si % 3]
                            si += 1
                            b.op("pe", lambda e: e.matmul(PS_[0:M, q0:512], lhsT=Kn[p0:p0 + 64, j, k0:k0 + M], rhs=Qn[p0:p0 + 64, j, q0:512],
                                                          start=True, stop=False), reads=[Kn, Qn], writes=[PS_])
                            b.op("pe", lambda e: e.matmul(PS_[0:M, q0:512], lhsT=Kr2[p0:p0 + 32, k0:k0 + M], rhs=Qr[p0:p0 + 32, j, q0:512],
                                                          start=False, stop=True), reads=[Kr2, Qr], writes=[PS_])
                            b.op("act", lambda e: e.activation(out=PTb[0:M, q0:512], in_=PS_[0:M, q0:512], func=AF.Exp, scale=scale),
                                 reads=[PS_], writes=[PTb])
                            if kt >= g0t:
                                b.op("dve", lambda e: e.tensor_tensor(out=PTb[:, q0:q0 + 128], in0=PTb[:, q0:q0 + 128], in1=tri[:], op=ALU.mult),
                                     reads=[PTb, tri], writes=[PTb])
                            for qt in range(qlo, 4):
                                b.op("pe", lambda e: e.matmul(POv[:, qt, :], lhsT=PTb[0:M, qt * 128:(qt + 1) * 128], rhs=V[0:M, kt, h, :],
                                                              start=(kt == 0 and qt == 0), stop=(kt == g0t + qt), skip_group_check=True),
                                     reads=[PTb, V], writes=[PO])
                        b.op("dve", lambda e: e.reciprocal(out=rec[:], in_=POv[:, :, 64]), reads=[PO], writes=[rec])
                        for qt in range(4):
                            b.op("dve", lambda e: e.tensor_scalar(out=o_tm[:, qt, h * 64:(h + 1) * 64], in0=POv[:, qt, 0:64],
                                                                  scalar1=rec[:, qt:qt + 1], scalar2=None, op0=ALU.mult),
                                 reads=[PO, rec], writes=[o_tm])
                    for qt in range(4):
                        for c in range(8):
                            b.op("pe", lambda e: e.transpose(out=pT[:, c * 128:(c + 1) * 128], in_=o_tm[:, qt, c * 128:(c + 1) * 128],
                                                             identity=ident[:]), reads=[o_tm, ident], writes=[pT])
                        b.op("dve", lambda e: e.tensor_copy(out=oT[:, :, qt * 128:(qt + 1) * 128],
                                                            in_=pT[:].rearrange("p (c t) -> p c t", c=8)), reads=[pT], writes=[oT])
                    b.dma("pool", st, lambda e: e.dma_start(out=S_oT.h.ap()[:, col:col + n].rearrange("(c p) n -> p c n", p=128), in_=oT[:]),
                          reads=[oT], writes=[S_oT])
        b.barrier()

    def phase3b():
        with ExitStack() as es:
            sb, ps = mk(es, "p3b_")
            WAO = sb("WAO", [128, 8, 1024], BF16)
            WO = sb("WO", [128, 8, 1024], BF16)
            load_w_cast(WAO, WAO[:], wao_d.ap().rearrange("(k p) f -> p k f", p=128))
            load_w_cast(WO, WO[:], wo_d.ap().rearrange("(k p) f -> p k f", p=128))
            oT = [sb("oT%d" % i, [128, 8, 512], BF16) for i in range(2)]
            ga = [sb("ga%d" % i, [128, 512], F32) for i in range(2)]
            mr = [sb("mr%d" % i, [128, 512], F32) for i in range(2)]
            tmp = sb("tmp", [128, 512], F32)
            mixT = sb("mixT", [128, 8, 512], BF16)
            xt = [sb("xt%d" % i, [128, 1024], F32) for i in range(2)]
            h2 = [sb("h2%d" % i, [128, 1024], F32) for i in range(2)]
            pk = [ps("pk%d" % i, [128, 512], F32) for i in range(4)]
            gi = 0
            k = 0
            ti = 0
            for s in range(NSEQ):
                sc = s * SEQC
                for g, (c0, n) in enumerate(GROUPS):
                    if g == 0:
                        continue
                    col = sc + c0
                    OT = oT[gi % 2]
                    gi += 1
                    b.dma("sp", ld, lambda e: e.dma_start(out=OT[:], in_=S_oT.h.ap()[:, col:col + n].rearrange("(c p) n -> p c n", p=128)),
                          reads=[S_oT], writes=[OT])
                    for dc in range(8):
                        GA = ga[k % 2]
                        MR = mr[k % 2]
                        PK = pk[k % 4]
                        k += 1
                        b.dma("sp", ld, lambda e: e.dma_start(out=GA[:], in_=S_gatt.h.ap()[dc * 128:(dc + 1) * 128, col:col + n]),
                              reads=[S_gatt], writes=[GA])
                        b.dma("sp", ld, lambda e: e.dma_start(out=MR[:], in_=S_mrnn.h.ap()[dc * 128:(dc + 1) * 128, col:col + n]),
                              reads=[S_mrnn], writes=[MR])
                        for kc in range(8):
                            b.op("pe", lambda e: e.matmul(PK[:], lhsT=WAO[:, kc, dc * 128:(dc + 1) * 128], rhs=OT[:, kc, :],
                                                          start=(kc == 0), stop=(kc == 7)), reads=[WAO, OT], writes=[PK])
                        b.op("dve", lambda e: e.tensor_tensor(out=tmp[:], in0=PK[:], in1=GA[:], op=ALU.mult), reads=[PK, GA], writes=[tmp])
                        b.op("dve", lambda e: e.tensor_tensor(out=mixT[:, dc, :], in0=tmp[:], in1=MR[:], op=ALU.add),
                             reads=[tmp, MR], writes=[mixT])
                    for qt in range(4):
                        X = xt[ti % 2]
                        H2 = h2[ti % 2]
                        ti += 1
                        r0 = (g - 1) * 512 + qt * 128
                        b.dma("sp", ld, lambda e: e.dma_start(out=X[:], in_=x_d.ap()[s, r0:r0 + 128, :]), writes=[X])
                        for half in range(2):
                            PK = pk[k % 4]
                            k += 1
                            for kc in range(8):
                                b.op("pe", lambda e: e.matmul(PK[:], lhsT=mixT[:, kc, qt * 128:(qt + 1) * 128],
                                                              rhs=WO[:, kc, half * 512:(half + 1) * 512],
                                                              start=(kc == 0), stop=(kc == 7)), reads=[WO, mixT], writes=[PK])
                            b.op("dve", lambda e: e.tensor_tensor(out=H2[:, half * 512:(half + 1) * 512], in0=PK[:],
                                                                  in1=X[:, half * 512:(half + 1) * 512], op=ALU.add),
                                 reads=[PK, X], writes=[H2])
                        b.dma("pool", st, lambda e: e.dma_start(out=S_h2.h.ap()[s * NREAL + r0:s * NREAL + r0 + 128, :], in_=H2[:]),
                              reads=[H2], writes=[S_h2])
        b.barrier()


    def phase5():
        with ExitStack() as es:
            sb, ps = mk(es, "p5_")
            WQ = sb("WQ", [128, 8, 2048], BF16)
            K1T = sb("K1T", [128, 128], BF16)
            K2T = sb("K2T", [128, 128], BF16)
            load_w_cast(WQ, WQ[:], wq_d.ap().rearrange("(k p) f -> p k f", p=128))
            load_w_cast(K1T, K1T[:], k1T_d.ap())
            load_w_cast(K2T, K2T[:], k2T_d.ap())
            g2b = sb("g2b", [128, 1024], F32)
            gfb = sb("gfb", [128, 1024], F32)
            b.dma("sp", None, lambda e: e.dma_start(out=g2b[:], in_=g2_d.ap().to_broadcast([128, 1024])), writes=[g2b])
            b.dma("sp", None, lambda e: e.dma_start(out=gfb[:], in_=gf_d.ap().to_broadcast([128, 1024])), writes=[gfb])
            ident, identf = make_ident(sb)
            iota_i = sb("iota_i", [128, 16], I32)
            iota16 = sb("iota16", [128, 16], F32)
            b.op("pool", lambda e: e.iota(iota_i[:], pattern=[[1, 16]], base=0, channel_multiplier=0), writes=[iota_i])
            b.op("dve", lambda e: e.tensor_copy(out=iota16[:], in_=iota_i[:]), reads=[iota_i], writes=[iota16])
            L = sb("L", [128, 128, 128], BF16)
            b.op("pool", lambda e: e.memset(L[:], 0.0), writes=[L])
            Lflat = L[:].rearrange("p a b -> p (a b)")
            Xs = [sb("X%d" % i, [128, 1024], F32) for i in range(2)]
            xnbs = [sb("xnb%d" % i, [128, 1024], BF16) for i in range(2)]
            GTs = [sb("GT%d" % i, [128, 128], F32) for i in range(2)]
            IDXTs = [sb("IDXT%d" % i, [128, 128], U32) for i in range(2)]
            Gs = [sb("G%d" % i, [128, 8, 16], F32) for i in range(2)]
            ssA = sb("ssA", [128, 1], F32)
            ssC = sb("ssC", [128, 1], F32)
            junkA = sb("junkA", [128, 1024], BF16)
            junkD = sb("junkD", [128, 1024], BF16)
            xT = sb("xT", [128, 8, 128], BF16)
            qT = sb("qT", [128, 16, 128], BF16)
            S = sb("S", [128, 16, 128], F32)
            eqv = S[:].rearrange("p (h x) (y a) -> p h (x y) a", x=2, a=16)
            T16 = sb("T16", [128, 16, 16], F32)
            I16 = sb("I16", [128, 16, 16], U32)
            I16f = sb("I16f", [128, 16, 16], F32)
            cand = sb("cand", [128, 8, 256], F32)
            TS = sb("TS", [128, 8, 16], F32)
            CI = sb("CI", [128, 8, 16], U32)
            CIa = sb("CIa", [128, 8, 16], U32)
            CIb = sb("CIb", [128, 8, 16], U32)
            Af = sb("Af", [128, 8, 16], F32)
            Bf = sb("Bf", [128, 8, 16], F32)
            i1s = sb("i1s", [128, 128], F32)
            i2s = sb("i2s", [128, 128], F32)
            i1b = sb("i1b", [128, 128], BF16)
            i2b = sb("i2b", [128, 128], BF16)
            idxf = sb("idxf", [128, 128], F32)
            idxTf = sb("idxTf", [128, 128], F32)
            iTf = sb("iTf", [128, 2, 128], F32)
            E = sb("E", [128, 8, 16], F32)
            Z = sb("Z", [128, 8], F32)
            ACTV = sb("ACTV", [128, 128], F32)
            coefb = sb("coefb", [128, 128], BF16)
            coefT = sb("coefT", [128, 128], BF16)
            UVG = [sb("UVG%d" % i, [128, 8, 2048], BF16) for i in range(2)]
            actT = sb("actT", [128, 128], F32)
            Ls = [T(L.h, "L%d" % i) for i in range(16)]
            for lt in Ls:
                lt.w = dict(L.w)
            h3 = sb("h3", [128, 1024], F32)
            pTa = ps("pTa", [128, 1024], BF16)
            pq = [ps("pq%d" % i, [128, 512], F32) for i in range(1)]
            pOut = [ps("pOut%d" % i, [128, 512], F32) for i in range(2)]
            pX = [ps("pX%d" % i, [128, 1024], F32) for i in range(2)]
            T16v = T16[:].rearrange("p (h two) a -> p h two a", two=2)
            I16fv = I16f[:].rearrange("p (h two) a -> p h two a", two=2)
            B4 = [128, 8, 16, 16]
            cnt = {"u": 0, "v": 0, "q": 0}
            ntiles = NR // 128 if p5tiles is None else p5tiles

            def sweep(eng, fns, reads, writes):
                for k_, f in enumerate(fns):
                    w = writes if (k_ == 0 or k_ == len(fns) - 1) else ()
                    b.op(eng, f, reads=reads, writes=w)

            def top16(vals, ng, tv, iv):
                sweep("dve", [(lambda e, g=g: e.max(out=tv[:, g, 0:8], in_=vals[:, g, :])) for g in range(ng)], [vals], [tv])
                sweep("dve", [(lambda e, g=g: e.max_index(out=iv[:, g, 0:8], in_max=tv[:, g, 0:8], in_values=vals[:, g, :])) for g in range(ng)],
                      [vals, tv], [iv])
                yield
                sweep("dve", [(lambda e, g=g: e.match_replace(out=vals[:, g, :], in_to_replace=tv[:, g, 0:8], in_values=vals[:, g, :],
                                                              imm_value=-1e30)) for g in range(ng)], [tv], [vals])
                yield
                sweep("dve", [(lambda e, g=g: e.max(out=tv[:, g, 8:16], in_=vals[:, g, :])) for g in range(ng)], [vals], [tv])
                sweep("dve", [(lambda e, g=g: e.max_index(out=iv[:, g, 8:16], in_max=tv[:, g, 8:16], in_values=vals[:, g, :])) for g in range(ng)],
                      [vals, tv], [iv])
                yield

            def stageA(i):
                par = i % 2
                X, xnb, GT, IDXT, G = Xs[par], xnbs[par], GTs[par], IDXTs[par], Gs[par]
                b.dma("sp", None, lambda e: e.dma_start(out=X[:], in_=S_h2.h.ap()[i * 128:(i + 1) * 128, :]), reads=[S_h2], writes=[X])
                b.op("act", lambda e: e.activation(out=junkA[:], in_=X[:], func=AF.Square, accum_out=ssA[:]), reads=[X], writes=[junkA, ssA])
                b.op("dve", lambda e: e.tensor_scalar(out=ssA[:], in0=ssA[:], scalar1=1.0 / 1024, scalar2=EPS, op0=ALU.mult, op1=ALU.add),
                     reads=[ssA], writes=[ssA])
                b.op("act", lambda e: e.activation(out=ssA[:], in_=ssA[:], func=AF.Sqrt), reads=[ssA], writes=[ssA])
                b.op("dve", lambda e: e.reciprocal(out=ssA[:], in_=ssA[:]), reads=[ssA], writes=[ssA])
                b.op("dve", lambda e: e.scalar_tensor_tensor(out=xnb[:], in0=X[:], scalar=ssA[:, 0:1], in1=g2b[:], op0=ALU.mult, op1=ALU.mult),
                     reads=[X, ssA, g2b], writes=[xnb])
                yield
                for c in range(8):
                    b.op("pe", lambda e: e.transpose(out=pTa[:, c * 128:(c + 1) * 128], in_=xnb[:, c * 128:(c + 1) * 128], identity=ident[:]),
                         reads=[xnb, ident], writes=[pTa])
                b.op("act", lambda e: e.activation(out=xT[:], in_=pTa[:].rearrange("p (c t) -> p c t", c=8), func=AF.Copy), reads=[pTa], writes=[xT])
                yield
                for bq in range(4):
                    PQ = pq[0]
                    for j in range(4):
                        hh = bq * 4 + j
                        for kc in range(8):
                            b.op("pe", lambda e: e.matmul(PQ[:, j * 128:(j + 1) * 128], lhsT=WQ[:, kc, hh * 128:(hh + 1) * 128], rhs=xT[:, kc, :],
                                                          start=(kc == 0), stop=(kc == 7), skip_group_check=True), reads=[WQ, xT], writes=[PQ])
                    b.op("act", lambda e: e.activation(out=qT[:, bq * 4:(bq + 1) * 4, :], in_=PQ[:].rearrange("p (j t) -> p j t", j=4), func=AF.Copy),
                         reads=[PQ], writes=[qT])
                    yield
                for bq in range(4):
                    PQ = pq[0]
                    for j in range(4):
                        hh = bq * 4 + j
                        KT = K1T if hh % 2 == 0 else K2T
                        b.op("pe", lambda e: e.matmul(PQ[:, j * 128:(j + 1) * 128], lhsT=qT[:, hh, :], rhs=KT[:], start=True, stop=True,
                                                      skip_group_check=True), reads=[qT, KT], writes=[PQ])
                    b.op("act", lambda e: e.activation(out=S[:, bq * 4:(bq + 1) * 4, :], in_=PQ[:].rearrange("p (j t) -> p j t", j=4), func=AF.Copy),
                         reads=[PQ], writes=[S])
                    yield
                yield from top16(S, 16, T16, I16)
                b.op("dve", lambda e: e.tensor_copy(out=I16f[:], in_=I16[:]), reads=[I16], writes=[I16f])
                b.op("dve", lambda e: e.tensor_tensor(out=cand[:].rearrange("p h (a c) -> p h a c", a=16),
                                                      in0=T16v[:, :, 0, :].unsqueeze(3).to_broadcast(B4),
                                                      in1=T16v[:, :, 1, :].unsqueeze(2).to_broadcast(B4), op=ALU.add),
                     reads=[T16], writes=[cand])
                yield
                yield from top16(cand, 8, TS, CI)
                b.op("dve", lambda e: e.tensor_single_scalar(out=CIa[:], in_=CI[:], scalar=4, op=ALU.logical_shift_right), reads=[CI], writes=[CIa])
                b.op("dve", lambda e: e.tensor_single_scalar(out=CIb[:], in_=CI[:], scalar=15, op=ALU.bitwise_and), reads=[CI], writes=[CIb])
                b.op("dve", lambda e: e.tensor_copy(out=Af[:], in_=CIa[:]), reads=[CIa], writes=[Af])
                b.op("dve", lambda e: e.tensor_copy(out=Bf[:], in_=CIb[:]), reads=[CIb], writes=[Bf])
                yield
                for (SEL, half, dst) in ((Af, 0, i1s), (Bf, 1, i2s)):
                    b.op("dve", lambda e: e.tensor_tensor(out=eqv, in0=SEL[:].unsqueeze(3).to_broadcast(B4),
                                                          in1=iota16[:].unsqueeze(1).unsqueeze(1).to_broadcast(B4), op=ALU.is_equal),
                         reads=[SEL, iota16], writes=[S])
                    b.op("dve", lambda e: e.tensor_tensor(out=eqv, in0=eqv, in1=I16fv[:, :, half, :].unsqueeze(2).to_broadcast(B4), op=ALU.mult),
                         reads=[S, I16f], writes=[S])
                    b.op("dve", lambda e: e.tensor_reduce(out=dst[:].rearrange("p (h k) -> p h k", h=8), in_=eqv, axis=AX.X, op=ALU.add),
                         reads=[S], writes=[dst])
                    yield
                b.op("act", lambda e: e.activation(out=i1b[:], in_=i1s[:], func=AF.Copy), reads=[i1s], writes=[i1b])
                b.op("act", lambda e: e.activation(out=i2b[:], in_=i2s[:], func=AF.Copy), reads=[i2s], writes=[i2b])
                b.op("pe", lambda e: e.transpose(out=pTa[:, 0:128], in_=i1b[:], identity=ident[:]), reads=[i1b, ident], writes=[pTa])
                b.op("pe", lambda e: e.transpose(out=pTa[:, 128:256], in_=i2b[:], identity=ident[:]), reads=[i2b, ident], writes=[pTa])
                b.op("act", lambda e: e.activation(out=iTf[:], in_=pTa[:, 0:256].rearrange("p (a t) -> p a t", a=2), func=AF.Copy),
                     reads=[pTa], writes=[iTf])
                yield
                b.op("dve", lambda e: e.scalar_tensor_tensor(out=idxTf[:], in0=iTf[:, 0, :], scalar=128.0, in1=iTf[:, 1, :], op0=ALU.mult, op1=ALU.add),
                     reads=[iTf], writes=[idxTf])
                b.op("dve", lambda e: e.tensor_copy(out=IDXT[:], in_=idxTf[:]), reads=[idxTf], writes=[IDXT])
                b.op("dve", lambda e: e.tensor_tensor(out=E[:], in0=TS[:], in1=TS[:, :, 0:1].to_broadcast([128, 8, 16]), op=ALU.subtract),
                     reads=[TS], writes=[E])
                b.op("act", lambda e: e.activation(out=E[:], in_=E[:], func=AF.Exp), reads=[E], writes=[E])
                b.op("dve", lambda e: e.tensor_reduce(out=Z[:], in_=E[:], axis=AX.X, op=ALU.add), reads=[E], writes=[Z])
                b.op("dve", lambda e: e.reciprocal(out=Z[:], in_=Z[:]), reads=[Z], writes=[Z])
                b.op("dve", lambda e: e.tensor_tensor(out=G[:], in0=E[:], in1=Z[:].unsqueeze(2).to_broadcast([128, 8, 16]), op=ALU.mult),
                     reads=[E, Z], writes=[G])
                b.op("pe", lambda e: e.transpose(out=pq[0][:, 0:128], in_=G[:].rearrange("p h k -> p (h k)"), identity=identf[:]), reads=[G, identf], writes=[pq[0]])
                b.op("act", lambda e: e.activation(out=GT[:], in_=pq[0][:, 0:128], func=AF.Copy), reads=[pq[0]], writes=[GT])
                yield

            def step(gen, k=1):
                if gen is None:
                    return
                for _ in range(k):
                    try:
                        next(gen)
                    except StopIteration:
                        return

            g0 = stageA(0)
            step(g0, 1000)
            for i in range(ntiles):
                par = i % 2
                X, xnb, GT, IDXT = Xs[par], xnbs[par], GTs[par], IDXTs[par]
                s, r0 = divmod(i * 128, NREAL)
                gN = stageA(i + 1) if i + 1 < ntiles else None
                pend = [None]

                def finish(tb_, UV_):
                    Lt = Ls[tb_]
                    tsl = slice(tb_ * 8, tb_ * 8 + 8)
                    b.op("act", lambda e: e.activation(out=actT[:, tsl], in_=actT[:, tsl], func=AF.Gelu_apprx_tanh), reads=[actT], writes=[actT])
                    b.op("dve", lambda e: e.tensor_tensor(out=coefT[:, tsl], in0=actT[:, tsl], in1=GT[:, tsl], op=ALU.mult),
                         reads=[actT, GT], writes=[coefT])
                    b.op("dve", lambda e: e.tensor_copy(out=Lflat[:, tb_ * 8 * 129:tb_ * 8 * 129 + 7 * 129 + 1:129], in_=coefT[:, tsl]),
                         reads=[coefT], writes=[Lt])
                    for q in range(8):
                        t = tb_ * 8 + q
                        for half in range(2):
                            b.op("pe", lambda e: e.matmul(pOut[half][:], lhsT=L[:, t, :], rhs=UV_[:, q, 1024 + half * 512:1024 + (half + 1) * 512],
                                                          start=(t == 0), stop=(t == 127)), reads=[Lt, UV_], writes=[pOut[half]])

                for tb in range(16):
                    UV = UVG[cnt["u"] % 2]
                    cnt["u"] += 1
                    for q in range(8):
                        t = tb * 8 + q
                        b.dma("pool", None, lambda e: e.indirect_dma_start(out=UV[:, q, :], out_offset=None, in_=UV16.h.ap(),
                                                                           in_offset=bass.IndirectOffsetOnAxis(ap=IDXT[:, t:t + 1], axis=0)),
                              reads=[IDXT, UV16], writes=[UV])
                    for q in range(8):
                        t = tb * 8 + q
                        PX = pX[cnt["v"] % 2]
                        cnt["v"] += 1
                        for half in range(2):
                            b.op("pe", lambda e: e.matmul(PX[:, half * 512:(half + 1) * 512], lhsT=ident[:, t:t + 1].to_broadcast([128, 128]),
                                                          rhs=xnb[:, half * 512:(half + 1) * 512], start=True, stop=True, skip_group_check=True),
                                 reads=[ident, xnb], writes=[PX])
                        b.op("dve", lambda e: e.scalar_tensor_tensor(out=junkD[:], in0=UV[:, q, 0:1024], scalar=1.0, in1=PX[:], op0=ALU.mult,
                                                                     op1=ALU.mult, accum_out=actT[:, t:t + 1]),
                             reads=[UV, PX], writes=[actT] if q in (0, 7) else ())
                        if q == 1 and pend[0] is not None:
                            finish(*pend[0])
                            pend[0] = None
                    pend[0] = (tb, UV)
                    step(gN, 2)
                finish(*pend[0])
                for half in range(2):
                    b.op("dve", lambda e: e.tensor_tensor(out=h3[:, half * 512:(half + 1) * 512], in0=pOut[half][:],
                                                          in1=X[:, half * 512:(half + 1) * 512], op=ALU.add), reads=[pOut[half], X], writes=[h3])
                b.op("act", lambda e: e.activation(out=junkA[:], in_=h3[:], func=AF.Square, accum_out=ssC[:]), reads=[h3], writes=[junkA, ssC])
                b.op("dve", lambda e: e.tensor_scalar(out=ssC[:], in0=ssC[:], scalar1=1.0 / 1024, scalar2=EPS, op0=ALU.mult, op1=ALU.add),
                     reads=[ssC], writes=[ssC])
                b.op("act", lambda e: e.activation(out=ssC[:], in_=ssC[:], func=AF.Sqrt), reads=[ssC], writes=[ssC])
                b.op("dve", lambda e: e.reciprocal(out=ssC[:], in_=ssC[:]), reads=[ssC], writes=[ssC])
                b.op("dve", lambda e: e.scalar_tensor_tensor(out=h3[:], in0=h3[:], scalar=ssC[:, 0:1], in1=gfb[:], op0=ALU.mult, op1=ALU.mult),
                     reads=[h3, ssC, gfb], writes=[h3])
                b.dma("sp", None, lambda e: e.dma_start(out=out_d.ap()[s, r0:r0 + 128, :], in_=h3[:]), reads=[h3], writes=[OUT])
                step(gN, 1000)
        b.barrier()

    progs = {1: phase1, 2: phase2, 3: phase3a, 4: phase3b, 5: phase5}
    for p in phases:
        if p in progs:
            progs[p]()
    b.barrier()
    return nc


def _pc(v, nchunk):
    return np.ascontiguousarray(np.asarray(v, np.float32).reshape(nchunk, 128).T)


def prep_common(inp):
    f = lambda a: np.asarray(a, np.float32)
    w_in = f(inp["w_in"])[0]
    z32 = np.zeros((1024, 32), np.float32)
    kr = w_in[:, 2688:2720]
    krs = np.concatenate([kr[:, 16:], kr[:, :16]], axis=1)
    w_in_r = np.concatenate([
        w_in[:, 0:1024], w_in[:, 1024:2048], w_in[:, 2048:2432], w_in[:, 2432:2688],
        kr, z32, kr, z32, krs, z32, krs, z32,
        w_in[:, 2720:3744], w_in[:, 3744:4768]], axis=1)
    assert w_in_r.shape == (1024, 4992)
    conv_w = f(inp["conv_w"])[0]
    cw = np.ascontiguousarray(conv_w.reshape(4, 8, 128).transpose(2, 1, 0))
    w_uq = f(inp["w_uq"])[0].reshape(384, 16, 96)
    nope = w_uq[:, :, :64].reshape(384, 1024)
    rope = w_uq[:, :, 64:]
    ropes = np.concatenate([rope[:, :, 16:], rope[:, :, :16]], axis=2)
    z = np.zeros((384, 16, 32), np.float32)
    rope_p = np.concatenate([rope, z], axis=2).reshape(384, 1024)
    ropes_p = np.concatenate([ropes, z], axis=2).reshape(384, 1024)
    w_uq_r = np.concatenate([nope, rope_p, ropes_p, np.zeros((384, 1024), np.float32)], axis=1)
    w_ukv = f(inp["w_ukv"])[0].reshape(256, 16, 128)
    w_ukv_r = np.concatenate([w_ukv[:, :, :64].reshape(256, 1024), w_ukv[:, :, 64:].reshape(256, 1024)], axis=1)
    pos = (np.arange(SEQC, dtype=np.float32) - 112.0).astype(np.float32)
    inv = np.power(np.float32(10000.0), -np.arange(16, dtype=np.float32) * np.float32(2.0 / 32)).astype(np.float32)
    ang = (pos[None, :] * inv[:, None]).astype(np.float32)
    c, s_ = np.cos(ang).astype(np.float32), np.sin(ang).astype(np.float32)
    cos32 = np.concatenate([c, c], axis=0)
    sin32 = np.concatenate([-s_, s_], axis=0)
    zz = np.zeros((32, SEQC), np.float32)
    cos2 = np.concatenate([cos32, zz, cos32, zz], axis=0)
    sin2s = np.concatenate([sin32, zz, sin32, zz], axis=0)
    return {
        "meta": f(inp["meta_tokens"]),
        "g1": _pc(inp["norm1_g"][0], 8),
        "w_in": np.ascontiguousarray(w_in_r),
        "convw": cw,
        "convb": _pc(inp["conv_b"][0], 8),
        "rg_wa": np.ascontiguousarray(f(inp["rg_wa"])[0].transpose(1, 0, 2)),
        "rg_wx": np.ascontiguousarray(f(inp["rg_wx"])[0].transpose(1, 0, 2)),
        "rg_ba": _pc(inp["rg_ba"][0], 8),
        "rg_bx": _pc(inp["rg_bx"][0], 8),
        "rg_lam": _pc(inp["rg_lambda"][0], 8),
        "w_rnn_out": f(inp["w_rnn_out"])[0],
        "qg": _pc(inp["q_norm_g"][0], 3),
        "kvg": _pc(inp["kv_norm_g"][0], 2),
        "w_uq": np.ascontiguousarray(w_uq_r),
        "w_ukv": np.ascontiguousarray(w_ukv_r),
        "w_attn_out": f(inp["w_attn_out"])[0],
        "w_out": f(inp["w_out"])[0],
        "g2": f(inp["norm2_g"]).reshape(1, 1024),
        "peer_wq": f(inp["peer_wq"])[0],
        "keys1T": np.ascontiguousarray(f(inp["peer_keys1"])[0].T),
        "keys2T": np.ascontiguousarray(f(inp["peer_keys2"])[0].T),
        "peer_u": f(inp["peer_u"])[0],
        "peer_v": f(inp["peer_v"])[0],
        "gf": f(inp["final_g"]).reshape(1, 1024),
        "cos2": cos2,
        "sin2s": sin2s,
    }


def kernel(**inputs):
    NC = 8
    NSEQ = 4
    common = prep_common(inputs)
    x = np.asarray(inputs["x"], np.float32)
    nc = build(NSEQ)
    in_maps = []
    for c in range(NC):
        m = dict(common)
        m["x"] = np.ascontiguousarray(x[c * NSEQ:(c + 1) * NSEQ])
        in_maps.append(m)
    res = run_bass_kernel_spmd(nc, in_maps, core_ids=list(range(NC)))
    return np.concatenate([r["out"] for r in res.results], axis=0)
```

```python
from contextlib import ExitStack
import numpy as np
import concourse.bass as bass
import concourse.mybir as mybir
from concourse.bass_utils import run_bass_kernel_spmd

F32 = mybir.dt.float32
BF16 = mybir.dt.bfloat16
I32 = mybir.dt.int32
U32 = mybir.dt.uint32
AF = mybir.ActivationFunctionType
ALU = mybir.AluOpType
AX = mybir.AxisListType

SEQC = 2176
NREAL = 2048
EPS = 1e-6
GROUPS = [(0, 128)] + [(128 + 512 * g, 512) for g in range(4)]


class T:
    def __init__(self, h, name, accum=False):
        self.h = h
        self.name = name
        self.w = {}
        self.r = {}
        self.accum = accum

    def __getitem__(self, k):
        return self.h[k]


class B:
    def __init__(self, nc):
        self.nc = nc
        self.E = {"pe": nc.tensor, "act": nc.scalar, "dve": nc.vector, "pool": nc.gpsimd, "sp": nc.sync}
        self.sems = {}
        self.cnt = {}
        self.seen = {k: {} for k in self.E}
        for k in self.E:
            self.sems[k] = nc.alloc_semaphore("s_" + k)
            self.cnt[k] = 0
        self.nins = 0

    def newsem(self, name):
        self.sems[name] = self.nc.alloc_semaphore("s_" + name)
        self.cnt[name] = 0
        return name

    def _wait(self, eng, evs):
        for key, val in evs.items():
            if val <= 0 or (eng == "pe" and key == "pe"):
                continue
            if self.seen[eng].get(key, 0) >= val:
                continue
            self.E[eng].wait_ge(self.sems[key], val)
            self.seen[eng][key] = val
            self.nins += 1

    @staticmethod
    def _merge(d, e):
        for k, v in e.items():
            if d.get(k, 0) < v:
                d[k] = v

    def _deps(self, reads, writes):
        evs = {}
        for t in reads:
            self._merge(evs, t.w)
        for t in writes:
            if not t.accum:
                self._merge(evs, t.w)
                self._merge(evs, t.r)
        return evs

    def _commit(self, ev, reads, writes):
        for t in reads:
            if not t.accum:
                self._merge(t.r, ev)
        for t in writes:
            if t.accum:
                self._merge(t.w, ev)
            else:
                t.w = dict(ev)
                t.r = {}

    def op(self, eng, fn, reads=(), writes=()):
        self._wait(eng, self._deps(reads, writes))
        ins = fn(self.E[eng])
        self.cnt[eng] += 1
        ins.then_inc(self.sems[eng], 1)
        self.nins += 1
        self._commit({eng: self.cnt[eng]}, reads, writes)

    def dma(self, q, sem, fn, reads=(), writes=()):
        own = [t for t in writes if not t.accum] or [t for t in reads if not t.accum]
        t0 = own[0]
        if getattr(t0, "sem", None) is None:
            t0.sem = {}
        if q not in t0.sem:
            t0.sem[q] = self.newsem("d%d" % len(self.sems))
        sem = t0.sem[q]
        deps = self._deps(reads, writes)
        if writes and not writes[0].accum:
            wr = {}
            for t in writes:
                self._merge(wr, t.r)
            if deps.get(sem, 0) > wr.get(sem, 0):
                v = wr.get(sem, 0)
                if v > 0:
                    deps[sem] = v
                else:
                    deps.pop(sem)
        self._wait(q, deps)
        ins = fn(self.E[q])
        self.cnt[sem] += 16
        ins.then_inc(self.sems[sem], 16)
        self.nins += 1
        self._commit({sem: self.cnt[sem]}, reads, writes)

    def barrier(self):
        allev = {k: v for k, v in self.cnt.items() if v > 0}
        for e in self.E:
            self._wait(e, allev)


def build(NSEQ, phases=(1, 2, 3, 4, 5), debug=False, p5tiles=None):
    nc = bass.Bass("TRN2", target_bir_lowering=False)
    b = B(nc)
    NT = NSEQ * SEQC
    NR = NSEQ * NREAL
    skind = "ExternalOutput" if debug else "Internal"

    def din(name, shape, dt=F32):
        return nc.dram_tensor(name, list(shape), dt, kind="ExternalInput")

    def dscr(name, shape, dt=F32):
        return T(nc.dram_tensor(name, list(shape), dt, kind=skind), name, accum=True)

    x_d = din("x", [NSEQ, NREAL, 1024])
    meta_d = din("meta", [16, 1024])
    g1_d = din("g1", [128, 8])
    win_d = din("w_in", [1024, 4992])
    cw_d = din("convw", [128, 8, 4])
    cb_d = din("convb", [128, 8])
    rgwa_d = din("rg_wa", [128, 8, 128])
    rgwx_d = din("rg_wx", [128, 8, 128])
    rgba_d = din("rg_ba", [128, 8])
    rgbx_d = din("rg_bx", [128, 8])
    lam_d = din("rg_lam", [128, 8])
    wro_d = din("w_rnn_out", [1024, 1024])
    qg_d = din("qg", [128, 3])
    kvg_d = din("kvg", [128, 2])
    wuq_d = din("w_uq", [384, 4096])
    wukv_d = din("w_ukv", [256, 2048])
    wao_d = din("w_attn_out", [1024, 1024])
    wo_d = din("w_out", [1024, 1024])
    g2_d = din("g2", [1, 1024])
    wq_d = din("peer_wq", [1024, 2048])
    k1T_d = din("keys1T", [128, 128])
    k2T_d = din("keys2T", [128, 128])
    pu_d = din("peer_u", [16384, 1024])
    pv_d = din("peer_v", [16384, 1024])
    gf_d = din("gf", [1, 1024])
    cos_d = din("cos2", [128, SEQC])
    sin_d = din("sin2s", [128, SEQC])
    out_d = nc.dram_tensor("out", [NSEQ, NREAL, 1024], F32, kind="ExternalOutput")
    OUT = T(out_d, "out", accum=True)

    S_xr = dscr("S_xr", [1024, NT])
    S_gr = dscr("S_gr", [1024, NT])
    S_cq = dscr("S_cq", [384, NT])
    S_ckv = dscr("S_ckv", [256, NT])
    S_kr = dscr("S_kr", [128, NT])
    S_krs = dscr("S_krs", [128, NT])
    S_grnn = dscr("S_grnn", [1024, NT])
    S_gatt = dscr("S_gatt", [1024, NT])
    S_mrnn = dscr("S_mrnn", [1024, NT])
    S_oT = dscr("S_oT", [1024, NT], BF16)
    S_h2 = dscr("S_h2", [NR, 1024])
    UV16 = T(nc.dram_tensor("UV16", [16384, 2048], BF16, kind="Internal"), "UV16", accum=True)

    ld = b.newsem("ld")
    st = b.newsem("st")
    wl = b.newsem("wl")

    def mk(es, pre):
        def sb(name, shape, dt):
            return T(es.enter_context(nc.sbuf_tensor(pre + name, list(shape), dt)), name)

        def ps(name, shape, dt):
            return T(es.enter_context(nc.psum_tensor(pre + name, list(shape), dt)), name)
        return sb, ps

    def make_ident(sb):
        identf = sb("identf", [128, 128], F32)
        ident = sb("ident", [128, 128], BF16)
        b.op("pool", lambda e: e.memset(identf[:], 1.0), writes=[identf])
        b.op("pool", lambda e: e.affine_select(out=identf[:], in_=identf[:], pattern=[[-1, 128]],
                                               compare_op=ALU.is_equal, fill=0.0, base=0, channel_multiplier=1),
             reads=[identf], writes=[identf])
        b.op("dve", lambda e: e.tensor_copy(out=ident[:], in_=identf[:]), reads=[identf], writes=[ident])
        return ident, identf

    def load_small(sb, name, d, shape):
        t = sb(name, shape, F32)
        b.dma("sp", wl, lambda e: e.dma_start(out=t[:], in_=d.ap()), writes=[t])
        return t

    def load_w_cast(dst, dst_ap, src_ap):
        b.dma("pool", wl, lambda e: e.dma_start(out=dst_ap, in_=src_ap), writes=[dst])

    def phase1():
        with ExitStack() as es:
            sb, ps = mk(es, "p1_")
            W = sb("W1", [128, 8, 4992], BF16)
            g1 = load_small(sb, "g1s", g1_d, [128, 8])
            stg = [sb("wstg%d" % i, [128, 4992], F32) for i in range(2)]
            for kc in range(8):
                s_ = stg[kc % 2]
                b.dma("sp", wl, lambda e: e.dma_start(out=s_[:], in_=win_d.ap()[kc * 128:(kc + 1) * 128, :]), writes=[s_])
                b.op("dve", lambda e: e.tensor_scalar(out=W[:, kc, :], in0=s_[:], scalar1=g1[:, kc:kc + 1], scalar2=None,
                                                      op0=ALU.mult), reads=[s_, g1], writes=[W])
            ident, _ = make_ident(sb)
            xt = [sb("xt%d" % i, [128, 1024], F32) for i in range(2)]
            junk = sb("junk", [128, 1024], BF16)
            ss = [sb("ss%d" % i, [128, 1], F32) for i in range(2)]
            xb = [sb("xb%d" % i, [128, 1024], BF16) for i in range(2)]
            n1T = [sb("n1T%d" % i, [128, 8, 512], BF16) for i in range(2)]
            pT = [ps("pT%d" % i, [128, 1024], BF16) for i in range(2)]
            pa = [ps("pa%d" % i, [128, 512], F32) for i in range(4)]
            stage = [sb("stage%d" % i, [128, 512], F32) for i in range(4)]
            chunks = []
            for i in range(8):
                chunks.append((S_xr, i * 128, None, 128))
            for i in range(8):
                chunks.append((S_gr, i * 128, AF.Gelu_apprx_tanh, 128))
            for i in range(3):
                chunks.append((S_cq, i * 128, None, 128))
            for i in range(2):
                chunks.append((S_ckv, i * 128, None, 128))
            chunks.append((S_kr, 0, None, 128))
            chunks.append((S_krs, 0, None, 128))
            for i in range(8):
                chunks.append((S_grnn, i * 128, AF.Sigmoid, 128))
            for i in range(8):
                chunks.append((S_gatt, i * 128, AF.Sigmoid, 128))
            ti = 0
            gi = 0
            ci = 0
            cv = [sb("cv%d" % i, [128, 8, 1024], BF16) for i in range(2)]
            conv = [(src, off, c) for (src, off) in ((pu_d, 0), (pv_d, 1024)) for c in range(16)]
            cvi = [0]

            def convert_some(k):
                for _ in range(k):
                    if cvi[0] >= len(conv):
                        return
                    src, off, c = conv[cvi[0]]
                    dst = UV16
                    CV = cv[cvi[0] % 2]
                    cvi[0] += 1
                    b.dma("pool", None, lambda e: e.dma_start(out=CV[:], in_=src.ap()[c * 1024:(c + 1) * 1024, :].rearrange("(p r) d -> p r d", p=128)),
                          writes=[CV])
                    b.dma("sp", None, lambda e: e.dma_start(out=dst.h.ap()[c * 1024:(c + 1) * 1024, off:off + 1024].rearrange("(p r) d -> p r d", p=128), in_=CV[:]),
                          reads=[CV], writes=[dst])

            for s in range(NSEQ):
                for (c0, n) in GROUPS:
                    nT = n1T[gi % 2]
                    gi += 1
                    convert_some(8 if NSEQ == 1 else 2)
                    for j in range(n // 128):
                        t = c0 // 128 + j
                        X = xt[ti % 2]
                        SS = ss[ti % 2]
                        XB = xb[ti % 2]
                        PT = pT[ti % 2]
                        ti += 1
                        if t == 0:
                            b.op("pool", lambda e: e.memset(X[:], 0.0), writes=[X])
                            b.dma("sp", ld, lambda e: e.dma_start(out=X[112:128, :], in_=meta_d.ap()), writes=[X])
                        else:
                            b.dma("sp", ld, lambda e: e.dma_start(out=X[:], in_=x_d.ap()[s, (t - 1) * 128:t * 128, :]), writes=[X])
                        b.op("act", lambda e: e.activation(out=junk[:], in_=X[:], func=AF.Square, accum_out=SS[:]),
                             reads=[X], writes=[junk, SS])
                        b.op("dve", lambda e: e.tensor_scalar(out=SS[:], in0=SS[:], scalar1=1.0 / 1024, scalar2=EPS,
                                                              op0=ALU.mult, op1=ALU.add), reads=[SS], writes=[SS])
                        b.op("act", lambda e: e.activation(out=SS[:], in_=SS[:], func=AF.Sqrt), reads=[SS], writes=[SS])
                        b.op("dve", lambda e: e.reciprocal(out=SS[:], in_=SS[:]), reads=[SS], writes=[SS])
                        b.op("act", lambda e: e.activation(out=XB[:], in_=X[:], func=AF.Copy, scale=SS[:]),
                             reads=[X, SS], writes=[XB])
                        for c in range(8):
                            b.op("pe", lambda e: e.transpose(out=PT[:, c * 128:(c + 1) * 128], in_=XB[:, c * 128:(c + 1) * 128],
                                                             identity=ident[:]), reads=[XB, ident], writes=[PT])
                        b.op("dve", lambda e: e.tensor_copy(out=nT[:, :, j * 128:(j + 1) * 128],
                                                            in_=PT[:].rearrange("p (c t) -> p c t", c=8)),
                             reads=[PT], writes=[nT])
                    for oc, (S_, r0, fn, m) in enumerate(chunks):
                        PA = pa[ci % 4]
                        SG = stage[ci % 4]
                        ci += 1
                        for kc in range(8):
                            b.op("pe", lambda e: e.matmul(PA[:, 0:n], lhsT=W[:, kc, oc * 128:(oc + 1) * 128], rhs=nT[:, kc, 0:n],
                                                          start=(kc == 0), stop=(kc == 7)), reads=[W, nT], writes=[PA])
                        if fn is None:
                            b.op("dve", lambda e: e.tensor_copy(out=SG[:, 0:n], in_=PA[:, 0:n]), reads=[PA], writes=[SG])
                        else:
                            b.op("act", lambda e: e.activation(out=SG[:, 0:n], in_=PA[:, 0:n], func=fn), reads=[PA], writes=[SG])
                        col = s * SEQC + c0
                        b.dma("pool", st, lambda e: e.dma_start(out=S_.h.ap()[r0:r0 + 128, col:col + n], in_=SG[:, 0:n]),
                              reads=[SG], writes=[S_])
            convert_some(len(conv))
        b.barrier()

    def phase2():
        with ExitStack() as es:
            sb, ps = mk(es, "p2_")
            WA = sb("WA", [128, 8, 128], BF16)
            WX = sb("WX", [128, 8, 128], BF16)
            WRO = sb("WRO", [128, 8, 1024], BF16)
            load_w_cast(WA, WA[:], rgwa_d.ap())
            load_w_cast(WX, WX[:], rgwx_d.ap())
            load_w_cast(WRO, WRO[:], wro_d.ap().rearrange("(k p) f -> p k f", p=128))
            cw = load_small(sb, "cw", cw_d, [128, 8, 4])
            cb = load_small(sb, "cb", cb_d, [128, 8])
            ba = load_small(sb, "ba", rgba_d, [128, 8])
            bx = load_small(sb, "bx", rgbx_d, [128, 8])
            lam = load_small(sb, "lam", lam_d, [128, 8])
            c8 = sb("c8", [128, 8], F32)
            c16 = sb("c16", [128, 8], F32)
            b.op("act", lambda e: e.activation(out=c8[:], in_=lam[:], func=AF.Exp, scale=-1.0), reads=[lam], writes=[c8])
            b.op("act", lambda e: e.activation(out=c8[:], in_=c8[:], func=AF.Ln, bias=1.0), reads=[c8], writes=[c8])
            b.op("dve", lambda e: e.tensor_scalar(out=c16[:], in0=c8[:], scalar1=-16.0, scalar2=None, op0=ALU.mult),
                 reads=[c8], writes=[c16])
            b.op("dve", lambda e: e.tensor_scalar(out=c8[:], in0=c8[:], scalar1=-8.0, scalar2=None, op0=ALU.mult),
                 reads=[c8, c16], writes=[c8])
            XR = sb("XR", [128, SEQC], F32)
            Y = sb("Y", [128, SEQC], F32)
            YB = sb("YB", [128, SEQC], BF16)
            A = sb("A", [128, SEQC], F32)
            U = sb("U", [128, SEQC], F32)
            H = sb("H", [128, SEQC], F32)
            GR = sb("GR", [128, SEQC], F32)
            ZT = sb("ZT", [128, 8, SEQC], BF16)
            tr = sb("tr", [128, 512], F32)
            ta2 = sb("ta2", [128, 512], F32)
            ti_ = sb("ti", [128, 512], F32)
            gs = [sb("gs%d" % i, [128, 512], F32) for i in range(2)]
            stage = [sb("stage%d" % i, [128, 512], F32) for i in range(2)]
            pA = ps("pA", [128, 512], F32)
            pX = ps("pX", [128, 512], F32)
            pO = [ps("pO%d" % i, [128, 512], F32) for i in range(2)]
            V0 = 112
            NV = SEQC - V0
            k = 0
            for s in range(NSEQ):
                sc = s * SEQC
                for n_ in range(8):
                    r0 = n_ * 128
                    b.dma("sp", ld, lambda e: e.dma_start(out=XR[:], in_=S_xr.h.ap()[r0:r0 + 128, sc:sc + SEQC]),
                          reads=[S_xr], writes=[XR])
                    b.dma("sp", ld, lambda e: e.dma_start(out=GR[:], in_=S_gr.h.ap()[r0:r0 + 128, sc:sc + SEQC]),
                          reads=[S_gr], writes=[GR])
                    b.op("dve", lambda e: e.tensor_scalar(out=Y[:, V0:SEQC], in0=XR[:, V0 - 3:SEQC - 3], scalar1=cw[:, n_, 0:1],
                                                          scalar2=cb[:, n_:n_ + 1], op0=ALU.mult, op1=ALU.add),
                         reads=[XR, cw, cb], writes=[Y])
                    for kk in range(1, 4):
                        b.op("dve", lambda e: e.scalar_tensor_tensor(out=Y[:, V0:SEQC], in0=XR[:, V0 - 3 + kk:SEQC - 3 + kk],
                                                                     scalar=cw[:, n_, kk:kk + 1], in1=Y[:, V0:SEQC],
                                                                     op0=ALU.mult, op1=ALU.add), reads=[XR, cw, Y], writes=[Y])
                    b.op("act", lambda e: e.activation(out=YB[:, V0:SEQC], in_=Y[:, V0:SEQC], func=AF.Copy), reads=[Y], writes=[YB])
                    for (c0, n) in GROUPS:
                        if c0 == 0:
                            c0, n = V0, 16
                        cs = slice(c0, c0 + n)
                        b.op("pe", lambda e: e.matmul(pA[:, 0:n], lhsT=WA[:, n_, :], rhs=YB[:, cs], start=True, stop=True),
                             reads=[WA, YB], writes=[pA])
                        b.op("pe", lambda e: e.matmul(pX[:, 0:n], lhsT=WX[:, n_, :], rhs=YB[:, cs], start=True, stop=True),
                             reads=[WX, YB], writes=[pX])
                        b.op("act", lambda e: e.activation(out=tr[:, 0:n], in_=pA[:, 0:n], func=AF.Sigmoid, bias=ba[:, n_:n_ + 1]),
                             reads=[pA, ba], writes=[tr])
                        b.op("act", lambda e: e.activation(out=ti_[:, 0:n], in_=pX[:, 0:n], func=AF.Sigmoid, bias=bx[:, n_:n_ + 1]),
                             reads=[pX, bx], writes=[ti_])
                        b.op("act", lambda e: e.activation(out=A[:, cs], in_=tr[:, 0:n], func=AF.Exp, scale=c8[:, n_:n_ + 1]),
                             reads=[tr, c8], writes=[A])
                        b.op("act", lambda e: e.activation(out=ta2[:, 0:n], in_=tr[:, 0:n], func=AF.Exp, scale=c16[:, n_:n_ + 1]),
                             reads=[tr, c16], writes=[ta2])
                        b.op("dve", lambda e: e.tensor_scalar(out=ta2[:, 0:n], in0=ta2[:, 0:n], scalar1=-1.0, scalar2=1.0,
                                                              op0=ALU.mult, op1=ALU.add), reads=[ta2], writes=[ta2])
                        b.op("act", lambda e: e.activation(out=ta2[:, 0:n], in_=ta2[:, 0:n], func=AF.Sqrt), reads=[ta2], writes=[ta2])
                        b.op("dve", lambda e: e.tensor_tensor(out=ti_[:, 0:n], in0=ti_[:, 0:n], in1=Y[:, cs], op=ALU.mult),
                             reads=[ti_, Y], writes=[ti_])
                        b.op("dve", lambda e: e.tensor_tensor(out=U[:, cs], in0=ti_[:, 0:n], in1=ta2[:, 0:n], op=ALU.mult),
                             reads=[ti_, ta2], writes=[U])
                    b.op("dve", lambda e: e.tensor_tensor_scan(out=H[:, V0:SEQC], data0=A[:, V0:SEQC], data1=U[:, V0:SEQC],
                                                               initial=0.0, op0=ALU.mult, op1=ALU.add), reads=[A, U], writes=[H])
                    b.op("dve", lambda e: e.tensor_tensor(out=ZT[:, n_, V0:SEQC], in0=H[:, V0:SEQC], in1=GR[:, V0:SEQC], op=ALU.mult),
                         reads=[H, GR], writes=[ZT])
                for (c0, n) in GROUPS[1:]:
                    cs = slice(c0, c0 + n)
                    for dc in range(8):
                        PO = pO[k % 2]
                        G = gs[k % 2]
                        SG = stage[k % 2]
                        k += 1
                        b.dma("sp", ld, lambda e: e.dma_start(out=G[:, 0:n], in_=S_grnn.h.ap()[dc * 128:(dc + 1) * 128, sc + c0:sc + c0 + n]),
                              reads=[S_grnn], writes=[G])
                        for kc in range(8):
                            b.op("pe", lambda e: e.matmul(PO[:, 0:n], lhsT=WRO[:, kc, dc * 128:(dc + 1) * 128], rhs=ZT[:, kc, cs],
                                                          start=(kc == 0), stop=(kc == 7)), reads=[WRO, ZT], writes=[PO])
                        b.op("dve", lambda e: e.tensor_tensor(out=SG[:, 0:n], in0=PO[:, 0:n], in1=G[:, 0:n], op=ALU.mult),
                             reads=[PO, G], writes=[SG])
                        b.dma("pool", st, lambda e: e.dma_start(out=S_mrnn.h.ap()[dc * 128:(dc + 1) * 128, sc + c0:sc + c0 + n],
                                                                in_=SG[:, 0:n]), reads=[SG], writes=[S_mrnn])
        b.barrier()


    def phase3a():
        with ExitStack() as es:
            sb, ps = mk(es, "p3_")
            WUQ = sb("WUQ", [128, 3, 3072], BF16)
            WUKV = sb("WUKV", [128, 2, 2048], BF16)
            qg = load_small(sb, "qg", qg_d, [128, 3])
            kvg = load_small(sb, "kvg", kvg_d, [128, 2])
            wst = sb("wst", [128, 3072], F32)
            for kc in range(3):
                b.dma("sp", wl, lambda e: e.dma_start(out=wst[:], in_=wuq_d.ap()[kc * 128:(kc + 1) * 128, 0:3072]), writes=[wst])
                b.op("dve", lambda e: e.tensor_scalar(out=WUQ[:, kc, :], in0=wst[:], scalar1=qg[:, kc:kc + 1], scalar2=None,
                                                      op0=ALU.mult), reads=[wst, qg], writes=[WUQ])
            for kc in range(2):
                b.dma("sp", wl, lambda e: e.dma_start(out=wst[:, 0:2048], in_=wukv_d.ap()[kc * 128:(kc + 1) * 128, :]), writes=[wst])
                b.op("dve", lambda e: e.tensor_scalar(out=WUKV[:, kc, :], in0=wst[:, 0:2048], scalar1=kvg[:, kc:kc + 1], scalar2=None,
                                                      op0=ALU.mult), reads=[wst, kvg], writes=[WUKV])
            ident, identf = make_ident(sb)
            ones = sb("ones", [128, 128], F32)
            b.op("pool", lambda e: e.memset(ones[:], 1.0), writes=[ones])
            tri = sb("tri", [128, 128], BF16)
            b.op("pool", lambda e: e.memset(identf[:], 1.0), reads=[identf], writes=[identf])
            b.op("pool", lambda e: e.affine_select(out=identf[:], in_=identf[:], pattern=[[1, 128]], compare_op=ALU.is_ge,
                                                   fill=0.0, base=0, channel_multiplier=-1), reads=[identf], writes=[identf])
            b.op("dve", lambda e: e.tensor_copy(out=tri[:], in_=identf[:]), reads=[identf], writes=[tri])
            Kn = sb("Kn", [128, 8, SEQC], BF16)
            Kr2 = sb("Kr2", [128, SEQC], BF16)
            V = sb("V", [128, 17, 16, 65], BF16)
            b.op("pool", lambda e: e.memset(V[:], 1.0), writes=[V])
            cq_t = sb("cq_t", [128, 3, 512], F32)
            ckv_t = sb("ckv_t", [128, 2, 512], F32)
            sq = sb("sq", [128, 3, 512], F32)
            rstd = sb("rstd", [128, 512], F32)
            cqn = sb("cqn", [128, 3, 512], BF16)
            ckvn = sb("ckvn", [128, 2, 512], BF16)
            kr_t = sb("kr_t", [128, 512], F32)
            krs_t = sb("krs_t", [128, 512], F32)
            cos_t = sb("cos_t", [128, 512], F32)
            sin_t = sb("sin_t", [128, 512], F32)
            tmp1 = sb("tmp1", [128, 512], F32)
            tmp2 = sb("tmp2", [128, 512], F32)
            Qn = sb("Qn", [128, 8, 512], BF16)
            Qr = sb("Qr", [128, 8, 512], BF16)
            PTs = [sb("PTs%d" % i, [128, 512], BF16) for i in range(3)]
            o_tm = sb("o_tm", [128, 4, 1024], BF16)
            oT = sb("oT", [128, 8, 512], BF16)
            rec = sb("rec", [128, 4], F32)
            pn = ps("pn", [128, 512], F32)
            pk = [ps("pk%d" % i, [128, 512], F32) for i in range(2)]
            pS = [ps("pS%d" % i, [128, 512], F32) for i in range(2)]
            pO = [ps("pO%d" % i, [128, 512], F32) for i in range(2)]
            pT = ps("pT", [128, 1024], BF16)
            scale = float(96 ** -0.5)
            ki = [0]

            def nextpk():
                ki[0] += 1
                return pk[ki[0] % 2]

            def rmsn(src, nch, dst, nfeat, n):
                b.op("act", lambda e: e.activation(out=sq[:, 0:nch, 0:n], in_=src[:, :, 0:n], func=AF.Square), reads=[src], writes=[sq])
                for c in range(nch):
                    b.op("pe", lambda e: e.matmul(pn[:, 0:n], lhsT=ones[:], rhs=sq[:, c, 0:n], start=(c == 0), stop=(c == nch - 1)),
                         reads=[ones, sq], writes=[pn])
                b.op("dve", lambda e: e.tensor_scalar(out=rstd[:, 0:n], in0=pn[:, 0:n], scalar1=1.0 / nfeat, scalar2=EPS,
                                                      op0=ALU.mult, op1=ALU.add), reads=[pn], writes=[rstd])
                b.op("act", lambda e: e.activation(out=rstd[:, 0:n], in_=rstd[:, 0:n], func=AF.Sqrt), reads=[rstd], writes=[rstd])
                b.op("dve", lambda e: e.reciprocal(out=rstd[:, 0:n], in_=rstd[:, 0:n]), reads=[rstd], writes=[rstd])
                for c in range(nch):
                    b.op("dve", lambda e: e.tensor_tensor(out=dst[:, c, 0:n], in0=src[:, c, 0:n], in1=rstd[:, 0:n], op=ALU.mult),
                         reads=[src, rstd], writes=[dst])

            si = 0
            for s in range(NSEQ):
                sc = s * SEQC
                for (c0, n) in GROUPS:
                    col = sc + c0
                    b.dma("sp", ld, lambda e: e.dma_start(out=cq_t[:, :, 0:n],
                                                          in_=S_cq.h.ap()[:, col:col + n].rearrange("(c p) n -> p c n", p=128)),
                          reads=[S_cq], writes=[cq_t])
                    b.dma("sp", ld, lambda e: e.dma_start(out=ckv_t[:, :, 0:n],
                                                          in_=S_ckv.h.ap()[:, col:col + n].rearrange("(c p) n -> p c n", p=128)),
                          reads=[S_ckv], writes=[ckv_t])
                    b.dma("sp", ld, lambda e: e.dma_start(out=kr_t[:, 0:n], in_=S_kr.h.ap()[:, col:col + n]), reads=[S_kr], writes=[kr_t])
                    b.dma("sp", ld, lambda e: e.dma_start(out=krs_t[:, 0:n], in_=S_krs.h.ap()[:, col:col + n]), reads=[S_krs], writes=[krs_t])
                    b.dma("sp", ld, lambda e: e.dma_start(out=cos_t[:, 0:n], in_=cos_d.ap()[:, c0:c0 + n]), writes=[cos_t])
                    b.dma("sp", ld, lambda e: e.dma_start(out=sin_t[:, 0:n], in_=sin_d.ap()[:, c0:c0 + n]), writes=[sin_t])
                    rmsn(cq_t, 3, cqn, 384.0, n)
                    rmsn(ckv_t, 2, ckvn, 256.0, n)
                    for j in range(8):
                        P_ = nextpk()
                        for kc in range(2):
                            b.op("pe", lambda e: e.matmul(P_[:, 0:n], lhsT=WUKV[:, kc, j * 128:(j + 1) * 128], rhs=ckvn[:, kc, 0:n],
                                                          start=(kc == 0), stop=(kc == 1)), reads=[WUKV, ckvn], writes=[P_])
                        b.op("act", lambda e: e.activation(out=Kn[:, j, c0:c0 + n], in_=P_[:, 0:n], func=AF.Copy), reads=[P_], writes=[Kn])
                    b.op("dve", lambda e: e.tensor_tensor(out=tmp1[:, 0:n], in0=kr_t[:, 0:n], in1=cos_t[:, 0:n], op=ALU.mult),
                         reads=[kr_t, cos_t], writes=[tmp1])
                    b.op("dve", lambda e: e.tensor_tensor(out=tmp2[:, 0:n], in0=krs_t[:, 0:n], in1=sin_t[:, 0:n], op=ALU.mult),
                         reads=[krs_t, sin_t], writes=[tmp2])
                    b.op("dve", lambda e: e.tensor_tensor(out=Kr2[:, c0:c0 + n], in0=tmp1[:, 0:n], in1=tmp2[:, 0:n], op=ALU.add),
                         reads=[tmp1, tmp2], writes=[Kr2])
                    for jt in range(n // 128):
                        t = c0 // 128 + jt
                        if t == 0:
                            lo, M = 112, 16
                        else:
                            lo, M = jt * 128, 128
                        for half in range(2):
                            P_ = nextpk()
                            for kc in range(2):
                                b.op("pe", lambda e: e.matmul(P_[0:M, :], lhsT=ckvn[:, kc, lo:lo + M],
                                                              rhs=WUKV[:, kc, 1024 + half * 512:1024 + (half + 1) * 512],
                                                              start=(kc == 0), stop=(kc == 1)), reads=[WUKV, ckvn], writes=[P_])
                            b.op("act", lambda e: e.activation(out=V[0:M, t, half * 8:(half + 1) * 8, 0:64],
                                                               in_=P_[0:M, :].rearrange("p (h d) -> p h d", h=8), func=AF.Copy),
                                 reads=[P_], writes=[V])
                    if c0 == 0:
                        continue
                    for j in range(8):
                        P_ = nextpk()
                        for kc in range(3):
                            b.op("pe", lambda e: e.matmul(P_[:, 0:n], lhsT=WUQ[:, kc, j * 128:(j + 1) * 128], rhs=cqn[:, kc, 0:n],
                                                          start=(kc == 0), stop=(kc == 2)), reads=[WUQ, cqn], writes=[P_])
                        b.op("act", lambda e: e.activation(out=Qn[:, j, 0:n], in_=P_[:, 0:n], func=AF.Copy), reads=[P_], writes=[Qn])
                    for j in range(8):
                        P1 = nextpk()
                        for kc in range(3):
                            b.op("pe", lambda e: e.matmul(P1[:, 0:n], lhsT=WUQ[:, kc, 1024 + j * 128:1024 + (j + 1) * 128], rhs=cqn[:, kc, 0:n],
                                                          start=(kc == 0), stop=(kc == 2)), reads=[WUQ, cqn], writes=[P1])
                        b.op("dve", lambda e: e.tensor_tensor(out=tmp1[:, 0:n], in0=P1[:, 0:n], in1=cos_t[:, 0:n], op=ALU.mult),
                             reads=[P1, cos_t], writes=[tmp1])
                        P2 = nextpk()
                        for kc in range(3):
                            b.op("pe", lambda e: e.matmul(P2[:, 0:n], lhsT=WUQ[:, kc, 2048 + j * 128:2048 + (j + 1) * 128], rhs=cqn[:, kc, 0:n],
                                                          start=(kc == 0), stop=(kc == 2)), reads=[WUQ, cqn], writes=[P2])
                        b.op("dve", lambda e: e.tensor_tensor(out=tmp2[:, 0:n], in0=P2[:, 0:n], in1=sin_t[:, 0:n], op=ALU.mult),
                             reads=[P2, sin_t], writes=[tmp2])
                        b.op("dve", lambda e: e.tensor_tensor(out=Qr[:, j, 0:n], in0=tmp1[:, 0:n], in1=tmp2[:, 0:n], op=ALU.add),
                             reads=[tmp1, tmp2], writes=[Qr])
                    g0t = c0 // 128
                    for h in range(16):
                        j = h // 2
                        p0 = (h % 2) * 64
                        PO = pO[h % 2]
                        POv = PO[:, 0:260].rearrange("p (q d) -> p q d", q=4)
                        for kt in range(0, g0t + 4):
                            if kt == 0:
                                k0, M = 112, 16
                            else:
                                k0, M = kt * 128, 128
                            qlo = max(0, kt - g0t)
                            q0 = qlo * 128
                            PS_ = pS[si % 2]
                            PTb = PTs[si % 3]
                            si += 1
                            b.op("pe", lambda e: e.matmul(PS_[0:M, q0:512], lhsT=Kn[p0:p0 + 64, j, k0:k0 + M], rhs=Qn[p0:p0 + 64, j, q0:512],
                                                          start=True, stop=False), reads=[Kn, Qn], writes=[PS_])
                            b.op("pe", lambda e: e.matmul(PS_[0:M, q0:512], lhsT=Kr2[p0:p0 + 32, k0:k0 + M], rhs=Qr[p0:p0 + 32, j, q0:512],
                                                          start=False, stop=True), reads=[Kr2, Qr], writes=[PS_])
                            b.op("act", lambda e: e.activation(out=PTb[0:M, q0:512], in_=PS_[0:M, q0:512], func=AF.Exp, scale=scale),
                                 reads=[PS_], writes=[PTb])
                            if kt >= g0t:
                                b.op("dve", lambda e: e.tensor_tensor(out=PTb[:, q0:q0 + 128], in0=PTb[:, q0:q0 + 128], in1=tri[:], op=ALU.mult),
                                     reads=[PTb, tri], writes=[PTb])
                            for qt in range(qlo, 4):
                                b.op("pe", lambda e: e.matmul(POv[:, qt, :], lhsT=PTb[0:M, qt * 128:(qt + 1) * 128], rhs=V[0:M, kt, h, :],
                                                              start=(kt == 0 and qt == 0), stop=(kt == g0t + qt), skip_group_check=True),
                                     reads=[PTb, V], writes=[PO])
                        b.op("dve", lambda e: e.reciprocal(out=rec[:], in_=POv[:, :, 64]), reads=[PO], writes=[rec])
                        for qt in range(4):
                            b.op("dve", lambda e: e.tensor_scalar(out=o_tm[:, qt, h * 64:(h + 1) * 64], in0=POv[:, qt, 0:64],
                                                                  scalar1=rec[:, qt:qt + 1], scalar2=None, op0=ALU.mult),
                                 reads=[PO, rec], writes=[o_tm])
                    for qt in range(4):
                        for c in range(8):
                            b.op("pe", lambda e: e.transpose(out=pT[:, c * 128:(c + 1) * 128], in_=o_tm[:, qt, c * 128:(c + 1) * 128],
                                                             identity=ident[:]), reads=[o_tm, ident], writes=[pT])
                        b.op("dve", lambda e: e.tensor_copy(out=oT[:, :, qt * 128:(qt + 1) * 128],
                                                            in_=pT[:].rearrange("p (c t) -> p c t", c=8)), reads=[pT], writes=[oT])
                    b.dma("pool", st, lambda e: e.dma_start(out=S_oT.h.ap()[:, col:col + n].rearrange("(c p) n -> p c n", p=128), in_=oT[:]),
                          reads=[oT], writes=[S_oT])
        b.barrier()

    def phase3b():
        with ExitStack() as es:
            sb, ps = mk(es, "p3b_")
            WAO = sb("WAO", [128, 8, 1024], BF16)
            WO = sb("WO", [128, 8, 1024], BF16)
            load_w_cast(WAO, WAO[:], wao_d.ap().rearrange("(k p) f -> p k f", p=128))
            load_w_cast(WO, WO[:], wo_d.ap().rearrange("(k p) f -> p k f", p=128))
            oT = [sb("oT%d" % i, [128, 8, 512], BF16) for i in range(2)]
            ga = [sb("ga%d" % i, [128, 512], F32) for i in range(2)]
            mr = [sb("mr%d" % i, [128, 512], F32) for i in range(2)]
            tmp = sb("tmp", [128, 512], F32)
            mixT = sb("mixT", [128, 8, 512], BF16)
            xt = [sb("xt%d" % i, [128, 1024], F32) for i in range(2)]
            h2 = [sb("h2%d" % i, [128, 1024], F32) for i in range(2)]
            pk = [ps("pk%d" % i, [128, 512], F32) for i in range(4)]
            gi = 0
            k = 0
            ti = 0
            for s in range(NSEQ):
                sc = s * SEQC
                for g, (c0, n) in enumerate(GROUPS):
                    if g == 0:
                        continue
                    col = sc + c0
                    OT = oT[gi % 2]
                    gi += 1
                    b.dma("sp", ld, lambda e: e.dma_start(out=OT[:], in_=S_oT.h.ap()[:, col:col + n].rearrange("(c p) n -> p c n", p=128)),
                          reads=[S_oT], writes=[OT])
                    for dc in range(8):
                        GA = ga[k % 2]
                        MR = mr[k % 2]
                        PK = pk[k % 4]
                        k += 1
                        b.dma("sp", ld, lambda e: e.dma_start(out=GA[:], in_=S_gatt.h.ap()[dc * 128:(dc + 1) * 128, col:col + n]),
                              reads=[S_gatt], writes=[GA])
                        b.dma("sp", ld, lambda e: e.dma_start(out=MR[:], in_=S_mrnn.h.ap()[dc * 128:(dc + 1) * 128, col:col + n]),
                              reads=[S_mrnn], writes=[MR])
                        for kc in range(8):
                            b.op("pe", lambda e: e.matmul(PK[:], lhsT=WAO[:, kc, dc * 128:(dc + 1) * 128], rhs=OT[:, kc, :],
                                                          start=(kc == 0), stop=(kc == 7)), reads=[WAO, OT], writes=[PK])
                        b.op("dve", lambda e: e.tensor_tensor(out=tmp[:], in0=PK[:], in1=GA[:], op=ALU.mult), reads=[PK, GA], writes=[tmp])
                        b.op("dve", lambda e: e.tensor_tensor(out=mixT[:, dc, :], in0=tmp[:], in1=MR[:], op=ALU.add),
                             reads=[tmp, MR], writes=[mixT])
                    for qt in range(4):
                        X = xt[ti % 2]
                        H2 = h2[ti % 2]
                        ti += 1
                        r0 = (g - 1) * 512 + qt * 128
                        b.dma("sp", ld, lambda e: e.dma_start(out=X[:], in_=x_d.ap()[s, r0:r0 + 128, :]), writes=[X])
                        for half in range(2):
                            PK = pk[k % 4]
                            k += 1
                            for kc in range(8):
                                b.op("pe", lambda e: e.matmul(PK[:], lhsT=mixT[:, kc, qt * 128:(qt + 1) * 128],
                                                              rhs=WO[:, kc, half * 512:(half + 1) * 512],
                                                              start=(kc == 0), stop=(kc == 7)), reads=[WO, mixT], writes=[PK])
                            b.op("dve", lambda e: e.tensor_tensor(out=H2[:, half * 512:(half + 1) * 512], in0=PK[:],
                                                                  in1=X[:, half * 512:(half + 1) * 512], op=ALU.add),
                                 reads=[PK, X], writes=[H2])
                        b.dma("pool", st, lambda e: e.dma_start(out=S_h2.h.ap()[s * NREAL + r0:s * NREAL + r0 + 128, :], in_=H2[:]),
                              reads=[H2], writes=[S_h2])
        b.barrier()


    def phase5():
        with ExitStack() as es:
            sb, ps = mk(es, "p5_")
            WQ = sb("WQ", [128, 8, 2048], BF16)
            K1T = sb("K1T", [128, 128], BF16)
            K2T = sb("K2T", [128, 128], BF16)
            load_w_cast(WQ, WQ[:], wq_d.ap().rearrange("(k p) f -> p k f", p=128))
            load_w_cast(K1T, K1T[:], k1T_d.ap())
            load_w_cast(K2T, K2T[:], k2T_d.ap())
            g2b = sb("g2b", [128, 1024], F32)
            gfb = sb("gfb", [128, 1024], F32)
            b.dma("sp", None, lambda e: e.dma_start(out=g2b[:], in_=g2_d.ap().to_broadcast([128, 1024])), writes=[g2b])
            b.dma("sp", None, lambda e: e.dma_start(out=gfb[:], in_=gf_d.ap().to_broadcast([128, 1024])), writes=[gfb])
            ident, identf = make_ident(sb)
            iota_i = sb("iota_i", [128, 16], I32)
            iota16 = sb("iota16", [128, 16], F32)
            b.op("pool", lambda e: e.iota(iota_i[:], pattern=[[1, 16]], base=0, channel_multiplier=0), writes=[iota_i])
            b.op("dve", lambda e: e.tensor_copy(out=iota16[:], in_=iota_i[:]), reads=[iota_i], writes=[iota16])
            L = sb("L", [128, 128, 128], BF16)
            b.op("pool", lambda e: e.memset(L[:], 0.0), writes=[L])
            Lflat = L[:].rearrange("p a b -> p (a b)")
            Xs = [sb("X%d" % i, [128, 1024], F32) for i in range(2)]
            xnbs = [sb("xnb%d" % i, [128, 1024], BF16) for i in range(2)]
            GTs = [sb("GT%d" % i, [128, 128], F32) for i in range(2)]
            IDXTs = [sb("IDXT%d" % i, [128, 128], U32) for i in range(2)]
            Gs = [sb("G%d" % i, [128, 8, 16], F32) for i in range(2)]
            ssA = sb("ssA", [128, 1], F32)
            ssC = sb("ssC", [128, 1], F32)
            junkA = sb("junkA", [128, 1024], BF16)
            junkD = sb("junkD", [128, 1024], BF16)
            xT = sb("xT", [128, 8, 128], BF16)
            qT = sb("qT", [128, 16, 128], BF16)
            S = sb("S", [128, 16, 128], F32)
            eqv = S[:].rearrange("p (h x) (y a) -> p h (x y) a", x=2, a=16)
            T16 = sb("T16", [128, 16, 16], F32)
            I16 = sb("I16", [128, 16, 16], U32)
            I16f = sb("I16f", [128, 16, 16], F32)
            cand = sb("cand", [128, 8, 256], F32)
            TS = sb("TS", [128, 8, 16], F32)
            CI = sb("CI", [128, 8, 16], U32)
            CIa = sb("CIa", [128, 8, 16], U32)
            CIb = sb("CIb", [128, 8, 16], U32)
            Af = sb("Af", [128, 8, 16], F32)
            Bf = sb("Bf", [128, 8, 16], F32)
            i1s = sb("i1s", [128, 128], F32)
            i2s = sb("i2s", [128, 128], F32)
            i1b = sb("i1b", [128, 128], BF16)
            i2b = sb("i2b", [128, 128], BF16)
            idxf = sb("idxf", [128, 128], F32)
            idxTf = sb("idxTf", [128, 128], F32)
            iTf = sb("iTf", [128, 2, 128], F32)
            E = sb("E", [128, 8, 16], F32)
            Z = sb("Z", [128, 8], F32)
            ACTV = sb("ACTV", [128, 128], F32)
            coefb = sb("coefb", [128, 128], BF16)
            coefT = sb("coefT", [128, 128], BF16)
            UVG = [sb("UVG%d" % i, [128, 8, 2048], BF16) for i in range(2)]
            actT = sb("actT", [128, 128], F32)
            Ls = [T(L.h, "L%d" % i) for i in range(16)]
            for lt in Ls:
                lt.w = dict(L.w)
            h3 = sb("h3", [128, 1024], F32)
            pTa = ps("pTa", [128, 1024], BF16)
            pq = [ps("pq%d" % i, [128, 512], F32) for i in range(1)]
            pOut = [ps("pOut%d" % i, [128, 512], F32) for i in range(2)]
            pX = [ps("pX%d" % i, [128, 1024], F32) for i in range(2)]
            T16v = T16[:].rearrange("p (h two) a -> p h two a", two=2)
            I16fv = I16f[:].rearrange("p (h two) a -> p h two a", two=2)
            B4 = [128, 8, 16, 16]
            cnt = {"u": 0, "v": 0, "q": 0}
            ntiles = NR // 128 if p5tiles is None else p5tiles

            def sweep(eng, fns, reads, writes):
                for k_, f in enumerate(fns):
                    w = writes if (k_ == 0 or k_ == len(fns) - 1) else ()
                    b.op(eng, f, reads=reads, writes=w)

            def top16(vals, ng, tv, iv):
                sweep("dve", [(lambda e, g=g: e.max(out=tv[:, g, 0:8], in_=vals[:, g, :])) for g in range(ng)], [vals], [tv])
                sweep("dve", [(lambda e, g=g: e.max_index(out=iv[:, g, 0:8], in_max=tv[:, g, 0:8], in_values=vals[:, g, :])) for g in range(ng)],
                      [vals, tv], [iv])
                yield
                sweep("dve", [(lambda e, g=g: e.match_replace(out=vals[:, g, :], in_to_replace=tv[:, g, 0:8], in_values=vals[:, g, :],
                                                              imm_value=-1e30)) for g in range(ng)], [tv], [vals])
                yield
                sweep("dve", [(lambda e, g=g: e.max(out=tv[:, g, 8:16], in_=vals[:, g, :])) for g in range(ng)], [vals], [tv])
                sweep("dve", [(lambda e, g=g: e.max_index(out=iv[:, g, 8:16], in_max=tv[:, g, 8:16], in_values=vals[:, g, :])) for g in range(ng)],
                      [vals, tv], [iv])
                yield

            def stageA(i):
                par = i % 2
                X, xnb, GT, IDXT, G = Xs[par], xnbs[par], GTs[par], IDXTs[par], Gs[par]
                b.dma("sp", None, lambda e: e.dma_start(out=X[:], in_=S_h2.h.ap()[i * 128:(i + 1) * 128, :]), reads=[S_h2], writes=[X])
                b.op("act", lambda e: e.activation(out=junkA[:], in_=X[:], func=AF.Square, accum_out=ssA[:]), reads=[X], writes=[junkA, ssA])
                b.op("dve", lambda e: e.tensor_scalar(out=ssA[:], in0=ssA[:], scalar1=1.0 / 1024, scalar2=EPS, op0=ALU.mult, op1=ALU.add),
                     reads=[ssA], writes=[ssA])
                b.op("act", lambda e: e.activation(out=ssA[:], in_=ssA[:], func=AF.Sqrt), reads=[ssA], writes=[ssA])
                b.op("dve", lambda e: e.reciprocal(out=ssA[:], in_=ssA[:]), reads=[ssA], writes=[ssA])
                b.op("dve", lambda e: e.scalar_tensor_tensor(out=xnb[:], in0=X[:], scalar=ssA[:, 0:1], in1=g2b[:], op0=ALU.mult, op1=ALU.mult),
                     reads=[X, ssA, g2b], writes=[xnb])
                yield
                for c in range(8):
                    b.op("pe", lambda e: e.transpose(out=pTa[:, c * 128:(c + 1) * 128], in_=xnb[:, c * 128:(c + 1) * 128], identity=ident[:]),
                         reads=[xnb, ident], writes=[pTa])
                b.op("act", lambda e: e.activation(out=xT[:], in_=pTa[:].rearrange("p (c t) -> p c t", c=8), func=AF.Copy), reads=[pTa], writes=[xT])
                yield
                for bq in range(4):
                    PQ = pq[0]
                    for j in range(4):
                        hh = bq * 4 + j
                        for kc in range(8):
                            b.op("pe", lambda e: e.matmul(PQ[:, j * 128:(j + 1) * 128], lhsT=WQ[:, kc, hh * 128:(hh + 1) * 128], rhs=xT[:, kc, :],
                                                          start=(kc == 0), stop=(kc == 7), skip_group_check=True), reads=[WQ, xT], writes=[PQ])
                    b.op("act", lambda e: e.activation(out=qT[:, bq * 4:(bq + 1) * 4, :], in_=PQ[:].rearrange("p (j t) -> p j t", j=4), func=AF.Copy),
                         reads=[PQ], writes=[qT])
                    yield
                for bq in range(4):
                    PQ = pq[0]
                    for j in range(4):
                        hh = bq * 4 + j
                        KT = K1T if hh % 2 == 0 else K2T
                        b.op("pe", lambda e: e.matmul(PQ[:, j * 128:(j + 1) * 128], lhsT=qT[:, hh, :], rhs=KT[:], start=True, stop=True,
                                                      skip_group_check=True), reads=[qT, KT], writes=[PQ])
                    b.op("act", lambda e: e.activation(out=S[:, bq * 4:(bq + 1) * 4, :], in_=PQ[:].rearrange("p (j t) -> p j t", j=4), func=AF.Copy),
                         reads=[PQ], writes=[S])
                    yield
                yield from top16(S, 16, T16, I16)
                b.op("dve", lambda e: e.tensor_copy(out=I16f[:], in_=I16[:]), reads=[I16], writes=[I16f])
                b.op("dve", lambda e: e.tensor_tensor(out=cand[:].rearrange("p h (a c) -> p h a c", a=16),
                                                      in0=T16v[:, :, 0, :].unsqueeze(3).to_broadcast(B4),
                                                      in1=T16v[:, :, 1, :].unsqueeze(2).to_broadcast(B4), op=ALU.add),
                     reads=[T16], writes=[cand])
                yield
                yield from top16(cand, 8, TS, CI)
                b.op("dve", lambda e: e.tensor_single_scalar(out=CIa[:], in_=CI[:], scalar=4, op=ALU.logical_shift_right), reads=[CI], writes=[CIa])
                b.op("dve", lambda e: e.tensor_single_scalar(out=CIb[:], in_=CI[:], scalar=15, op=ALU.bitwise_and), reads=[CI], writes=[CIb])
                b.op("dve", lambda e: e.tensor_copy(out=Af[:], in_=CIa[:]), reads=[CIa], writes=[Af])
                b.op("dve", lambda e: e.tensor_copy(out=Bf[:], in_=CIb[:]), reads=[CIb], writes=[Bf])
                yield
                for (SEL, half, dst) in ((Af, 0, i1s), (Bf, 1, i2s)):
                    b.op("dve", lambda e: e.tensor_tensor(out=eqv, in0=SEL[:].unsqueeze(3).to_broadcast(B4),
                                                          in1=iota16[:].unsqueeze(1).unsqueeze(1).to_broadcast(B4), op=ALU.is_equal),
                         reads=[SEL, iota16], writes=[S])
                    b.op("dve", lambda e: e.tensor_tensor(out=eqv, in0=eqv, in1=I16fv[:, :, half, :].unsqueeze(2).to_broadcast(B4), op=ALU.mult),
                         reads=[S, I16f], writes=[S])
                    b.op("dve", lambda e: e.tensor_reduce(out=dst[:].rearrange("p (h k) -> p h k", h=8), in_=eqv, axis=AX.X, op=ALU.add),
                         reads=[S], writes=[dst])
                    yield
                b.op("act", lambda e: e.activation(out=i1b[:], in_=i1s[:], func=AF.Copy), reads=[i1s], writes=[i1b])
                b.op("act", lambda e: e.activation(out=i2b[:], in_=i2s[:], func=AF.Copy), reads=[i2s], writes=[i2b])
                b.op("pe", lambda e: e.transpose(out=pTa[:, 0:128], in_=i1b[:], identity=ident[:]), reads=[i1b, ident], writes=[pTa])
                b.op("pe", lambda e: e.transpose(out=pTa[:, 128:256], in_=i2b[:], identity=ident[:]), reads=[i2b, ident], writes=[pTa])
                b.op("act", lambda e: e.activation(out=iTf[:], in_=pTa[:, 0:256].rearrange("p (a t) -> p a t", a=2), func=AF.Copy),
                     reads=[pTa], writes=[iTf])
                yield
                b.op("dve", lambda e: e.scalar_tensor_tensor(out=idxTf[:], in0=iTf[:, 0, :], scalar=128.0, in1=iTf[:, 1, :], op0=ALU.mult, op1=ALU.add),
                     reads=[iTf], writes=[idxTf])
                b.op("dve", lambda e: e.tensor_copy(out=IDXT[:], in_=idxTf[:]), reads=[idxTf], writes=[IDXT])
                b.op("dve", lambda e: e.tensor_tensor(out=E[:], in0=TS[:], in1=TS[:, :, 0:1].to_broadcast([128, 8, 16]), op=ALU.subtract),
                     reads=[TS], writes=[E])
                b.op("act", lambda e: e.activation(out=E[:], in_=E[:], func=AF.Exp), reads=[E], writes=[E])
                b.op("dve", lambda e: e.tensor_reduce(out=Z[:], in_=E[:], axis=AX.X, op=ALU.add), reads=[E], writes=[Z])
                b.op("dve", lambda e: e.reciprocal(out=Z[:], in_=Z[:]), reads=[Z], writes=[Z])
                b.op("dve", lambda e: e.tensor_tensor(out=G[:], in0=E[:], in1=Z[:].unsqueeze(2).to_broadcast([128, 8, 16]), op=ALU.mult),
                     reads=[E, Z], writes=[G])
                b.op("pe", lambda e: e.transpose(out=pq[0][:, 0:128], in_=G[:].rearrange("p h k -> p (h k)"), identity=identf[:]), reads=[G, identf], writes=[pq[0]])
                b.op("act", lambda e: e.activation(out=GT[:], in_=pq[0][:, 0:128], func=AF.Copy), reads=[pq[0]], writes=[GT])
                yield

            def step(gen, k=1):
                if gen is None:
                    return
                for _ in range(k):
                    try:
                        next(gen)
                    except StopIteration:
                        return

            g0 = stageA(0)
            step(g0, 1000)
            for i in range(ntiles):
                par = i % 2
                X, xnb, GT, IDXT = Xs[par], xnbs[par], GTs[par], IDXTs[par]
                s, r0 = divmod(i * 128, NREAL)
                gN = stageA(i + 1) if i + 1 < ntiles else None
                pend = [None]

                def finish(tb_, UV_):
                    Lt = Ls[tb_]
                    tsl = slice(tb_ * 8, tb_ * 8 + 8)
                    b.op("act", lambda e: e.activation(out=actT[:, tsl], in_=actT[:, tsl], func=AF.Gelu_apprx_tanh), reads=[actT], writes=[actT])
                    b.op("dve", lambda e: e.tensor_tensor(out=coefT[:, tsl], in0=actT[:, tsl], in1=GT[:, tsl], op=ALU.mult),
                         reads=[actT, GT], writes=[coefT])
                    b.op("dve", lambda e: e.tensor_copy(out=Lflat[:, tb_ * 8 * 129:tb_ * 8 * 129 + 7 * 129 + 1:129], in_=coefT[:, tsl]),
                         reads=[coefT], writes=[Lt])
                    for q in range(8):
                        t = tb_ * 8 + q
                        for half in range(2):
                            b.op("pe", lambda e: e.matmul(pOut[half][:], lhsT=L[:, t, :], rhs=UV_[:, q, 1024 + half * 512:1024 + (half + 1) * 512],
                                                          start=(t == 0), stop=(t == 127)), reads=[Lt, UV_], writes=[pOut[half]])

                for tb in range(16):
                    UV = UVG[cnt["u"] % 2]
                    cnt["u"] += 1
                    for q in range(8):
                        t = tb * 8 + q
                        b.dma("pool", None, lambda e: e.indirect_dma_start(out=UV[:, q, :], out_offset=None, in_=UV16.h.ap(),
                                                                           in_offset=bass.IndirectOffsetOnAxis(ap=IDXT[:, t:t + 1], axis=0)),
                              reads=[IDXT, UV16], writes=[UV])
                    for q in range(8):
                        t = tb * 8 + q
                        PX = pX[cnt["v"] % 2]
                        cnt["v"] += 1
                        for half in range(2):
                            b.op("pe", lambda e: e.matmul(PX[:, half * 512:(half + 1) * 512], lhsT=ident[:, t:t + 1].to_broadcast([128, 128]),
                                                          rhs=xnb[:, half * 512:(half + 1) * 512], start=True, stop=True, skip_group_check=True),
                                 reads=[ident, xnb], writes=[PX])
                        b.op("dve", lambda e: e.scalar_tensor_tensor(out=junkD[:], in0=UV[:, q, 0:1024], scalar=1.0, in1=PX[:], op0=ALU.mult,
                                                                     op1=ALU.mult, accum_out=actT[:, t:t + 1]),
                             reads=[UV, PX], writes=[actT] if q in (0, 7) else ())
                        if q == 1 and pend[0] is not None:
                            finish(*pend[0])
                            pend[0] = None
                    pend[0] = (tb, UV)
                    step(gN, 2)
                finish(*pend[0])
                for half in range(2):
                    b.op("dve", lambda e: e.tensor_tensor(out=h3[:, half * 512:(half + 1) * 512], in0=pOut[half][:],
                                                          in1=X[:, half * 512:(half + 1) * 512], op=ALU.add), reads=[pOut[half], X], writes=[h3])
                b.op("act", lambda e: e.activation(out=junkA[:], in_=h3[:], func=AF.Square, accum_out=ssC[:]), reads=[h3], writes=[junkA, ssC])
                b.op("dve", lambda e: e.tensor_scalar(out=ssC[:], in0=ssC[:], scalar1=1.0 / 1024, scalar2=EPS, op0=ALU.mult, op1=ALU.add),
                     reads=[ssC], writes=[ssC])
                b.op("act", lambda e: e.activation(out=ssC[:], in_=ssC[:], func=AF.Sqrt), reads=[ssC], writes=[ssC])
                b.op("dve", lambda e: e.reciprocal(out=ssC[:], in_=ssC[:]), reads=[ssC], writes=[ssC])
                b.op("dve", lambda e: e.scalar_tensor_tensor(out=h3[:], in0=h3[:], scalar=ssC[:, 0:1], in1=gfb[:], op0=ALU.mult, op1=ALU.mult),
                     reads=[h3, ssC, gfb], writes=[h3])
                b.dma("sp", None, lambda e: e.dma_start(out=out_d.ap()[s, r0:r0 + 128, :], in_=h3[:]), reads=[h3], writes=[OUT])
                step(gN, 1000)
        b.barrier()

    progs = {1: phase1, 2: phase2, 3: phase3a, 4: phase3b, 5: phase5}
    for p in phases:
        if p in progs:
            progs[p]()
    b.barrier()
    return nc


def _pc(v, nchunk):
    return np.ascontiguousarray(np.asarray(v, np.float32).reshape(nchunk, 128).T)


def prep_common(inp):
    f = lambda a: np.asarray(a, np.float32)
    w_in = f(inp["w_in"])[0]
    z32 = np.zeros((1024, 32), np.float32)
    kr = w_in[:, 2688:2720]
    krs = np.concatenate([kr[:, 16:], kr[:, :16]], axis=1)
    w_in_r = np.concatenate([
        w_in[:, 0:1024], w_in[:, 1024:2048], w_in[:, 2048:2432], w_in[:, 2432:2688],
        kr, z32, kr, z32, krs, z32, krs, z32,
        w_in[:, 2720:3744], w_in[:, 3744:4768]], axis=1)
    assert w_in_r.shape == (1024, 4992)
    conv_w = f(inp["conv_w"])[0]
    cw = np.ascontiguousarray(conv_w.reshape(4, 8, 128).transpose(2, 1, 0))
    w_uq = f(inp["w_uq"])[0].reshape(384, 16, 96)
    nope = w_uq[:, :, :64].reshape(384, 1024)
    rope = w_uq[:, :, 64:]
    ropes = np.concatenate([rope[:, :, 16:], rope[:, :, :16]], axis=2)
    z = np.zeros((384, 16, 32), np.float32)
    rope_p = np.concatenate([rope, z], axis=2).reshape(384, 1024)
    ropes_p = np.concatenate([ropes, z], axis=2).reshape(384, 1024)
    w_uq_r = np.concatenate([nope, rope_p, ropes_p, np.zeros((384, 1024), np.float32)], axis=1)
    w_ukv = f(inp["w_ukv"])[0].reshape(256, 16, 128)
    w_ukv_r = np.concatenate([w_ukv[:, :, :64].reshape(256, 1024), w_ukv[:, :, 64:].reshape(256, 1024)], axis=1)
    pos = (np.arange(SEQC, dtype=np.float32) - 112.0).astype(np.float32)
    inv = np.power(np.float32(10000.0), -np.arange(16, dtype=np.float32) * np.float32(2.0 / 32)).astype(np.float32)
    ang = (pos[None, :] * inv[:, None]).astype(np.float32)
    c, s_ = np.cos(ang).astype(np.float32), np.sin(ang).astype(np.float32)
    cos32 = np.concatenate([c, c], axis=0)
    sin32 = np.concatenate([-s_, s_], axis=0)
    zz = np.zeros((32, SEQC), np.float32)
    cos2 = np.concatenate([cos32, zz, cos32, zz], axis=0)
    sin2s = np.concatenate([sin32, zz, sin32, zz], axis=0)
    return {
        "meta": f(inp["meta_tokens"]),
        "g1": _pc(inp["norm1_g"][0], 8),
        "w_in": np.ascontiguousarray(w_in_r),
        "convw": cw,
        "convb": _pc(inp["conv_b"][0], 8),
        "rg_wa": np.ascontiguousarray(f(inp["rg_wa"])[0].transpose(1, 0, 2)),
        "rg_wx": np.ascontiguousarray(f(inp["rg_wx"])[0].transpose(1, 0, 2)),
        "rg_ba": _pc(inp["rg_ba"][0], 8),
        "rg_bx": _pc(inp["rg_bx"][0], 8),
        "rg_lam": _pc(inp["rg_lambda"][0], 8),
        "w_rnn_out": f(inp["w_rnn_out"])[0],
        "qg": _pc(inp["q_norm_g"][0], 3),
        "kvg": _pc(inp["kv_norm_g"][0], 2),
        "w_uq": np.ascontiguousarray(w_uq_r),
        "w_ukv": np.ascontiguousarray(w_ukv_r),
        "w_attn_out": f(inp["w_attn_out"])[0],
        "w_out": f(inp["w_out"])[0],
        "g2": f(inp["norm2_g"]).reshape(1, 1024),
        "peer_wq": f(inp["peer_wq"])[0],
        "keys1T": np.ascontiguousarray(f(inp["peer_keys1"])[0].T),
        "keys2T": np.ascontiguousarray(f(inp["peer_keys2"])[0].T),
        "peer_u": f(inp["peer_u"])[0],
        "peer_v": f(inp["peer_v"])[0],
        "gf": f(inp["final_g"]).reshape(1, 1024),
        "cos2": cos2,
        "sin2s": sin2s,
    }


def kernel(**inputs):
    NC = 8
    NSEQ = 4
    common = prep_common(inputs)
    x = np.asarray(inputs["x"], np.float32)
    nc = build(NSEQ)
    in_maps = []
    for c in range(NC):
        m = dict(common)
        m["x"] = np.ascontiguousarray(x[c * NSEQ:(c + 1) * NSEQ])
        in_maps.append(m)
    res = run_bass_kernel_spmd(nc, in_maps, core_ids=list(range(NC)))
    return np.concatenate([r["out"] for r in res.results], axis=0)
```

```python
from contextlib import ExitStack
import numpy as np
import concourse.bass as bass
import concourse.mybir as mybir
from concourse.bass_utils import run_bass_kernel_spmd

F32 = mybir.dt.float32
BF16 = mybir.dt.bfloat16
I32 = mybir.dt.int32
U32 = mybir.dt.uint32
AF = mybir.ActivationFunctionType
ALU = mybir.AluOpType
AX = mybir.AxisListType

SEQC = 2176
NREAL = 2048
EPS = 1e-6
GROUPS = [(0, 128)] + [(128 + 512 * g, 512) for g in range(4)]


class T:
    def __init__(self, h, name, accum=False):
        self.h = h
        self.name = name
        self.w = {}
        self.r = {}
        self.accum = accum

    def __getitem__(self, k):
        return self.h[k]


class B:
    def __init__(self, nc):
        self.nc = nc
        self.E = {"pe": nc.tensor, "act": nc.scalar, "dve": nc.vector, "pool": nc.gpsimd, "sp": nc.sync}
        self.sems = {}
        self.cnt = {}
        self.seen = {k: {} for k in self.E}
        for k in self.E:
            self.sems[k] = nc.alloc_semaphore("s_" + k)
            self.cnt[k] = 0
        self.nins = 0

    def newsem(self, name):
        self.sems[name] = self.nc.alloc_semaphore("s_" + name)
        self.cnt[name] = 0
        return name

    def _wait(self, eng, evs):
        for key, val in evs.items():
            if val <= 0 or (eng == "pe" and key == "pe"):
                continue
            if self.seen[eng].get(key, 0) >= val:
                continue
            self.E[eng].wait_ge(self.sems[key], val)
            self.seen[eng][key] = val
            self.nins += 1

    @staticmethod
    def _merge(d, e):
        for k, v in e.items():
            if d.get(k, 0) < v:
                d[k] = v

    def _deps(self, reads, writes):
        evs = {}
        for t in reads:
            self._merge(evs, t.w)
        for t in writes:
            if not t.accum:
                self._merge(evs, t.w)
                self._merge(evs, t.r)
        return evs

    def _commit(self, ev, reads, writes):
        for t in reads:
            if not t.accum:
                self._merge(t.r, ev)
        for t in writes:
            if t.accum:
                self._merge(t.w, ev)
            else:
                t.w = dict(ev)
                t.r = {}

    def op(self, eng, fn, reads=(), writes=()):
        self._wait(eng, self._deps(reads, writes))
        ins = fn(self.E[eng])
        self.cnt[eng] += 1
        ins.then_inc(self.sems[eng], 1)
        self.nins += 1
        self._commit({eng: self.cnt[eng]}, reads, writes)

    def dma(self, q, sem, fn, reads=(), writes=()):
        own = [t for t in writes if not t.accum] or [t for t in reads if not t.accum]
        t0 = own[0]
        if getattr(t0, "sem", None) is None:
            t0.sem = {}
        if q not in t0.sem:
            t0.sem[q] = self.newsem("d%d" % len(self.sems))
        sem = t0.sem[q]
        deps = self._deps(reads, writes)
        if writes and not writes[0].accum:
            wr = {}
            for t in writes:
                self._merge(wr, t.r)
            if deps.get(sem, 0) > wr.get(sem, 0):
                v = wr.get(sem, 0)
                if v > 0:
                    deps[sem] = v
                else:
                    deps.pop(sem)
        self._wait(q, deps)
        ins = fn(self.E[q])
        self.cnt[sem] += 16
        ins.then_inc(self.sems[sem], 16)
        self.nins += 1
        self._commit({sem: self.cnt[sem]}, reads, writes)

    def barrier(self):
        allev = {k: v for k, v in self.cnt.items() if v > 0}
        for e in self.E:
            self._wait(e, allev)


def build(NSEQ, phases=(1, 2, 3, 4, 5), debug=False, p5tiles=None):
    nc = bass.Bass("TRN2", target_bir_lowering=False)
    b = B(nc)
    NT = NSEQ * SEQC
    NR = NSEQ * NREAL
    skind = "ExternalOutput" if debug else "Internal"

    def din(name, shape, dt=F32):
        return nc.dram_tensor(name, list(shape), dt, kind="ExternalInput")

    def dscr(name, shape, dt=F32):
        return T(nc.dram_tensor(name, list(shape), dt, kind=skind), name, accum=True)

    x_d = din("x", [NSEQ, NREAL, 1024])
    meta_d = din("meta", [16, 1024])
    g1_d = din("g1", [128, 8])
    win_d = din("w_in", [1024, 4992])
    cw_d = din("convw", [128, 8, 4])
    cb_d = din("convb", [128, 8])
    rgwa_d = din("rg_wa", [128, 8, 128])
    rgwx_d = din("rg_wx", [128, 8, 128])
    rgba_d = din("rg_ba", [128, 8])
    rgbx_d = din("rg_bx", [128, 8])
    lam_d = din("rg_lam", [128, 8])
    wro_d = din("w_rnn_out", [1024, 1024])
    qg_d = din("qg", [128, 3])
    kvg_d = din("kvg", [128, 2])
    wuq_d = din("w_uq", [384, 4096])
    wukv_d = din("w_ukv", [256, 2048])
    wao_d = din("w_attn_out", [1024, 1024])
    wo_d = din("w_out", [1024, 1024])
    g2_d = din("g2", [1, 1024])
    wq_d = din("peer_wq", [1024, 2048])
    k1T_d = din("keys1T", [128, 128])
    k2T_d = din("keys2T", [128, 128])
    pu_d = din("peer_u", [16384, 1024])
    pv_d = din("peer_v", [16384, 1024])
    gf_d = din("gf", [1, 1024])
    cos_d = din("cos2", [128, SEQC])
    sin_d = din("sin2s", [128, SEQC])
    out_d = nc.dram_tensor("out", [NSEQ, NREAL, 1024], F32, kind="ExternalOutput")
    OUT = T(out_d, "out", accum=True)

    S_xr = dscr("S_xr", [1024, NT])
    S_gr = dscr("S_gr", [1024, NT])
    S_cq = dscr("S_cq", [384, NT])
    S_ckv = dscr("S_ckv", [256, NT])
    S_kr = dscr("S_kr", [128, NT])
    S_krs = dscr("S_krs", [128, NT])
    S_grnn = dscr("S_grnn", [1024, NT])
    S_gatt = dscr("S_gatt", [1024, NT])
    S_mrnn = dscr("S_mrnn", [1024, NT])
    S_oT = dscr("S_oT", [1024, NT], BF16)
    S_h2 = dscr("S_h2", [NR, 1024])
    UV16 = T(nc.dram_tensor("UV16", [16384, 2048], BF16, kind="Internal"), "UV16", accum=True)

    ld = b.newsem("ld")
    st = b.newsem("st")
    wl = b.newsem("wl")

    def mk(es, pre):
        def sb(name, shape, dt):
            return T(es.enter_context(nc.sbuf_tensor(pre + name, list(shape), dt)), name)

        def ps(name, shape, dt):
            return T(es.enter_context(nc.psum_tensor(pre + name, list(shape), dt)), name)
        return sb, ps

    def make_ident(sb):
        identf = sb("identf", [128, 128], F32)
        ident = sb("ident", [128, 128], BF16)
        b.op("pool", lambda e: e.memset(identf[:], 1.0), writes=[identf])
        b.op("pool", lambda e: e.affine_select(out=identf[:], in_=identf[:], pattern=[[-1, 128]],
                                               compare_op=ALU.is_equal, fill=0.0, base=0, channel_multiplier=1),
             reads=[identf], writes=[identf])
        b.op("dve", lambda e: e.tensor_copy(out=ident[:], in_=identf[:]), reads=[identf], writes=[ident])
        return ident, identf

    def load_small(sb, name, d, shape):
        t = sb(name, shape, F32)
        b.dma("sp", wl, lambda e: e.dma_start(out=t[:], in_=d.ap()), writes=[t])
        return t

    def load_w_cast(dst, dst_ap, src_ap):
        b.dma("pool", wl, lambda e: e.dma_start(out=dst_ap, in_=src_ap), writes=[dst])

    def phase1():
        with ExitStack() as es:
            sb, ps = mk(es, "p1_")
            W = sb("W1", [128, 8, 4992], BF16)
            g1 = load_small(sb, "g1s", g1_d, [128, 8])
            stg = [sb("wstg%d" % i, [128, 4992], F32) for i in range(2)]
            for kc in range(8):
                s_ = stg[kc % 2]
                b.dma("sp", wl, lambda e: e.dma_start(out=s_[:], in_=win_d.ap()[kc * 128:(kc + 1) * 128, :]), writes=[s_])
                b.op("dve", lambda e: e.tensor_scalar(out=W[:, kc, :], in0=s_[:], scalar1=g1[:, kc:kc + 1], scalar2=None,
                                                      op0=ALU.mult), reads=[s_, g1], writes=[W])
            ident, _ = make_ident(sb)
            xt = [sb("xt%d" % i, [128, 1024], F32) for i in range(2)]
            junk = sb("junk", [128, 1024], BF16)
            ss = [sb("ss%d" % i, [128, 1], F32) for i in range(2)]
            xb = [sb("xb%d" % i, [128, 1024], BF16) for i in range(2)]
            n1T = [sb("n1T%d" % i, [128, 8, 512], BF16) for i in range(2)]
            pT = [ps("pT%d" % i, [128, 1024], BF16) for i in range(2)]
            pa = [ps("pa%d" % i, [128, 512], F32) for i in range(4)]
            stage = [sb("stage%d" % i, [128, 512], F32) for i in range(4)]
            chunks = []
            for i in range(8):
                chunks.append((S_xr, i * 128, None, 128))
            for i in range(8):
                chunks.append((S_gr, i * 128, AF.Gelu_apprx_tanh, 128))
            for i in range(3):
                chunks.append((S_cq, i * 128, None, 128))
            for i in range(2):
                chunks.append((S_ckv, i * 128, None, 128))
            chunks.append((S_kr, 0, None, 128))
            chunks.append((S_krs, 0, None, 128))
            for i in range(8):
                chunks.append((S_grnn, i * 128, AF.Sigmoid, 128))
            for i in range(8):
                chunks.append((S_gatt, i * 128, AF.Sigmoid, 128))
            ti = 0
            gi = 0
            ci = 0
            cv = [sb("cv%d" % i, [128, 8, 1024], BF16) for i in range(2)]
            conv = [(src, off, c) for (src, off) in ((pu_d, 0), (pv_d, 1024)) for c in range(16)]
            cvi = [0]

            def convert_some(k):
                for _ in range(k):
                    if cvi[0] >= len(conv):
                        return
                    src, off, c = conv[cvi[0]]
                    dst = UV16
                    CV = cv[cvi[0] % 2]
                    cvi[0] += 1
                    b.dma("pool", None, lambda e: e.dma_start(out=CV[:], in_=src.ap()[c * 1024:(c + 1) * 1024, :].rearrange("(p r) d -> p r d", p=128)),
                          writes=[CV])
                    b.dma("sp", None, lambda e: e.dma_start(out=dst.h.ap()[c * 1024:(c + 1) * 1024, off:off + 1024].rearrange("(p r) d -> p r d", p=128), in_=CV[:]),
                          reads=[CV], writes=[dst])

            for s in range(NSEQ):
                for (c0, n) in GROUPS:
                    nT = n1T[gi % 2]
                    gi += 1
                    convert_some(8 if NSEQ == 1 else 2)
                    for j in range(n // 128):
                        t = c0 // 128 + j
                        X = xt[ti % 2]
                        SS = ss[ti % 2]
                        XB = xb[ti % 2]
                        PT = pT[ti % 2]
                        ti += 1
                        if t == 0:
                            b.op("pool", lambda e: e.memset(X[:], 0.0), writes=[X])
                            b.dma("sp", ld, lambda e: e.dma_start(out=X[112:128, :], in_=meta_d.ap()), writes=[X])
                        else:
                            b.dma("sp", ld, lambda e: e.dma_start(out=X[:], in_=x_d.ap()[s, (t - 1) * 128:t * 128, :]), writes=[X])
                        b.op("act", lambda e: e.activation(out=junk[:], in_=X[:], func=AF.Square, accum_out=SS[:]),
                             reads=[X], writes=[junk, SS])
                        b.op("dve", lambda e: e.tensor_scalar(out=SS[:], in0=SS[:], scalar1=1.0 / 1024, scalar2=EPS,
                                                              op0=ALU.mult, op1=ALU.add), reads=[SS], writes=[SS])
                        b.op("act", lambda e: e.activation(out=SS[:], in_=SS[:], func=AF.Sqrt), reads=[SS], writes=[SS])
                        b.op("dve", lambda e: e.reciprocal(out=SS[:], in_=SS[:]), reads=[SS], writes=[SS])
                        b.op("act", lambda e: e.activation(out=XB[:], in_=X[:], func=AF.Copy, scale=SS[:]),
                             reads=[X, SS], writes=[XB])
                        for c in range(8):
                            b.op("pe", lambda e: e.transpose(out=PT[:, c * 128:(c + 1) * 128], in_=XB[:, c * 128:(c + 1) * 128],
                                                             identity=ident[:]), reads=[XB, ident], writes=[PT])
                        b.op("dve", lambda e: e.tensor_copy(out=nT[:, :, j * 128:(j + 1) * 128],
                                                            in_=PT[:].rearrange("p (c t) -> p c t", c=8)),
                             reads=[PT], writes=[nT])
                    for oc, (S_, r0, fn, m) in enumerate(chunks):
                        PA = pa[ci % 4]
                        SG = stage[ci % 4]
                        ci += 1
                        for kc in range(8):
                            b.op("pe", lambda e: e.matmul(PA[:, 0:n], lhsT=W[:, kc, oc * 128:(oc + 1) * 128], rhs=nT[:, kc, 0:n],
                                                          start=(kc == 0), stop=(kc == 7)), reads=[W, nT], writes=[PA])
                        if fn is None:
                            b.op("dve", lambda e: e.tensor_copy(out=SG[:, 0:n], in_=PA[:, 0:n]), reads=[PA], writes=[SG])
                        else:
                            b.op("act", lambda e: e.activation(out=SG[:, 0:n], in_=PA[:, 0:n], func=fn), reads=[PA], writes=[SG])
                        col = s * SEQC + c0
                        b.dma("pool", st, lambda e: e.dma_start(out=S_.h.ap()[r0:r0 + 128, col:col + n], in_=SG[:, 0:n]),
                              reads=[SG], writes=[S_])
            convert_some(len(conv))
        b.barrier()

    def phase2():
        with ExitStack() as es:
            sb, ps = mk(es, "p2_")
            WA = sb("WA", [128, 8, 128], BF16)
            WX = sb("WX", [128, 8, 128], BF16)
            WRO = sb("WRO", [128, 8, 1024], BF16)
            load_w_cast(WA, WA[:], rgwa_d.ap())
            load_w_cast(WX, WX[:], rgwx_d.ap())
            load_w_cast(WRO, WRO[:], wro_d.ap().rearrange("(k p) f -> p k f", p=128))
            cw = load_small(sb, "cw", cw_d, [128, 8, 4])
            cb = load_small(sb, "cb", cb_d, [128, 8])
            ba = load_small(sb, "ba", rgba_d, [128, 8])
            bx = load_small(sb, "bx", rgbx_d, [128, 8])
            lam = load_small(sb, "lam", lam_d, [128, 8])
            c8 = sb("c8", [128, 8], F32)
            c16 = sb("c16", [128, 8], F32)
            b.op("act", lambda e: e.activation(out=c8[:], in_=lam[:], func=AF.Exp, scale=-1.0), reads=[lam], writes=[c8])
            b.op("act", lambda e: e.activation(out=c8[:], in_=c8[:], func=AF.Ln, bias=1.0), reads=[c8], writes=[c8])
            b.op("dve", lambda e: e.tensor_scalar(out=c16[:], in0=c8[:], scalar1=-16.0, scalar2=None, op0=ALU.mult),
                 reads=[c8], writes=[c16])
            b.op("dve", lambda e: e.tensor_scalar(out=c8[:], in0=c8[:], scalar1=-8.0, scalar2=None, op0=ALU.mult),
                 reads=[c8, c16], writes=[c8])
            XR = sb("XR", [128, SEQC], F32)
            Y = sb("Y", [128, SEQC], F32)
            YB = sb("YB", [128, SEQC], BF16)
            A = sb("A", [128, SEQC], F32)
            U = sb("U", [128, SEQC], F32)
            H = sb("H", [128, SEQC], F32)
            GR = sb("GR", [128, SEQC], F32)
            ZT = sb("ZT", [128, 8, SEQC], BF16)
            tr = sb("tr", [128, 512], F32)
            ta2 = sb("ta2", [128, 512], F32)
            ti_ = sb("ti", [128, 512], F32)
            gs = [sb("gs%d" % i, [128, 512], F32) for i in range(2)]
            stage = [sb("stage%d" % i, [128, 512], F32) for i in range(2)]
            pA = ps("pA", [128, 512], F32)
            pX = ps("pX", [128, 512], F32)
            pO = [ps("pO%d" % i, [128, 512], F32) for i in range(2)]
            V0 = 112
            NV = SEQC - V0
            k = 0
            for s in range(NSEQ):
                sc = s * SEQC
                for n_ in range(8):
                    r0 = n_ * 128
                    b.dma("sp", ld, lambda e: e.dma_start(out=XR[:], in_=S_xr.h.ap()[r0:r0 + 128, sc:sc + SEQC]),
                          reads=[S_xr], writes=[XR])
                    b.dma("sp", ld, lambda e: e.dma_start(out=GR[:], in_=S_gr.h.ap()[r0:r0 + 128, sc:sc + SEQC]),
                          reads=[S_gr], writes=[GR])
                    b.op("dve", lambda e: e.tensor_scalar(out=Y[:, V0:SEQC], in0=XR[:, V0 - 3:SEQC - 3], scalar1=cw[:, n_, 0:1],
                                                          scalar2=cb[:, n_:n_ + 1], op0=ALU.mult, op1=ALU.add),
                         reads=[XR, cw, cb], writes=[Y])
                    for kk in range(1, 4):
                        b.op("dve", lambda e: e.scalar_tensor_tensor(out=Y[:, V0:SEQC], in0=XR[:, V0 - 3 + kk:SEQC - 3 + kk],
                                                                     scalar=cw[:, n_, kk:kk + 1], in1=Y[:, V0:SEQC],
                                                                     op0=ALU.mult, op1=ALU.add), reads=[XR, cw, Y], writes=[Y])
                    b.op("act", lambda e: e.activation(out=YB[:, V0:SEQC], in_=Y[:, V0:SEQC], func=AF.Copy), reads=[Y], writes=[YB])
                    for (c0, n) in GROUPS:
                        if c0 == 0:
                            c0, n = V0, 16
                        cs = slice(c0, c0 + n)
                        b.op("pe", lambda e: e.matmul(pA[:, 0:n], lhsT=WA[:, n_, :], rhs=YB[:, cs], start=True, stop=True),
                             reads=[WA, YB], writes=[pA])
                        b.op("pe", lambda e: e.matmul(pX[:, 0:n], lhsT=WX[:, n_, :], rhs=YB[:, cs], start=True, stop=True),
                             reads=[WX, YB], writes=[pX])
                        b.op("act", lambda e: e.activation(out=tr[:, 0:n], in_=pA[:, 0:n], func=AF.Sigmoid, bias=ba[:, n_:n_ + 1]),
                             reads=[pA, ba], writes=[tr])
                        b.op("act", lambda e: e.activation(out=ti_[:, 0:n], in_=pX[:, 0:n], func=AF.Sigmoid, bias=bx[:, n_:n_ + 1]),
                             reads=[pX, bx], writes=[ti_])
                        b.op("act", lambda e: e.activation(out=A[:, cs], in_=tr[:, 0:n], func=AF.Exp, scale=c8[:, n_:n_ + 1]),
                             reads=[tr, c8], writes=[A])
                        b.op("act", lambda e: e.activation(out=ta2[:, 0:n], in_=tr[:, 0:n], func=AF.Exp, scale=c16[:, n_:n_ + 1]),
                             reads=[tr, c16], writes=[ta2])
                        b.op("dve", lambda e: e.tensor_scalar(out=ta2[:, 0:n], in0=ta2[:, 0:n], scalar1=-1.0, scalar2=1.0,
                                                              op0=ALU.mult, op1=ALU.add), reads=[ta2], writes=[ta2])
                        b.op("act", lambda e: e.activation(out=ta2[:, 0:n], in_=ta2[:, 0:n], func=AF.Sqrt), reads=[ta2], writes=[ta2])
                        b.op("dve", lambda e: e.tensor_tensor(out=ti_[:, 0:n], in0=ti_[:, 0:n], in1=Y[:, cs], op=ALU.mult),
                             reads=[ti_, Y], writes=[ti_])
                        b.op("dve", lambda e: e.tensor_tensor(out=U[:, cs], in0=ti_[:, 0:n], in1=ta2[:, 0:n], op=ALU.mult),
                             reads=[ti_, ta2], writes=[U])
                    b.op("dve", lambda e: e.tensor_tensor_scan(out=H[:, V0:SEQC], data0=A[:, V0:SEQC], data1=U[:, V0:SEQC],
                                                               initial=0.0, op0=ALU.mult, op1=ALU.add), reads=[A, U], writes=[H])
                    b.op("dve", lambda e: e.tensor_tensor(out=ZT[:, n_, V0:SEQC], in0=H[:, V0:SEQC], in1=GR[:, V0:SEQC], op=ALU.mult),
                         reads=[H, GR], writes=[ZT])
                for (c0, n) in GROUPS[1:]:
                    cs = slice(c0, c0 + n)
                    for dc in range(8):
                        PO = pO[k % 2]
                        G = gs[k % 2]
                        SG = stage[k % 2]
                        k += 1
                        b.dma("sp", ld, lambda e: e.dma_start(out=G[:, 0:n], in_=S_grnn.h.ap()[dc * 128:(dc + 1) * 128, sc + c0:sc + c0 + n]),
                              reads=[S_grnn], writes=[G])
                        for kc in range(8):
                            b.op("pe", lambda e: e.matmul(PO[:, 0:n], lhsT=WRO[:, kc, dc * 128:(dc + 1) * 128], rhs=ZT[:, kc, cs],
                                                          start=(kc == 0), stop=(kc == 7)), reads=[WRO, ZT], writes=[PO])
                        b.op("dve", lambda e: e.tensor_tensor(out=SG[:, 0:n], in0=PO[:, 0:n], in1=G[:, 0:n], op=ALU.mult),
                             reads=[PO, G], writes=[SG])
                        b.dma("pool", st, lambda e: e.dma_start(out=S_mrnn.h.ap()[dc * 128:(dc + 1) * 128, sc + c0:sc + c0 + n],
                                                                in_=SG[:, 0:n]), reads=[SG], writes=[S_mrnn])
        b.barrier()


    def phase3a():
        with ExitStack() as es:
            sb, ps = mk(es, "p3_")
            WUQ = sb("WUQ", [128, 3, 3072], BF16)
            WUKV = sb("WUKV", [128, 2, 2048], BF16)
            qg = load_small(sb, "qg", qg_d, [128, 3])
            kvg = load_small(sb, "kvg", kvg_d, [128, 2])
            wst = sb("wst", [128, 3072], F32)
            for kc in range(3):
                b.dma("sp", wl, lambda e: e.dma_start(out=wst[:], in_=wuq_d.ap()[kc * 128:(kc + 1) * 128, 0:3072]), writes=[wst])
                b.op("dve", lambda e: e.tensor_scalar(out=WUQ[:, kc, :], in0=wst[:], scalar1=qg[:, kc:kc + 1], scalar2=None,
                                                      op0=ALU.mult), reads=[wst, qg], writes=[WUQ])
            for kc in range(2):
                b.dma("sp", wl, lambda e: e.dma_start(out=wst[:, 0:2048], in_=wukv_d.ap()[kc * 128:(kc + 1) * 128, :]), writes=[wst])
                b.op("dve", lambda e: e.tensor_scalar(out=WUKV[:, kc, :], in0=wst[:, 0:2048], scalar1=kvg[:, kc:kc + 1], scalar2=None,
                                                      op0=ALU.mult), reads=[wst, kvg], writes=[WUKV])
            ident, identf = make_ident(sb)
            ones = sb("ones", [128, 128], F32)
            b.op("pool", lambda e: e.memset(ones[:], 1.0), writes=[ones])
            tri = sb("tri", [128, 128], BF16)
            b.op("pool", lambda e: e.memset(identf[:], 1.0), reads=[identf], writes=[identf])
            b.op("pool", lambda e: e.affine_select(out=identf[:], in_=identf[:], pattern=[[1, 128]], compare_op=ALU.is_ge,
                                                   fill=0.0, base=0, channel_multiplier=-1), reads=[identf], writes=[identf])
            b.op("dve", lambda e: e.tensor_copy(out=tri[:], in_=identf[:]), reads=[identf], writes=[tri])
            Kn = sb("Kn", [128, 8, SEQC], BF16)
            Kr2 = sb("Kr2", [128, SEQC], BF16)
            V = sb("V", [128, 17, 16, 65], BF16)
            b.op("pool", lambda e: e.memset(V[:], 1.0), writes=[V])
            cq_t = sb("cq_t", [128, 3, 512], F32)
            ckv_t = sb("ckv_t", [128, 2, 512], F32)
            sq = sb("sq", [128, 3, 512], F32)
            rstd = sb("rstd", [128, 512], F32)
            cqn = sb("cqn", [128, 3, 512], BF16)
            ckvn = sb("ckvn", [128, 2, 512], BF16)
            kr_t = sb("kr_t", [128, 512], F32)
            krs_t = sb("krs_t", [128, 512], F32)
            cos_t = sb("cos_t", [128, 512], F32)
            sin_t = sb("sin_t", [128, 512], F32)
            tmp1 = sb("tmp1", [128, 512], F32)
            tmp2 = sb("tmp2", [128, 512], F32)
            Qn = sb("Qn", [128, 8, 512], BF16)
            Qr = sb("Qr", [128, 8, 512], BF16)
            PTs = [sb("PTs%d" % i, [128, 512], BF16) for i in range(3)]
            o_tm = sb("o_tm", [128, 4, 1024], BF16)
            oT = sb("oT", [128, 8, 512], BF16)
            rec = sb("rec", [128, 4], F32)
            pn = ps("pn", [128, 512], F32)
            pk = [ps("pk%d" % i, [128, 512], F32) for i in range(2)]
            pS = [ps("pS%d" % i, [128, 512], F32) for i in range(2)]
            pO = [ps("pO%d" % i, [128, 512], F32) for i in range(2)]
            pT = ps("pT", [128, 1024], BF16)
            scale = float(96 ** -0.5)
            ki = [0]

            def nextpk():
                ki[0] += 1
                return pk[ki[0] % 2]

            def rmsn(src, nch, dst, nfeat, n):
                b.op("act", lambda e: e.activation(out=sq[:, 0:nch, 0:n], in_=src[:, :, 0:n], func=AF.Square), reads=[src], writes=[sq])
                for c in range(nch):
                    b.op("pe", lambda e: e.matmul(pn[:, 0:n], lhsT=ones[:], rhs=sq[:, c, 0:n], start=(c == 0), stop=(c == nch - 1)),
                         reads=[ones, sq], writes=[pn])
                b.op("dve", lambda e: e.tensor_scalar(out=rstd[:, 0:n], in0=pn[:, 0:n], scalar1=1.0 / nfeat, scalar2=EPS,
                                                      op0=ALU.mult, op1=ALU.add), reads=[pn], writes=[rstd])
                b.op("act", lambda e: e.activation(out=rstd[:, 0:n], in_=rstd[:, 0:n], func=AF.Sqrt), reads=[rstd], writes=[rstd])
                b.op("dve", lambda e: e.reciprocal(out=rstd[:, 0:n], in_=rstd[:, 0:n]), reads=[rstd], writes=[rstd])
                for c in range(nch):
                    b.op("dve", lambda e: e.tensor_tensor(out=dst[:, c, 0:n], in0=src[:, c, 0:n], in1=rstd[:, 0:n], op=ALU.mult),
                         reads=[src, rstd], writes=[dst])

            si = 0
            for s in range(NSEQ):
                sc = s * SEQC
                for (c0, n) in GROUPS:
                    col = sc + c0
                    b.dma("sp", ld, lambda e: e.dma_start(out=cq_t[:, :, 0:n],
                                                          in_=S_cq.h.ap()[:, col:col + n].rearrange("(c p) n -> p c n", p=128)),
                          reads=[S_cq], writes=[cq_t])
                    b.dma("sp", ld, lambda e: e.dma_start(out=ckv_t[:, :, 0:n],
                                                          in_=S_ckv.h.ap()[:, col:col + n].rearrange("(c p) n -> p c n", p=128)),
                          reads=[S_ckv], writes=[ckv_t])
                    b.dma("sp", ld, lambda e: e.dma_start(out=kr_t[:, 0:n], in_=S_kr.h.ap()[:, col:col + n]), reads=[S_kr], writes=[kr_t])
                    b.dma("sp", ld, lambda e: e.dma_start(out=krs_t[:, 0:n], in_=S_krs.h.ap()[:, col:col + n]), reads=[S_krs], writes=[krs_t])
                    b.dma("sp", ld, lambda e: e.dma_start(out=cos_t[:, 0:n], in_=cos_d.ap()[:, c0:c0 + n]), writes=[cos_t])
                    b.dma("sp", ld, lambda e: e.dma_start(out=sin_t[:, 0:n], in_=sin_d.ap()[:, c0:c0 + n]), writes=[sin_t])
                    rmsn(cq_t, 3, cqn, 384.0, n)
                    rmsn(ckv_t, 2, ckvn, 256.0, n)
                    for j in range(8):
                        P_ = nextpk()
                        for kc in range(2):
                            b.op("pe", lambda e: e.matmul(P_[:, 0:n], lhsT=WUKV[:, kc, j * 128:(j + 1) * 128], rhs=ckvn[:, kc, 0:n],
                                                          start=(kc == 0), stop=(kc == 1)), reads=[WUKV, ckvn], writes=[P_])
                        b.op("act", lambda e: e.activation(out=Kn[:, j, c0:c0 + n], in_=P_[:, 0:n], func=AF.Copy), reads=[P_], writes=[Kn])
                    b.op("dve", lambda e: e.tensor_tensor(out=tmp1[:, 0:n], in0=kr_t[:, 0:n], in1=cos_t[:, 0:n], op=ALU.mult),
                         reads=[kr_t, cos_t], writes=[tmp1])
                    b.op("dve", lambda e: e.tensor_tensor(out=tmp2[:, 0:n], in0=krs_t[:, 0:n], in1=sin_t[:, 0:n], op=ALU.mult),
                         reads=[krs_t, sin_t], writes=[tmp2])
                    b.op("dve", lambda e: e.tensor_tensor(out=Kr2[:, c0:c0 + n], in0=tmp1[:, 0:n], in1=tmp2[:, 0:n], op=ALU.add),
                         reads=[tmp1, tmp2], writes=[Kr2])
                    for jt in range(n // 128):
                        t = c0 // 128 + jt
                        if t == 0:
                            lo, M = 112, 16
                        else:
                            lo, M = jt * 128, 128
                        for half in range(2):
                            P_ = nextpk()
                            for kc in range(2):
                                b.op("pe", lambda e: e.matmul(P_[0:M, :], lhsT=ckvn[:, kc, lo:lo + M],
                                                              rhs=WUKV[:, kc, 1024 + half * 512:1024 + (half + 1) * 512],
                                                              start=(kc == 0), stop=(kc == 1)), reads=[WUKV, ckvn], writes=[P_])
                            b.op("act", lambda e: e.activation(out=V[0:M, t, half * 8:(half + 1) * 8, 0:64],
                                                               in_=P_[0:M, :].rearrange("p (h d) -> p h d", h=8), func=AF.Copy),
                                 reads=[P_], writes=[V])
                    if c0 == 0:
                        continue
                    for j in range(8):
                        P_ = nextpk()
                        for kc in range(3):
                            b.op("pe", lambda e: e.matmul(P_[:, 0:n], lhsT=WUQ[:, kc, j * 128:(j + 1) * 128], rhs=cqn[:, kc, 0:n],
                                                          start=(kc == 0), stop=(kc == 2)), reads=[WUQ, cqn], writes=[P_])
                        b.op("act", lambda e: e.activation(out=Qn[:, j, 0:n], in_=P_[:, 0:n], func=AF.Copy), reads=[P_], writes=[Qn])
                    for j in range(8):
                        P1 = nextpk()
                        for kc in range(3):
                            b.op("pe", lambda e: e.matmul(P1[:, 0:n], lhsT=WUQ[:, kc, 1024 + j * 128:1024 + (j + 1) * 128], rhs=cqn[:, kc, 0:n],
                                                          start=(kc == 0), stop=(kc == 2)), reads=[WUQ, cqn], writes=[P1])
                        b.op("dve", lambda e: e.tensor_tensor(out=tmp1[:, 0:n], in0=P1[:, 0:n], in1=cos_t[:, 0:n], op=ALU.mult),
                             reads=[P1, cos_t], writes=[tmp1])
                        P2 = nextpk()
                        for kc in range(3):
                            b.op("pe", lambda e: e.matmul(P2[:, 0:n], lhsT=WUQ[:, kc, 2048 + j * 128:2048 + (j + 1) * 128], rhs=cqn[:, kc, 0:n],
                                                          start=(kc == 0), stop=(kc == 2)), reads=[WUQ, cqn], writes=[P2])
                        b.op("dve", lambda e: e.tensor_tensor(out=tmp2[:, 0:n], in0=P2[:, 0:n], in1=sin_t[:, 0:n], op=ALU.mult),
                             reads=[P2, sin_t], writes=[tmp2])
                        b.op("dve", lambda e: e.tensor_tensor(out=Qr[:, j, 0:n], in0=tmp1[:, 0:n], in1=tmp2[:, 0:n], op=ALU.add),
                             reads=[tmp1, tmp2], writes=[Qr])
                    g0t = c0 // 128
                    for h in range(16):
                        j = h // 2
                        p0 = (h % 2) * 64
                        PO = pO[h % 2]
                        POv = PO[:, 0:260].rearrange("p (q d) -> p q d", q=4)
                        for kt in range(0, g0t + 4):
                            if kt == 0:
                                k0, M = 112, 16
                            else:
                                k0, M = kt * 128, 128
                            qlo = max(0, kt - g0t)
                            q0 = qlo * 128
                            PS_ = pS[si % 2]
                            PTb = PTs[si % 3]
                            si += 1
                            b.op("pe", lambda e: e.matmul(PS_[0:M, q0:512], lhsT=Kn[p0:p0 + 64, j, k0:k0 + M], rhs=Qn[p0:p0 + 64, j, q0:512],
                                                          start=True, stop=False), reads=[Kn, Qn], writes=[PS_])
                            b.op("pe", lambda e: e.matmul(PS_[0:M, q0:512], lhsT=Kr2[p0:p0 + 32, k0:k0 + M], rhs=Qr[p0:p0 + 32, j, q0:512],
                                                          start=False, stop=True), reads=[Kr2, Qr], writes=[PS_])
                            b.op("act", lambda e: e.activation(out=PTb[0:M, q0:512], in_=PS_[0:M, q0:512], func=AF.Exp, scale=scale),
                                 reads=[PS_], writes=[PTb])
                            if kt >= g0t:
                                b.op("dve", lambda e: e.tensor_tensor(out=PTb[:, q0:q0 + 128], in0=PTb[:, q0:q0 + 128], in1=tri[:], op=ALU.mult),
                                     reads=[PTb, tri], writes=[PTb])
                            for qt in range(qlo, 4):
                                b.op("pe", lambda e: e.matmul(POv[:, qt, :], lhsT=PTb[0:M, qt * 128:(qt + 1) * 128], rhs=V[0:M, kt, h, :],
                                                              start=(kt == 0 and qt == 0), stop=(kt == g0t + qt), skip_group_check=True),
                                     reads=[PTb, V], writes=[PO])
                        b.op("dve", lambda e: e.reciprocal(out=rec[:], in_=POv[:, :, 64]), reads=[PO], writes=[rec])
                        for qt in range(4):
                            b.op("dve", lambda e: e.tensor_scalar(out=o_tm[:, qt, h * 64:(h + 1) * 64], in0=POv[:, qt, 0:64],
                                                                  scalar1=rec[:, qt:qt + 1], scalar2=None, op0=ALU.mult),
                                 reads=[PO, rec], writes=[o_tm])
                    for qt in range(4):
                        for c in range(8):
                            b.op("pe", lambda e: e.transpose(out=pT[:, c * 128:(c + 1) * 128], in_=o_tm[:, qt, c * 128:(c + 1) * 128],
                                                             identity=ident[:]), reads=[o_tm, ident], writes=[pT])
                        b.op("dve", lambda e: e.tensor_copy(out=oT[:, :, qt * 128:(qt + 1) * 128],
                                                            in_=pT[:].rearrange("p (c t) -> p c t", c=8)), reads=[pT], writes=[oT])
                    b.dma("pool", st, lambda e: e.dma_start(out=S_oT.h.ap()[:, col:col + n].rearrange("(c p) n -> p c n", p=128), in_=oT[:]),
                          reads=[oT], writes=[S_oT])
        b.barrier()

    def phase3b():
        with ExitStack() as es:
            sb, ps = mk(es, "p3b_")
            WAO = sb("WAO", [128, 8, 1024], BF16)
            WO = sb("WO", [128, 8, 1024], BF16)
            load_w_cast(WAO, WAO[:], wao_d.ap().rearrange("(k p) f -> p k f", p=128))
            load_w_cast(WO, WO[:], wo_d.ap().rearrange("(k p) f -> p k f", p=128))
            oT = [sb("oT%d" % i, [128, 8, 512], BF16) for i in range(2)]
            ga = [sb("ga%d" % i, [128, 512], F32) for i in range(2)]
            mr = [sb("mr%d" % i, [128, 512], F32) for i in range(2)]
            tmp = sb("tmp", [128, 512], F32)
            mixT = sb("mixT", [128, 8, 512], BF16)
            xt = [sb("xt%d" % i, [128, 1024], F32) for i in range(2)]
            h2 = [sb("h2%d" % i, [128, 1024], F32) for i in range(2)]
            pk = [ps("pk%d" % i, [128, 512], F32) for i in range(4)]
            gi = 0
            k = 0
            ti = 0
            for s in range(NSEQ):
                sc = s * SEQC
                for g, (c0, n) in enumerate(GROUPS):
                    if g == 0:
                        continue
                    col = sc + c0
                    OT = oT[gi % 2]
                    gi += 1
                    b.dma("sp", ld, lambda e: e.dma_start(out=OT[:], in_=S_oT.h.ap()[:, col:col + n].rearrange("(c p) n -> p c n", p=128)),
                          reads=[S_oT], writes=[OT])
                    for dc in range(8):
                        GA = ga[k % 2]
                        MR = mr[k % 2]
                        PK = pk[k % 4]
                        k += 1
                        b.dma("sp", ld, lambda e: e.dma_start(out=GA[:], in_=S_gatt.h.ap()[dc * 128:(dc + 1) * 128, col:col + n]),
                              reads=[S_gatt], writes=[GA])
                        b.dma("sp", ld, lambda e: e.dma_start(out=MR[:], in_=S_mrnn.h.ap()[dc * 128:(dc + 1) * 128, col:col + n]),
                              reads=[S_mrnn], writes=[MR])
                        for kc in range(8):
                            b.op("pe", lambda e: e.matmul(PK[:], lhsT=WAO[:, kc, dc * 128:(dc + 1) * 128], rhs=OT[:, kc, :],
                                                          start=(kc == 0), stop=(kc == 7)), reads=[WAO, OT], writes=[PK])
                        b.op("dve", lambda e: e.tensor_tensor(out=tmp[:], in0=PK[:], in1=GA[:], op=ALU.mult), reads=[PK, GA], writes=[tmp])
                        b.op("dve", lambda e: e.tensor_tensor(out=mixT[:, dc, :], in0=tmp[:], in1=MR[:], op=ALU.add),
                             reads=[tmp, MR], writes=[mixT])
                    for qt in range(4):
                        X = xt[ti % 2]
                        H2 = h2[ti % 2]
                        ti += 1
                        r0 = (g - 1) * 512 + qt * 128
                        b.dma("sp", ld, lambda e: e.dma_start(out=X[:], in_=x_d.ap()[s, r0:r0 + 128, :]), writes=[X])
                        for half in range(2):
                            PK = pk[k % 4]
                            k += 1
                            for kc in range(8):
                                b.op("pe", lambda e: e.matmul(PK[:], lhsT=mixT[:, kc, qt * 128:(qt + 1) * 128],
                                                              rhs=WO[:, kc, half * 512:(half + 1) * 512],
                                                              start=(kc == 0), stop=(kc == 7)), reads=[WO, mixT], writes=[PK])
                            b.op("dve", lambda e: e.tensor_tensor(out=H2[:, half * 512:(half + 1) * 512], in0=PK[:],
                                                                  in1=X[:, half * 512:(half + 1) * 512], op=ALU.add),
                                 reads=[PK, X], writes=[H2])
                        b.dma("pool", st, lambda e: e.dma_start(out=S_h2.h.ap()[s * NREAL + r0:s * NREAL + r0 + 128, :], in_=H2[:]),
                              reads=[H2], writes=[S_h2])
        b.barrier()


    def phase5():
        with ExitStack() as es:
            sb, ps = mk(es, "p5_")
            WQ = sb("WQ", [128, 8, 2048], BF16)
            K1T = sb("K1T", [128, 128], BF16)
            K2T = sb("K2T", [128, 128], BF16)
            load_w_cast(WQ, WQ[:], wq_d.ap().rearrange("(k p) f -> p k f", p=128))
            load_w_cast(K1T, K1T[:], k1T_d.ap())
            load_w_cast(K2T, K2T[:], k2T_d.ap())
            g2b = sb("g2b", [128, 1024], F32)
            gfb = sb("gfb", [128, 1024], F32)
            b.dma("sp", None, lambda e: e.dma_start(out=g2b[:], in_=g2_d.ap().to_broadcast([128, 1024])), writes=[g2b])
            b.dma("sp", None, lambda e: e.dma_start(out=gfb[:], in_=gf_d.ap().to_broadcast([128, 1024])), writes=[gfb])
            ident, identf = make_ident(sb)
            iota_i = sb("iota_i", [128, 16], I32)
            iota16 = sb("iota16", [128, 16], F32)
            b.op("pool", lambda e: e.iota(iota_i[:], pattern=[[1, 16]], base=0, channel_multiplier=0), writes=[iota_i])
            b.op("dve", lambda e: e.tensor_copy(out=iota16[:], in_=iota_i[:]), reads=[iota_i], writes=[iota16])
            L = sb("L", [128, 128, 128], BF16)
            b.op("pool", lambda e: e.memset(L[:], 0.0), writes=[L])
            Lflat = L[:].rearrange("p a b -> p (a b)")
            Xs = [sb("X%d" % i, [128, 1024], F32) for i in range(2)]
            xnbs = [sb("xnb%d" % i, [128, 1024], BF16) for i in range(2)]
            GTs = [sb("GT%d" % i, [128, 128], F32) for i in range(2)]
            IDXTs = [sb("IDXT%d" % i, [128, 128], U32) for i in range(2)]
            Gs = [sb("G%d" % i, [128, 8, 16], F32) for i in range(2)]
            ssA = sb("ssA", [128, 1], F32)
            ssC = sb("ssC", [128, 1], F32)
            junkA = sb("junkA", [128, 1024], BF16)
            junkD = sb("junkD", [128, 1024], BF16)
            xT = sb("xT", [128, 8, 128], BF16)
            qT = sb("qT", [128, 16, 128], BF16)
            S = sb("S", [128, 16, 128], F32)
            eqv = S[:].rearrange("p (h x) (y a) -> p h (x y) a", x=2, a=16)
            T16 = sb("T16", [128, 16, 16], F32)
            I16 = sb("I16", [128, 16, 16], U32)
            I16f = sb("I16f", [128, 16, 16], F32)
            cand = sb("cand", [128, 8, 256], F32)
            TS = sb("TS", [128, 8, 16], F32)
            CI = sb("CI", [128, 8, 16], U32)
            CIa = sb("CIa", [128, 8, 16], U32)
            CIb = sb("CIb", [128, 8, 16], U32)
            Af = sb("Af", [128, 8, 16], F32)
            Bf = sb("Bf", [128, 8, 16], F32)
            i1s = sb("i1s", [128, 128], F32)
            i2s = sb("i2s", [128, 128], F32)
            i1b = sb("i1b", [128, 128], BF16)
            i2b = sb("i2b", [128, 128], BF16)
            idxf = sb("idxf", [128, 128], F32)
            idxTf = sb("idxTf", [128, 128], F32)
            iTf = sb("iTf", [128, 2, 128], F32)
            E = sb("E", [128, 8, 16], F32)
            Z = sb("Z", [128, 8], F32)
            ACTV = sb("ACTV", [128, 128], F32)
            coefb = sb("coefb", [128, 128], BF16)
            coefT = sb("coefT", [128, 128], BF16)
            UVG = [sb("UVG%d" % i, [128, 4, 2048], BF16) for i in range(4)]
            actT = sb("actT", [128, 128], F32)
            Ls = [T(L.h, "L%d" % i) for i in range(32)]
            for lt in Ls:
                lt.w = dict(L.w)
            h3 = sb("h3", [128, 1024], F32)
            pTa = ps("pTa", [128, 1024], BF16)
            pq = [ps("pq%d" % i, [128, 512], F32) for i in range(1)]
            pOut = [ps("pOut%d" % i, [128, 512], F32) for i in range(2)]
            pX = [ps("pX%d" % i, [128, 1024], F32) for i in range(2)]
            T16v = T16[:].rearrange("p (h two) a -> p h two a", two=2)
            I16fv = I16f[:].rearrange("p (h two) a -> p h two a", two=2)
            B4 = [128, 8, 16, 16]
            cnt = {"u": 0, "v": 0, "q": 0}
            ntiles = NR // 128 if p5tiles is None else p5tiles

            def sweep(eng, fns, reads, writes):
                for k_, f in enumerate(fns):
                    w = writes if (k_ == 0 or k_ == len(fns) - 1) else ()
                    b.op(eng, f, reads=reads, writes=w)

            def top16(vals, ng, tv, iv):
                sweep("dve", [(lambda e, g=g: e.max(out=tv[:, g, 0:8], in_=vals[:, g, :])) for g in range(ng)], [vals], [tv])
                sweep("dve", [(lambda e, g=g: e.max_index(out=iv[:, g, 0:8], in_max=tv[:, g, 0:8], in_values=vals[:, g, :])) for g in range(ng)],
                      [vals, tv], [iv])
                yield
                sweep("dve", [(lambda e, g=g: e.match_replace(out=vals[:, g, :], in_to_replace=tv[:, g, 0:8], in_values=vals[:, g, :],
                                                              imm_value=-1e30)) for g in range(ng)], [tv], [vals])
                yield
                sweep("dve", [(lambda e, g=g: e.max(out=tv[:, g, 8:16], in_=vals[:, g, :])) for g in range(ng)], [vals], [tv])
                sweep("dve", [(lambda e, g=g: e.max_index(out=iv[:, g, 8:16], in_max=tv[:, g, 8:16], in_values=vals[:, g, :])) for g in range(ng)],
                      [vals, tv], [iv])
                yield

            def stageA(i):
                par = i % 2
                X, xnb, GT, IDXT, G = Xs[par], xnbs[par], GTs[par], IDXTs[par], Gs[par]
                b.dma("sp", None, lambda e: e.dma_start(out=X[:], in_=S_h2.h.ap()[i * 128:(i + 1) * 128, :]), reads=[S_h2], writes=[X])
                b.op("act", lambda e: e.activation(out=junkA[:], in_=X[:], func=AF.Square, accum_out=ssA[:]), reads=[X], writes=[junkA, ssA])
                b.op("dve", lambda e: e.tensor_scalar(out=ssA[:], in0=ssA[:], scalar1=1.0 / 1024, scalar2=EPS, op0=ALU.mult, op1=ALU.add),
                     reads=[ssA], writes=[ssA])
                b.op("act", lambda e: e.activation(out=ssA[:], in_=ssA[:], func=AF.Sqrt), reads=[ssA], writes=[ssA])
                b.op("dve", lambda e: e.reciprocal(out=ssA[:], in_=ssA[:]), reads=[ssA], writes=[ssA])
                b.op("dve", lambda e: e.scalar_tensor_tensor(out=xnb[:], in0=X[:], scalar=ssA[:, 0:1], in1=g2b[:], op0=ALU.mult, op1=ALU.mult),
                     reads=[X, ssA, g2b], writes=[xnb])
                yield
                for c in range(8):
                    b.op("pe", lambda e: e.transpose(out=pTa[:, c * 128:(c + 1) * 128], in_=xnb[:, c * 128:(c + 1) * 128], identity=ident[:]),
                         reads=[xnb, ident], writes=[pTa])
                b.op("act", lambda e: e.activation(out=xT[:], in_=pTa[:].rearrange("p (c t) -> p c t", c=8), func=AF.Copy), reads=[pTa], writes=[xT])
                yield
                for bq in range(4):
                    PQ = pq[0]
                    for j in range(4):
                        hh = bq * 4 + j
                        for kc in range(8):
                            b.op("pe", lambda e: e.matmul(PQ[:, j * 128:(j + 1) * 128], lhsT=WQ[:, kc, hh * 128:(hh + 1) * 128], rhs=xT[:, kc, :],
                                                          start=(kc == 0), stop=(kc == 7), skip_group_check=True), reads=[WQ, xT], writes=[PQ])
                    b.op("act", lambda e: e.activation(out=qT[:, bq * 4:(bq + 1) * 4, :], in_=PQ[:].rearrange("p (j t) -> p j t", j=4), func=AF.Copy),
                         reads=[PQ], writes=[qT])
                    yield
                for bq in range(4):
                    PQ = pq[0]
                    for j in range(4):
                        hh = bq * 4 + j
                        KT = K1T if hh % 2 == 0 else K2T
                        b.op("pe", lambda e: e.matmul(PQ[:, j * 128:(j + 1) * 128], lhsT=qT[:, hh, :], rhs=KT[:], start=True, stop=True,
                                                      skip_group_check=True), reads=[qT, KT], writes=[PQ])
                    b.op("act", lambda e: e.activation(out=S[:, bq * 4:(bq + 1) * 4, :], in_=PQ[:].rearrange("p (j t) -> p j t", j=4), func=AF.Copy),
                         reads=[PQ], writes=[S])
                    yield
                yield from top16(S, 16, T16, I16)
                b.op("dve", lambda e: e.tensor_copy(out=I16f[:], in_=I16[:]), reads=[I16], writes=[I16f])
                b.op("dve", lambda e: e.tensor_tensor(out=cand[:].rearrange("p h (a c) -> p h a c", a=16),
                                                      in0=T16v[:, :, 0, :].unsqueeze(3).to_broadcast(B4),
                                                      in1=T16v[:, :, 1, :].unsqueeze(2).to_broadcast(B4), op=ALU.add),
                     reads=[T16], writes=[cand])
                yield
                yield from top16(cand, 8, TS, CI)
                b.op("dve", lambda e: e.tensor_single_scalar(out=CIa[:], in_=CI[:], scalar=4, op=ALU.logical_shift_right), reads=[CI], writes=[CIa])
                b.op("dve", lambda e: e.tensor_single_scalar(out=CIb[:], in_=CI[:], scalar=15, op=ALU.bitwise_and), reads=[CI], writes=[CIb])
                b.op("dve", lambda e: e.tensor_copy(out=Af[:], in_=CIa[:]), reads=[CIa], writes=[Af])
                b.op("dve", lambda e: e.tensor_copy(out=Bf[:], in_=CIb[:]), reads=[CIb], writes=[Bf])
                yield
                for (SEL, half, dst) in ((Af, 0, i1s), (Bf, 1, i2s)):
                    b.op("dve", lambda e: e.tensor_tensor(out=eqv, in0=SEL[:].unsqueeze(3).to_broadcast(B4),
                                                          in1=iota16[:].unsqueeze(1).unsqueeze(1).to_broadcast(B4), op=ALU.is_equal),
                         reads=[SEL, iota16], writes=[S])
                    b.op("dve", lambda e: e.tensor_tensor(out=eqv, in0=eqv, in1=I16fv[:, :, half, :].unsqueeze(2).to_broadcast(B4), op=ALU.mult),
                         reads=[S, I16f], writes=[S])
                    b.op("dve", lambda e: e.tensor_reduce(out=dst[:].rearrange("p (h k) -> p h k", h=8), in_=eqv, axis=AX.X, op=ALU.add),
                         reads=[S], writes=[dst])
                    yield
                b.op("act", lambda e: e.activation(out=i1b[:], in_=i1s[:], func=AF.Copy), reads=[i1s], writes=[i1b])
                b.op("act", lambda e: e.activation(out=i2b[:], in_=i2s[:], func=AF.Copy), reads=[i2s], writes=[i2b])
                b.op("pe", lambda e: e.transpose(out=pTa[:, 0:128], in_=i1b[:], identity=ident[:]), reads=[i1b, ident], writes=[pTa])
                b.op("pe", lambda e: e.transpose(out=pTa[:, 128:256], in_=i2b[:], identity=ident[:]), reads=[i2b, ident], writes=[pTa])
                b.op("act", lambda e: e.activation(out=iTf[:], in_=pTa[:, 0:256].rearrange("p (a t) -> p a t", a=2), func=AF.Copy),
                     reads=[pTa], writes=[iTf])
                yield
                b.op("dve", lambda e: e.scalar_tensor_tensor(out=idxTf[:], in0=iTf[:, 0, :], scalar=128.0, in1=iTf[:, 1, :], op0=ALU.mult, op1=ALU.add),
                     reads=[iTf], writes=[idxTf])
                b.op("dve", lambda e: e.tensor_copy(out=IDXT[:], in_=idxTf[:]), reads=[idxTf], writes=[IDXT])
                b.op("dve", lambda e: e.tensor_tensor(out=E[:], in0=TS[:], in1=TS[:, :, 0:1].to_broadcast([128, 8, 16]), op=ALU.subtract),
                     reads=[TS], writes=[E])
                b.op("act", lambda e: e.activation(out=E[:], in_=E[:], func=AF.Exp), reads=[E], writes=[E])
                b.op("dve", lambda e: e.tensor_reduce(out=Z[:], in_=E[:], axis=AX.X, op=ALU.add), reads=[E], writes=[Z])
                b.op("dve", lambda e: e.reciprocal(out=Z[:], in_=Z[:]), reads=[Z], writes=[Z])
                b.op("dve", lambda e: e.tensor_tensor(out=G[:], in0=E[:], in1=Z[:].unsqueeze(2).to_broadcast([128, 8, 16]), op=ALU.mult),
                     reads=[E, Z], writes=[G])
                b.op("pe", lambda e: e.transpose(out=pq[0][:, 0:128], in_=G[:].rearrange("p h k -> p (h k)"), identity=identf[:]), reads=[G, identf], writes=[pq[0]])
                b.op("act", lambda e: e.activation(out=GT[:], in_=pq[0][:, 0:128], func=AF.Copy), reads=[pq[0]], writes=[GT])
                yield

            def step(gen, k=1):
                if gen is None:
                    return
                for _ in range(k):
                    try:
                        next(gen)
                    except StopIteration:
                        return

            g0 = stageA(0)
            step(g0, 1000)
            for i in range(ntiles):
                par = i % 2
                X, xnb, GT, IDXT = Xs[par], xnbs[par], GTs[par], IDXTs[par]
                s, r0 = divmod(i * 128, NREAL)
                gN = stageA(i + 1) if i + 1 < ntiles else None
                pend = [None]

                def finish(tb_, UV_):
                    Lt = Ls[tb_]
                    tsl = slice(tb_ * 4, tb_ * 4 + 4)
                    b.op("act", lambda e: e.activation(out=actT[:, tsl], in_=actT[:, tsl], func=AF.Gelu_apprx_tanh), reads=[actT], writes=[actT])
                    b.op("dve", lambda e: e.tensor_tensor(out=coefT[:, tsl], in0=actT[:, tsl], in1=GT[:, tsl], op=ALU.mult),
                         reads=[actT, GT], writes=[coefT])
                    b.op("dve", lambda e: e.tensor_copy(out=Lflat[:, tb_ * 4 * 129:tb_ * 4 * 129 + 3 * 129 + 1:129], in_=coefT[:, tsl]),
                         reads=[coefT], writes=[Lt])
                    for q in range(4):
                        t = tb_ * 4 + q
                        for half in range(2):
                            b.op("pe", lambda e: e.matmul(pOut[half][:], lhsT=L[:, t, :], rhs=UV_[:, q, 1024 + half * 512:1024 + (half + 1) * 512],
                                                          start=(t == 0), stop=(t == 127)), reads=[Lt, UV_], writes=[pOut[half]])

                for tb in range(32):
                    UV = UVG[cnt["u"] % 4]
                    cnt["u"] += 1
                    for q in range(4):
                        t = tb * 4 + q
                        b.dma("pool", None, lambda e: e.indirect_dma_start(out=UV[:, q, :], out_offset=None, in_=UV16.h.ap(),
                                                                           in_offset=bass.IndirectOffsetOnAxis(ap=IDXT[:, t:t + 1], axis=0)),
                              reads=[IDXT, UV16], writes=[UV])
                    for q in range(4):
                        t = tb * 4 + q
                        PX = pX[cnt["v"] % 2]
                        cnt["v"] += 1
                        for half in range(2):
                            b.op("pe", lambda e: e.matmul(PX[:, half * 512:(half + 1) * 512], lhsT=ident[:, t:t + 1].to_broadcast([128, 128]),
                                                          rhs=xnb[:, half * 512:(half + 1) * 512], start=True, stop=True, skip_group_check=True),
                                 reads=[ident, xnb], writes=[PX])
                        b.op("dve", lambda e: e.scalar_tensor_tensor(out=junkD[:], in0=UV[:, q, 0:1024], scalar=1.0, in1=PX[:], op0=ALU.mult,
                                                                     op1=ALU.mult, accum_out=actT[:, t:t + 1]),
                             reads=[UV, PX], writes=[actT] if q in (0, 3) else ())
                        if q == 1 and pend[0] is not None:
                            finish(*pend[0])
                            pend[0] = None
                    pend[0] = (tb, UV)
                    step(gN, 1)
                finish(*pend[0])
                for half in range(2):
                    b.op("dve", lambda e: e.tensor_tensor(out=h3[:, half * 512:(half + 1) * 512], in0=pOut[half][:],
                                                          in1=X[:, half * 512:(half + 1) * 512], op=ALU.add), reads=[pOut[half], X], writes=[h3])
                b.op("act", lambda e: e.activation(out=junkA[:], in_=h3[:], func=AF.Square, accum_out=ssC[:]), reads=[h3], writes=[junkA, ssC])
                b.op("dve", lambda e: e.tensor_scalar(out=ssC[:], in0=ssC[:], scalar1=1.0 / 1024, scalar2=EPS, op0=ALU.mult, op1=ALU.add),
                     reads=[ssC], writes=[ssC])
                b.op("act", lambda e: e.activation(out=ssC[:], in_=ssC[:], func=AF.Sqrt), reads=[ssC], writes=[ssC])
                b.op("dve", lambda e: e.reciprocal(out=ssC[:], in_=ssC[:]), reads=[ssC], writes=[ssC])
                b.op("dve", lambda e: e.scalar_tensor_tensor(out=h3[:], in0=h3[:], scalar=ssC[:, 0:1], in1=gfb[:], op0=ALU.mult, op1=ALU.mult),
                     reads=[h3, ssC, gfb], writes=[h3])
                b.dma("sp", None, lambda e: e.dma_start(out=out_d.ap()[s, r0:r0 + 128, :], in_=h3[:]), reads=[h3], writes=[OUT])
                step(gN, 1000)
        b.barrier()

    progs = {1: phase1, 2: phase2, 3: phase3a, 4: phase3b, 5: phase5}
    for p in phases:
        if p in progs:
            progs[p]()
    b.barrier()
    return nc


def _pc(v, nchunk):
    return np.ascontiguousarray(np.asarray(v, np.float32).reshape(nchunk, 128).T)


def prep_common(inp):
    f = lambda a: np.asarray(a, np.float32)
    w_in = f(inp["w_in"])[0]
    z32 = np.zeros((1024, 32), np.float32)
    kr = w_in[:, 2688:2720]
    krs = np.concatenate([kr[:, 16:], kr[:, :16]], axis=1)
    w_in_r = np.concatenate([
        w_in[:, 0:1024], w_in[:, 1024:2048], w_in[:, 2048:2432], w_in[:, 2432:2688],
        kr, z32, kr, z32, krs, z32, krs, z32,
        w_in[:, 2720:3744], w_in[:, 3744:4768]], axis=1)
    assert w_in_r.shape == (1024, 4992)
    conv_w = f(inp["conv_w"])[0]
    cw = np.ascontiguousarray(conv_w.reshape(4, 8, 128).transpose(2, 1, 0))
    w_uq = f(inp["w_uq"])[0].reshape(384, 16, 96)
    nope = w_uq[:, :, :64].reshape(384, 1024)
    rope = w_uq[:, :, 64:]
    ropes = np.concatenate([rope[:, :, 16:], rope[:, :, :16]], axis=2)
    z = np.zeros((384, 16, 32), np.float32)
    rope_p = np.concatenate([rope, z], axis=2).reshape(384, 1024)
    ropes_p = np.concatenate([ropes, z], axis=2).reshape(384, 1024)
    w_uq_r = np.concatenate([nope, rope_p, ropes_p, np.zeros((384, 1024), np.float32)], axis=1)
    w_ukv = f(inp["w_ukv"])[0].reshape(256, 16, 128)
    w_ukv_r = np.concatenate([w_ukv[:, :, :64].reshape(256, 1024), w_ukv[:, :, 64:].reshape(256, 1024)], axis=1)
    pos = (np.arange(SEQC, dtype=np.float32) - 112.0).astype(np.float32)
    inv = np.power(np.float32(10000.0), -np.arange(16, dtype=np.float32) * np.float32(2.0 / 32)).astype(np.float32)
    ang = (pos[None, :] * inv[:, None]).astype(np.float32)
    c, s_ = np.cos(ang).astype(np.float32), np.sin(ang).astype(np.float32)
    cos32 = np.concatenate([c, c], axis=0)
    sin32 = np.concatenate([-s_, s_], axis=0)
    zz = np.zeros((32, SEQC), np.float32)
    cos2 = np.concatenate([cos32, zz, cos32, zz], axis=0)
    sin2s = np.concatenate([sin32, zz, sin32, zz], axis=0)
    return {
        "meta": f(inp["meta_tokens"]),
        "g1": _pc(inp["norm1_g"][0], 8),
        "w_in": np.ascontiguousarray(w_in_r),
        "convw": cw,
        "convb": _pc(inp["conv_b"][0], 8),
        "rg_wa": np.ascontiguousarray(f(inp["rg_wa"])[0].transpose(1, 0, 2)),
        "rg_wx": np.ascontiguousarray(f(inp["rg_wx"])[0].transpose(1, 0, 2)),
        "rg_ba": _pc(inp["rg_ba"][0], 8),
        "rg_bx": _pc(inp["rg_bx"][0], 8),
        "rg_lam": _pc(inp["rg_lambda"][0], 8),
        "w_rnn_out": f(inp["w_rnn_out"])[0],
        "qg": _pc(inp["q_norm_g"][0], 3),
        "kvg": _pc(inp["kv_norm_g"][0], 2),
        "w_uq": np.ascontiguousarray(w_uq_r),
        "w_ukv": np.ascontiguousarray(w_ukv_r),
        "w_attn_out": f(inp["w_attn_out"])[0],
        "w_out": f(inp["w_out"])[0],
        "g2": f(inp["norm2_g"]).reshape(1, 1024),
        "peer_wq": f(inp["peer_wq"])[0],
        "keys1T": np.ascontiguousarray(f(inp["peer_keys1"])[0].T),
        "keys2T": np.ascontiguousarray(f(inp["peer_keys2"])[0].T),
        "peer_u": f(inp["peer_u"])[0],
        "peer_v": f(inp["peer_v"])[0],
        "gf": f(inp["final_g"]).reshape(1, 1024),
        "cos2": cos2,
        "sin2s": sin2s,
    }


def kernel(**inputs):
    NC = 8
    NSEQ = 4
    common = prep_common(inputs)
    x = np.asarray(inputs["x"], np.float32)
    nc = build(NSEQ)
    in_maps = []
    for c in range(NC):
        m = dict(common)
        m["x"] = np.ascontiguousarray(x[c * NSEQ:(c + 1) * NSEQ])
        in_maps.append(m)
    res = run_bass_kernel_spmd(nc, in_maps, core_ids=list(range(NC)))
    return np.concatenate([r["out"] for r in res.results], axis=0)
```

```python
from contextlib import ExitStack
import numpy as np
import concourse.bass as bass
import concourse.mybir as mybir
from concourse.bass_utils import run_bass_kernel_spmd

F32 = mybir.dt.float32
BF16 = mybir.dt.bfloat16
I32 = mybir.dt.int32
U32 = mybir.dt.uint32
AF = mybir.ActivationFunctionType
ALU = mybir.AluOpType
AX = mybir.AxisListType

SEQC = 2176
NREAL = 2048
EPS = 1e-6
GROUPS = [(0, 128)] + [(128 + 512 * g, 512) for g in range(4)]


class T:
    def __init__(self, h, name, accum=False):
        self.h = h
        self.name = name
        self.w = {}
        self.r = {}
        self.accum = accum

    def __getitem__(self, k):
        return self.h[k]


class B:
    def __init__(self, nc):
        self.nc = nc
        self.E = {"pe": nc.tensor, "act": nc.scalar, "dve": nc.vector, "pool": nc.gpsimd, "sp": nc.sync}
        self.sems = {}
        self.cnt = {}
        self.seen = {k: {} for k in self.E}
        for k in self.E:
            self.sems[k] = nc.alloc_semaphore("s_" + k)
            self.cnt[k] = 0
        self.nins = 0

    def newsem(self, name):
        self.sems[name] = self.nc.alloc_semaphore("s_" + name)
        self.cnt[name] = 0
        return name

    def _wait(self, eng, evs):
        for key, val in evs.items():
            if val <= 0 or (eng == "pe" and key == "pe"):
                continue
            if self.seen[eng].get(key, 0) >= val:
                continue
            self.E[eng].wait_ge(self.sems[key], val)
            self.seen[eng][key] = val
            self.nins += 1

    @staticmethod
    def _merge(d, e):
        for k, v in e.items():
            if d.get(k, 0) < v:
                d[k] = v

    def _deps(self, reads, writes):
        evs = {}
        for t in reads:
            self._merge(evs, t.w)
        for t in writes:
            if not t.accum:
                self._merge(evs, t.w)
                self._merge(evs, t.r)
        return evs

    def _commit(self, ev, reads, writes):
        for t in reads:
            if not t.accum:
                self._merge(t.r, ev)
        for t in writes:
            if t.accum:
                self._merge(t.w, ev)
            else:
                t.w = dict(ev)
                t.r = {}

    def op(self, eng, fn, reads=(), writes=()):
        self._wait(eng, self._deps(reads, writes))
        ins = fn(self.E[eng])
        self.cnt[eng] += 1
        ins.then_inc(self.sems[eng], 1)
        self.nins += 1
        self._commit({eng: self.cnt[eng]}, reads, writes)

    def dma(self, q, sem, fn, reads=(), writes=()):
        own = [t for t in writes if not t.accum] or [t for t in reads if not t.accum]
        t0 = own[0]
        if getattr(t0, "sem", None) is None:
            t0.sem = {}
        if q not in t0.sem:
            t0.sem[q] = self.newsem("d%d" % len(self.sems))
        sem = t0.sem[q]
        deps = self._deps(reads, writes)
        if writes and not writes[0].accum:
            wr = {}
            for t in writes:
                self._merge(wr, t.r)
            if deps.get(sem, 0) > wr.get(sem, 0):
                v = wr.get(sem, 0)
                if v > 0:
                    deps[sem] = v
                else:
                    deps.pop(sem)
        self._wait(q, deps)
        ins = fn(self.E[q])
        self.cnt[sem] += 16
        ins.then_inc(self.sems[sem], 16)
        self.nins += 1
        self._commit({sem: self.cnt[sem]}, reads, writes)

    def barrier(self):
        allev = {k: v for k, v in self.cnt.items() if v > 0}
        for e in self.E:
            self._wait(e, allev)


def build(NSEQ, phases=(1, 2, 3, 4, 5), debug=False, p5tiles=None):
    nc = bass.Bass("TRN2", target_bir_lowering=False)
    b = B(nc)
    NT = NSEQ * SEQC
    NR = NSEQ * NREAL
    skind = "ExternalOutput" if debug else "Internal"

    def din(name, shape, dt=F32):
        return nc.dram_tensor(name, list(shape), dt, kind="ExternalInput")

    def dscr(name, shape, dt=F32):
        return T(nc.dram_tensor(name, list(shape), dt, kind=skind), name, accum=True)

    x_d = din("x", [NSEQ, NREAL, 1024])
    meta_d = din("meta", [16, 1024])
    g1_d = din("g1", [128, 8])
    win_d = din("w_in", [1024, 4992])
    cw_d = din("convw", [128, 8, 4])
    cb_d = din("convb", [128, 8])
    rgwa_d = din("rg_wa", [128, 8, 128])
    rgwx_d = din("rg_wx", [128, 8, 128])
    rgba_d = din("rg_ba", [128, 8])
    rgbx_d = din("rg_bx", [128, 8])
    lam_d = din("rg_lam", [128, 8])
    wro_d = din("w_rnn_out", [1024, 1024])
    qg_d = din("qg", [128, 3])
    kvg_d = din("kvg", [128, 2])
    wuq_d = din("w_uq", [384, 4096])
    wukv_d = din("w_ukv", [256, 2048])
    wao_d = din("w_attn_out", [1024, 1024])
    wo_d = din("w_out", [1024, 1024])
    g2_d = din("g2", [1, 1024])
    wq_d = din("peer_wq", [1024, 2048])
    k1T_d = din("keys1T", [128, 128])
    k2T_d = din("keys2T", [128, 128])
    pu_d = din("peer_u", [16384, 1024])
    pv_d = din("peer_v", [16384, 1024])
    gf_d = din("gf", [1, 1024])
    cos_d = din("cos2", [128, SEQC])
    sin_d = din("sin2s", [128, SEQC])
    out_d = nc.dram_tensor("out", [NSEQ, NREAL, 1024], F32, kind="ExternalOutput")
    OUT = T(out_d, "out", accum=True)

    S_xr = dscr("S_xr", [1024, NT])
    S_gr = dscr("S_gr", [1024, NT])
    S_cq = dscr("S_cq", [384, NT])
    S_ckv = dscr("S_ckv", [256, NT])
    S_kr = dscr("S_kr", [128, NT])
    S_krs = dscr("S_krs", [128, NT])
    S_grnn = dscr("S_grnn", [1024, NT])
    S_gatt = dscr("S_gatt", [1024, NT])
    S_mrnn = dscr("S_mrnn", [1024, NT])
    S_oT = dscr("S_oT", [1024, NT], BF16)
    S_h2 = dscr("S_h2", [NR, 1024])
    UV16 = T(nc.dram_tensor("UV16", [16384, 2048], BF16, kind="Internal"), "UV16", accum=True)

    ld = b.newsem("ld")
    st = b.newsem("st")
    wl = b.newsem("wl")

    def mk(es, pre):
        def sb(name, shape, dt):
            return T(es.enter_context(nc.sbuf_tensor(pre + name, list(shape), dt)), name)

        def ps(name, shape, dt):
            return T(es.enter_context(nc.psum_tensor(pre + name, list(shape), dt)), name)
        return sb, ps

    def make_ident(sb):
        identf = sb("identf", [128, 128], F32)
        ident = sb("ident", [128, 128], BF16)
        b.op("pool", lambda e: e.memset(identf[:], 1.0), writes=[identf])
        b.op("pool", lambda e: e.affine_select(out=identf[:], in_=identf[:], pattern=[[-1, 128]],
                                               compare_op=ALU.is_equal, fill=0.0, base=0, channel_multiplier=1),
             reads=[identf], writes=[identf])
        b.op("dve", lambda e: e.tensor_copy(out=ident[:], in_=identf[:]), reads=[identf], writes=[ident])
        return ident, identf

    def load_small(sb, name, d, shape):
        t = sb(name, shape, F32)
        b.dma("sp", wl, lambda e: e.dma_start(out=t[:], in_=d.ap()), writes=[t])
        return t

    def load_w_cast(dst, dst_ap, src_ap):
        b.dma("pool", wl, lambda e: e.dma_start(out=dst_ap, in_=src_ap), writes=[dst])

    def phase1():
        with ExitStack() as es:
            sb, ps = mk(es, "p1_")
            W = sb("W1", [128, 8, 4992], BF16)
            g1 = load_small(sb, "g1s", g1_d, [128, 8])
            stg = [sb("wstg%d" % i, [128, 4992], F32) for i in range(2)]
            for kc in range(8):
                s_ = stg[kc % 2]
                b.dma("sp", wl, lambda e: e.dma_start(out=s_[:], in_=win_d.ap()[kc * 128:(kc + 1) * 128, :]), writes=[s_])
                b.op("dve", lambda e: e.tensor_scalar(out=W[:, kc, :], in0=s_[:], scalar1=g1[:, kc:kc + 1], scalar2=None,
                                                      op0=ALU.mult), reads=[s_, g1], writes=[W])
            ident, _ = make_ident(sb)
            xt = [sb("xt%d" % i, [128, 1024], F32) for i in range(2)]
            junk = sb("junk", [128, 1024], BF16)
            ss = [sb("ss%d" % i, [128, 1], F32) for i in range(2)]
            xb = [sb("xb%d" % i, [128, 1024], BF16) for i in range(2)]
            n1T = [sb("n1T%d" % i, [128, 8, 512], BF16) for i in range(2)]
            pT = [ps("pT%d" % i, [128, 1024], BF16) for i in range(2)]
            pa = [ps("pa%d" % i, [128, 512], F32) for i in range(4)]
            stage = [sb("stage%d" % i, [128, 512], F32) for i in range(4)]
            chunks = []
            for i in range(8):
                chunks.append((S_xr, i * 128, None, 128))
            for i in range(8):
                chunks.append((S_gr, i * 128, AF.Gelu_apprx_tanh, 128))
            for i in range(3):
                chunks.append((S_cq, i * 128, None, 128))
            for i in range(2):
                chunks.append((S_ckv, i * 128, None, 128))
            chunks.append((S_kr, 0, None, 128))
            chunks.append((S_krs, 0, None, 128))
            for i in range(8):
                chunks.append((S_grnn, i * 128, AF.Sigmoid, 128))
            for i in range(8):
                chunks.append((S_gatt, i * 128, AF.Sigmoid, 128))
            ti = 0
            gi = 0
            ci = 0
            cv = [sb("cv%d" % i, [128, 8, 1024], BF16) for i in range(2)]
            conv = [(src, off, c) for (src, off) in ((pu_d, 0), (pv_d, 1024)) for c in range(16)]
            cvi = [0]

            def convert_some(k):
                for _ in range(k):
                    if cvi[0] >= len(conv):
                        return
                    src, off, c = conv[cvi[0]]
                    dst = UV16
                    CV = cv[cvi[0] % 2]
                    cvi[0] += 1
                    b.dma("pool", None, lambda e: e.dma_start(out=CV[:], in_=src.ap()[c * 1024:(c + 1) * 1024, :].rearrange("(p r) d -> p r d", p=128)),
                          writes=[CV])
                    b.dma("sp", None, lambda e: e.dma_start(out=dst.h.ap()[c * 1024:(c + 1) * 1024, off:off + 1024].rearrange("(p r) d -> p r d", p=128), in_=CV[:]),
                          reads=[CV], writes=[dst])

            for s in range(NSEQ):
                for (c0, n) in GROUPS:
                    nT = n1T[gi % 2]
                    gi += 1
                    convert_some(8 if NSEQ == 1 else 2)
                    for j in range(n // 128):
                        t = c0 // 128 + j
                        X = xt[ti % 2]
                        SS = ss[ti % 2]
                        XB = xb[ti % 2]
                        PT = pT[ti % 2]
                        ti += 1
                        if t == 0:
                            b.op("pool", lambda e: e.memset(X[:], 0.0), writes=[X])
                            b.dma("sp", ld, lambda e: e.dma_start(out=X[112:128, :], in_=meta_d.ap()), writes=[X])
                        else:
                            b.dma("sp", ld, lambda e: e.dma_start(out=X[:], in_=x_d.ap()[s, (t - 1) * 128:t * 128, :]), writes=[X])
                        b.op("act", lambda e: e.activation(out=junk[:], in_=X[:], func=AF.Square, accum_out=SS[:]),
                             reads=[X], writes=[junk, SS])
                        b.op("dve", lambda e: e.tensor_scalar(out=SS[:], in0=SS[:], scalar1=1.0 / 1024, scalar2=EPS,
                                                              op0=ALU.mult, op1=ALU.add), reads=[SS], writes=[SS])
                        b.op("act", lambda e: e.activation(out=SS[:], in_=SS[:], func=AF.Sqrt), reads=[SS], writes=[SS])
                        b.op("dve", lambda e: e.reciprocal(out=SS[:], in_=SS[:]), reads=[SS], writes=[SS])
                        b.op("act", lambda e: e.activation(out=XB[:], in_=X[:], func=AF.Copy, scale=SS[:]),
                             reads=[X, SS], writes=[XB])
                        for c in range(8):
                            b.op("pe", lambda e: e.transpose(out=PT[:, c * 128:(c + 1) * 128], in_=XB[:, c * 128:(c + 1) * 128],
                                                             identity=ident[:]), reads=[XB, ident], writes=[PT])
                        b.op("dve", lambda e: e.tensor_copy(out=nT[:, :, j * 128:(j + 1) * 128],
                                                            in_=PT[:].rearrange("p (c t) -> p c t", c=8)),
                             reads=[PT], writes=[nT])
                    for oc, (S_, r0, fn, m) in enumerate(chunks):
                        PA = pa[ci % 4]
                        SG = stage[ci % 4]
                        ci += 1
                        for kc in range(8):
                            b.op("pe", lambda e: e.matmul(PA[:, 0:n], lhsT=W[:, kc, oc * 128:(oc + 1) * 128], rhs=nT[:, kc, 0:n],
                                                          start=(kc == 0), stop=(kc == 7)), reads=[W, nT], writes=[PA])
                        if fn is None:
                            b.op("dve", lambda e: e.tensor_copy(out=SG[:, 0:n], in_=PA[:, 0:n]), reads=[PA], writes=[SG])
                        else:
                            b.op("act", lambda e: e.activation(out=SG[:, 0:n], in_=PA[:, 0:n], func=fn), reads=[PA], writes=[SG])
                        col = s * SEQC + c0
                        b.dma("pool", st, lambda e: e.dma_start(out=S_.h.ap()[r0:r0 + 128, col:col + n], in_=SG[:, 0:n]),
                              reads=[SG], writes=[S_])
            convert_some(len(conv))
        b.barrier()

    def phase2():
        with ExitStack() as es:
            sb, ps = mk(es, "p2_")
            WA = sb("WA", [128, 8, 128], BF16)
            WX = sb("WX", [128, 8, 128], BF16)
            WRO = sb("WRO", [128, 8, 1024], BF16)
            load_w_cast(WA, WA[:], rgwa_d.ap())
            load_w_cast(WX, WX[:], rgwx_d.ap())
            load_w_cast(WRO, WRO[:], wro_d.ap().rearrange("(k p) f -> p k f", p=128))
            cw = load_small(sb, "cw", cw_d, [128, 8, 4])
            cb = load_small(sb, "cb", cb_d, [128, 8])
            ba = load_small(sb, "ba", rgba_d, [128, 8])
            bx = load_small(sb, "bx", rgbx_d, [128, 8])
            lam = load_small(sb, "lam", lam_d, [128, 8])
            c8 = sb("c8", [128, 8], F32)
            c16 = sb("c16", [128, 8], F32)
            b.op("act", lambda e: e.activation(out=c8[:], in_=lam[:], func=AF.Exp, scale=-1.0), reads=[lam], writes=[c8])
            b.op("act", lambda e: e.activation(out=c8[:], in_=c8[:], func=AF.Ln, bias=1.0), reads=[c8], writes=[c8])
            b.op("dve", lambda e: e.tensor_scalar(out=c16[:], in0=c8[:], scalar1=-16.0, scalar2=None, op0=ALU.mult),
                 reads=[c8], writes=[c16])
            b.op("dve", lambda e: e.tensor_scalar(out=c8[:], in0=c8[:], scalar1=-8.0, scalar2=None, op0=ALU.mult),
                 reads=[c8, c16], writes=[c8])
            XR = sb("XR", [128, SEQC], F32)
            Y = sb("Y", [128, SEQC], F32)
            YB = sb("YB", [128, SEQC], BF16)
            A = sb("A", [128, SEQC], F32)
            U = sb("U", [128, SEQC], F32)
            H = sb("H", [128, SEQC], F32)
            GR = sb("GR", [128, SEQC], F32)
            ZT = sb("ZT", [128, 8, SEQC], BF16)
            tr = sb("tr", [128, 512], F32)
            ta2 = sb("ta2", [128, 512], F32)
            ti_ = sb("ti", [128, 512], F32)
            gs = [sb("gs%d" % i, [128, 512], F32) for i in range(2)]
            stage = [sb("stage%d" % i, [128, 512], F32) for i in range(2)]
            pA = ps("pA", [128, 512], F32)
            pX = ps("pX", [128, 512], F32)
            pO = [ps("pO%d" % i, [128, 512], F32) for i in range(2)]
            V0 = 112
            NV = SEQC - V0
            k = 0
            for s in range(NSEQ):
                sc = s * SEQC
                for n_ in range(8):
                    r0 = n_ * 128
                    b.dma("sp", ld, lambda e: e.dma_start(out=XR[:], in_=S_xr.h.ap()[r0:r0 + 128, sc:sc + SEQC]),
                          reads=[S_xr], writes=[XR])
                    b.dma("sp", ld, lambda e: e.dma_start(out=GR[:], in_=S_gr.h.ap()[r0:r0 + 128, sc:sc + SEQC]),
                          reads=[S_gr], writes=[GR])
                    b.op("dve", lambda e: e.tensor_scalar(out=Y[:, V0:SEQC], in0=XR[:, V0 - 3:SEQC - 3], scalar1=cw[:, n_, 0:1],
                                                          scalar2=cb[:, n_:n_ + 1], op0=ALU.mult, op1=ALU.add),
                         reads=[XR, cw, cb], writes=[Y])
                    for kk in range(1, 4):
                        b.op("dve", lambda e: e.scalar_tensor_tensor(out=Y[:, V0:SEQC], in0=XR[:, V0 - 3 + kk:SEQC - 3 + kk],
                                                                     scalar=cw[:, n_, kk:kk + 1], in1=Y[:, V0:SEQC],
                                                                     op0=ALU.mult, op1=ALU.add), reads=[XR, cw, Y], writes=[Y])
                    b.op("act", lambda e: e.activation(out=YB[:, V0:SEQC], in_=Y[:, V0:SEQC], func=AF.Copy), reads=[Y], writes=[YB])
                    for (c0, n) in GROUPS:
                        if c0 == 0:
                            c0, n = V0, 16
                        cs = slice(c0, c0 + n)
                        b.op("pe", lambda e: e.matmul(pA[:, 0:n], lhsT=WA[:, n_, :], rhs=YB[:, cs], start=True, stop=True),
                             reads=[WA, YB], writes=[pA])
                        b.op("pe", lambda e: e.matmul(pX[:, 0:n], lhsT=WX[:, n_, :], rhs=YB[:, cs], start=True, stop=True),
                             reads=[WX, YB], writes=[pX])
                        b.op("act", lambda e: e.activation(out=tr[:, 0:n], in_=pA[:, 0:n], func=AF.Sigmoid, bias=ba[:, n_:n_ + 1]),
                             reads=[pA, ba], writes=[tr])
                        b.op("act", lambda e: e.activation(out=ti_[:, 0:n], in_=pX[:, 0:n], func=AF.Sigmoid, bias=bx[:, n_:n_ + 1]),
                             reads=[pX, bx], writes=[ti_])
                        b.op("act", lambda e: e.activation(out=A[:, cs], in_=tr[:, 0:n], func=AF.Exp, scale=c8[:, n_:n_ + 1]),
                             reads=[tr, c8], writes=[A])
                        b.op("act", lambda e: e.activation(out=ta2[:, 0:n], in_=tr[:, 0:n], func=AF.Exp, scale=c16[:, n_:n_ + 1]),
                             reads=[tr, c16], writes=[ta2])
                        b.op("dve", lambda e: e.tensor_scalar(out=ta2[:, 0:n], in0=ta2[:, 0:n], scalar1=-1.0, scalar2=1.0,
                                                              op0=ALU.mult, op1=ALU.add), reads=[ta2], writes=[ta2])
                        b.op("act", lambda e: e.activation(out=ta2[:, 0:n], in_=ta2[:, 0:n], func=AF.Sqrt), reads=[ta2], writes=[ta2])
                        b.op("dve", lambda e: e.tensor_tensor(out=ti_[:, 0:n], in0=ti_[:, 0:n], in1=Y[:, cs], op=ALU.mult),
                             reads=[ti_, Y], writes=[ti_])
                        b.op("dve", lambda e: e.tensor_tensor(out=U[:, cs], in0=ti_[:, 0:n], in1=ta2[:, 0:n], op=ALU.mult),
                             reads=[ti_, ta2], writes=[U])
                    b.op("dve", lambda e: e.tensor_tensor_scan(out=H[:, V0:SEQC], data0=A[:, V0:SEQC], data1=U[:, V0:SEQC],
                                                               initial=0.0, op0=ALU.mult, op1=ALU.add), reads=[A, U], writes=[H])
                    b.op("dve", lambda e: e.tensor_tensor(out=ZT[:, n_, V0:SEQC], in0=H[:, V0:SEQC], in1=GR[:, V0:SEQC], op=ALU.mult),
                         reads=[H, GR], writes=[ZT])
                for (c0, n) in GROUPS[1:]:
                    cs = slice(c0, c0 + n)
                    for dc in range(8):
                        PO = pO[k % 2]
                        G = gs[k % 2]
                        SG = stage[k % 2]
                        k += 1
                        b.dma("sp", ld, lambda e: e.dma_start(out=G[:, 0:n], in_=S_grnn.h.ap()[dc * 128:(dc + 1) * 128, sc + c0:sc + c0 + n]),
                              reads=[S_grnn], writes=[G])
                        for kc in range(8):
                            b.op("pe", lambda e: e.matmul(PO[:, 0:n], lhsT=WRO[:, kc, dc * 128:(dc + 1) * 128], rhs=ZT[:, kc, cs],
                                                          start=(kc == 0), stop=(kc == 7)), reads=[WRO, ZT], writes=[PO])
                        b.op("dve", lambda e: e.tensor_tensor(out=SG[:, 0:n], in0=PO[:, 0:n], in1=G[:, 0:n], op=ALU.mult),
                             reads=[PO, G], writes=[SG])
                        b.dma("pool", st, lambda e: e.dma_start(out=S_mrnn.h.ap()[dc * 128:(dc + 1) * 128, sc + c0:sc + c0 + n],
                                                                in_=SG[:, 0:n]), reads=[SG], writes=[S_mrnn])
        b.barrier()


    def phase3a():
        with ExitStack() as es:
            sb, ps = mk(es, "p3_")
            WUQ = sb("WUQ", [128, 3, 3072], BF16)
            WUKV = sb("WUKV", [128, 2, 2048], BF16)
            qg = load_small(sb, "qg", qg_d, [128, 3])
            kvg = load_small(sb, "kvg", kvg_d, [128, 2])
            wst = sb("wst", [128, 3072], F32)
            for kc in range(3):
                b.dma("sp", wl, lambda e: e.dma_start(out=wst[:], in_=wuq_d.ap()[kc * 128:(kc + 1) * 128, 0:3072]), writes=[wst])
                b.op("dve", lambda e: e.tensor_scalar(out=WUQ[:, kc, :], in0=wst[:], scalar1=qg[:, kc:kc + 1], scalar2=None,
                                                      op0=ALU.mult), reads=[wst, qg], writes=[WUQ])
            for kc in range(2):
                b.dma("sp", wl, lambda e: e.dma_start(out=wst[:, 0:2048], in_=wukv_d.ap()[kc * 128:(kc + 1) * 128, :]), writes=[wst])
                b.op("dve", lambda e: e.tensor_scalar(out=WUKV[:, kc, :], in0=wst[:, 0:2048], scalar1=kvg[:, kc:kc + 1], scalar2=None,
                                                      op0=ALU.mult), reads=[wst, kvg], writes=[WUKV])
            ident, identf = make_ident(sb)
            ones = sb("ones", [128, 128], F32)
            b.op("pool", lambda e: e.memset(ones[:], 1.0), writes=[ones])
            tri = sb("tri", [128, 128], BF16)
            b.op("pool", lambda e: e.memset(identf[:], 1.0), reads=[identf], writes=[identf])
            b.op("pool", lambda e: e.affine_select(out=identf[:], in_=identf[:], pattern=[[1, 128]], compare_op=ALU.is_ge,
                                                   fill=0.0, base=0, channel_multiplier=-1), reads=[identf], writes=[identf])
            b.op("dve", lambda e: e.tensor_copy(out=tri[:], in_=identf[:]), reads=[identf], writes=[tri])
            Kn = sb("Kn", [128, 8, SEQC], BF16)
            Kr2 = sb("Kr2", [128, SEQC], BF16)
            V = sb("V", [128, 17, 16, 65], BF16)
            b.op("pool", lambda e: e.memset(V[:], 1.0), writes=[V])
            cq_t = sb("cq_t", [128, 3, 512], F32)
            ckv_t = sb("ckv_t", [128, 2, 512], F32)
            sq = sb("sq", [128, 3, 512], F32)
            rstd = sb("rstd", [128, 512], F32)
            cqn = sb("cqn", [128, 3, 512], BF16)
            ckvn = sb("ckvn", [128, 2, 512], BF16)
            kr_t = sb("kr_t", [128, 512], F32)
            krs_t = sb("krs_t", [128, 512], F32)
            cos_t = sb("cos_t", [128, 512], F32)
            sin_t = sb("sin_t", [128, 512], F32)
            tmp1 = sb("tmp1", [128, 512], F32)
            tmp2 = sb("tmp2", [128, 512], F32)
            Qn = sb("Qn", [128, 8, 512], BF16)
            Qr = sb("Qr", [128, 8, 512], BF16)
            PTs = [sb("PTs%d" % i, [128, 512], BF16) for i in range(3)]
            o_tm = sb("o_tm", [128, 4, 1024], BF16)
            oT = sb("oT", [128, 8, 512], BF16)
            rec = sb("rec", [128, 4], F32)
            pn = ps("pn", [128, 512], F32)
            pk = [ps("pk%d" % i, [128, 512], F32) for i in range(2)]
            pS = [ps("pS%d" % i, [128, 512], F32) for i in range(2)]
            pO = [ps("pO%d" % i, [128, 512], F32) for i in range(2)]
            pT = ps("pT", [128, 1024], BF16)
            scale = float(96 ** -0.5)
            ki = [0]

            def nextpk():
                ki[0] += 1
                return pk[ki[0] % 2]

            def rmsn(src, nch, dst, nfeat, n):
                b.op("act", lambda e: e.activation(out=sq[:, 0:nch, 0:n], in_=src[:, :, 0:n], func=AF.Square), reads=[src], writes=[sq])
                for c in range(nch):
                    b.op("pe", lambda e: e.matmul(pn[:, 0:n], lhsT=ones[:], rhs=sq[:, c, 0:n], start=(c == 0), stop=(c == nch - 1)),
                         reads=[ones, sq], writes=[pn])
                b.op("dve", lambda e: e.tensor_scalar(out=rstd[:, 0:n], in0=pn[:, 0:n], scalar1=1.0 / nfeat, scalar2=EPS,
                                                      op0=ALU.mult, op1=ALU.add), reads=[pn], writes=[rstd])
                b.op("act", lambda e: e.activation(out=rstd[:, 0:n], in_=rstd[:, 0:n], func=AF.Sqrt), reads=[rstd], writes=[rstd])
                b.op("dve", lambda e: e.reciprocal(out=rstd[:, 0:n], in_=rstd[:, 0:n]), reads=[rstd], writes=[rstd])
                for c in range(nch):
                    b.op("dve", lambda e: e.tensor_tensor(out=dst[:, c, 0:n], in0=src[:, c, 0:n], in1=rstd[:, 0:n], op=ALU.mult),
                         reads=[src, rstd], writes=[dst])

            si = 0
            for s in range(NSEQ):
                sc = s * SEQC
                for (c0, n) in GROUPS:
                    col = sc + c0
                    b.dma("sp", ld, lambda e: e.dma_start(out=cq_t[:, :, 0:n],
                                                          in_=S_cq.h.ap()[:, col:col + n].rearrange("(c p) n -> p c n", p=128)),
                          reads=[S_cq], writes=[cq_t])
                    b.dma("sp", ld, lambda e: e.dma_start(out=ckv_t[:, :, 0:n],
                                                          in_=S_ckv.h.ap()[:, col:col + n].rearrange("(c p) n -> p c n", p=128)),
                          reads=[S_ckv], writes=[ckv_t])
                    b.dma("sp", ld, lambda e: e.dma_start(out=kr_t[:, 0:n], in_=S_kr.h.ap()[:, col:col + n]), reads=[S_kr], writes=[kr_t])
                    b.dma("sp", ld, lambda e: e.dma_start(out=krs_t[:, 0:n], in_=S_krs.h.ap()[:, col:col + n]), reads=[S_krs], writes=[krs_t])
                    b.dma("sp", ld, lambda e: e.dma_start(out=cos_t[:, 0:n], in_=cos_d.ap()[:, c0:c0 + n]), writes=[cos_t])
                    b.dma("sp", ld, lambda e: e.dma_start(out=sin_t[:, 0:n], in_=sin_d.ap()[:, c0:c0 + n]), writes=[sin_t])
                    rmsn(cq_t, 3, cqn, 384.0, n)
                    rmsn(ckv_t, 2, ckvn, 256.0, n)
                    for j in range(8):
                        P_ = nextpk()
                        for kc in range(2):
                            b.op("pe", lambda e: e.matmul(P_[:, 0:n], lhsT=WUKV[:, kc, j * 128:(j + 1) * 128], rhs=ckvn[:, kc, 0:n],
                                                          start=(kc == 0), stop=(kc == 1)), reads=[WUKV, ckvn], writes=[P_])
                        b.op("act", lambda e: e.activation(out=Kn[:, j, c0:c0 + n], in_=P_[:, 0:n], func=AF.Copy), reads=[P_], writes=[Kn])
                    b.op("dve", lambda e: e.tensor_tensor(out=tmp1[:, 0:n], in0=kr_t[:, 0:n], in1=cos_t[:, 0:n], op=ALU.mult),
                         reads=[kr_t, cos_t], writes=[tmp1])
                    b.op("dve", lambda e: e.tensor_tensor(out=tmp2[:, 0:n], in0=krs_t[:, 0:n], in1=sin_t[:, 0:n], op=ALU.mult),
                         reads=[krs_t, sin_t], writes=[tmp2])
                    b.op("dve", lambda e: e.tensor_tensor(out=Kr2[:, c0:c0 + n], in0=tmp1[:, 0:n], in1=tmp2[:, 0:n], op=ALU.add),
                         reads=[tmp1, tmp2], writes=[Kr2])
                    for jt in range(n // 128):
                        t = c0 // 128 + jt
                        if t == 0:
                            lo, M = 112, 16
                        else:
                            lo, M = jt * 128, 128
                        for half in range(2):
                            P_ = nextpk()
                            for kc in range(2):
                                b.op("pe", lambda e: e.matmul(P_[0:M, :], lhsT=ckvn[:, kc, lo:lo + M],
                                                              rhs=WUKV[:, kc, 1024 + half * 512:1024 + (half + 1) * 512],
                                                              start=(kc == 0), stop=(kc == 1)), reads=[WUKV, ckvn], writes=[P_])
                            b.op("act", lambda e: e.activation(out=V[0:M, t, half * 8:(half + 1) * 8, 0:64],
                                                               in_=P_[0:M, :].rearrange("p (h d) -> p h d", h=8), func=AF.Copy),
                                 reads=[P_], writes=[V])
                    if c0 == 0:
                        continue
                    for j in range(8):
                        P_ = nextpk()
                        for kc in range(3):
                            b.op("pe", lambda e: e.matmul(P_[:, 0:n], lhsT=WUQ[:, kc, j * 128:(j + 1) * 128], rhs=cqn[:, kc, 0:n],
                                                          start=(kc == 0), stop=(kc == 2)), reads=[WUQ, cqn], writes=[P_])
                        b.op("act", lambda e: e.activation(out=Qn[:, j, 0:n], in_=P_[:, 0:n], func=AF.Copy), reads=[P_], writes=[Qn])
                    for j in range(8):
                        P1 = nextpk()
                        for kc in range(3):
                            b.op("pe", lambda e: e.matmul(P1[:, 0:n], lhsT=WUQ[:, kc, 1024 + j * 128:1024 + (j + 1) * 128], rhs=cqn[:, kc, 0:n],
                                                          start=(kc == 0), stop=(kc == 2)), reads=[WUQ, cqn], writes=[P1])
                        b.op("dve", lambda e: e.tensor_tensor(out=tmp1[:, 0:n], in0=P1[:, 0:n], in1=cos_t[:, 0:n], op=ALU.mult),
                             reads=[P1, cos_t], writes=[tmp1])
                        P2 = nextpk()
                        for kc in range(3):
                            b.op("pe", lambda e: e.matmul(P2[:, 0:n], lhsT=WUQ[:, kc, 2048 + j * 128:2048 + (j + 1) * 128], rhs=cqn[:, kc, 0:n],
                                                          start=(kc == 0), stop=(kc == 2)), reads=[WUQ, cqn], writes=[P2])
                        b.op("dve", lambda e: e.tensor_tensor(out=tmp2[:, 0:n], in0=P2[:, 0:n], in1=sin_t[:, 0:n], op=ALU.mult),
                             reads=[P2, sin_t], writes=[tmp2])
                        b.op("dve", lambda e: e.tensor_tensor(out=Qr[:, j, 0:n], in0=tmp1[:, 0:n], in1=tmp2[:, 0:n], op=ALU.add),
                             reads=[tmp1, tmp2], writes=[Qr])
                    g0t = c0 // 128
                    for h in range(16):
                        j = h // 2
                        p0 = (h % 2) * 64
                        PO = pO[h % 2]
                        POv = PO[:, 0:260].rearrange("p (q d) -> p q d", q=4)
                        for kt in range(0, g0t + 4):
                            if kt == 0:
                                k0, M = 112, 16
                            else:
                                k0, M = kt * 128, 128
                            qlo = max(0, kt - g0t)
                            q0 = qlo * 128
                            PS_ = pS[si % 2]
                            PTb = PTs[si % 3]
                            si += 1
                            b.op("pe", lambda e: e.matmul(PS_[0:M, q0:512], lhsT=Kn[p0:p0 + 64, j, k0:k0 + M], rhs=Qn[p0:p0 + 64, j, q0:512],
                                                          start=True, stop=False), reads=[Kn, Qn], writes=[PS_])
                            b.op("pe", lambda e: e.matmul(PS_[0:M, q0:512], lhsT=Kr2[p0:p0 + 32, k0:k0 + M], rhs=Qr[p0:p0 + 32, j, q0:512],
                                                          start=False, stop=True), reads=[Kr2, Qr], writes=[PS_])
                            b.op("act", lambda e: e.activation(out=PTb[0:M, q0:512], in_=PS_[0:M, q0:512], func=AF.Exp, scale=scale),
                                 reads=[PS_], writes=[PTb])
                            if kt >= g0t:
                                b.op("dve", lambda e: e.tensor_tensor(out=PTb[:, q0:q0 + 128], in0=PTb[:, q0:q0 + 128], in1=tri[:], op=ALU.mult),
                                     reads=[PTb, tri], writes=[PTb])
                            for qt in range(qlo, 4):
                                b.op("pe", lambda e: e.matmul(POv[:, qt, :], lhsT=PTb[0:M, qt * 128:(qt + 1) * 128], rhs=V[0:M, kt, h, :],
                                                              start=(kt == 0 and qt == 0), stop=(kt == g0t + qt), skip_group_check=True),
                                     reads=[PTb, V], writes=[PO])
                        b.op("dve", lambda e: e.reciprocal(out=rec[:], in_=POv[:, :, 64]), reads=[PO], writes=[rec])
                        for qt in range(4):
                            b.op("dve", lambda e: e.tensor_scalar(out=o_tm[:, qt, h * 64:(h + 1) * 64], in0=POv[:, qt, 0:64],
                                                                  scalar1=rec[:, qt:qt + 1], scalar2=None, op0=ALU.mult),
                                 reads=[PO, rec], writes=[o_tm])
                    for qt in range(4):
                        for c in range(8):
                            b.op("pe", lambda e: e.transpose(out=pT[:, c * 128:(c + 1) * 128], in_=o_tm[:, qt, c * 128:(c + 1) * 128],
                                                             identity=ident[:]), reads=[o_tm, ident], writes=[pT])
                        b.op("dve", lambda e: e.tensor_copy(out=oT[:, :, qt * 128:(qt + 1) * 128],
                                                            in_=pT[:].rearrange("p (c t) -> p c t", c=8)), reads=[pT], writes=[oT])
                    b.dma("pool", st, lambda e: e.dma_start(out=S_oT.h.ap()[:, col:col + n].rearrange("(c p) n -> p c n", p=128), in_=oT[:]),
                          reads=[oT], writes=[S_oT])
        b.barrier()

    def phase3b():
        with ExitStack() as es:
            sb, ps = mk(es, "p3b_")
            WAO = sb("WAO", [128, 8, 1024], BF16)
            WO = sb("WO", [128, 8, 1024], BF16)
            load_w_cast(WAO, WAO[:], wao_d.ap().rearrange("(k p) f -> p k f", p=128))
            load_w_cast(WO, WO[:], wo_d.ap().rearrange("(k p) f -> p k f", p=128))
            oT = [sb("oT%d" % i, [128, 8, 512], BF16) for i in range(2)]
            ga = [sb("ga%d" % i, [128, 512], F32) for i in range(2)]
            mr = [sb("mr%d" % i, [128, 512], F32) for i in range(2)]
            tmp = sb("tmp", [128, 512], F32)
            mixT = sb("mixT", [128, 8, 512], BF16)
            xt = [sb("xt%d" % i, [128, 1024], F32) for i in range(2)]
            h2 = [sb("h2%d" % i, [128, 1024], F32) for i in range(2)]
            pk = [ps("pk%d" % i, [128, 512], F32) for i in range(4)]
            gi = 0
            k = 0
            ti = 0
            for s in range(NSEQ):
                sc = s * SEQC
                for g, (c0, n) in enumerate(GROUPS):
                    if g == 0:
                        continue
                    col = sc + c0
                    OT = oT[gi % 2]
                    gi += 1
                    b.dma("sp", ld, lambda e: e.dma_start(out=OT[:], in_=S_oT.h.ap()[:, col:col + n].rearrange("(c p) n -> p c n", p=128)),
                          reads=[S_oT], writes=[OT])
                    for dc in range(8):
                        GA = ga[k % 2]
                        MR = mr[k % 2]
                        PK = pk[k % 4]
                        k += 1
                        b.dma("sp", ld, lambda e: e.dma_start(out=GA[:], in_=S_gatt.h.ap()[dc * 128:(dc + 1) * 128, col:col + n]),
                              reads=[S_gatt], writes=[GA])
                        b.dma("sp", ld, lambda e: e.dma_start(out=MR[:], in_=S_mrnn.h.ap()[dc * 128:(dc + 1) * 128, col:col + n]),
                              reads=[S_mrnn], writes=[MR])
                        for kc in range(8):
                            b.op("pe", lambda e: e.matmul(PK[:], lhsT=WAO[:, kc, dc * 128:(dc + 1) * 128], rhs=OT[:, kc, :],
                                                          start=(kc == 0), stop=(kc == 7)), reads=[WAO, OT], writes=[PK])
                        b.op("dve", lambda e: e.tensor_tensor(out=tmp[:], in0=PK[:], in1=GA[:], op=ALU.mult), reads=[PK, GA], writes=[tmp])
                        b.op("dve", lambda e: e.tensor_tensor(out=mixT[:, dc, :], in0=tmp[:], in1=MR[:], op=ALU.add),
                             reads=[tmp, MR], writes=[mixT])
                    for qt in range(4):
                        X = xt[ti % 2]
                        H2 = h2[ti % 2]
                        ti += 1
                        r0 = (g - 1) * 512 + qt * 128
                        b.dma("sp", ld, lambda e: e.dma_start(out=X[:], in_=x_d.ap()[s, r0:r0 + 128, :]), writes=[X])
                        for half in range(2):
                            PK = pk[k % 4]
                            k += 1
                            for kc in range(8):
                                b.op("pe", lambda e: e.matmul(PK[:], lhsT=mixT[:, kc, qt * 128:(qt + 1) * 128],
                                                              rhs=WO[:, kc, half * 512:(half + 1) * 512],
                                                              start=(kc == 0), stop=(kc == 7)), reads=[WO, mixT], writes=[PK])
                            b.op("dve", lambda e: e.tensor_tensor(out=H2[:, half * 512:(half + 1) * 512], in0=PK[:],
                                                                  in1=X[:, half * 512:(half + 1) * 512], op=ALU.add),
                                 reads=[PK, X], writes=[H2])
                        b.dma("pool", st, lambda e: e.dma_start(out=S_h2.h.ap()[s * NREAL + r0:s * NREAL + r0 + 128, :], in_=H2[:]),
                              reads=[H2], writes=[S_h2])
        b.barrier()


    def phase5():
        with ExitStack() as es:
            sb, ps = mk(es, "p5_")
            WQ = sb("WQ", [128, 8, 2048], BF16)
            K1T = sb("K1T", [128, 128], BF16)
            K2T = sb("K2T", [128, 128], BF16)
            load_w_cast(WQ, WQ[:], wq_d.ap().rearrange("(k p) f -> p k f", p=128))
            load_w_cast(K1T, K1T[:], k1T_d.ap())
            load_w_cast(K2T, K2T[:], k2T_d.ap())
            g2b = sb("g2b", [128, 1024], F32)
            gfb = sb("gfb", [128, 1024], F32)
            b.dma("sp", None, lambda e: e.dma_start(out=g2b[:], in_=g2_d.ap().to_broadcast([128, 1024])), writes=[g2b])
            b.dma("sp", None, lambda e: e.dma_start(out=gfb[:], in_=gf_d.ap().to_broadcast([128, 1024])), writes=[gfb])
            ident, identf = make_ident(sb)
            iota_i = sb("iota_i", [128, 16], I32)
            iota16 = sb("iota16", [128, 16], F32)
            b.op("pool", lambda e: e.iota(iota_i[:], pattern=[[1, 16]], base=0, channel_multiplier=0), writes=[iota_i])
            b.op("dve", lambda e: e.tensor_copy(out=iota16[:], in_=iota_i[:]), reads=[iota_i], writes=[iota16])
            L = sb("L", [128, 128, 128], BF16)
            b.op("pool", lambda e: e.memset(L[:], 0.0), writes=[L])
            Lflat = L[:].rearrange("p a b -> p (a b)")
            Xs = [sb("X%d" % i, [128, 1024], F32) for i in range(2)]
            xnbs = [sb("xnb%d" % i, [128, 1024], BF16) for i in range(2)]
            GTs = [sb("GT%d" % i, [128, 128], F32) for i in range(2)]
            IDXTs = [sb("IDXT%d" % i, [128, 128], U32) for i in range(2)]
            Gs = [sb("G%d" % i, [128, 8, 16], F32) for i in range(2)]
            ssA = sb("ssA", [128, 1], F32)
            ssC = sb("ssC", [128, 1], F32)
            junkA = sb("junkA", [128, 1024], BF16)
            junkD = sb("junkD", [128, 1024], BF16)
            xT = sb("xT", [128, 8, 128], BF16)
            qT = sb("qT", [128, 16, 128], BF16)
            S = sb("S", [128, 16, 128], F32)
            eqv = S[:].rearrange("p (h x) (y a) -> p h (x y) a", x=2, a=16)
            T16 = sb("T16", [128, 16, 16], F32)
            I16 = sb("I16", [128, 16, 16], U32)
            I16f = sb("I16f", [128, 16, 16], F32)
            cand = sb("cand", [128, 8, 256], F32)
            TS = sb("TS", [128, 8, 16], F32)
            CI = sb("CI", [128, 8, 16], U32)
            CIa = sb("CIa", [128, 8, 16], U32)
            CIb = sb("CIb", [128, 8, 16], U32)
            Af = sb("Af", [128, 8, 16], F32)
            Bf = sb("Bf", [128, 8, 16], F32)
            i1s = sb("i1s", [128, 128], F32)
            i2s = sb("i2s", [128, 128], F32)
            i1b = sb("i1b", [128, 128], BF16)
            i2b = sb("i2b", [128, 128], BF16)
            idxf = sb("idxf", [128, 128], F32)
            idxTf = sb("idxTf", [128, 128], F32)
            iTf = sb("iTf", [128, 2, 128], F32)
            E = sb("E", [128, 8, 16], F32)
            Z = sb("Z", [128, 8], F32)
            ACTV = sb("ACTV", [128, 128], F32)
            coefb = sb("coefb", [128, 128], BF16)
            coefT = sb("coefT", [128, 128], BF16)
            UVG = [sb("UVG%d" % i, [128, 8, 2048], BF16) for i in range(2)]
            UVGt = [[T(u.h, "UVG%d_%d" % (i_, q_)) for q_ in range(8)] for i_, u in enumerate(UVG)]
            actT = sb("actT", [128, 128], F32)
            Ls = [T(L.h, "L%d" % i) for i in range(16)]
            for lt in Ls:
                lt.w = dict(L.w)
            h3 = sb("h3", [128, 1024], F32)
            pTa = ps("pTa", [128, 1024], BF16)
            pq = [ps("pq%d" % i, [128, 512], F32) for i in range(1)]
            pOut = [ps("pOut%d" % i, [128, 512], F32) for i in range(2)]
            pX = [ps("pX%d" % i, [128, 1024], F32) for i in range(2)]
            T16v = T16[:].rearrange("p (h two) a -> p h two a", two=2)
            I16fv = I16f[:].rearrange("p (h two) a -> p h two a", two=2)
            B4 = [128, 8, 16, 16]
            cnt = {"u": 0, "v": 0, "q": 0}
            ntiles = NR // 128 if p5tiles is None else p5tiles

            def sweep(eng, fns, reads, writes):
                for k_, f in enumerate(fns):
                    w = writes if (k_ == 0 or k_ == len(fns) - 1) else ()
                    b.op(eng, f, reads=reads, writes=w)

            def top16(vals, ng, tv, iv):
                sweep("dve", [(lambda e, g=g: e.max(out=tv[:, g, 0:8], in_=vals[:, g, :])) for g in range(ng)], [vals], [tv])
                sweep("dve", [(lambda e, g=g: e.max_index(out=iv[:, g, 0:8], in_max=tv[:, g, 0:8], in_values=vals[:, g, :])) for g in range(ng)],
                      [vals, tv], [iv])
                yield
                sweep("dve", [(lambda e, g=g: e.match_replace(out=vals[:, g, :], in_to_replace=tv[:, g, 0:8], in_values=vals[:, g, :],
                                                              imm_value=-1e30)) for g in range(ng)], [tv], [vals])
                yield
                sweep("dve", [(lambda e, g=g: e.max(out=tv[:, g, 8:16], in_=vals[:, g, :])) for g in range(ng)], [vals], [tv])
                sweep("dve", [(lambda e, g=g: e.max_index(out=iv[:, g, 8:16], in_max=tv[:, g, 8:16], in_values=vals[:, g, :])) for g in range(ng)],
                      [vals, tv], [iv])
                yield

            def stageA(i):
                par = i % 2
                X, xnb, GT, IDXT, G = Xs[par], xnbs[par], GTs[par], IDXTs[par], Gs[par]
                b.dma("sp", None, lambda e: e.dma_start(out=X[:], in_=S_h2.h.ap()[i * 128:(i + 1) * 128, :]), reads=[S_h2], writes=[X])
                b.op("act", lambda e: e.activation(out=junkA[:], in_=X[:], func=AF.Square, accum_out=ssA[:]), reads=[X], writes=[junkA, ssA])
                b.op("dve", lambda e: e.tensor_scalar(out=ssA[:], in0=ssA[:], scalar1=1.0 / 1024, scalar2=EPS, op0=ALU.mult, op1=ALU.add),
                     reads=[ssA], writes=[ssA])
                b.op("act", lambda e: e.activation(out=ssA[:], in_=ssA[:], func=AF.Sqrt), reads=[ssA], writes=[ssA])
                b.op("dve", lambda e: e.reciprocal(out=ssA[:], in_=ssA[:]), reads=[ssA], writes=[ssA])
                b.op("dve", lambda e: e.scalar_tensor_tensor(out=xnb[:], in0=X[:], scalar=ssA[:, 0:1], in1=g2b[:], op0=ALU.mult, op1=ALU.mult),
                     reads=[X, ssA, g2b], writes=[xnb])
                yield
                for c in range(8):
                    b.op("pe", lambda e: e.transpose(out=pTa[:, c * 128:(c + 1) * 128], in_=xnb[:, c * 128:(c + 1) * 128], identity=ident[:]),
                         reads=[xnb, ident], writes=[pTa])
                b.op("act", lambda e: e.activation(out=xT[:], in_=pTa[:].rearrange("p (c t) -> p c t", c=8), func=AF.Copy), reads=[pTa], writes=[xT])
                yield
                for bq in range(4):
                    PQ = pq[0]
                    for j in range(4):
                        hh = bq * 4 + j
                        for kc in range(8):
                            b.op("pe", lambda e: e.matmul(PQ[:, j * 128:(j + 1) * 128], lhsT=WQ[:, kc, hh * 128:(hh + 1) * 128], rhs=xT[:, kc, :],
                                                          start=(kc == 0), stop=(kc == 7), skip_group_check=True), reads=[WQ, xT], writes=[PQ])
                    b.op("act", lambda e: e.activation(out=qT[:, bq * 4:(bq + 1) * 4, :], in_=PQ[:].rearrange("p (j t) -> p j t", j=4), func=AF.Copy),
                         reads=[PQ], writes=[qT])
                    yield
                for bq in range(4):
                    PQ = pq[0]
                    for j in range(4):
                        hh = bq * 4 + j
                        KT = K1T if hh % 2 == 0 else K2T
                        b.op("pe", lambda e: e.matmul(PQ[:, j * 128:(j + 1) * 128], lhsT=qT[:, hh, :], rhs=KT[:], start=True, stop=True,
                                                      skip_group_check=True), reads=[qT, KT], writes=[PQ])
                    b.op("act", lambda e: e.activation(out=S[:, bq * 4:(bq + 1) * 4, :], in_=PQ[:].rearrange("p (j t) -> p j t", j=4), func=AF.Copy),
                         reads=[PQ], writes=[S])
                    yield
                yield from top16(S, 16, T16, I16)
                b.op("dve", lambda e: e.tensor_copy(out=I16f[:], in_=I16[:]), reads=[I16], writes=[I16f])
                b.op("dve", lambda e: e.tensor_tensor(out=cand[:].rearrange("p h (a c) -> p h a c", a=16),
                                                      in0=T16v[:, :, 0, :].unsqueeze(3).to_broadcast(B4),
                                                      in1=T16v[:, :, 1, :].unsqueeze(2).to_broadcast(B4), op=ALU.add),
                     reads=[T16], writes=[cand])
                yield
                yield from top16(cand, 8, TS, CI)
                b.op("dve", lambda e: e.tensor_single_scalar(out=CIa[:], in_=CI[:], scalar=4, op=ALU.logical_shift_right), reads=[CI], writes=[CIa])
                b.op("dve", lambda e: e.tensor_single_scalar(out=CIb[:], in_=CI[:], scalar=15, op=ALU.bitwise_and), reads=[CI], writes=[CIb])
                b.op("dve", lambda e: e.tensor_copy(out=Af[:], in_=CIa[:]), reads=[CIa], writes=[Af])
                b.op("dve", lambda e: e.tensor_copy(out=Bf[:], in_=CIb[:]), reads=[CIb], writes=[Bf])
                yield
                for (SEL, half, dst) in ((Af, 0, i1s), (Bf, 1, i2s)):
                    b.op("dve", lambda e: e.tensor_tensor(out=eqv, in0=SEL[:].unsqueeze(3).to_broadcast(B4),
                                                          in1=iota16[:].unsqueeze(1).unsqueeze(1).to_broadcast(B4), op=ALU.is_equal),
                         reads=[SEL, iota16], writes=[S])
                    b.op("dve", lambda e: e.tensor_tensor(out=eqv, in0=eqv, in1=I16fv[:, :, half, :].unsqueeze(2).to_broadcast(B4), op=ALU.mult),
                         reads=[S, I16f], writes=[S])
                    b.op("dve", lambda e: e.tensor_reduce(out=dst[:].rearrange("p (h k) -> p h k", h=8), in_=eqv, axis=AX.X, op=ALU.add),
                         reads=[S], writes=[dst])
                    yield
                b.op("act", lambda e: e.activation(out=i1b[:], in_=i1s[:], func=AF.Copy), reads=[i1s], writes=[i1b])
                b.op("act", lambda e: e.activation(out=i2b[:], in_=i2s[:], func=AF.Copy), reads=[i2s], writes=[i2b])
                b.op("pe", lambda e: e.transpose(out=pTa[:, 0:128], in_=i1b[:], identity=ident[:]), reads=[i1b, ident], writes=[pTa])
                b.op("pe", lambda e: e.transpose(out=pTa[:, 128:256], in_=i2b[:], identity=ident[:]), reads=[i2b, ident], writes=[pTa])
                b.op("act", lambda e: e.activation(out=iTf[:], in_=pTa[:, 0:256].rearrange("p (a t) -> p a t", a=2), func=AF.Copy),
                     reads=[pTa], writes=[iTf])
                yield
                b.op("dve", lambda e: e.scalar_tensor_tensor(out=idxTf[:], in0=iTf[:, 0, :], scalar=128.0, in1=iTf[:, 1, :], op0=ALU.mult, op1=ALU.add),
                     reads=[iTf], writes=[idxTf])
                b.op("dve", lambda e: e.tensor_copy(out=IDXT[:], in_=idxTf[:]), reads=[idxTf], writes=[IDXT])
                b.op("dve", lambda e: e.tensor_tensor(out=E[:], in0=TS[:], in1=TS[:, :, 0:1].to_broadcast([128, 8, 16]), op=ALU.subtract),
                     reads=[TS], writes=[E])
                b.op("act", lambda e: e.activation(out=E[:], in_=E[:], func=AF.Exp), reads=[E], writes=[E])
                b.op("dve", lambda e: e.tensor_reduce(out=Z[:], in_=E[:], axis=AX.X, op=ALU.add), reads=[E], writes=[Z])
                b.op("dve", lambda e: e.reciprocal(out=Z[:], in_=Z[:]), reads=[Z], writes=[Z])
                b.op("dve", lambda e: e.tensor_tensor(out=G[:], in0=E[:], in1=Z[:].unsqueeze(2).to_broadcast([128, 8, 16]), op=ALU.mult),
                     reads=[E, Z], writes=[G])
                b.op("pe", lambda e: e.transpose(out=pq[0][:, 0:128], in_=G[:].rearrange("p h k -> p (h k)"), identity=identf[:]), reads=[G, identf], writes=[pq[0]])
                b.op("act", lambda e: e.activation(out=GT[:], in_=pq[0][:, 0:128], func=AF.Copy), reads=[pq[0]], writes=[GT])
                yield

            def step(gen, k=1):
                if gen is None:
                    return
                for _ in range(k):
                    try:
                        next(gen)
                    except StopIteration:
                        return

            g0 = stageA(0)
            step(g0, 1000)
            for i in range(ntiles):
                par = i % 2
                X, xnb, GT, IDXT = Xs[par], xnbs[par], GTs[par], IDXTs[par]
                s, r0 = divmod(i * 128, NREAL)
                gN = stageA(i + 1) if i + 1 < ntiles else None
                pend = [None]

                def finish(tb_, UV_, UVt_):
                    Lt = Ls[tb_]
                    tsl = slice(tb_ * 8, tb_ * 8 + 8)
                    b.op("act", lambda e: e.activation(out=actT[:, tsl], in_=actT[:, tsl], func=AF.Gelu_apprx_tanh), reads=[actT], writes=[actT])
                    b.op("dve", lambda e: e.tensor_tensor(out=coefT[:, tsl], in0=actT[:, tsl], in1=GT[:, tsl], op=ALU.mult),
                         reads=[actT, GT], writes=[coefT])
                    b.op("dve", lambda e: e.tensor_copy(out=Lflat[:, tb_ * 8 * 129:tb_ * 8 * 129 + 7 * 129 + 1:129], in_=coefT[:, tsl]),
                         reads=[coefT], writes=[Lt])
                    for q in range(8):
                        t = tb_ * 8 + q
                        for half in range(2):
                            b.op("pe", lambda e: e.matmul(pOut[half][:], lhsT=L[:, t, :], rhs=UV_[:, q, 1024 + half * 512:1024 + (half + 1) * 512],
                                                          start=(t == 0), stop=(t == 127)), reads=[Lt, UVt_[q]], writes=[pOut[half]])

                for tb in range(16):
                    UV = UVG[cnt["u"] % 2]
                    UVt = UVGt[cnt["u"] % 2]
                    cnt["u"] += 1
                    for q in range(8):
                        t = tb * 8 + q
                        b.dma("pool", None, lambda e: e.indirect_dma_start(out=UV[:, q, :], out_offset=None, in_=UV16.h.ap(),
                                                                           in_offset=bass.IndirectOffsetOnAxis(ap=IDXT[:, t:t + 1], axis=0)),
                              reads=[IDXT, UV16], writes=[UVt[q]])
                    for q in range(8):
                        t = tb * 8 + q
                        PX = pX[cnt["v"] % 2]
                        cnt["v"] += 1
                        for half in range(2):
                            b.op("pe", lambda e: e.matmul(PX[:, half * 512:(half + 1) * 512], lhsT=ident[:, t:t + 1].to_broadcast([128, 128]),
                                                          rhs=xnb[:, half * 512:(half + 1) * 512], start=True, stop=True, skip_group_check=True),
                                 reads=[ident, xnb], writes=[PX])
                        b.op("dve", lambda e: e.scalar_tensor_tensor(out=junkD[:], in0=UV[:, q, 0:1024], scalar=1.0, in1=PX[:], op0=ALU.mult,
                                                                     op1=ALU.mult, accum_out=actT[:, t:t + 1]),
                             reads=[UVt[q], PX], writes=[actT] if q in (0, 7) else ())
                        if q == 1 and pend[0] is not None:
                            finish(*pend[0])
                            pend[0] = None
                    pend[0] = (tb, UV, UVt)
                    step(gN, 2)
                finish(*pend[0])
                for half in range(2):
                    b.op("dve", lambda e: e.tensor_tensor(out=h3[:, half * 512:(half + 1) * 512], in0=pOut[half][:],
                                                          in1=X[:, half * 512:(half + 1) * 512], op=ALU.add), reads=[pOut[half], X], writes=[h3])
                b.op("act", lambda e: e.activation(out=junkA[:], in_=h3[:], func=AF.Square, accum_out=ssC[:]), reads=[h3], writes=[junkA, ssC])
                b.op("dve", lambda e: e.tensor_scalar(out=ssC[:], in0=ssC[:], scalar1=1.0 / 1024, scalar2=EPS, op0=ALU.mult, op1=ALU.add),
                     reads=[ssC], writes=[ssC])
                b.op("act", lambda e: e.activation(out=ssC[:], in_=ssC[:], func=AF.Sqrt), reads=[ssC], writes=[ssC])
                b.op("dve", lambda e: e.reciprocal(out=ssC[:], in_=ssC[:]), reads=[ssC], writes=[ssC])
                b.op("dve", lambda e: e.scalar_tensor_tensor(out=h3[:], in0=h3[:], scalar=ssC[:, 0:1], in1=gfb[:], op0=ALU.mult, op1=ALU.mult),
                     reads=[h3, ssC, gfb], writes=[h3])
                b.dma("sp", None, lambda e: e.dma_start(out=out_d.ap()[s, r0:r0 + 128, :], in_=h3[:]), reads=[h3], writes=[OUT])
                step(gN, 1000)
        b.barrier()

    progs = {1: phase1, 2: phase2, 3: phase3a, 4: phase3b, 5: phase5}
    for p in phases:
        if p in progs:
            progs[p]()
    b.barrier()
    return nc


def _pc(v, nchunk):
    return np.ascontiguousarray(np.asarray(v, np.float32).reshape(nchunk, 128).T)


def prep_common(inp):
    f = lambda a: np.asarray(a, np.float32)
    w_in = f(inp["w_in"])[0]
    z32 = np.zeros((1024, 32), np.float32)
    kr = w_in[:, 2688:2720]
    krs = np.concatenate([kr[:, 16:], kr[:, :16]], axis=1)
    w_in_r = np.concatenate([
        w_in[:, 0:1024], w_in[:, 1024:2048], w_in[:, 2048:2432], w_in[:, 2432:2688],
        kr, z32, kr, z32, krs, z32, krs, z32,
        w_in[:, 2720:3744], w_in[:, 3744:4768]], axis=1)
    assert w_in_r.shape == (1024, 4992)
    conv_w = f(inp["conv_w"])[0]
    cw = np.ascontiguousarray(conv_w.reshape(4, 8, 128).transpose(2, 1, 0))
    w_uq = f(inp["w_uq"])[0].reshape(384, 16, 96)
    nope = w_uq[:, :, :64].reshape(384, 1024)
    rope = w_uq[:, :, 64:]
    ropes = np.concatenate([rope[:, :, 16:], rope[:, :, :16]], axis=2)
    z = np.zeros((384, 16, 32), np.float32)
    rope_p = np.concatenate([rope, z], axis=2).reshape(384, 1024)
    ropes_p = np.concatenate([ropes, z], axis=2).reshape(384, 1024)
    w_uq_r = np.concatenate([nope, rope_p, ropes_p, np.zeros((384, 1024), np.float32)], axis=1)
    w_ukv = f(inp["w_ukv"])[0].reshape(256, 16, 128)
    w_ukv_r = np.concatenate([w_ukv[:, :, :64].reshape(256, 1024), w_ukv[:, :, 64:].reshape(256, 1024)], axis=1)
    pos = (np.arange(SEQC, dtype=np.float32) - 112.0).astype(np.float32)
    inv = np.power(np.float32(10000.0), -np.arange(16, dtype=np.float32) * np.float32(2.0 / 32)).astype(np.float32)
    ang = (pos[None, :] * inv[:, None]).astype(np.float32)
    c, s_ = np.cos(ang).astype(np.float32), np.sin(ang).astype(np.float32)
    cos32 = np.concatenate([c, c], axis=0)
    sin32 = np.concatenate([-s_, s_], axis=0)
    zz = np.zeros((32, SEQC), np.float32)
    cos2 = np.concatenate([cos32, zz, cos32, zz], axis=0)
    sin2s = np.concatenate([sin32, zz, sin32, zz], axis=0)
    return {
        "meta": f(inp["meta_tokens"]),
        "g1": _pc(inp["norm1_g"][0], 8),
        "w_in": np.ascontiguousarray(w_in_r),
        "convw": cw,
        "convb": _pc(inp["conv_b"][0], 8),
        "rg_wa": np.ascontiguousarray(f(inp["rg_wa"])[0].transpose(1, 0, 2)),
        "rg_wx": np.ascontiguousarray(f(inp["rg_wx"])[0].transpose(1, 0, 2)),
        "rg_ba": _pc(inp["rg_ba"][0], 8),
        "rg_bx": _pc(inp["rg_bx"][0], 8),
        "rg_lam": _pc(inp["rg_lambda"][0], 8),
        "w_rnn_out": f(inp["w_rnn_out"])[0],
        "qg": _pc(inp["q_norm_g"][0], 3),
        "kvg": _pc(inp["kv_norm_g"][0], 2),
        "w_uq": np.ascontiguousarray(w_uq_r),
        "w_ukv": np.ascontiguousarray(w_ukv_r),
        "w_attn_out": f(inp["w_attn_out"])[0],
        "w_out": f(inp["w_out"])[0],
        "g2": f(inp["norm2_g"]).reshape(1, 1024),
        "peer_wq": f(inp["peer_wq"])[0],
        "keys1T": np.ascontiguousarray(f(inp["peer_keys1"])[0].T),
        "keys2T": np.ascontiguousarray(f(inp["peer_keys2"])[0].T),
        "peer_u": f(inp["peer_u"])[0],
        "peer_v": f(inp["peer_v"])[0],
        "gf": f(inp["final_g"]).reshape(1, 1024),
        "cos2": cos2,
        "sin2s": sin2s,
    }


def kernel(**inputs):
    NC = 8
    NSEQ = 4
    common = prep_common(inputs)
    x = np.asarray(inputs["x"], np.float32)
    nc = build(NSEQ)
    in_maps = []
    for c in range(NC):
        m = dict(common)
        m["x"] = np.ascontiguousarray(x[c * NSEQ:(c + 1) * NSEQ])
        in_maps.append(m)
    res = run_bass_kernel_spmd(nc, in_maps, core_ids=list(range(NC)))
    return np.concatenate([r["out"] for r in res.results], axis=0)
```

```python
from contextlib import ExitStack
import numpy as np
import concourse.bass as bass
import concourse.mybir as mybir
from concourse.bass_utils import run_bass_kernel_spmd

F32 = mybir.dt.float32
BF16 = mybir.dt.bfloat16
I32 = mybir.dt.int32
U32 = mybir.dt.uint32
AF = mybir.ActivationFunctionType
ALU = mybir.AluOpType
AX = mybir.AxisListType

SEQC = 2176
NREAL = 2048
EPS = 1e-6
GROUPS = [(0, 128)] + [(128 + 512 * g, 512) for g in range(4)]


class T:
    def __init__(self, h, name, accum=False):
        self.h = h
        self.name = name
        self.w = {}
        self.r = {}
        self.accum = accum

    def __getitem__(self, k):
        return self.h[k]


class B:
    def __init__(self, nc):
        self.nc = nc
        self.E = {"pe": nc.tensor, "act": nc.scalar, "dve": nc.vector, "pool": nc.gpsimd, "sp": nc.sync}
        self.sems = {}
        self.cnt = {}
        self.seen = {k: {} for k in self.E}
        for k in self.E:
            self.sems[k] = nc.alloc_semaphore("s_" + k)
            self.cnt[k] = 0
        self.nins = 0

    def newsem(self, name):
        self.sems[name] = self.nc.alloc_semaphore("s_" + name)
        self.cnt[name] = 0
        return name

    def _wait(self, eng, evs):
        for key, val in evs.items():
            if val <= 0 or (eng == "pe" and key == "pe"):
                continue
            if self.seen[eng].get(key, 0) >= val:
                continue
            self.E[eng].wait_ge(self.sems[key], val)
            self.seen[eng][key] = val
            self.nins += 1

    @staticmethod
    def _merge(d, e):
        for k, v in e.items():
            if d.get(k, 0) < v:
                d[k] = v

    def _deps(self, reads, writes):
        evs = {}
        for t in reads:
            self._merge(evs, t.w)
        for t in writes:
            if not t.accum:
                self._merge(evs, t.w)
                self._merge(evs, t.r)
        return evs

    def _commit(self, ev, reads, writes):
        for t in reads:
            if not t.accum:
                self._merge(t.r, ev)
        for t in writes:
            if t.accum:
                self._merge(t.w, ev)
            else:
                t.w = dict(ev)
                t.r = {}

    def op(self, eng, fn, reads=(), writes=()):
        self._wait(eng, self._deps(reads, writes))
        ins = fn(self.E[eng])
        self.cnt[eng] += 1
        ins.then_inc(self.sems[eng], 1)
        self.nins += 1
        self._commit({eng: self.cnt[eng]}, reads, writes)

    def dma(self, q, sem, fn, reads=(), writes=()):
        own = [t for t in writes if not t.accum] or [t for t in reads if not t.accum]
        t0 = own[0]
        if getattr(t0, "sem", None) is None:
            t0.sem = {}
        if q not in t0.sem:
            t0.sem[q] = self.newsem("d%d" % len(self.sems))
        sem = t0.sem[q]
        deps = self._deps(reads, writes)
        if writes and not writes[0].accum:
            wr = {}
            for t in writes:
                self._merge(wr, t.r)
            if deps.get(sem, 0) > wr.get(sem, 0):
                v = wr.get(sem, 0)
                if v > 0:
                    deps[sem] = v
                else:
                    deps.pop(sem)
        self._wait(q, deps)
        ins = fn(self.E[q])
        self.cnt[sem] += 16
        ins.then_inc(self.sems[sem], 16)
        self.nins += 1
        self._commit({sem: self.cnt[sem]}, reads, writes)

    def barrier(self):
        allev = {k: v for k, v in self.cnt.items() if v > 0}
        for e in self.E:
            self._wait(e, allev)


def build(NSEQ, phases=(1, 2, 3, 4, 5), debug=False, p5tiles=None):
    nc = bass.Bass("TRN2", target_bir_lowering=False)
    b = B(nc)
    NT = NSEQ * SEQC
    NR = NSEQ * NREAL
    skind = "ExternalOutput" if debug else "Internal"

    def din(name, shape, dt=F32):
        return nc.dram_tensor(name, list(shape), dt, kind="ExternalInput")

    def dscr(name, shape, dt=F32):
        return T(nc.dram_tensor(name, list(shape), dt, kind=skind), name, accum=True)

    x_d = din("x", [NSEQ, NREAL, 1024])
    meta_d = din("meta", [16, 1024])
    g1_d = din("g1", [128, 8])
    win_d = din("w_in", [1024, 4992])
    cw_d = din("convw", [128, 8, 4])
    cb_d = din("convb", [128, 8])
    rgwa_d = din("rg_wa", [128, 8, 128])
    rgwx_d = din("rg_wx", [128, 8, 128])
    rgba_d = din("rg_ba", [128, 8])
    rgbx_d = din("rg_bx", [128, 8])
    lam_d = din("rg_lam", [128, 8])
    wro_d = din("w_rnn_out", [1024, 1024])
    qg_d = din("qg", [128, 3])
    kvg_d = din("kvg", [128, 2])
    wuq_d = din("w_uq", [384, 4096])
    wukv_d = din("w_ukv", [256, 2048])
    wao_d = din("w_attn_out", [1024, 1024])
    wo_d = din("w_out", [1024, 1024])
    g2_d = din("g2", [1, 1024])
    wq_d = din("peer_wq", [1024, 2048])
    k1T_d = din("keys1T", [128, 128])
    k2T_d = din("keys2T", [128, 128])
    pu_d = din("peer_u", [16384, 1024])
    pv_d = din("peer_v", [16384, 1024])
    gf_d = din("gf", [1, 1024])
    cos_d = din("cos2", [128, SEQC])
    sin_d = din("sin2s", [128, SEQC])
    out_d = nc.dram_tensor("out", [NSEQ, NREAL, 1024], F32, kind="ExternalOutput")
    OUT = T(out_d, "out", accum=True)

    S_xr = dscr("S_xr", [1024, NT])
    S_gr = dscr("S_gr", [1024, NT])
    S_cq = dscr("S_cq", [384, NT])
    S_ckv = dscr("S_ckv", [256, NT])
    S_kr = dscr("S_kr", [128, NT])
    S_krs = dscr("S_krs", [128, NT])
    S_grnn = dscr("S_grnn", [1024, NT])
    S_gatt = dscr("S_gatt", [1024, NT])
    S_mrnn = dscr("S_mrnn", [1024, NT])
    S_oT = dscr("S_oT", [1024, NT], BF16)
    S_h2 = dscr("S_h2", [NR, 1024])
    UV16 = T(nc.dram_tensor("UV16", [16384, 2048], BF16, kind="Internal"), "UV16", accum=True)

    ld = b.newsem("ld")
    st = b.newsem("st")
    wl = b.newsem("wl")

    def mk(es, pre):
        def sb(name, shape, dt):
            return T(es.enter_context(nc.sbuf_tensor(pre + name, list(shape), dt)), name)

        def ps(name, shape, dt):
            return T(es.enter_context(nc.psum_tensor(pre + name, list(shape), dt)), name)
        return sb, ps

    def make_ident(sb):
        identf = sb("identf", [128, 128], F32)
        ident = sb("ident", [128, 128], BF16)
        b.op("pool", lambda e: e.memset(identf[:], 1.0), writes=[identf])
        b.op("pool", lambda e: e.affine_select(out=identf[:], in_=identf[:], pattern=[[-1, 128]],
                                               compare_op=ALU.is_equal, fill=0.0, base=0, channel_multiplier=1),
             reads=[identf], writes=[identf])
        b.op("dve", lambda e: e.tensor_copy(out=ident[:], in_=identf[:]), reads=[identf], writes=[ident])
        return ident, identf

    def load_small(sb, name, d, shape):
        t = sb(name, shape, F32)
        b.dma("sp", wl, lambda e: e.dma_start(out=t[:], in_=d.ap()), writes=[t])
        return t

    def load_w_cast(dst, dst_ap, src_ap):
        b.dma("pool", wl, lambda e: e.dma_start(out=dst_ap, in_=src_ap), writes=[dst])

    def phase1():
        with ExitStack() as es:
            sb, ps = mk(es, "p1_")
            W = sb("W1", [128, 8, 4992], BF16)
            g1 = load_small(sb, "g1s", g1_d, [128, 8])
            stg = [sb("wstg%d" % i, [128, 4992], F32) for i in range(2)]
            for kc in range(8):
                s_ = stg[kc % 2]
                b.dma("sp", wl, lambda e: e.dma_start(out=s_[:], in_=win_d.ap()[kc * 128:(kc + 1) * 128, :]), writes=[s_])
                b.op("dve", lambda e: e.tensor_scalar(out=W[:, kc, :], in0=s_[:], scalar1=g1[:, kc:kc + 1], scalar2=None,
                                                      op0=ALU.mult), reads=[s_, g1], writes=[W])
            ident, _ = make_ident(sb)
            xt = [sb("xt%d" % i, [128, 1024], F32) for i in range(2)]
            junk = sb("junk", [128, 1024], BF16)
            ss = [sb("ss%d" % i, [128, 1], F32) for i in range(2)]
            xb = [sb("xb%d" % i, [128, 1024], BF16) for i in range(2)]
            n1T = [sb("n1T%d" % i, [128, 8, 512], BF16) for i in range(2)]
            pT = [ps("pT%d" % i, [128, 1024], BF16) for i in range(2)]
            pa = [ps("pa%d" % i, [128, 512], F32) for i in range(4)]
            stage = [sb("stage%d" % i, [128, 512], F32) for i in range(4)]
            chunks = []
            for i in range(8):
                chunks.append((S_xr, i * 128, None, 128))
            for i in range(8):
                chunks.append((S_gr, i * 128, AF.Gelu_apprx_tanh, 128))
            for i in range(3):
                chunks.append((S_cq, i * 128, None, 128))
            for i in range(2):
                chunks.append((S_ckv, i * 128, None, 128))
            chunks.append((S_kr, 0, None, 128))
            chunks.append((S_krs, 0, None, 128))
            for i in range(8):
                chunks.append((S_grnn, i * 128, AF.Sigmoid, 128))
            for i in range(8):
                chunks.append((S_gatt, i * 128, AF.Sigmoid, 128))
            ti = 0
            gi = 0
            ci = 0
            cv = [sb("cv%d" % i, [128, 8, 1024], BF16) for i in range(2)]
            conv = [(src, off, c) for (src, off) in ((pu_d, 0), (pv_d, 1024)) for c in range(16)]
            cvi = [0]

            def convert_some(k):
                for _ in range(k):
                    if cvi[0] >= len(conv):
                        return
                    src, off, c = conv[cvi[0]]
                    dst = UV16
                    CV = cv[cvi[0] % 2]
                    cvi[0] += 1
                    b.dma("pool", None, lambda e: e.dma_start(out=CV[:], in_=src.ap()[c * 1024:(c + 1) * 1024, :].rearrange("(p r) d -> p r d", p=128)),
                          writes=[CV])
                    b.dma("sp", None, lambda e: e.dma_start(out=dst.h.ap()[c * 1024:(c + 1) * 1024, off:off + 1024].rearrange("(p r) d -> p r d", p=128), in_=CV[:]),
                          reads=[CV], writes=[dst])

            for s in range(NSEQ):
                for (c0, n) in GROUPS:
                    nT = n1T[gi % 2]
                    gi += 1
                    convert_some(8 if NSEQ == 1 else 2)
                    for j in range(n // 128):
                        t = c0 // 128 + j
                        X = xt[ti % 2]
                        SS = ss[ti % 2]
                        XB = xb[ti % 2]
                        PT = pT[ti % 2]
                        ti += 1
                        if t == 0:
                            b.op("pool", lambda e: e.memset(X[:], 0.0), writes=[X])
                            b.dma("sp", ld, lambda e: e.dma_start(out=X[112:128, :], in_=meta_d.ap()), writes=[X])
                        else:
                            b.dma("sp", ld, lambda e: e.dma_start(out=X[:], in_=x_d.ap()[s, (t - 1) * 128:t * 128, :]), writes=[X])
                        b.op("act", lambda e: e.activation(out=junk[:], in_=X[:], func=AF.Square, accum_out=SS[:]),
                             reads=[X], writes=[junk, SS])
                        b.op("dve", lambda e: e.tensor_scalar(out=SS[:], in0=SS[:], scalar1=1.0 / 1024, scalar2=EPS,
                                                              op0=ALU.mult, op1=ALU.add), reads=[SS], writes=[SS])
                        b.op("act", lambda e: e.activation(out=SS[:], in_=SS[:], func=AF.Sqrt), reads=[SS], writes=[SS])
                        b.op("dve", lambda e: e.reciprocal(out=SS[:], in_=SS[:]), reads=[SS], writes=[SS])
                        b.op("act", lambda e: e.activation(out=XB[:], in_=X[:], func=AF.Copy, scale=SS[:]),
                             reads=[X, SS], writes=[XB])
                        for c in range(8):
                            b.op("pe", lambda e: e.transpose(out=PT[:, c * 128:(c + 1) * 128], in_=XB[:, c * 128:(c + 1) * 128],
                                                             identity=ident[:]), reads=[XB, ident], writes=[PT])
                        b.op("dve", lambda e: e.tensor_copy(out=nT[:, :, j * 128:(j + 1) * 128],
                                                            in_=PT[:].rearrange("p (c t) -> p c t", c=8)),
                             reads=[PT], writes=[nT])
                    for oc, (S_, r0, fn, m) in enumerate(chunks):
                        PA = pa[ci % 4]
                        SG = stage[ci % 4]
                        ci += 1
                        for kc in range(8):
                            b.op("pe", lambda e: e.matmul(PA[:, 0:n], lhsT=W[:, kc, oc * 128:(oc + 1) * 128], rhs=nT[:, kc, 0:n],
                                                          start=(kc == 0), stop=(kc == 7)), reads=[W, nT], writes=[PA])
                        if fn is None:
                            b.op("dve", lambda e: e.tensor_copy(out=SG[:, 0:n], in_=PA[:, 0:n]), reads=[PA], writes=[SG])
                        else:
                            b.op("act", lambda e: e.activation(out=SG[:, 0:n], in_=PA[:, 0:n], func=fn), reads=[PA], writes=[SG])
                        col = s * SEQC + c0
                        b.dma("pool", st, lambda e: e.dma_start(out=S_.h.ap()[r0:r0 + 128, col:col + n], in_=SG[:, 0:n]),
                              reads=[SG], writes=[S_])
            convert_some(len(conv))
        b.barrier()

    def phase2():
        with ExitStack() as es:
            sb, ps = mk(es, "p2_")
            WA = sb("WA", [128, 8, 128], BF16)
            WX = sb("WX", [128, 8, 128], BF16)
            WRO = sb("WRO", [128, 8, 1024], BF16)
            load_w_cast(WA, WA[:], rgwa_d.ap())
            load_w_cast(WX, WX[:], rgwx_d.ap())
            load_w_cast(WRO, WRO[:], wro_d.ap().rearrange("(k p) f -> p k f", p=128))
            cw = load_small(sb, "cw", cw_d, [128, 8, 4])
            cb = load_small(sb, "cb", cb_d, [128, 8])
            ba = load_small(sb, "ba", rgba_d, [128, 8])
            bx = load_small(sb, "bx", rgbx_d, [128, 8])
            lam = load_small(sb, "lam", lam_d, [128, 8])
            c8 = sb("c8", [128, 8], F32)
            c16 = sb("c16", [128, 8], F32)
            b.op("act", lambda e: e.activation(out=c8[:], in_=lam[:], func=AF.Exp, scale=-1.0), reads=[lam], writes=[c8])
            b.op("act", lambda e: e.activation(out=c8[:], in_=c8[:], func=AF.Ln, bias=1.0), reads=[c8], writes=[c8])
            b.op("dve", lambda e: e.tensor_scalar(out=c16[:], in0=c8[:], scalar1=-16.0, scalar2=None, op0=ALU.mult),
                 reads=[c8], writes=[c16])
            b.op("dve", lambda e: e.tensor_scalar(out=c8[:], in0=c8[:], scalar1=-8.0, scalar2=None, op0=ALU.mult),
                 reads=[c8, c16], writes=[c8])
            XR = sb("XR", [128, SEQC], F32)
            Y = sb("Y", [128, SEQC], F32)
            YB = sb("YB", [128, SEQC], BF16)
            A = sb("A", [128, SEQC], F32)
            U = sb("U", [128, SEQC], F32)
            H = sb("H", [128, SEQC], F32)
            GR = sb("GR", [128, SEQC], F32)
            ZT = sb("ZT", [128, 8, SEQC], BF16)
            tr = sb("tr", [128, 512], F32)
            ta2 = sb("ta2", [128, 512], F32)
            ti_ = sb("ti", [128, 512], F32)
            gs = [sb("gs%d" % i, [128, 512], F32) for i in range(2)]
            stage = [sb("stage%d" % i, [128, 512], F32) for i in range(2)]
            pA = ps("pA", [128, 512], F32)
            pX = ps("pX", [128, 512], F32)
            pO = [ps("pO%d" % i, [128, 512], F32) for i in range(2)]
            V0 = 112
            NV = SEQC - V0
            k = 0
            for s in range(NSEQ):
                sc = s * SEQC
                for n_ in range(8):
                    r0 = n_ * 128
                    b.dma("sp", ld, lambda e: e.dma_start(out=XR[:], in_=S_xr.h.ap()[r0:r0 + 128, sc:sc + SEQC]),
                          reads=[S_xr], writes=[XR])
                    b.dma("sp", ld, lambda e: e.dma_start(out=GR[:], in_=S_gr.h.ap()[r0:r0 + 128, sc:sc + SEQC]),
                          reads=[S_gr], writes=[GR])
                    b.op("dve", lambda e: e.tensor_scalar(out=Y[:, V0:SEQC], in0=XR[:, V0 - 3:SEQC - 3], scalar1=cw[:, n_, 0:1],
                                                          scalar2=cb[:, n_:n_ + 1], op0=ALU.mult, op1=ALU.add),
                         reads=[XR, cw, cb], writes=[Y])
                    for kk in range(1, 4):
                        b.op("dve", lambda e: e.scalar_tensor_tensor(out=Y[:, V0:SEQC], in0=XR[:, V0 - 3 + kk:SEQC - 3 + kk],
                                                                     scalar=cw[:, n_, kk:kk + 1], in1=Y[:, V0:SEQC],
                                                                     op0=ALU.mult, op1=ALU.add), reads=[XR, cw, Y], writes=[Y])
                    b.op("act", lambda e: e.activation(out=YB[:, V0:SEQC], in_=Y[:, V0:SEQC], func=AF.Copy), reads=[Y], writes=[YB])
                    for (c0, n) in GROUPS:
                        if c0 == 0:
                            c0, n = V0, 16
                        cs = slice(c0, c0 + n)
                        b.op("pe", lambda e: e.matmul(pA[:, 0:n], lhsT=WA[:, n_, :], rhs=YB[:, cs], start=True, stop=True),
                             reads=[WA, YB], writes=[pA])
                        b.op("pe", lambda e: e.matmul(pX[:, 0:n], lhsT=WX[:, n_, :], rhs=YB[:, cs], start=True, stop=True),
                             reads=[WX, YB], writes=[pX])
                        b.op("act", lambda e: e.activation(out=tr[:, 0:n], in_=pA[:, 0:n], func=AF.Sigmoid, bias=ba[:, n_:n_ + 1]),
                             reads=[pA, ba], writes=[tr])
                        b.op("act", lambda e: e.activation(out=ti_[:, 0:n], in_=pX[:, 0:n], func=AF.Sigmoid, bias=bx[:, n_:n_ + 1]),
                             reads=[pX, bx], writes=[ti_])
                        b.op("act", lambda e: e.activation(out=A[:, cs], in_=tr[:, 0:n], func=AF.Exp, scale=c8[:, n_:n_ + 1]),
                             reads=[tr, c8], writes=[A])
                        b.op("act", lambda e: e.activation(out=ta2[:, 0:n], in_=tr[:, 0:n], func=AF.Exp, scale=c16[:, n_:n_ + 1]),
                             reads=[tr, c16], writes=[ta2])
                        b.op("dve", lambda e: e.tensor_scalar(out=ta2[:, 0:n], in0=ta2[:, 0:n], scalar1=-1.0, scalar2=1.0,
                                                              op0=ALU.mult, op1=ALU.add), reads=[ta2], writes=[ta2])
                        b.op("act", lambda e: e.activation(out=ta2[:, 0:n], in_=ta2[:, 0:n], func=AF.Sqrt), reads=[ta2], writes=[ta2])
                        b.op("dve", lambda e: e.tensor_tensor(out=ti_[:, 0:n], in0=ti_[:, 0:n], in1=Y[:, cs], op=ALU.mult),
                             reads=[ti_, Y], writes=[ti_])
                        b.op("dve", lambda e: e.tensor_tensor(out=U[:, cs], in0=ti_[:, 0:n], in1=ta2[:, 0:n], op=ALU.mult),
                             reads=[ti_, ta2], writes=[U])
                    b.op("dve", lambda e: e.tensor_tensor_scan(out=H[:, V0:SEQC], data0=A[:, V0:SEQC], data1=U[:, V0:SEQC],
                                                               initial=0.0, op0=ALU.mult, op1=ALU.add), reads=[A, U], writes=[H])
                    b.op("dve", lambda e: e.tensor_tensor(out=ZT[:, n_, V0:SEQC], in0=H[:, V0:SEQC], in1=GR[:, V0:SEQC], op=ALU.mult),
                         reads=[H, GR], writes=[ZT])
                for (c0, n) in GROUPS[1:]:
                    cs = slice(c0, c0 + n)
                    for dc in range(8):
                        PO = pO[k % 2]
                        G = gs[k % 2]
                        SG = stage[k % 2]
                        k += 1
                        b.dma("sp", ld, lambda e: e.dma_start(out=G[:, 0:n], in_=S_grnn.h.ap()[dc * 128:(dc + 1) * 128, sc + c0:sc + c0 + n]),
                              reads=[S_grnn], writes=[G])
                        for kc in range(8):
                            b.op("pe", lambda e: e.matmul(PO[:, 0:n], lhsT=WRO[:, kc, dc * 128:(dc + 1) * 128], rhs=ZT[:, kc, cs],
                                                          start=(kc == 0), stop=(kc == 7)), reads=[WRO, ZT], writes=[PO])
                        b.op("dve", lambda e: e.tensor_tensor(out=SG[:, 0:n], in0=PO[:, 0:n], in1=G[:, 0:n], op=ALU.mult),
                             reads=[PO, G], writes=[SG])
                        b.dma("pool", st, lambda e: e.dma_start(out=S_mrnn.h.ap()[dc * 128:(dc + 1) * 128, sc + c0:sc + c0 + n],
                                                                in_=SG[:, 0:n]), reads=[SG], writes=[S_mrnn])
        b.barrier()


    def phase3a():
        with ExitStack() as es:
            sb, ps = mk(es, "p3_")
            WUQ = sb("WUQ", [128, 3, 3072], BF16)
            WUKV = sb("WUKV", [128, 2, 2048], BF16)
            qg = load_small(sb, "qg", qg_d, [128, 3])
            kvg = load_small(sb, "kvg", kvg_d, [128, 2])
            wst = sb("wst", [128, 3072], F32)
            for kc in range(3):
                b.dma("sp", wl, lambda e: e.dma_start(out=wst[:], in_=wuq_d.ap()[kc * 128:(kc + 1) * 128, 0:3072]), writes=[wst])
                b.op("dve", lambda e: e.tensor_scalar(out=WUQ[:, kc, :], in0=wst[:], scalar1=qg[:, kc:kc + 1], scalar2=None,
                                                      op0=ALU.mult), reads=[wst, qg], writes=[WUQ])
            for kc in range(2):
                b.dma("sp", wl, lambda e: e.dma_start(out=wst[:, 0:2048], in_=wukv_d.ap()[kc * 128:(kc + 1) * 128, :]), writes=[wst])
                b.op("dve", lambda e: e.tensor_scalar(out=WUKV[:, kc, :], in0=wst[:, 0:2048], scalar1=kvg[:, kc:kc + 1], scalar2=None,
                                                      op0=ALU.mult), reads=[wst, kvg], writes=[WUKV])
            ident, identf = make_ident(sb)
            ones = sb("ones", [128, 128], F32)
            b.op("pool", lambda e: e.memset(ones[:], 1.0), writes=[ones])
            tri = sb("tri", [128, 128], BF16)
            b.op("pool", lambda e: e.memset(identf[:], 1.0), reads=[identf], writes=[identf])
            b.op("pool", lambda e: e.affine_select(out=identf[:], in_=identf[:], pattern=[[1, 128]], compare_op=ALU.is_ge,
                                                   fill=0.0, base=0, channel_multiplier=-1), reads=[identf], writes=[identf])
            b.op("dve", lambda e: e.tensor_copy(out=tri[:], in_=identf[:]), reads=[identf], writes=[tri])
            Kn = sb("Kn", [128, 8, SEQC], BF16)
            Kr2 = sb("Kr2", [128, SEQC], BF16)
            V = sb("V", [128, 17, 16, 65], BF16)
            b.op("pool", lambda e: e.memset(V[:], 1.0), writes=[V])
            cq_t = sb("cq_t", [128, 3, 512], F32)
            ckv_t = sb("ckv_t", [128, 2, 512], F32)
            sq = sb("sq", [128, 3, 512], F32)
            rstd = sb("rstd", [128, 512], F32)
            cqn = sb("cqn", [128, 3, 512], BF16)
            ckvn = sb("ckvn", [128, 2, 512], BF16)
            kr_t = sb("kr_t", [128, 512], F32)
            krs_t = sb("krs_t", [128, 512], F32)
            cos_t = sb("cos_t", [128, 512], F32)
            sin_t = sb("sin_t", [128, 512], F32)
            tmp1 = sb("tmp1", [128, 512], F32)
            tmp2 = sb("tmp2", [128, 512], F32)
            Qn = sb("Qn", [128, 8, 512], BF16)
            Qr = sb("Qr", [128, 8, 512], BF16)
            PTs = [sb("PTs%d" % i, [128, 512], BF16) for i in range(3)]
            o_tm = sb("o_tm", [128, 4, 1024], BF16)
            oT = sb("oT", [128, 8, 512], BF16)
            rec = sb("rec", [128, 4], F32)
            pn = ps("pn", [128, 512], F32)
            pk = [ps("pk%d" % i, [128, 512], F32) for i in range(2)]
            pS = [ps("pS%d" % i, [128, 512], F32) for i in range(2)]
            pO = [ps("pO%d" % i, [128, 512], F32) for i in range(2)]
            pT = ps("pT", [128, 1024], BF16)
            scale = float(96 ** -0.5)
            ki = [0]

            def nextpk():
                ki[0] += 1
                return pk[ki[0] % 2]

            def rmsn(src, nch, dst, nfeat, n):
                b.op("act", lambda e: e.activation(out=sq[:, 0:nch, 0:n], in_=src[:, :, 0:n], func=AF.Square), reads=[src], writes=[sq])
                for c in range(nch):
                    b.op("pe", lambda e: e.matmul(pn[:, 0:n], lhsT=ones[:], rhs=sq[:, c, 0:n], start=(c == 0), stop=(c == nch - 1)),
                         reads=[ones, sq], writes=[pn])
                b.op("dve", lambda e: e.tensor_scalar(out=rstd[:, 0:n], in0=pn[:, 0:n], scalar1=1.0 / nfeat, scalar2=EPS,
                                                      op0=ALU.mult, op1=ALU.add), reads=[pn], writes=[rstd])
                b.op("act", lambda e: e.activation(out=rstd[:, 0:n], in_=rstd[:, 0:n], func=AF.Sqrt), reads=[rstd], writes=[rstd])
                b.op("dve", lambda e: e.reciprocal(out=rstd[:, 0:n], in_=rstd[:, 0:n]), reads=[rstd], writes=[rstd])
                for c in range(nch):
                    b.op("dve", lambda e: e.tensor_tensor(out=dst[:, c, 0:n], in0=src[:, c, 0:n], in1=rstd[:, 0:n], op=ALU.mult),
                         reads=[src, rstd], writes=[dst])

            si = 0
            for s in range(NSEQ):
                sc = s * SEQC
                for (c0, n) in GROUPS:
                    col = sc + c0
                    b.dma("sp", ld, lambda e: e.dma_start(out=cq_t[:, :, 0:n],
                                                          in_=S_cq.h.ap()[:, col:col + n].rearrange("(c p) n -> p c n", p=128)),
                          reads=[S_cq], writes=[cq_t])
                    b.dma("sp", ld, lambda e: e.dma_start(out=ckv_t[:, :, 0:n],
                                                          in_=S_ckv.h.ap()[:, col:col + n].rearrange("(c p) n -> p c n", p=128)),
                          reads=[S_ckv], writes=[ckv_t])
                    b.dma("sp", ld, lambda e: e.dma_start(out=kr_t[:, 0:n], in_=S_kr.h.ap()[:, col:col + n]), reads=[S_kr], writes=[kr_t])
                    b.dma("sp", ld, lambda e: e.dma_start(out=krs_t[:, 0:n], in_=S_krs.h.ap()[:, col:col + n]), reads=[S_krs], writes=[krs_t])
                    b.dma("sp", ld, lambda e: e.dma_start(out=cos_t[:, 0:n], in_=cos_d.ap()[:, c0:c0 + n]), writes=[cos_t])
                    b.dma("sp", ld, lambda e: e.dma_start(out=sin_t[:, 0:n], in_=sin_d.ap()[:, c0:c0 + n]), writes=[sin_t])
                    rmsn(cq_t, 3, cqn, 384.0, n)
                    rmsn(ckv_t, 2, ckvn, 256.0, n)
                    for j in range(8):
                        P_ = nextpk()
                        for kc in range(2):
                            b.op("pe", lambda e: e.matmul(P_[:, 0:n], lhsT=WUKV[:, kc, j * 128:(j + 1) * 128], rhs=ckvn[:, kc, 0:n],
                                                          start=(kc == 0), stop=(kc == 1)), reads=[WUKV, ckvn], writes=[P_])
                        b.op("act", lambda e: e.activation(out=Kn[:, j, c0:c0 + n], in_=P_[:, 0:n], func=AF.Copy), reads=[P_], writes=[Kn])
                    b.op("dve", lambda e: e.tensor_tensor(out=tmp1[:, 0:n], in0=kr_t[:, 0:n], in1=cos_t[:, 0:n], op=ALU.mult),
                         reads=[kr_t, cos_t], writes=[tmp1])
                    b.op("dve", lambda e: e.tensor_tensor(out=tmp2[:, 0:n], in0=krs_t[:, 0:n], in1=sin_t[:, 0:n], op=ALU.mult),
                         reads=[krs_t, sin_t], writes=[tmp2])
                    b.op("dve", lambda e: e.tensor_tensor(out=Kr2[:, c0:c0 + n], in0=tmp1[:, 0:n], in1=tmp2[:, 0:n], op=ALU.add),
                         reads=[tmp1, tmp2], writes=[Kr2])
                    for jt in range(n // 128):
                        t = c0 // 128 + jt
                        if t == 0:
                            lo, M = 112, 16
                        else:
                            lo, M = jt * 128, 128
                        for half in range(2):
                            P_ = nextpk()
                            for kc in range(2):
                                b.op("pe", lambda e: e.matmul(P_[0:M, :], lhsT=ckvn[:, kc, lo:lo + M],
                                                              rhs=WUKV[:, kc, 1024 + half * 512:1024 + (half + 1) * 512],
                                                              start=(kc == 0), stop=(kc == 1)), reads=[WUKV, ckvn], writes=[P_])
                            b.op("act", lambda e: e.activation(out=V[0:M, t, half * 8:(half + 1) * 8, 0:64],
                                                               in_=P_[0:M, :].rearrange("p (h d) -> p h d", h=8), func=AF.Copy),
                                 reads=[P_], writes=[V])
                    if c0 == 0:
                        continue
                    for j in range(8):
                        P_ = nextpk()
                        for kc in range(3):
                            b.op("pe", lambda e: e.matmul(P_[:, 0:n], lhsT=WUQ[:, kc, j * 128:(j + 1) * 128], rhs=cqn[:, kc, 0:n],
                                                          start=(kc == 0), stop=(kc == 2)), reads=[WUQ, cqn], writes=[P_])
                        b.op("act", lambda e: e.activation(out=Qn[:, j, 0:n], in_=P_[:, 0:n], func=AF.Copy), reads=[P_], writes=[Qn])
                    for j in range(8):
                        P1 = nextpk()
                        for kc in range(3):
                            b.op("pe", lambda e: e.matmul(P1[:, 0:n], lhsT=WUQ[:, kc, 1024 + j * 128:1024 + (j + 1) * 128], rhs=cqn[:, kc, 0:n],
                                                          start=(kc == 0), stop=(kc == 2)), reads=[WUQ, cqn], writes=[P1])
                        b.op("dve", lambda e: e.tensor_tensor(out=tmp1[:, 0:n], in0=P1[:, 0:n], in1=cos_t[:, 0:n], op=ALU.mult),
                             reads=[P1, cos_t], writes=[tmp1])
                        P2 = nextpk()
                        for kc in range(3):
                            b.op("pe", lambda e: e.matmul(P2[:, 0:n], lhsT=WUQ[:, kc, 2048 + j * 128:2048 + (j + 1) * 128], rhs=cqn[:, kc, 0:n],
                                                          start=(kc == 0), stop=(kc == 2)), reads=[WUQ, cqn], writes=[P2])
                        b.op("dve", lambda e: e.tensor_tensor(out=tmp2[:, 0:n], in0=P2[:, 0:n], in1=sin_t[:, 0:n], op=ALU.mult),
                             reads=[P2, sin_t], writes=[tmp2])
                        b.op("dve", lambda e: e.tensor_tensor(out=Qr[:, j, 0:n], in0=tmp1[:, 0:n], in1=tmp2[:, 0:n], op=ALU.add),
                             reads=[tmp1, tmp2], writes=[Qr])
                    g0t = c0 // 128
                    for h in range(16):
                        j = h // 2
                        p0 = (h % 2) * 64
                        PO = pO[h % 2]
                        POv = PO[:, 0:260].rearrange("p (q d) -> p q d", q=4)
                        for kt in range(0, g0t + 4):
                            if kt == 0:
                                k0, M = 112, 16
                            else:
                                k0, M = kt * 128, 128
                            qlo = max(0, kt - g0t)
                            q0 = qlo * 128
                            PS_ = pS[si % 2]
                            PTb = PTs[si % 3]
                            si += 1
                            b.op("pe", lambda e: e.matmul(PS_[0:M, q0:512], lhsT=Kn[p0:p0 + 64, j, k0:k0 + M], rhs=Qn[p0:p0 + 64, j, q0:512],
                                                          start=True, stop=False), reads=[Kn, Qn], writes=[PS_])
                            b.op("pe", lambda e: e.matmul(PS_[0:M, q0:512], lhsT=Kr2[p0:p0 + 32, k0:k0 + M], rhs=Qr[p0:p0 + 32, j, q0:512],
                                                          start=False, stop=True), reads=[Kr2, Qr], writes=[PS_])
                            b.op("act", lambda e: e.activation(out=PTb[0:M, q0:512], in_=PS_[0:M, q0:512], func=AF.Exp, scale=scale),
                                 reads=[PS_], writes=[PTb])
                            if kt >= g0t:
                                b.op("dve", lambda e: e.tensor_tensor(out=PTb[:, q0:q0 + 128], in0=PTb[:, q0:q0 + 128], in1=tri[:], op=ALU.mult),
                                     reads=[PTb, tri], writes=[PTb])
                            for qt in range(qlo, 4):
                                b.op("pe", lambda e: e.matmul(POv[:, qt, :], lhsT=PTb[0:M, qt * 128:(qt + 1) * 128], rhs=V[0:M, kt, h, :],
                                                              start=(kt == 0 and qt == 0), stop=(kt == g0t + qt), skip_group_check=True),
                                     reads=[PTb, V], writes=[PO])
                        b.op("dve", lambda e: e.reciprocal(out=rec[:], in_=POv[:, :, 64]), reads=[PO], writes=[rec])
                        for qt in range(4):
                            b.op("dve", lambda e: e.tensor_scalar(out=o_tm[:, qt, h * 64:(h + 1) * 64], in0=POv[:, qt, 0:64],
                                                                  scalar1=rec[:, qt:qt + 1], scalar2=None, op0=ALU.mult),
                                 reads=[PO, rec], writes=[o_tm])
                    for qt in range(4):
                        for c in range(8):
                            b.op("pe", lambda e: e.transpose(out=pT[:, c * 128:(c + 1) * 128], in_=o_tm[:, qt, c * 128:(c + 1) * 128],
                                                             identity=ident[:]), reads=[o_tm, ident], writes=[pT])
                        b.op("dve", lambda e: e.tensor_copy(out=oT[:, :, qt * 128:(qt + 1) * 128],
                                                            in_=pT[:].rearrange("p (c t) -> p c t", c=8)), reads=[pT], writes=[oT])
                    b.dma("pool", st, lambda e: e.dma_start(out=S_oT.h.ap()[:, col:col + n].rearrange("(c p) n -> p c n", p=128), in_=oT[:]),
                          reads=[oT], writes=[S_oT])
        b.barrier()

    def phase3b():
        with ExitStack() as es:
            sb, ps = mk(es, "p3b_")
            WAO = sb("WAO", [128, 8, 1024], BF16)
            WO = sb("WO", [128, 8, 1024], BF16)
            load_w_cast(WAO, WAO[:], wao_d.ap().rearrange("(k p) f -> p k f", p=128))
            load_w_cast(WO, WO[:], wo_d.ap().rearrange("(k p) f -> p k f", p=128))
            oT = [sb("oT%d" % i, [128, 8, 512], BF16) for i in range(2)]
            ga = [sb("ga%d" % i, [128, 512], F32) for i in range(2)]
            mr = [sb("mr%d" % i, [128, 512], F32) for i in range(2)]
            tmp = sb("tmp", [128, 512], F32)
            mixT = sb("mixT", [128, 8, 512], BF16)
            xt = [sb("xt%d" % i, [128, 1024], F32) for i in range(2)]
            h2 = [sb("h2%d" % i, [128, 1024], F32) for i in range(2)]
            pk = [ps("pk%d" % i, [128, 512], F32) for i in range(4)]
            gi = 0
            k = 0
            ti = 0
            for s in range(NSEQ):
                sc = s * SEQC
                for g, (c0, n) in enumerate(GROUPS):
                    if g == 0:
                        continue
                    col = sc + c0
                    OT = oT[gi % 2]
                    gi += 1
                    b.dma("sp", ld, lambda e: e.dma_start(out=OT[:], in_=S_oT.h.ap()[:, col:col + n].rearrange("(c p) n -> p c n", p=128)),
                          reads=[S_oT], writes=[OT])
                    for dc in range(8):
                        GA = ga[k % 2]
                        MR = mr[k % 2]
                        PK = pk[k % 4]
                        k += 1
                        b.dma("sp", ld, lambda e: e.dma_start(out=GA[:], in_=S_gatt.h.ap()[dc * 128:(dc + 1) * 128, col:col + n]),
                              reads=[S_gatt], writes=[GA])
                        b.dma("sp", ld, lambda e: e.dma_start(out=MR[:], in_=S_mrnn.h.ap()[dc * 128:(dc + 1) * 128, col:col + n]),
                              reads=[S_mrnn], writes=[MR])
                        for kc in range(8):
                            b.op("pe", lambda e: e.matmul(PK[:], lhsT=WAO[:, kc, dc * 128:(dc + 1) * 128], rhs=OT[:, kc, :],
                                                          start=(kc == 0), stop=(kc == 7)), reads=[WAO, OT], writes=[PK])
                        b.op("dve", lambda e: e.tensor_tensor(out=tmp[:], in0=PK[:], in1=GA[:], op=ALU.mult), reads=[PK, GA], writes=[tmp])
                        b.op("dve", lambda e: e.tensor_tensor(out=mixT[:, dc, :], in0=tmp[:], in1=MR[:], op=ALU.add),
                             reads=[tmp, MR], writes=[mixT])
                    for qt in range(4):
                        X = xt[ti % 2]
                        H2 = h2[ti % 2]
                        ti += 1
                        r0 = (g - 1) * 512 + qt * 128
                        b.dma("sp", ld, lambda e: e.dma_start(out=X[:], in_=x_d.ap()[s, r0:r0 + 128, :]), writes=[X])
                        for half in range(2):
                            PK = pk[k % 4]
                            k += 1
                            for kc in range(8):
                                b.op("pe", lambda e: e.matmul(PK[:], lhsT=mixT[:, kc, qt * 128:(qt + 1) * 128],
                                                              rhs=WO[:, kc, half * 512:(half + 1) * 512],
                                                              start=(kc == 0), stop=(kc == 7)), reads=[WO, mixT], writes=[PK])
                            b.op("dve", lambda e: e.tensor_tensor(out=H2[:, half * 512:(half + 1) * 512], in0=PK[:],
                                                                  in1=X[:, half * 512:(half + 1) * 512], op=ALU.add),
                                 reads=[PK, X], writes=[H2])
                        b.dma("pool", st, lambda e: e.dma_start(out=S_h2.h.ap()[s * NREAL + r0:s * NREAL + r0 + 128, :], in_=H2[:]),
                              reads=[H2], writes=[S_h2])
        b.barrier()


    def phase5():
        with ExitStack() as es:
            sb, ps = mk(es, "p5_")
            WQ = sb("WQ", [128, 8, 2048], BF16)
            K1T = sb("K1T", [128, 128], BF16)
            K2T = sb("K2T", [128, 128], BF16)
            load_w_cast(WQ, WQ[:], wq_d.ap().rearrange("(k p) f -> p k f", p=128))
            load_w_cast(K1T, K1T[:], k1T_d.ap())
            load_w_cast(K2T, K2T[:], k2T_d.ap())
            g2b = sb("g2b", [128, 1024], F32)
            gfb = sb("gfb", [128, 1024], F32)
            b.dma("sp", None, lambda e: e.dma_start(out=g2b[:], in_=g2_d.ap().to_broadcast([128, 1024])), writes=[g2b])
            b.dma("sp", None, lambda e: e.dma_start(out=gfb[:], in_=gf_d.ap().to_broadcast([128, 1024])), writes=[gfb])
            ident, identf = make_ident(sb)
            iota_i = sb("iota_i", [128, 16], I32)
            iota16 = sb("iota16", [128, 16], F32)
            b.op("pool", lambda e: e.iota(iota_i[:], pattern=[[1, 16]], base=0, channel_multiplier=0), writes=[iota_i])
            b.op("dve", lambda e: e.tensor_copy(out=iota16[:], in_=iota_i[:]), reads=[iota_i], writes=[iota16])
            L = sb("L", [128, 128, 128], BF16)
            b.op("pool", lambda e: e.memset(L[:], 0.0), writes=[L])
            Lflat = L[:].rearrange("p a b -> p (a b)")
            Xs = [sb("X%d" % i, [128, 1024], F32) for i in range(2)]
            xnbs = [sb("xnb%d" % i, [128, 1024], BF16) for i in range(2)]
            GTs = [sb("GT%d" % i, [128, 128], F32) for i in range(2)]
            IDXTs = [sb("IDXT%d" % i, [128, 128], U32) for i in range(2)]
            Gs = [sb("G%d" % i, [128, 8, 16], F32) for i in range(2)]
            ssA = sb("ssA", [128, 1], F32)
            ssC = sb("ssC", [128, 1], F32)
            junkA = sb("junkA", [128, 1024], BF16)
            junkD = sb("junkD", [128, 1024], BF16)
            xT = sb("xT", [128, 8, 128], BF16)
            qT = sb("qT", [128, 16, 128], BF16)
            S = sb("S", [128, 16, 128], F32)
            eqv = S[:].rearrange("p (h x) (y a) -> p h (x y) a", x=2, a=16)
            T16 = sb("T16", [128, 16, 16], F32)
            I16 = sb("I16", [128, 16, 16], U32)
            I16f = sb("I16f", [128, 16, 16], F32)
            cand = sb("cand", [128, 8, 256], F32)
            TS = sb("TS", [128, 8, 16], F32)
            CI = sb("CI", [128, 8, 16], U32)
            CIa = sb("CIa", [128, 8, 16], U32)
            CIb = sb("CIb", [128, 8, 16], U32)
            Af = sb("Af", [128, 8, 16], F32)
            Bf = sb("Bf", [128, 8, 16], F32)
            i1s = sb("i1s", [128, 128], F32)
            i2s = sb("i2s", [128, 128], F32)
            i1b = sb("i1b", [128, 128], BF16)
            i2b = sb("i2b", [128, 128], BF16)
            idxf = sb("idxf", [128, 128], F32)
            idxTf = sb("idxTf", [128, 128], F32)
            iTf = sb("iTf", [128, 2, 128], F32)
            E = sb("E", [128, 8, 16], F32)
            Z = sb("Z", [128, 8], F32)
            ACTV = sb("ACTV", [128, 128], F32)
            coefb = sb("coefb", [128, 128], BF16)
            coefT = sb("coefT", [128, 128], BF16)
            UVG = [sb("UVG%d" % i, [128, 8, 2048], BF16) for i in range(2)]
            UVGt = [[T(u.h, "UVG%d_%d" % (i_, q_)) for q_ in range(8)] for i_, u in enumerate(UVG)]
            actT = sb("actT", [128, 128], F32)
            Ls = [T(L.h, "L%d" % i) for i in range(16)]
            for lt in Ls:
                lt.w = dict(L.w)
            h3 = sb("h3", [128, 1024], F32)
            pTa = ps("pTa", [128, 1024], BF16)
            pq = [ps("pq%d" % i, [128, 512], F32) for i in range(1)]
            pOut = [ps("pOut%d" % i, [128, 512], F32) for i in range(2)]
            pX = [ps("pX%d" % i, [128, 1024], F32) for i in range(2)]
            T16v = T16[:].rearrange("p (h two) a -> p h two a", two=2)
            I16fv = I16f[:].rearrange("p (h two) a -> p h two a", two=2)
            B4 = [128, 8, 16, 16]
            cnt = {"u": 0, "v": 0, "q": 0}
            ntiles = NR // 128 if p5tiles is None else p5tiles

            def sweep(eng, fns, reads, writes):
                for k_, f in enumerate(fns):
                    w = writes if (k_ == 0 or k_ == len(fns) - 1) else ()
                    b.op(eng, f, reads=reads, writes=w)

            def top16(vals, ng, tv, iv):
                sweep("dve", [(lambda e, g=g: e.max(out=tv[:, g, 0:8], in_=vals[:, g, :])) for g in range(ng)], [vals], [tv])
                sweep("dve", [(lambda e, g=g: e.max_index(out=iv[:, g, 0:8], in_max=tv[:, g, 0:8], in_values=vals[:, g, :])) for g in range(ng)],
                      [vals, tv], [iv])
                yield
                sweep("dve", [(lambda e, g=g: e.match_replace(out=vals[:, g, :], in_to_replace=tv[:, g, 0:8], in_values=vals[:, g, :],
                                                              imm_value=-1e30)) for g in range(ng)], [tv], [vals])
                yield
                sweep("dve", [(lambda e, g=g: e.max(out=tv[:, g, 8:16], in_=vals[:, g, :])) for g in range(ng)], [vals], [tv])
                sweep("dve", [(lambda e, g=g: e.max_index(out=iv[:, g, 8:16], in_max=tv[:, g, 8:16], in_values=vals[:, g, :])) for g in range(ng)],
                      [vals, tv], [iv])
                yield

            def stageA(i):
                par = i % 2
                X, xnb, GT, IDXT, G = Xs[par], xnbs[par], GTs[par], IDXTs[par], Gs[par]
                b.dma("sp", None, lambda e: e.dma_start(out=X[:], in_=S_h2.h.ap()[i * 128:(i + 1) * 128, :]), reads=[S_h2], writes=[X])
                b.op("act", lambda e: e.activation(out=junkA[:], in_=X[:], func=AF.Square, accum_out=ssA[:]), reads=[X], writes=[junkA, ssA])
                b.op("dve", lambda e: e.tensor_scalar(out=ssA[:], in0=ssA[:], scalar1=1.0 / 1024, scalar2=EPS, op0=ALU.mult, op1=ALU.add),
                     reads=[ssA], writes=[ssA])
                b.op("act", lambda e: e.activation(out=ssA[:], in_=ssA[:], func=AF.Sqrt), reads=[ssA], writes=[ssA])
                b.op("dve", lambda e: e.reciprocal(out=ssA[:], in_=ssA[:]), reads=[ssA], writes=[ssA])
                b.op("dve", lambda e: e.scalar_tensor_tensor(out=xnb[:], in0=X[:], scalar=ssA[:, 0:1], in1=g2b[:], op0=ALU.mult, op1=ALU.mult),
                     reads=[X, ssA, g2b], writes=[xnb])
                yield
                for c in range(8):
                    b.op("pe", lambda e: e.transpose(out=pTa[:, c * 128:(c + 1) * 128], in_=xnb[:, c * 128:(c + 1) * 128], identity=ident[:]),
                         reads=[xnb, ident], writes=[pTa])
                b.op("act", lambda e: e.activation(out=xT[:], in_=pTa[:].rearrange("p (c t) -> p c t", c=8), func=AF.Copy), reads=[pTa], writes=[xT])
                yield
                for bq in range(4):
                    PQ = pq[0]
                    for j in range(4):
                        hh = bq * 4 + j
                        for kc in range(8):
                            b.op("pe", lambda e: e.matmul(PQ[:, j * 128:(j + 1) * 128], lhsT=WQ[:, kc, hh * 128:(hh + 1) * 128], rhs=xT[:, kc, :],
                                                          start=(kc == 0), stop=(kc == 7), skip_group_check=True), reads=[WQ, xT], writes=[PQ])
                    b.op("act", lambda e: e.activation(out=qT[:, bq * 4:(bq + 1) * 4, :], in_=PQ[:].rearrange("p (j t) -> p j t", j=4), func=AF.Copy),
                         reads=[PQ], writes=[qT])
                    yield
                for bq in range(4):
                    PQ = pq[0]
                    for j in range(4):
                        hh = bq * 4 + j
                        KT = K1T if hh % 2 == 0 else K2T
                        b.op("pe", lambda e: e.matmul(PQ[:, j * 128:(j + 1) * 128], lhsT=qT[:, hh, :], rhs=KT[:], start=True, stop=True,
                                                      skip_group_check=True), reads=[qT, KT], writes=[PQ])
                    b.op("act", lambda e: e.activation(out=S[:, bq * 4:(bq + 1) * 4, :], in_=PQ[:].rearrange("p (j t) -> p j t", j=4), func=AF.Copy),
                         reads=[PQ], writes=[S])
                    yield
                yield from top16(S, 16, T16, I16)
                b.op("dve", lambda e: e.tensor_copy(out=I16f[:], in_=I16[:]), reads=[I16], writes=[I16f])
                b.op("dve", lambda e: e.tensor_tensor(out=cand[:].rearrange("p h (a c) -> p h a c", a=16),
                                                      in0=T16v[:, :, 0, :].unsqueeze(3).to_broadcast(B4),
                                                      in1=T16v[:, :, 1, :].unsqueeze(2).to_broadcast(B4), op=ALU.add),
                     reads=[T16], writes=[cand])
                yield
                yield from top16(cand, 8, TS, CI)
                b.op("dve", lambda e: e.tensor_single_scalar(out=CIa[:], in_=CI[:], scalar=4, op=ALU.logical_shift_right), reads=[CI], writes=[CIa])
                b.op("dve", lambda e: e.tensor_single_scalar(out=CIb[:], in_=CI[:], scalar=15, op=ALU.bitwise_and), reads=[CI], writes=[CIb])
                b.op("dve", lambda e: e.tensor_copy(out=Af[:], in_=CIa[:]), reads=[CIa], writes=[Af])
                b.op("dve", lambda e: e.tensor_copy(out=Bf[:], in_=CIb[:]), reads=[CIb], writes=[Bf])
                yield
                for (SEL, half, dst) in ((Af, 0, i1s), (Bf, 1, i2s)):
                    b.op("dve", lambda e: e.tensor_tensor(out=eqv, in0=SEL[:].unsqueeze(3).to_broadcast(B4),
                                                          in1=iota16[:].unsqueeze(1).unsqueeze(1).to_broadcast(B4), op=ALU.is_equal),
                         reads=[SEL, iota16], writes=[S])
                    b.op("dve", lambda e: e.tensor_tensor(out=eqv, in0=eqv, in1=I16fv[:, :, half, :].unsqueeze(2).to_broadcast(B4), op=ALU.mult),
                         reads=[S, I16f], writes=[S])
                    b.op("dve", lambda e: e.tensor_reduce(out=dst[:].rearrange("p (h k) -> p h k", h=8), in_=eqv, axis=AX.X, op=ALU.add),
                         reads=[S], writes=[dst])
                    yield
                b.op("act", lambda e: e.activation(out=i1b[:], in_=i1s[:], func=AF.Copy), reads=[i1s], writes=[i1b])
                b.op("act", lambda e: e.activation(out=i2b[:], in_=i2s[:], func=AF.Copy), reads=[i2s], writes=[i2b])
                b.op("pe", lambda e: e.transpose(out=pTa[:, 0:128], in_=i1b[:], identity=ident[:]), reads=[i1b, ident], writes=[pTa])
                b.op("pe", lambda e: e.transpose(out=pTa[:, 128:256], in_=i2b[:], identity=ident[:]), reads=[i2b, ident], writes=[pTa])
                b.op("act", lambda e: e.activation(out=iTf[:], in_=pTa[:, 0:256].rearrange("p (a t) -> p a t", a=2), func=AF.Copy),
                     reads=[pTa], writes=[iTf])
                yield
                b.op("dve", lambda e: e.scalar_tensor_tensor(out=idxTf[:], in0=iTf[:, 0, :], scalar=128.0, in1=iTf[:, 1, :], op0=ALU.mult, op1=ALU.add),
                     reads=[iTf], writes=[idxTf])
                b.op("dve", lambda e: e.tensor_copy(out=IDXT[:], in_=idxTf[:]), reads=[idxTf], writes=[IDXT])
                b.op("dve", lambda e: e.tensor_tensor(out=E[:], in0=TS[:], in1=TS[:, :, 0:1].to_broadcast([128, 8, 16]), op=ALU.subtract),
                     reads=[TS], writes=[E])
                b.op("act", lambda e: e.activation(out=E[:], in_=E[:], func=AF.Exp), reads=[E], writes=[E])
                b.op("dve", lambda e: e.tensor_reduce(out=Z[:], in_=E[:], axis=AX.X, op=ALU.add), reads=[E], writes=[Z])
                b.op("dve", lambda e: e.reciprocal(out=Z[:], in_=Z[:]), reads=[Z], writes=[Z])
                b.op("dve", lambda e: e.tensor_tensor(out=G[:], in0=E[:], in1=Z[:].unsqueeze(2).to_broadcast([128, 8, 16]), op=ALU.mult),
                     reads=[E, Z], writes=[G])
                b.op("pe", lambda e: e.transpose(out=pq[0][:, 0:128], in_=G[:].rearrange("p h k -> p (h k)"), identity=identf[:]), reads=[G, identf], writes=[pq[0]])
                b.op("act", lambda e: e.activation(out=GT[:], in_=pq[0][:, 0:128], func=AF.Copy), reads=[pq[0]], writes=[GT])
                yield

            def step(gen, k=1):
                if gen is None:
                    return
                for _ in range(k):
                    try:
                        next(gen)
                    except StopIteration:
                        return

            g0 = stageA(0)
            step(g0, 1000)
            for i in range(ntiles):
                par = i % 2
                X, xnb, GT, IDXT = Xs[par], xnbs[par], GTs[par], IDXTs[par]
                s, r0 = divmod(i * 128, NREAL)
                gN = stageA(i + 1) if i + 1 < ntiles else None
                pend = [None]

                def finish(tb_, UV_, UVt_, vlater=False):
                    Lt = Ls[tb_]
                    tsl = slice(tb_ * 8, tb_ * 8 + 8)
                    b.op("act", lambda e: e.activation(out=actT[:, tsl], in_=actT[:, tsl], func=AF.Gelu_apprx_tanh), reads=[actT], writes=[actT])
                    b.op("dve", lambda e: e.tensor_tensor(out=coefT[:, tsl], in0=actT[:, tsl], in1=GT[:, tsl], op=ALU.mult),
                         reads=[actT, GT], writes=[coefT])
                    b.op("dve", lambda e: e.tensor_copy(out=Lflat[:, tb_ * 8 * 129:tb_ * 8 * 129 + 7 * 129 + 1:129], in_=coefT[:, tsl]),
                         reads=[coefT], writes=[Lt])
                    if not vlater:
                        vmm(tb_, UV_, UVt_, range(8))

                def vmm(tb_, UV_, UVt_, qs):
                    Lt = Ls[tb_]
                    for q in qs:
                        t = tb_ * 8 + q
                        for half in range(2):
                            b.op("pe", lambda e: e.matmul(pOut[half][:], lhsT=L[:, t, :], rhs=UV_[:, q, 1024 + half * 512:1024 + (half + 1) * 512],
                                                          start=(t == 0), stop=(t == 127)), reads=[Lt, UVt_[q]], writes=[pOut[half]])

                VSPREAD = {2: (0, 1), 3: (2,), 4: (3,), 5: (4, 5), 6: (6,), 7: (7,)}

                for tb in range(16):
                    UV = UVG[cnt["u"] % 2]
                    UVt = UVGt[cnt["u"] % 2]
                    cnt["u"] += 1
                    for q in range(8):
                        t = tb * 8 + q
                        b.dma("pool", None, lambda e: e.indirect_dma_start(out=UV[:, q, :], out_offset=None, in_=UV16.h.ap(),
                                                                           in_offset=bass.IndirectOffsetOnAxis(ap=IDXT[:, t:t + 1], axis=0)),
                              reads=[IDXT, UV16], writes=[UVt[q]])
                    for q in range(8):
                        t = tb * 8 + q
                        PX = pX[cnt["v"] % 2]
                        cnt["v"] += 1
                        for half in range(2):
                            b.op("pe", lambda e: e.matmul(PX[:, half * 512:(half + 1) * 512], lhsT=ident[:, t:t + 1].to_broadcast([128, 128]),
                                                          rhs=xnb[:, half * 512:(half + 1) * 512], start=True, stop=True, skip_group_check=True),
                                 reads=[ident, xnb], writes=[PX])
                        b.op("dve", lambda e: e.scalar_tensor_tensor(out=junkD[:], in0=UV[:, q, 0:1024], scalar=1.0, in1=PX[:], op0=ALU.mult,
                                                                     op1=ALU.mult, accum_out=actT[:, t:t + 1]),
                             reads=[UVt[q], PX], writes=[actT] if q in (0, 7) else ())
                        if pend[0] is not None:
                            if q == 1:
                                finish(*pend[0], vlater=True)
                            elif q >= 2:
                                vmm(*pend[0], VSPREAD[q])
                                if q == 7:
                                    pend[0] = None
                    pend[0] = (tb, UV, UVt)
                    step(gN, 2)
                finish(*pend[0])
                for half in range(2):
                    b.op("dve", lambda e: e.tensor_tensor(out=h3[:, half * 512:(half + 1) * 512], in0=pOut[half][:],
                                                          in1=X[:, half * 512:(half + 1) * 512], op=ALU.add), reads=[pOut[half], X], writes=[h3])
                b.op("act", lambda e: e.activation(out=junkA[:], in_=h3[:], func=AF.Square, accum_out=ssC[:]), reads=[h3], writes=[junkA, ssC])
                b.op("dve", lambda e: e.tensor_scalar(out=ssC[:], in0=ssC[:], scalar1=1.0 / 1024, scalar2=EPS, op0=ALU.mult, op1=ALU.add),
                     reads=[ssC], writes=[ssC])
                b.op("act", lambda e: e.activation(out=ssC[:], in_=ssC[:], func=AF.Sqrt), reads=[ssC], writes=[ssC])
                b.op("dve", lambda e: e.reciprocal(out=ssC[:], in_=ssC[:]), reads=[ssC], writes=[ssC])
                b.op("dve", lambda e: e.scalar_tensor_tensor(out=h3[:], in0=h3[:], scalar=ssC[:, 0:1], in1=gfb[:], op0=ALU.mult, op1=ALU.mult),
                     reads=[h3, ssC, gfb], writes=[h3])
                b.dma("sp", None, lambda e: e.dma_start(out=out_d.ap()[s, r0:r0 + 128, :], in_=h3[:]), reads=[h3], writes=[OUT])
                step(gN, 1000)
        b.barrier()

    progs = {1: phase1, 2: phase2, 3: phase3a, 4: phase3b, 5: phase5}
    for p in phases:
        if p in progs:
            progs[p]()
    b.barrier()
    return nc


def _pc(v, nchunk):
    return np.ascontiguousarray(np.asarray(v, np.float32).reshape(nchunk, 128).T)


def prep_common(inp):
    f = lambda a: np.asarray(a, np.float32)
    w_in = f(inp["w_in"])[0]
    z32 = np.zeros((1024, 32), np.float32)
    kr = w_in[:, 2688:2720]
    krs = np.concatenate([kr[:, 16:], kr[:, :16]], axis=1)
    w_in_r = np.concatenate([
        w_in[:, 0:1024], w_in[:, 1024:2048], w_in[:, 2048:2432], w_in[:, 2432:2688],
        kr, z32, kr, z32, krs, z32, krs, z32,
        w_in[:, 2720:3744], w_in[:, 3744:4768]], axis=1)
    assert w_in_r.shape == (1024, 4992)
    conv_w = f(inp["conv_w"])[0]
    cw = np.ascontiguousarray(conv_w.reshape(4, 8, 128).transpose(2, 1, 0))
    w_uq = f(inp["w_uq"])[0].reshape(384, 16, 96)
    nope = w_uq[:, :, :64].reshape(384, 1024)
    rope = w_uq[:, :, 64:]
    ropes = np.concatenate([rope[:, :, 16:], rope[:, :, :16]], axis=2)
    z = np.zeros((384, 16, 32), np.float32)
    rope_p = np.concatenate([rope, z], axis=2).reshape(384, 1024)
    ropes_p = np.concatenate([ropes, z], axis=2).reshape(384, 1024)
    w_uq_r = np.concatenate([nope, rope_p, ropes_p, np.zeros((384, 1024), np.float32)], axis=1)
    w_ukv = f(inp["w_ukv"])[0].reshape(256, 16, 128)
    w_ukv_r = np.concatenate([w_ukv[:, :, :64].reshape(256, 1024), w_ukv[:, :, 64:].reshape(256, 1024)], axis=1)
    pos = (np.arange(SEQC, dtype=np.float32) - 112.0).astype(np.float32)
    inv = np.power(np.float32(10000.0), -np.arange(16, dtype=np.float32) * np.float32(2.0 / 32)).astype(np.float32)
    ang = (pos[None, :] * inv[:, None]).astype(np.float32)
    c, s_ = np.cos(ang).astype(np.float32), np.sin(ang).astype(np.float32)
    cos32 = np.concatenate([c, c], axis=0)
    sin32 = np.concatenate([-s_, s_], axis=0)
    zz = np.zeros((32, SEQC), np.float32)
    cos2 = np.concatenate([cos32, zz, cos32, zz], axis=0)
    sin2s = np.concatenate([sin32, zz, sin32, zz], axis=0)
    return {
        "meta": f(inp["meta_tokens"]),
        "g1": _pc(inp["norm1_g"][0], 8),
        "w_in": np.ascontiguousarray(w_in_r),
        "convw": cw,
        "convb": _pc(inp["conv_b"][0], 8),
        "rg_wa": np.ascontiguousarray(f(inp["rg_wa"])[0].transpose(1, 0, 2)),
        "rg_wx": np.ascontiguousarray(f(inp["rg_wx"])[0].transpose(1, 0, 2)),
        "rg_ba": _pc(inp["rg_ba"][0], 8),
        "rg_bx": _pc(inp["rg_bx"][0], 8),
        "rg_lam": _pc(inp["rg_lambda"][0], 8),
        "w_rnn_out": f(inp["w_rnn_out"])[0],
        "qg": _pc(inp["q_norm_g"][0], 3),
        "kvg": _pc(inp["kv_norm_g"][0], 2),
        "w_uq": np.ascontiguousarray(w_uq_r),
        "w_ukv": np.ascontiguousarray(w_ukv_r),
        "w_attn_out": f(inp["w_attn_out"])[0],
        "w_out": f(inp["w_out"])[0],
        "g2": f(inp["norm2_g"]).reshape(1, 1024),
        "peer_wq": f(inp["peer_wq"])[0],
        "keys1T": np.ascontiguousarray(f(inp["peer_keys1"])[0].T),
        "keys2T": np.ascontiguousarray(f(inp["peer_keys2"])[0].T),
        "peer_u": f(inp["peer_u"])[0],
        "peer_v": f(inp["peer_v"])[0],
        "gf": f(inp["final_g"]).reshape(1, 1024),
        "cos2": cos2,
        "sin2s": sin2s,
    }


def kernel(**inputs):
    NC = 8
    NSEQ = 4
    common = prep_common(inputs)
    x = np.asarray(inputs["x"], np.float32)
    nc = build(NSEQ)
    in_maps = []
    for c in range(NC):
        m = dict(common)
        m["x"] = np.ascontiguousarray(x[c * NSEQ:(c + 1) * NSEQ])
        in_maps.append(m)
    res = run_bass_kernel_spmd(nc, in_maps, core_ids=list(range(NC)))
    return np.concatenate([r["out"] for r in res.results], axis=0)
```
